# Optimizing a Trainium2 kernel written in Bass

```python
import jax, jax.numpy as jnp
from jax import lax
import numpy as np


D_MODEL = 1024
BATCH = 8
SEQ = 8192
DEPTH = 1

MLA_HEADS = 8
MLA_Q_LORA = 256
MLA_KV_LORA = 128
MLA_NOPE = 64
MLA_ROPE = 32
MLA_QK = MLA_NOPE + MLA_ROPE
MLA_V = 64
ROPE_THETA = 10000.0
Q_BLOCK = 128
HG_HEADS = 4
HG_DK = 128
HG_DV = 128
HG_CHUNK = 64
MIX_WIDTH = MLA_HEADS * MLA_V + HG_HEADS * HG_DV
IN_SIZES = (MLA_Q_LORA, MLA_KV_LORA, MLA_ROPE,
            HG_HEADS * HG_DK, HG_HEADS * HG_DK, HG_HEADS * HG_DK,
            HG_HEADS * HG_DV, HG_HEADS * HG_DV)
N_IN = MLA_Q_LORA + MLA_KV_LORA + MLA_ROPE + 3 * HG_HEADS * HG_DK + 2 * HG_HEADS * HG_DV
N_GROUPS = 4
EXPERTS_PER_GROUP = 8
N_EXPERTS = N_GROUPS * EXPERTS_PER_GROUP
TOP_K = 2
D_EXPERT = 256
MOE_BLOCK = 256
EPS = 1e-6

kernel_name = "hybrid_mla_hgrn2_hiermoe_encoder"


def rms_norm(x, gain):
    xf = x.astype(jnp.float32)
    y = xf * lax.rsqrt(jnp.mean(xf * xf, axis=-1, keepdims=True) + EPS)
    return (y * gain.astype(jnp.float32)).astype(x.dtype)


def split_cols(t, sizes):
    out, off = [], 0
    for s in sizes:
        out.append(t[..., off:off + s])
        off += s
    return out


def rope(x, pos):
    half = x.shape[-1] // 2
    inv = 1.0 / (ROPE_THETA ** (jnp.arange(half, dtype=jnp.float32) / half))
    ang = pos.astype(jnp.float32)[:, None] * inv[None, :]
    cos = jnp.cos(ang)[:, None, :]
    sin = jnp.sin(ang)[:, None, :]
    xf = x.astype(jnp.float32)
    x1, x2 = xf[..., :half], xf[..., half:]
    return jnp.concatenate([x1 * cos - x2 * sin, x2 * cos + x1 * sin], -1).astype(x.dtype)


def mla_mixer(q_lat, kv_lat, k_rope, q_lat_norm, kv_lat_norm, w_uq, w_ukv, q_norm, k_norm):
    B, S, _ = q_lat.shape
    H = MLA_HEADS
    q = (rms_norm(q_lat, q_lat_norm) @ w_uq).reshape(B, S, H, MLA_QK)
    kv = (rms_norm(kv_lat, kv_lat_norm) @ w_ukv).reshape(B, S, H, MLA_NOPE + MLA_V)
    k_nope, v = kv[..., :MLA_NOPE], kv[..., MLA_NOPE:]
    k = jnp.concatenate([k_nope, jnp.broadcast_to(k_rope[:, :, None, :], (B, S, H, MLA_ROPE))], -1)
    q = rms_norm(q, q_norm)
    k = rms_norm(k, k_norm)
    pos = jnp.arange(S)
    q = jnp.concatenate([q[..., :MLA_NOPE], rope(q[..., MLA_NOPE:], pos)], -1)
    k = jnp.concatenate([k[..., :MLA_NOPE], rope(k[..., MLA_NOPE:], pos)], -1)
    q = q * (MLA_QK ** -0.5)
    qb = q.reshape(B, S // Q_BLOCK, Q_BLOCK, H, MLA_QK).transpose(1, 0, 2, 3, 4)

    def attend(q_blk):
        s = jnp.einsum('bqhd,bkhd->bhqk', q_blk, k, preferred_element_type=jnp.float32)
        p = jax.nn.softmax(s, axis=-1).astype(v.dtype)
        return jnp.einsum('bhqk,bkhd->bqhd', p, v)

    o = lax.map(attend, qb)
    return o.transpose(1, 0, 2, 3, 4).reshape(B, S, H * MLA_V)


def hgrn2_mixer(hq, f_fwd, f_bwd, hi, hg, lb_logits, layer, out_norm):
    B, S, _ = hq.shape
    H, C = HG_HEADS, HG_CHUNK
    NC = S // C
    lb = jnp.cumsum(jax.nn.softmax(lb_logits.astype(jnp.float32), axis=0), axis=0)[layer]
    lb = lb[:, None, None, :]
    z = jnp.stack([f_fwd, jnp.flip(f_bwd, 1)]).astype(jnp.float32)
    f = lb + (1.0 - lb) * jax.nn.sigmoid(z)
    log_f = jnp.log(f)
    k = (1.0 - lb) * jax.nn.sigmoid(-z)
    qf = jax.nn.silu(hq.astype(jnp.float32))
    vf = hi.astype(jnp.float32)
    q = jnp.stack([qf, jnp.flip(qf, 1)])
    v = jnp.stack([vf, jnp.flip(vf, 1)])

    def to_chunks(t, d):
        return t.reshape(2, B, NC, C, H, d).transpose(2, 0, 1, 4, 3, 5)

    qc, kc, vc, gc = to_chunks(q, HG_DK), to_chunks(k, HG_DK), to_chunks(v, HG_DV), to_chunks(log_f, HG_DK)
    causal_in_scan = jnp.tril(jnp.ones((C, C), dtype=bool))[:, :, None]

    def step(state, inp):
        q_t, k_t, v_t, g_t = inp
        b = jnp.cumsum(g_t, axis=-2)
        o_inter = jnp.einsum('zbhtd,zbhde->zbhte', q_t * jnp.exp(b), state)
        diff = b[..., :, None, :] - b[..., None, :, :]
        decay = jnp.exp(jnp.where(causal_in_scan, diff, -jnp.inf))
        a = jnp.einsum('zbhtd,zbhsd,zbhtsd->zbhts', q_t, k_t, decay)
        o_intra = jnp.einsum('zbhts,zbhse->zbhte', a, v_t)
        b_last = b[..., -1:, :]
        new_state = jnp.exp(b_last[..., 0, :])[..., None] * state + \
            jnp.einsum('zbhsd,zbhse->zbhde', k_t * jnp.exp(b_last - b), v_t)
        return new_state, o_inter + o_intra

    s0 = jnp.zeros((2, B, H, HG_DK, HG_DV), jnp.float32)
    _, o = lax.scan(step, s0, (qc, kc, vc, gc))
    o = o.transpose(1, 2, 0, 4, 3, 5).reshape(2, B, S, H, HG_DV)
    o = o[0] + jnp.flip(o[1], axis=1)
    o = rms_norm(o, out_norm) * jax.nn.silu(hg.reshape(B, S, H, HG_DV).astype(jnp.float32))
    return o.reshape(B, S, H * HG_DV).astype(hq.dtype)


def hier_moe(h, w_group, b_group, w_router, b_router, w_gate, w_up, w_down):
    B, S, D = h.shape
    T = B * S
    M = T * TOP_K
    x = h.reshape(T, D)
    g_prob = jax.nn.softmax((x @ w_group).astype(jnp.float32) + b_group.astype(jnp.float32), axis=-1)
    g_w, g_idx = lax.top_k(g_prob, 1)
    e_logits = ((x @ w_router).astype(jnp.float32) + b_router.astype(jnp.float32)).reshape(T, N_GROUPS, EXPERTS_PER_GROUP)
    e_logits = jnp.take_along_axis(e_logits, g_idx[:, :, None], axis=1)[:, 0]
    e_w, e_idx = lax.top_k(jax.nn.softmax(e_logits, axis=-1), TOP_K)
    e_w = e_w / jnp.sum(e_w, axis=-1, keepdims=True)
    gate = (g_w * e_w).astype(h.dtype).reshape(-1)
    eid = (g_idx * EXPERTS_PER_GROUP + e_idx).reshape(-1)
    tok = jnp.repeat(jnp.arange(T, dtype=jnp.int32), TOP_K)
    order = jnp.argsort(eid)
    eid_s, tok_s, gate_s = eid[order], tok[order], gate[order]
    counts = jnp.bincount(eid, length=N_EXPERTS)
    starts = jnp.cumsum(counts) - counts
    padded = (counts + MOE_BLOCK - 1) // MOE_BLOCK * MOE_BLOCK
    pend = jnp.cumsum(padded)
    pstart = pend - padded
    dest = pstart[eid_s] + (jnp.arange(M) - starts[eid_s])
    P = -(-M // MOE_BLOCK) * MOE_BLOCK + N_EXPERTS * MOE_BLOCK
    NB = P // MOE_BLOCK
    tok_buf = jnp.zeros((P,), jnp.int32).at[dest].set(tok_s)
    gate_buf = jnp.zeros((P,), h.dtype).at[dest].set(gate_s)
    block_e = jnp.minimum(jnp.searchsorted(pend, jnp.arange(NB) * MOE_BLOCK, side='right'), N_EXPERTS - 1)
    xb = x[tok_buf].reshape(NB, MOE_BLOCK, D)

    def expert_block(args):
        x_blk, e = args
        return (jax.nn.silu(x_blk @ w_gate[e]) * (x_blk @ w_up[e])) @ w_down[e]

    yb = lax.map(expert_block, (xb, block_e))
    y = jnp.zeros((T, D), h.dtype).at[tok_buf].add(yb.reshape(P, D) * gate_buf[:, None])
    return y.reshape(B, S, D)


def setup_inputs(seed: int = 0) -> dict:
    key = jax.random.key(seed)
    ks = jax.random.split(key, 24)
    f32 = jnp.float32
    L = DEPTH

    def nrm(k, shape, fan_in):
        return jax.random.normal(k, shape, f32) * (fan_in ** -0.5)

    def gain(k, shape):
        return 1.0 + 0.02 * jax.random.normal(k, shape, f32)

    return {
        "x": jax.random.normal(ks[0], (BATCH, SEQ, D_MODEL), f32),
        "norm_mix": gain(ks[1], (L, D_MODEL)),
        "w_in": nrm(ks[2], (L, D_MODEL, N_IN), D_MODEL),
        "q_lat_norm": gain(ks[3], (L, MLA_Q_LORA)),
        "kv_lat_norm": gain(ks[4], (L, MLA_KV_LORA)),
        "w_uq": nrm(ks[5], (L, MLA_Q_LORA, MLA_HEADS * MLA_QK), MLA_Q_LORA),
        "w_ukv": nrm(ks[6], (L, MLA_KV_LORA, MLA_HEADS * (MLA_NOPE + MLA_V)), MLA_KV_LORA),
        "q_norm": gain(ks[7], (L, MLA_QK)),
        "k_norm": gain(ks[8], (L, MLA_QK)),
        "lb_logits": 0.1 * jax.random.normal(ks[9], (DEPTH + 1, 2, HG_HEADS * HG_DK), f32),
        "hg_out_norm": gain(ks[10], (L, HG_DV)),
        "w_out": nrm(ks[11], (L, MIX_WIDTH, D_MODEL), MIX_WIDTH),
        "norm_ffn": gain(ks[12], (L, D_MODEL)),
        "w_group": nrm(ks[13], (L, D_MODEL, N_GROUPS), D_MODEL),
        "b_group": 0.01 * jax.random.normal(ks[14], (L, N_GROUPS), f32),
        "w_router": nrm(ks[15], (L, D_MODEL, N_EXPERTS), D_MODEL),
        "b_router": 0.01 * jax.random.normal(ks[16], (L, N_EXPERTS), f32),
        "w_gate": nrm(ks[17], (L, N_EXPERTS, D_MODEL, D_EXPERT), D_MODEL),
        "w_up": nrm(ks[18], (L, N_EXPERTS, D_MODEL, D_EXPERT), D_MODEL),
        "w_down": nrm(ks[19], (L, N_EXPERTS, D_EXPERT, D_MODEL), D_EXPERT),
    }


def reference(x, norm_mix, w_in, q_lat_norm, kv_lat_norm, w_uq, w_ukv, q_norm, k_norm,
              lb_logits, hg_out_norm, w_out, norm_ffn, w_group, b_group, w_router, b_router,
              w_gate, w_up, w_down):
    h = x
    for l in range(DEPTH):
        n = rms_norm(h, norm_mix[l])
        proj = n @ w_in[l]
        q_lat, kv_lat, k_rope, hq, f_fwd, f_bwd, hi, hg = split_cols(proj, IN_SIZES)
        a = mla_mixer(q_lat, kv_lat, k_rope, q_lat_norm[l], kv_lat_norm[l], w_uq[l], w_ukv[l],
                      q_norm[l], k_norm[l])
        r = hgrn2_mixer(hq, f_fwd, f_bwd, hi, hg, lb_logits, l, hg_out_norm[l])
        h = h + jnp.concatenate([a, r], axis=-1) @ w_out[l]
        h = h + hier_moe(rms_norm(h, norm_ffn[l]), w_group[l], b_group[l], w_router[l], b_router[l],
                         w_gate[l], w_up[l], w_down[l])
    return h
```

```python
import numpy as np
from contextlib import ExitStack
import concourse.bass as bass
import concourse.mybir as mybir
from concourse.bass_utils import run_bass_kernel_spmd

F32 = mybir.dt.float32
BF16 = mybir.dt.bfloat16
I32 = mybir.dt.int32
U32 = mybir.dt.uint32
U8 = mybir.dt.uint8
AF = mybir.ActivationFunctionType
ALU = mybir.AluOpType
AX = mybir.AxisListType

ENGS = ("pe", "act", "dve", "pool", "sp")
EPS = 1e-6
D = 1024
NIN = 2976
H = 8
QK = 96
NOPE = 64
ROPE = 32
DV = 64
HGH = 4
NE = 32
NG = 4
DE = 256
MOE_B = 256
MOE_LOG2B = 8
import os
MOE_DBG = int(os.environ.get('MOE_DBG', '0'))
class Buf:
    __slots__ = ("name", "last_w", "readers", "multi", "writers")

    def __init__(self, name="", multi=False):
        self.name = name
        self.last_w = None
        self.readers = {}
        self.multi = multi
        self.writers = []


class Op:
    __slots__ = ("eng", "fn", "waits", "signal", "ticket", "is_dma", "sem", "target", "idx", "emitted", "drained")


class Prog:
    def __init__(self, nc, n_dma_sems=40, n_sw_sems=8):
        self.nc = nc
        self.eng_obj = {"pe": nc.tensor, "act": nc.scalar, "dve": nc.vector,
                        "pool": nc.gpsimd, "sp": nc.sync}
        self.sem = {}
        self.count = {e: 0 for e in ENGS}
        self._stack = []
        for e in ENGS:
            g = nc.semaphore("s_" + e)
            self.sem[e] = g.__enter__()
            self._stack.append(g)
        self.dma_sems = []
        self.dma_uses = []
        self.dma_last = []
        for i in range(n_dma_sems):
            g = nc.semaphore("d_%d" % i)
            self.dma_sems.append(g.__enter__())
            self._stack.append(g)
            self.dma_uses.append(0)
            self.dma_last.append(None)
        self.dma_rr = 0
        self.sw_sems = []
        self.sw_uses = []
        self.sw_last = []
        for i in range(n_sw_sems):
            g = nc.semaphore("w_%d" % i)
            self.sw_sems.append(g.__enter__())
            self._stack.append(g)
            self.sw_uses.append(0)
            self.sw_last.append(None)
        self.sw_rr = 0
        self.ops = {e: [] for e in ENGS}
        self.waited = {e: {} for e in ENGS}
        self.nops = 0
        self.pending_dma = []
        self._deferred = []
        self._def_src = set()

    def defer_dma(self, eng, out, in_, reads=(), writes=(), **kw):
        self._deferred.append((eng, out, in_, tuple(reads), tuple(writes), kw))
        for b in reads:
            self._def_src.add(id(b))

    def flush(self):
        d, self._deferred = self._deferred, []
        self._def_src = set()
        for eng, out, in_, r, w, kw in d:
            self.dma(eng, out, in_, r, w, **kw)

    def add(self, eng, fn, reads=(), writes=(), dma=False):
        if self._deferred and any(id(b) in self._def_src for b in writes):
            self.flush()
        op = Op()
        op.eng = eng
        op.fn = fn
        op.waits = []
        op.signal = False
        op.ticket = None
        op.is_dma = dma
        op.sem = None
        op.target = None
        op.idx = self.nops
        op.emitted = False
        op.drained = False
        self.nops += 1
        deps = {}
        raw = set()
        for b in reads:
            if b.multi:
                b.writers = [w_ for w_ in b.writers if not (w_.emitted and (w_.drained or not w_.is_dma))]
                for w_ in b.writers:
                    deps[id(w_)] = w_
            elif b.last_w is not None:
                deps[id(b.last_w)] = b.last_w
                raw.add(id(b.last_w))
        for b in writes:
            if (not b.multi) and b.last_w is not None:
                deps[id(b.last_w)] = b.last_w
            for r in b.readers.values():
                deps[id(r)] = r
        for k, d in deps.items():
            if d is op:
                continue
            if d.is_dma:
                if not (d.emitted and d.drained):
                    op.waits.append(d)
            elif d.emitted:
                continue
            elif d.eng == eng and not dma:
                if eng == "pe":
                    continue
                d.signal = True
                op.waits.append(d)
            else:
                d.signal = True
                op.waits.append(d)
        if dma and eng == "pool":
            i = self.sw_rr
            self.sw_rr = (self.sw_rr + 1) % len(self.sw_sems)
            prev = self.sw_last[i]
            if prev is not None:
                op.waits.append(prev)
            self.sw_uses[i] += 1
            op.sem = self.sw_sems[i]
            op.target = 16 * self.sw_uses[i]
            self.sw_last[i] = op
            self.pending_dma.append(op)
        elif dma:
            i = self.dma_rr
            self.dma_rr = (self.dma_rr + 1) % len(self.dma_sems)
            prev = self.dma_last[i]
            if prev is not None:
                op.waits.append(prev)
            self.dma_uses[i] += 1
            op.sem = self.dma_sems[i]
            op.target = 16 * self.dma_uses[i]
            self.dma_last[i] = op
            self.pending_dma.append(op)
        for b in reads:
            key = ("d", op.idx) if dma else eng
            b.readers[key] = op
        for b in writes:
            if b.multi:
                b.writers.append(op)
                b.readers = {k_: r_ for k_, r_ in b.readers.items() if not (r_.emitted and (r_.drained or not r_.is_dma))}
            else:
                b.last_w = op
                b.readers = {}
        self.ops[eng].append(op)
        return op

    def dma(self, eng, out, in_, reads=(), writes=(), **kw):
        return self.add(eng, lambda e: e.dma_start(out=out, in_=in_, **kw), reads, writes, dma=True)

    def drain_dmas(self, eng="sp"):
        op = self.add(eng, None)
        seen = {}
        for d in self.pending_dma:
            d.drained = True
            seen[id(d.sem)] = d
        op.waits.extend(seen.values())
        self.pending_dma = []
        return op

    def emit(self, name=None):
        for e in ENGS:
            c = self.count[e]
            for op in self.ops[e]:
                if op.is_dma:
                    continue
                if op.signal:
                    c += 1
                    op.ticket = c
            self.count[e] = c
        prog = self

        def run(e, engine):
            waited = prog.waited[e]
            for op in prog.ops[e]:
                for d in op.waits:
                    if d.is_dma:
                        key, sem, val = id(d.sem), d.sem, d.target
                    else:
                        key, sem, val = d.eng, prog.sem[d.eng], d.ticket
                    if waited.get(key, 0) >= val:
                        continue
                    waited[key] = val
                    engine.wait_ge(sem, val)
                if op.fn is None:
                    continue
                ins = op.fn(engine)
                if op.is_dma:
                    ins.then_inc(op.sem, 16)
                elif op.signal:
                    ins.then_inc(prog.sem[e], 1)

        with self.nc.Block() as block:
            @block.tensor
            def _(eng):
                run("pe", eng)

            @block.scalar
            def _(eng):
                run("act", eng)

            @block.vector
            def _(eng):
                run("dve", eng)

            @block.gpsimd
            def _(eng):
                run("pool", eng)

            @block.sync
            def _(eng):
                run("sp", eng)
        for e in ENGS:
            for op in self.ops[e]:
                op.emitted = True
        self.ops = {e: [] for e in ENGS}


def _bf(a):
    import ml_dtypes
    return np.asarray(a, dtype=np.float32).astype(ml_dtypes.bfloat16)


def moe_cap(S):
    return max(MOE_B, S // 8)


def make_consts(S):
    c = {}
    c["ident_bf"] = _bf(np.eye(128))
    c["ident_f"] = np.eye(128, dtype=np.float32)
    c["ones_bf"] = _bf(np.ones((128, 128)))
    c["ones_f"] = np.ones((128, 128), dtype=np.float32)
    rot = np.zeros((96, 96), np.float32)
    for i in range(16):
        rot[80 + i, 64 + i] = -1.0
        rot[64 + i, 80 + i] = 1.0
    c["rotT"] = _bf(rot)
    sel = np.zeros((32, 96), np.float32)
    for i in range(32):
        sel[i, 64 + i] = 1.0
    c["kr_sel"] = _bf(sel)
    half = 16
    inv = (1.0 / (10000.0 ** (np.arange(half, dtype=np.float32) / half))).astype(np.float32)
    ang = (np.arange(S, dtype=np.float32)[None, :] * inv[:, None]).astype(np.float32)
    cs = np.zeros((96, S), np.float32)
    sn = np.zeros((96, S), np.float32)
    cs[64:80] = np.cos(ang); cs[80:96] = np.cos(ang)
    sn[64:80] = np.sin(ang); sn[80:96] = np.sin(ang)
    c["rope_cos"] = cs
    c["rope_sin"] = sn
    s_ = np.arange(128)[:, None]
    t_ = np.arange(128)[None, :]
    c["hg_Lc_f"] = ((s_ <= t_).astype(np.float32) - (s_ <= 63).astype(np.float32))
    c["hg_Lr_f"] = (s_ > t_).astype(np.float32)
    c["hg_Lc_b"] = ((s_ >= t_).astype(np.float32) - (s_ >= 64).astype(np.float32))
    c["hg_Lr_b"] = (s_ < t_).astype(np.float32)
    c["hg_selm_f"] = np.stack([np.ones(128), (np.arange(128) <= 63)], 1).astype(np.float32)
    c["hg_selm_b"] = np.stack([np.ones(128), (np.arange(128) >= 64)], 1).astype(np.float32)
    mf = (s_ <= t_).astype(np.uint32)
    mb = (s_ >= t_).astype(np.uint32)
    c["hg_mask_f"] = np.tile(mf, (1, 4))
    c["hg_mask_b"] = np.tile(mb, (1, 4))
    c["ustrict"] = (s_ < t_).astype(np.float32)
    c["u32strict"] = (np.arange(32)[:, None] < np.arange(32)[None, :]).astype(np.float32)
    c["ident32"] = np.eye(32, dtype=np.float32)
    cap = moe_cap(S)
    c["blk_iota"] = np.stack([np.arange(32) * cap, np.zeros(32)], 1).astype(np.float32)
    c["lim_row"] = np.tile(((np.arange(32) + 1) * cap).astype(np.float32)[None, :], (128, 1))
    c["p_iota"] = np.arange(128, dtype=np.float32).reshape(128, 1)
    c["tok_iota"] = (np.arange(S // 128, dtype=np.int32)[None, :] * 128 + np.arange(128, dtype=np.int32)[:, None]).astype(np.int32)
    return c


CONST_SPECS = {
    "ustrict": ([128, 128], F32), "u32strict": ([32, 32], F32), "ident32": ([32, 32], F32),
    "blk_iota": ([32, 2], F32), "lim_row": ([128, 32], F32), "p_iota": ([128, 1], F32), "tok_iota": "tok",
    "ident_bf": ([128, 128], BF16), "ident_f": ([128, 128], F32),
    "ones_bf": ([128, 128], BF16), "ones_f": ([128, 128], F32),
    "rotT": ([96, 96], BF16), "kr_sel": ([32, 96], BF16),
    "rope_cos": None, "rope_sin": None,
    "hg_Lc_f": ([128, 128], F32), "hg_Lr_f": ([128, 128], F32),
    "hg_Lc_b": ([128, 128], F32), "hg_Lr_b": ([128, 128], F32),
    "hg_selm_f": ([128, 2], F32), "hg_selm_b": ([128, 2], F32),
    "hg_mask_f": ([128, 512], U32), "hg_mask_b": ([128, 512], U32),
}


def layout_params(inp):
    f = lambda a: np.ascontiguousarray(np.asarray(a, dtype=np.float32))
    p = {}
    p["norm_mix_l"] = f(inp["norm_mix"][0].reshape(8, 128).T)
    p["q_lat_norm_l"] = f(inp["q_lat_norm"][0].reshape(2, 128).T)
    p["kv_lat_norm_l"] = f(inp["kv_lat_norm"][0].reshape(1, 128).T)
    p["q_norm_l"] = f(inp["q_norm"][0].reshape(96, 1))
    p["k_norm_l"] = f(inp["k_norm"][0].reshape(96, 1))
    p["lb_logits_l"] = f(inp["lb_logits"].reshape(2, 2 * 512))
    p["hg_out_norm_l"] = f(inp["hg_out_norm"][0].reshape(1, 128))
    p["norm_ffn_l"] = f(inp["norm_ffn"][0].reshape(1, 1024))
    p["w_rt"] = f(np.concatenate([inp["w_group"][0], inp["w_router"][0]], axis=1))
    p["b_rt"] = f(np.concatenate([inp["b_group"][0], inp["b_router"][0]]).reshape(1, 36))
    p["w_in"] = f(inp["w_in"][0])
    p["w_uq"] = f(inp["w_uq"][0])
    p["w_ukv"] = f(inp["w_ukv"][0])
    p["w_out"] = f(inp["w_out"][0])
    p["w_gate"] = f(inp["w_gate"][0])
    p["w_up"] = f(inp["w_up"][0])
    p["w_down"] = f(inp["w_down"][0])
    return p


PARAM_SPECS = {
    "norm_mix_l": [128, 8], "q_lat_norm_l": [128, 2], "kv_lat_norm_l": [128, 1],
    "q_norm_l": [96, 1], "k_norm_l": [96, 1], "lb_logits_l": [2, 1024],
    "hg_out_norm_l": [1, 128], "norm_ffn_l": [1, 1024], "w_rt": [1024, 36], "b_rt": [1, 36],
    "w_in": [D, NIN], "w_uq": [256, 768], "w_ukv": [128, 1024], "w_out": [1024, 1024],
    "w_gate": [NE, D, DE], "w_up": [NE, D, DE], "w_down": [NE, DE, D],
}


class K:
    def __init__(self, S, debug=(), phases=None):
        self.S = S
        self.NT = S // 128
        self.NB = S // 512
        self.debug = set(debug)
        self.phases = phases
        self.nc = nc = bass.Bass("TRN2", target_bir_lowering=False)
        self.P = Prog(nc)
        self.din = {}
        self.x = nc.dram_tensor("x", [S, D], F32, kind="ExternalInput").ap()
        for k, shp in PARAM_SPECS.items():
            self.din[k] = nc.dram_tensor(k, shp, F32, kind="ExternalInput").ap()
        for k, spec in CONST_SPECS.items():
            if spec is None:
                shp, dt = [96, S], F32
            elif spec == "tok":
                shp, dt = [128, S // 128], I32
            else:
                shp, dt = spec
            self.din[k] = nc.dram_tensor(k, shp, dt, kind="ExternalInput").ap()
        self.y = nc.dram_tensor("y", [S, D], F32, kind="ExternalOutput").ap()
        self.scr = {}
        self.scr_buf = {}
        self.deferred = []
        self.fence_t = nc.alloc_sbuf_tensor("fence_scratch", [128, 1], F32)
        self.b_fence = Buf("fence")

    def scratch(self, name, shape, dt):
        kind = "ExternalOutput" if name in self.debug else "Internal"
        t = self.nc.dram_tensor(name, shape, dt, kind=kind).ap()
        self.scr[name] = t
        self.scr_buf[name] = Buf(name, multi=True)
        return t

    def mm(self, out, lhsT, rhs, start=True, stop=True, r=(), w=()):
        return self.P.add("pe", lambda e: e.matmul(out, lhsT, rhs, start=start, stop=stop), r, w)

    def tr(self, out, in_, ident, r=(), w=()):
        return self.P.add("pe", lambda e: e.transpose(out, in_, ident), r, w)

    def act(self, out, in_, func, r=(), w=(), eng="act", **kw):
        return self.P.add(eng, lambda e: e.activation(out, in_, func, **kw), r, w)

    def ts(self, eng, out, in0, s1, s2, op0, op1=None, r=(), w=(), **kw):
        if op1 is None:
            return self.P.add(eng, lambda e: e.tensor_scalar(out, in0, s1, s2, op0, **kw), r, w)
        return self.P.add(eng, lambda e: e.tensor_scalar(out, in0, s1, s2, op0, op1, **kw), r, w)

    def tt(self, eng, out, in0, in1, op, r=(), w=()):
        return self.P.add(eng, lambda e: e.tensor_tensor(out, in0, in1, op), r, w)

    def cp(self, eng, out, in_, r=(), w=()):
        if eng == "act":
            return self.P.add(eng, lambda e: e.copy(out, in_), r, w)
        return self.P.add(eng, lambda e: e.tensor_copy(out, in_), r, w)

    def dma(self, eng, out, in_, r=(), w=(), **kw):
        return self.P.dma(eng, out, in_, r, w, **kw)

    def load(self, out, in_, r=(), w=(), **kw):
        op = self.P.dma("sp", out, in_, r, w, **kw)
        self.flush()
        return op

    def store(self, out, in_, r=(), w=(), **kw):
        self.P.defer_dma("sp", out, in_, r, w, **kw)

    def fence(self, bufs):
        t = self.fence_t
        self.P.add("pool", lambda e: e.memset(t[0:1, 0:1], 0.0), list(bufs), [self.b_fence])

    def flush(self):
        self.P.flush()

    def make_eps(self, es):
        self.epst = {}
        self.b_epst = Buf("eps")
        for n in (96, 128, 256, 1024):
            t = es.enter_context(self.nc.sbuf_tensor("sb_eps%d" % n, [128, 1], F32))
            self.epst[n] = t
            self.P.add("pool", lambda e, t=t, n=n: e.memset(t[:], float(n * EPS)), (), [self.b_epst])

    def rstd(self, out, in_, neps, bout, r=()):
        np_ = out.shape[0]
        self.P.add("act", lambda e: e.activation(out, in_, AF.Ln, bias=self.epst[neps][0:np_, 0:1]), list(r) + [self.b_epst], [bout])
        self.P.add("act", lambda e: e.activation(out, out, AF.Exp, scale=-0.5), [bout], [bout])


def run_pipelined(gens, depth):
    active = []
    it = iter(gens)
    while True:
        while len(active) < depth:
            g = next(it, None)
            if g is None:
                break
            active.append(g)
        if not active:
            break
        for g in list(active):
            if next(g, "done") == "done":
                active.remove(g)


class Pool_:
    def __init__(self, tiles):
        self.tiles = tiles
        self.bufs = [Buf() for _ in tiles]
        self.i = 0

    def next(self):
        i = self.i
        self.i = (self.i + 1) % len(self.tiles)
        return self.tiles[i], self.bufs[i]


class FreePool:
    def __init__(self, tiles):
        self.tiles = tiles
        self.bufs = [Buf() for _ in tiles]
        self.free = list(range(len(tiles)))

    def get(self):
        while not self.free:
            yield
        i = self.free.pop(0)
        return self.tiles[i], self.bufs[i], i

    def put(self, i):
        self.free.append(i)

    def take(self):
        i = self.free.pop(0)
        return self.tiles[i], self.bufs[i], i


def sb_free(es, nc, name, n, shape, dt):
    return FreePool([es.enter_context(nc.sbuf_tensor("sb_%s%d" % (name, i), shape, dt)) for i in range(n)])


def ps_free(es, nc, name, n, shape=(128, 512), dt=F32):
    return FreePool([es.enter_context(nc.psum_tensor("%s%d" % (name, i), list(shape), dt)) for i in range(n)])


def sb_ring(es, nc, name, n, shape, dt):
    return Pool_([es.enter_context(nc.sbuf_tensor("sb_%s%d" % (name, i), shape, dt)) for i in range(n)])


def ps_ring(es, nc, name, n, shape=(128, 512), dt=F32):
    return Pool_([es.enter_context(nc.psum_tensor("%s%d" % (name, i), list(shape), dt)) for i in range(n)])


def phase1(k):
    nc, P, S = k.nc, k.P, k.S
    QT = k.scratch("QT", [H, QK, S], BF16)
    KT = k.scratch("KT", [H, QK, S], BF16)
    VV = k.scratch("VV", [S, H * DV], BF16)
    HG = k.scratch("HG", [S, 2560], F32)
    bQT, bKT, bVV, bHG = (k.scr_buf[n] for n in ("QT", "KT", "VV", "HG"))
    with ExitStack() as es:
        def sb(name, shape, dt):
            return es.enter_context(nc.sbuf_tensor("sb_" + name, list(shape), dt)), Buf(name)
        ident, b_ident = sb("ident", [128, 128], BF16)
        ones, b_ones = sb("ones", [128, 128], BF16)
        rotT, b_rotT = sb("rotT", [96, 96], BF16)
        krsel, b_krsel = sb("krsel", [32, 96], BF16)
        for t_, b_, nm in ((ident, b_ident, "ident_bf"), (ones, b_ones, "ones_bf"),
                           (rotT, b_rotT, "rotT"), (krsel, b_krsel, "kr_sel")):
            k.dma("sp", t_[:], k.din[nm], w=[b_])
        k.make_eps(es)
        gmix, b_gmix = sb("gmix", [128, 8], F32)
        gql, b_gql = sb("gql", [128, 2], F32)
        gkvl, b_gkvl = sb("gkvl", [128, 1], F32)
        gq, b_gq = sb("gq", [96, 1], F32)
        gk, b_gk = sb("gk", [96, 1], F32)
        for t_, b_, nm in ((gmix, b_gmix, "norm_mix_l"), (gql, b_gql, "q_lat_norm_l"),
                           (gkvl, b_gkvl, "kv_lat_norm_l"), (gq, b_gq, "q_norm_l"), (gk, b_gk, "k_norm_l")):
            k.dma("sp", t_[:], k.din[nm], w=[b_])
        k.ts("dve", gmix[:], gmix[:], 32.0, None, ALU.mult, r=[b_gmix], w=[b_gmix])
        k.ts("dve", gql[:], gql[:], 16.0, None, ALU.mult, r=[b_gql], w=[b_gql])
        k.ts("dve", gkvl[:], gkvl[:], float(np.sqrt(128.0)), None, ALU.mult, r=[b_gkvl], w=[b_gkvl])
        k.ts("dve", gq[:], gq[:], float(np.sqrt(96.0) * 96.0 ** -0.5), None, ALU.mult, r=[b_gq], w=[b_gq])
        k.ts("dve", gk[:], gk[:], float(np.sqrt(96.0)), None, ALU.mult, r=[b_gk], w=[b_gk])

        win, b_win = sb("win", [128, 8, NIN], BF16)
        wst = sb_ring(es, nc, "wst", 1, [128, NIN], F32)
        w_in_v = k.din["w_in"].rearrange("(kc p) n -> p kc n", p=128)
        for kc in range(8):
            st, bst = wst.next()
            k.dma("sp", st[:], w_in_v[:, kc, :], w=[bst])
            k.ts("dve" if kc % 2 == 0 else "pool", win[:, kc, :], st[:], gmix[:, kc:kc + 1], None, ALU.mult,
                 r=[bst, b_gmix], w=[b_win])
        wuq, b_wuq = sb("wuq", [128, 2, 768], BF16)
        w_uq_v = k.din["w_uq"].rearrange("(kc p) n -> p kc n", p=128)
        for kc in range(2):
            st, bst = wst.next()
            k.dma("sp", st[:, 0:768], w_uq_v[:, kc, :], w=[bst])
            k.ts("dve", wuq[:, kc, :], st[:, 0:768], gql[:, kc:kc + 1], None, ALU.mult, r=[bst, b_gql], w=[b_wuq])
        wk, b_wk = sb("wk", [128, 8, 96], BF16)
        wv, b_wv = sb("wv", [128, 8, 64], BF16)
        st, bst = wst.next()
        k.dma("sp", st[:, 0:1024], k.din["w_ukv"], w=[bst])
        P.add("pool", lambda e: e.memset(wk[:], 0.0), (), [b_wk])
        stv = st[:, 0:1024].rearrange("p (h c) -> p h c", c=128)
        k.ts("dve", wk[:, :, 0:64], stv[:, :, 0:64], gkvl[:, 0:1], None, ALU.mult, r=[bst, b_gkvl], w=[b_wk])
        k.ts("dve", wv[:], stv[:, :, 64:128], gkvl[:, 0:1], None, ALU.mult, r=[bst, b_gkvl], w=[b_wv])

        xr = sb_ring(es, nc, "xt", 3, [128, D], F32)
        junkr = sb_ring(es, nc, "junk", 3, [128, D], BF16)
        ssr = sb_ring(es, nc, "ss", 4, [128, 2], F32)
        nr = sb_ring(es, nc, "nbf", 2, [128, D], BF16)
        nTr = sb_ring(es, nc, "nT", 2, [128, 8, 512], BF16)
        stg = sb_ring(es, nc, "stg", 2, [128, 2560], F32)
        sqq, b_sqq = sb("sqq", [128, 3, 512], BF16)
        rsl, b_rsl = sb("rsl", [128, 2, 512], F32)
        qnT, b_qnT = sb("qnT", [128, 2, 512], BF16)
        kvnT, b_kvnT = sb("kvnT", [128, 512], BF16)
        krT, b_krT = sb("krT", [32, 512], BF16)
        vsb = sb_ring(es, nc, "vsb", 2, [128, 512], BF16)
        cosr = sb_ring(es, nc, "cos", 2, [96, 512], F32)
        sinr = sb_ring(es, nc, "sin", 2, [96, 512], F32)
        sqh = sb_ring(es, nc, "sqh", 3, [96, 512], BF16)
        qgh = sb_ring(es, nc, "qgh", 3, [96, 512], BF16)
        qg32 = sb_ring(es, nc, "qg32", 3, [96, 512], F32)
        rsh = sb_ring(es, nc, "rsh", 3, [96, 512], F32)
        t1r = sb_ring(es, nc, "t1r", 3, [96, 512], F32)
        t2r = sb_ring(es, nc, "t2r", 3, [96, 512], F32)
        qfr = sb_ring(es, nc, "qfr", 3, [96, 512], BF16)
        psf = ps_ring(es, nc, "psf", 6)
        psb = ps_ring(es, nc, "psb", 2, (128, 1024), BF16)

        eflip = [0]

        def evac_eng():
            eflip[0] ^= 1
            return "act" if eflip[0] else "dve"

        for j in range(k.NB):
            tok0 = j * 512
            nT, b_nT = nTr.next()
            def xt_gen(t, tok0=tok0, nT=nT, b_nT=b_nT):
                r0 = tok0 + t * 128
                xt, bx = xr.next()
                k.load(xt[:], k.x[r0:r0 + 128, :], w=[bx])
                yield
                ss, bss = ssr.next()
                junk, b_junk = junkr.next()
                k.act(junk[:], xt[:], AF.Square, r=[bx], w=[b_junk, bss], accum_out=ss[:, 0:1])
                k.rstd(ss[:, 1:2], ss[:, 0:1], 1024, bss, r=[bss])
                yield
                nb_, bn = nr.next()
                k.act(nb_[:], xt[:], AF.Copy, r=[bx, bss], w=[bn], scale=ss[:, 1:2])
                yield
                pb, bpb = psb.next()
                for kc in range(8):
                    k.tr(pb[:, kc * 128:(kc + 1) * 128], nb_[:, kc * 128:(kc + 1) * 128], ident[:],
                         r=[bn, b_ident], w=[bpb])
                yield
                k.cp("act" if t % 2 == 0 else "dve", nT[:, :, t * 128:(t + 1) * 128],
                     pb[:].rearrange("p (kc t) -> p kc t", kc=8), r=[bpb], w=[b_nT])

            run_pipelined([xt_gen(t) for t in range(4)], 2)
            lat = []
            for (c0, ncol) in ((0, 128), (128, 128), (256, 128), (384, 32)):
                ps, bps = psf.next()
                for kc in range(8):
                    k.mm(ps[0:ncol, :], win[:, kc, c0:c0 + ncol], nT[:, kc, :], start=(kc == 0), stop=(kc == 7),
                         r=[b_win, b_nT], w=[bps])
                lat.append((ps, bps))
            for i in range(3):
                k.act(sqq[:, i, :], lat[i][0][:], AF.Square, r=[lat[i][1]], w=[b_sqq])
            k.cp("dve", krT[:], lat[3][0][0:32, :], r=[lat[3][1]], w=[b_krT])
            msq, bmsq = psf.next()
            k.mm(msq[:], ones[:], sqq[:, 0, :], start=True, stop=False, r=[b_ones, b_sqq], w=[bmsq])
            k.mm(msq[:], ones[:], sqq[:, 1, :], start=False, stop=True, r=[b_ones, b_sqq], w=[bmsq])
            msk, bmsk = psf.next()
            k.mm(msk[:], ones[:], sqq[:, 2, :], r=[b_ones, b_sqq], w=[bmsk])
            k.rstd(rsl[:, 0, :], msq[:], 256, b_rsl, r=[bmsq])
            k.rstd(rsl[:, 1, :], msk[:], 128, b_rsl, r=[bmsk])
            k.tt("dve", qnT[:, 0, :], lat[0][0][:], rsl[:, 0, :], ALU.mult, r=[lat[0][1], b_rsl], w=[b_qnT])
            k.tt("dve", qnT[:, 1, :], lat[1][0][:], rsl[:, 0, :], ALU.mult, r=[lat[1][1], b_rsl], w=[b_qnT])
            k.tt("dve", kvnT[:], lat[2][0][:], rsl[:, 1, :], ALU.mult, r=[lat[2][1], b_rsl], w=[b_kvnT])
            for t in range(4):
                ps, bps = psf.next()
                k.mm(ps[:], kvnT[:, t * 128:(t + 1) * 128], wv[:].rearrange("p h c -> p (h c)"),
                     r=[b_kvnT, b_wv], w=[bps])
                v_, bv = vsb.next()
                k.cp("dve", v_[:], ps[:], r=[bps], w=[bv])
                k.store(VV[tok0 + t * 128: tok0 + (t + 1) * 128, :], v_[:], r=[bv], w=[bVV])
            cs, bcs = cosr.next()
            sn, bsn = sinr.next()
            k.load(cs[64:96, :], k.din["rope_cos"][64:96, tok0:tok0 + 512], w=[bcs])
            k.load(sn[64:96, :], k.din["rope_sin"][64:96, tok0:tok0 + 512], w=[bsn])

            def head_gen(h, which, tok0=tok0, cs=cs, bcs=bcs, sn=sn, bsn=bsn):
                ps, bps = psf.next()
                if which == 0:
                    k.mm(ps[0:96, :], wuq[:, 0, h * 96:(h + 1) * 96], qnT[:, 0, :], start=True, stop=False,
                         r=[b_wuq, b_qnT], w=[bps])
                    k.mm(ps[0:96, :], wuq[:, 1, h * 96:(h + 1) * 96], qnT[:, 1, :], start=False, stop=True,
                         r=[b_wuq, b_qnT], w=[bps])
                    g_, bg_, dst, bdst = gq, b_gq, QT, bQT
                else:
                    k.mm(ps[0:96, :], wk[:, h, :], kvnT[:], start=True, stop=False, r=[b_wk, b_kvnT], w=[bps])
                    k.mm(ps[0:96, :], krsel[:], krT[:], start=False, stop=True, r=[b_krsel, b_krT], w=[bps])
                    g_, bg_, dst, bdst = gk, b_gk, KT, bKT
                yield
                sq, bsq = sqh.next()
                k.act(sq[:], ps[0:96, :], AF.Square, r=[bps], w=[bsq])
                q32, bq32 = qg32.next()
                k.act(q32[:], ps[0:96, :], AF.Copy, r=[bps, bg_], w=[bq32], scale=g_[:, 0:1])
                yield
                ms, bms = psf.next()
                k.mm(ms[0:96, :], ones[0:96, 0:96], sq[:], r=[b_ones, bsq], w=[bms])
                qg, bqg = qgh.next()
                k.act(qg[:], ps[0:96, :], AF.Copy, r=[bps, bg_], w=[bqg], scale=g_[:, 0:1])
                t1, bt1 = t1r.next()
                k.tt("pool", t1[64:96, :], q32[64:96, :], cs[64:96, :], ALU.mult, r=[bq32, bcs], w=[bt1])
                yield
                rt, brt = psf.next()
                k.mm(rt[0:96, :], rotT[:], qg[:], r=[b_rotT, bqg], w=[brt])
                rs, brs = rsh.next()
                k.rstd(rs[:], ms[0:96, :], 96, brs, r=[bms])
                yield
                t2, bt2 = t2r.next()
                k.tt("dve", t2[64:96, :], rt[64:96, :], sn[64:96, :], ALU.mult, r=[brt, bsn], w=[bt2])
                yield
                k.tt("pool", q32[64:96, :], t1[64:96, :], t2[64:96, :], ALU.add, r=[bt1, bt2], w=[bq32])
                yield
                qf, bqf = qfr.next()
                k.tt("dve", qf[:], q32[:], rs[:], ALU.mult, r=[bq32, brs], w=[bqf])
                k.store(dst[h, :, tok0:tok0 + 512], qf[:], r=[bqf], w=[bdst])

            def tm_gen(t, tok0=tok0, nT=nT, b_nT=b_nT):
                sg, bsg = stg.next()
                for g in range(5):
                    ps, bps = psf.next()
                    c0 = 416 + g * 512
                    for kc in range(8):
                        k.mm(ps[:], nT[:, kc, t * 128:(t + 1) * 128], win[:, kc, c0:c0 + 512],
                             start=(kc == 0), stop=(kc == 7), r=[b_nT, b_win], w=[bps])
                    yield
                    k.cp(evac_eng(), sg[:, g * 512:(g + 1) * 512], ps[:], r=[bps], w=[bsg])
                k.store(HG[tok0 + t * 128: tok0 + (t + 1) * 128, :], sg[:], r=[bsg], w=[bHG])

            gens = []
            hw = [(h, w_) for h in range(H) for w_ in range(2)]
            for t in range(4):
                gens += [head_gen(h, w_) for (h, w_) in hw[4 * t:4 * t + 4]]
                gens.append(tm_gen(t))
            run_pipelined(gens, 3)
        k.flush()
        P.drain_dmas()
        P.emit()


def moe_scratch_init(k, es):
    nc, P, S = k.nc, k.P, k.S
    CAP = moe_cap(S)
    DUMP = NE * CAP
    PSL = DUMP + 128
    k.CAP, k.PSL = CAP, PSL
    XS = k.scratch("XS", [PSL, D], BF16)
    ROUTE = k.scratch("ROUTE", [PSL, 16], I32)
    YT = k.scratch("YT", [2 * S + PSL, D], BF16)
    bXS, bROUTE, bYT = (k.scr_buf[n] for n in ("XS", "ROUTE", "YT"))
    RW = PSL // 128 * 16
    zt = es.enter_context(nc.sbuf_tensor("sb_i_zero", [128, RW], I32))
    b_zt = Buf("i_zero")
    P.add("pool", lambda e: e.iota(zt[:], pattern=[[128, PSL // 128], [0, 16]], base=2 * S, channel_multiplier=1),
          (), [b_zt])
    k.dma("sp", ROUTE.rearrange("(r p) c -> p r c", p=128), zt[:].rearrange("p (r c) -> p r c", c=16),
          r=[b_zt], w=[bROUTE])
    zx = es.enter_context(nc.sbuf_tensor("sb_i_zx", [128, 4 * D], BF16))
    b_zx = Buf("i_zx")
    P.add("pool", lambda e: e.memset(zx[:], 0.0), (), [b_zx])
    XSz = XS.rearrange("(p r) n -> p r n", p=128)
    RX = PSL // 128
    for r_ in range(0, RX, 4):
        n_ = min(4, RX - r_)
        k.dma("sp", XSz[:, r_:r_ + n_, :], zx[:, 0:n_ * D].rearrange("p (r n) -> p r n", r=n_), r=[b_zx], w=[bXS])
    YTz = YT[0:2 * S, :].rearrange("(p r) n -> p r n", p=128)
    RZ = 2 * S // 128
    for r_ in range(0, RZ, 4):
        n_ = min(4, RZ - r_)
        k.dma("sp", YTz[:, r_:r_ + n_, :], zx[:, 0:n_ * D].rearrange("p (r n) -> p r n", r=n_), r=[b_zx], w=[bYT])


def phase_attn(k, with_hgrn=False):
    nc, P, S, NT = k.nc, k.P, k.S, k.NT
    QT, KT, VV = k.scr["QT"], k.scr["KT"], k.scr["VV"]
    bQT, bKT, bVV = k.scr_buf["QT"], k.scr_buf["KT"], k.scr_buf["VV"]
    MIXT = k.scratch("MIXT", [D, S], BF16)
    bMIXT = k.scr_buf["MIXT"]
    VVv = VV.rearrange("(t p) (h c) -> p t h c", p=128, c=DV)
    RD = k.scratch("RD", [H * (S // 512), 512], F32)
    bRD = [Buf("rd%d" % i) for i in range(4)]
    with ExitStack() as es:
        def sb(name, shape, dt):
            return es.enter_context(nc.sbuf_tensor("sb_" + name, list(shape), dt)), Buf(name)
        onesf, b_onesf = sb("a_onesf", [128, 128], F32)
        k.dma("sp", onesf[:], k.din["ones_f"], w=[b_onesf])
        kth = sb_ring(es, nc, "a_kt", 2, [96, S], BF16)
        qth = sb_ring(es, nc, "a_qt", 3, [96, 512], BF16)
        vh = sb_ring(es, nc, "a_v", 2, [128, NT, DV + 1], BF16)
        for t_, b_ in zip(vh.tiles, vh.bufs):
            P.add("pool", lambda e, t_=t_: e.memset(t_[:], 1.0), (), [b_])
        ptr = sb_ring(es, nc, "a_pt", 3 if with_hgrn else 4, [128, 1024], BF16)
        ocr = sb_ring(es, nc, "a_oc", 2, [128, 512], F32)
        osb = sb_ring(es, nc, "a_o", 2, [64, 512], F32)
        aout = sb_ring(es, nc, "a_a", 2, [64, 512], BF16)
        scr_ = ps_ring(es, nc, "a_sc", 2 if with_hgrn else 3, (128, 1024), F32)
        accr = ps_ring(es, nc, "a_acc", 1 if with_hgrn else 2)
        hstep = None
        if with_hgrn:
            h_psf = ps_ring(es, nc, "h_psf", 2)
            h_psb = ps_ring(es, nc, "h_psb", 1, (128, 1024), BF16)
            hstep = hgrn_build(k, es, h_psf, h_psb)
            hsteps = hgrn_order(NT)
        NP2 = NT // 2
        NQB = S // 512
        heads = {}

        def load_head(h):
            kt_, bkt = kth.next()
            v_, bv = vh.next()
            k.load(kt_[:], KT[h], r=[bKT], w=[bkt])
            k.load(v_[:, :, 0:DV], VVv[:, :, h, :], r=[bVV], w=[bv])
            heads[h] = (kt_, bkt, v_, bv)

        qbs = {}

        def load_q(h, qb):
            qt_, bqt = qth.next()
            k.load(qt_[:], QT[h, :, qb * 512:(qb + 1) * 512], r=[bQT], w=[bqt])
            qbs[(h, qb)] = (qt_, bqt)

        steps = [(h, qb, kp) for h in range(H) for qb in range(NQB) for kp in range(NP2)]
        scs = {}

        def emit_qk(i):
            h, qb, kp = steps[i]
            if kp == 0:
                if qb == 0 and h not in heads:
                    load_head(h)
                if (h, qb) not in qbs:
                    load_q(h, qb)
                nxt = (h, qb + 1) if qb + 1 < NQB else ((h + 1, 0) if h + 1 < H else None)
                if nxt is not None:
                    if nxt[1] == 0 and nxt[0] not in heads:
                        load_head(nxt[0])
                    if nxt not in qbs:
                        load_q(*nxt)
            kt_, bkt, v_, bv = heads[h]
            qt_, bqt = qbs[(h, qb)]
            sc, bsc = scr_.next()
            for u in range(2):
                kt = 2 * kp + u
                k.mm(sc[:, u * 512:(u + 1) * 512], kt_[:, kt * 128:(kt + 1) * 128], qt_[:],
                     r=[bkt, bqt], w=[bsc])
            scs[i] = (sc, bsc)

        LOOK = 1 if with_hgrn else 2
        for i in range(min(LOOK, len(steps))):
            emit_qk(i)
        if "XS" not in k.scr:
            moe_scratch_init(k, es)
        acc = bacc = None
        gen = None
        for i, (h, qb, kp) in enumerate(steps):
            kt_, bkt, v_, bv = heads[h]
            if kp == 0:
                acc, bacc = accr.next()
                if hstep is not None and h * NQB + qb < len(hsteps):
                    gen = hstep(*hsteps[h * NQB + qb])
            sc, bsc = scs.pop(i)
            pt, bpt = ptr.next()
            k.act(pt[:], sc[:], AF.Exp, r=[bsc], w=[bpt])
            if i + LOOK < len(steps):
                emit_qk(i + LOOK)
            for u in range(2):
                kt = 2 * kp + u
                k.mm(acc[0:DV + 1, :], v_[:, kt, :], pt[:, u * 512:(u + 1) * 512],
                     start=(kt == 0), stop=(kt == NT - 1), r=[bv, bpt], w=[bacc])
            if gen is not None:
                if next(gen, "done") == "done":
                    gen = None
            if kp == NP2 - 1:
                if gen is not None:
                    for _ in gen:
                        pass
                    gen = None
                oc, boc = ocr.next()
                k.cp("dve", oc[0:DV + 1, :], acc[0:DV + 1, :], r=[bacc], w=[boc])
                P.add("dve", lambda e, oc=oc: e.reciprocal(oc[64:65, :], oc[64:65, :]), [boc], [boc])
                o_, bo = osb.next()
                ridx = h * NQB + qb
                k.dma("sp", RD[ridx:ridx + 1, :], oc[64:65, :], r=[boc], w=[bRD[ridx % 4]])
                k.dma("sp", o_[:], RD[ridx:ridx + 1, :].partition_broadcast(64), r=[bRD[ridx % 4]], w=[bo])
                a_, ba = aout.next()
                k.tt("dve", a_[:], oc[0:64, :], o_[:], ALU.mult, r=[boc, bo], w=[ba])
                k.store(MIXT[h * 64:(h + 1) * 64, qb * 512:(qb + 1) * 512], a_[:], r=[ba], w=[bMIXT])
        k.flush()
        P.drain_dmas()
        P.emit()


def hgrn_build(k, es, psf, psb):
    nc, P, S, NT = k.nc, k.P, k.S, k.NT
    HG, MIXT = k.scr["HG"], k.scr["MIXT"]
    bHG, bMIXT = k.scr_buf["HG"], k.scr_buf["MIXT"]
    OF = k.scratch("OF", [S, 512], F32)
    bOF = k.scr_buf["OF"]
    MIXr = MIXT[512:1024, :].rearrange("(c p) t -> p c t", p=128)
    def sb(name, shape, dt):
        return es.enter_context(nc.sbuf_tensor("sb_" + name, list(shape), dt)), Buf(name)
    identb, b_identb = sb("h_ident", [128, 128], BF16)
    k.dma("sp", identb[:], k.din["ident_bf"], w=[b_identb])
    cst = {}
    for nm, shp, dt in (("hg_Lc_f", [128, 128], F32), ("hg_Lr_f", [128, 128], F32), ("hg_Lc_b", [128, 128], F32),
                        ("hg_Lr_b", [128, 128], F32), ("hg_selm_f", [128, 2], F32), ("hg_selm_b", [128, 2], F32),
                        ("hg_mask_f", [128, 512], U32), ("hg_mask_b", [128, 512], U32)):
        t_, b_ = sb(nm, shp, dt)
        k.dma("sp", t_[:], k.din[nm], w=[b_])
        cst[nm] = (t_, b_)
    l0, b_l0 = sb("h_l0", [128, 1024], F32)
    l1, b_l1 = sb("h_l1", [128, 1024], F32)
    k.dma("sp", l0[:], k.din["lb_logits_l"][0:1, :].partition_broadcast(128), w=[b_l0])
    k.dma("sp", l1[:], k.din["lb_logits_l"][1:2, :].partition_broadcast(128), w=[b_l1])
    lbb, b_lbb = sb("h_lb", [128, 1024], F32)
    oml, b_oml = sb("h_oml", [128, 1024], F32)
    k.tt("dve", l1[:], l1[:], l0[:], ALU.subtract, r=[b_l0, b_l1], w=[b_l1])
    k.act(l1[:], l1[:], AF.Exp, r=[b_l1], w=[b_l1])
    k.ts("dve", l1[:], l1[:], 1.0, None, ALU.add, r=[b_l1], w=[b_l1])
    P.add("dve", lambda e: e.reciprocal(lbb[:], l1[:]), [b_l1], [b_lbb])
    k.ts("dve", oml[:], lbb[:], -1.0, 1.0, ALU.mult, ALU.add, r=[b_lbb], w=[b_oml])
    gn, b_gn = sb("h_gn", [128, 4, 128], F32)
    for hh in range(4):
        k.dma("sp", gn[:, hh, :], k.din["hg_out_norm_l"][0:1, :].partition_broadcast(128), w=[b_gn])
    k.ts("dve", gn[:], gn[:], float(np.sqrt(128.0)), None, ALU.mult, r=[b_gn], w=[b_gn])
    eps128, b_eps = sb("h_eps", [128, 1], F32)
    P.add("pool", lambda e: e.memset(eps128[:], float(128 * EPS)), (), [b_eps])
    one1, b_one = sb("h_one", [128, 1], F32)
    P.add("pool", lambda e: e.memset(one1[:], 1.0), (), [b_one])

    hgr = sb_ring(es, nc, "h_in", 2, [128, 2560], F32)
    e1r = sb_ring(es, nc, "h_e1", 2, [128, 512], F32)
    e2r = sb_ring(es, nc, "h_e2", 2, [128, 512], F32)
    e3r = sb_ring(es, nc, "h_e3", 2, [128, 512], F32)
    fr = sb_ring(es, nc, "h_f", 2, [128, 512], F32)
    kkr = sb_ring(es, nc, "h_kk", 2, [128, 512], F32)
    gr = sb_ring(es, nc, "h_g", 2, [128, 512], F32)
    qr = sb_ring(es, nc, "h_q", 2, [128, 512], F32)
    vr = sb_ring(es, nc, "h_v", 2, [128, 512], BF16)
    ecr = sb_ring(es, nc, "h_ec", 2, [128, 512], F32)
    encr = sb_ring(es, nc, "h_enc", 2, [128, 512], F32)
    err_ = sb_ring(es, nc, "h_er", 2, [128, 512], F32)
    eblr = sb_ring(es, nc, "h_ebl", 2, [128, 8], F32)
    qkbr = sb_ring(es, nc, "h_qkb", 2, [128, 1024], BF16)
    kdr = sb_ring(es, nc, "h_kd", 2, [128, 512], BF16)
    qkTr = sb_ring(es, nc, "h_qkT", 2, [128, 1024], BF16)
    atr = sb_ring(es, nc, "h_at", 2, [128, 512], BF16)
    sbr = sb_ring(es, nc, "h_sb", 2, [128, 512], BF16)
    Sst, b_S = sb("h_S", [128, 512], F32)
    osr = sb_ring(es, nc, "h_os", 2, [128, 512], F32)
    ofr = sb_ring(es, nc, "h_of", 2, [128, 512], F32)
    junkr = sb_ring(es, nc, "h_junk", 4, [128, 128], BF16)
    ssq = sb_ring(es, nc, "h_ssq", 2, [128, 8], F32)
    rbr = sb_ring(es, nc, "h_rb", 2, [128, 512], BF16)
    rTr = sb_ring(es, nc, "h_rT", 2, [128, 512], BF16)

    def silu_from(dst, src, rd, wr, tmp, btmp):
        k.act(tmp[:], src, AF.Exp, r=rd, w=[btmp], scale=-1.0)
        k.ts("dve", tmp[:], tmp[:], 1.0, None, ALU.add, r=[btmp], w=[btmp])
        P.add("dve", lambda e: e.reciprocal(tmp[:], tmp[:]), [btmp], [btmp])
        k.tt("dve", dst, tmp[:], src, ALU.mult, r=[btmp] + list(rd), w=wr)

    dirs = []
    for d_ in range(2):
        sfx = "_f" if d_ == 0 else "_b"
        dirs.append((cst["hg_Lc" + sfx], cst["hg_Lr" + sfx], cst["hg_selm" + sfx], cst["hg_mask" + sfx]))

    prog_state = {"s_done": 0, "started": 0}

    def step(d, ti, first):
        (Lc, b_Lc), (Lr, b_Lr), (selm, b_selm), (mask, b_mask) = dirs[d]
        my = prog_state["started"]
        prog_state["started"] += 1
        if first:
            while prog_state["s_done"] < my:
                yield
            P.add("pool", lambda e: e.memset(Sst[:], 0.0), (), [b_S])
            for t_, b_ in zip(atr.tiles, atr.bufs):
                P.add("pool", lambda e, t_=t_: e.memset(t_[:], 0.0), (), [b_])
        r0 = ti * 128
        hg_, bhg = hgr.next()
        k.load(hg_[:], HG[r0:r0 + 128, :], r=[bHG], w=[bhg])
        if d == 1:
            of_, bof = ofr.next()
            k.load(of_[:], OF[r0:r0 + 128, :], r=[bOF], w=[bof])
        z = hg_[:, 512 + d * 512: 1024 + d * 512]
        yield
        e1, be1 = e1r.next()
        e2, be2 = e2r.next()
        k.act(e1[:], z, AF.Sigmoid, r=[bhg], w=[be1])
        k.act(e2[:], hg_[:, 0:512], AF.Sigmoid, r=[bhg], w=[be2])
        if d == 1:
            e3, be3 = e3r.next()
            k.act(e3[:], hg_[:, 2048:2560], AF.Sigmoid, r=[bhg], w=[be3])
        v_, bv = vr.next()
        k.act(v_[:], hg_[:, 1536:2048], AF.Copy, r=[bhg], w=[bv], scale=-1.0)
        yield
        f_, bf_ = fr.next()
        k.tt("dve", f_[:], e1[:], oml[:, d * 512:(d + 1) * 512], ALU.mult, r=[be1, b_oml], w=[bf_])
        k.tt("dve", f_[:], f_[:], lbb[:, d * 512:(d + 1) * 512], ALU.add, r=[bf_, b_lbb], w=[bf_])
        q_, bq = qr.next()
        k.tt("dve", q_[:], e2[:], hg_[:, 0:512], ALU.mult, r=[be2, bhg], w=[bq])
        yield
        g_, bg = gr.next()
        k.act(g_[:], f_[:], AF.Ln, r=[bf_], w=[bg])
        yield
        cps, bcps = psf.next()
        k.mm(cps[:], Lc[:], g_[:], r=[b_Lc, bg], w=[bcps])
        rps, brps = psf.next()
        k.mm(rps[:], Lr[:], g_[:], r=[b_Lr, bg], w=[brps])
        yield
        ec, bec = ecr.next()
        enc, benc = encr.next()
        er, ber = err_.next()
        k.act(ec[:], cps[:], AF.Exp, r=[bcps], w=[bec])
        k.act(enc[:], cps[:], AF.Exp, r=[bcps], w=[benc], scale=-1.0)
        k.act(er[:], rps[:], AF.Exp, r=[brps], w=[ber])
        yield
        blm, bblm = psf.next()
        for hh in range(4):
            k.mm(blm[:, 2 * hh:2 * hh + 2], g_[:, hh * 128:(hh + 1) * 128], selm[:], r=[bg, b_selm], w=[bblm])
        qkb, bqkb = qkbr.next()
        kd, bkd = kdr.next()
        k.tt("dve", qkb[:, 0:512], q_[:], ec[:], ALU.mult, r=[bq, bec], w=[bqkb])
        P.add("dve", lambda e, qkb=qkb, f_=f_, enc=enc: e.scalar_tensor_tensor(
            qkb[:, 512:1024], f_[:], 1.0, enc[:], ALU.subtract, ALU.mult), [bf_, benc], [bqkb])
        P.add("dve", lambda e, kd=kd, f_=f_, er=er: e.scalar_tensor_tensor(
            kd[:], f_[:], 1.0, er[:], ALU.subtract, ALU.mult), [bf_, ber], [bkd])
        yield
        ebl, bebl = eblr.next()
        k.act(ebl[:], blm[:, 0:8], AF.Exp, r=[bblm], w=[bebl])
        yield
        pb, bpb = psb.next()
        for c8 in range(8):
            k.tr(pb[:, c8 * 128:(c8 + 1) * 128], qkb[:, c8 * 128:(c8 + 1) * 128], identb[:],
                 r=[bqkb, b_identb], w=[bpb])
        while prog_state["s_done"] < my:
            yield
        sb_, bsb = sbr.next()
        for hh in range(4):
            k.ts("dve", sb_[:, hh * 128:(hh + 1) * 128], Sst[:, hh * 128:(hh + 1) * 128],
                 ebl[:, 2 * hh + 1:2 * hh + 2], None, ALU.mult, r=[b_S, bebl], w=[bsb])
        yield
        qkT, bqkT = qkTr.next()
        k.cp("dve", qkT[:], pb[:], r=[bpb], w=[bqkT])
        yield
        atp, batp = psf.next()
        for hh in range(4):
            k.mm(atp[:, hh * 128:(hh + 1) * 128], qkT[:, 512 + hh * 128: 512 + (hh + 1) * 128],
                 qkT[:, hh * 128:(hh + 1) * 128], r=[bqkT], w=[batp])
        snp, bsnp = psf.next()
        for hh in range(4):
            sl = slice(hh * 128, (hh + 1) * 128)
            k.mm(snp[:, sl], kd[:, sl], v_[:, sl], r=[bkd, bv], w=[bsnp])
        yield
        at, bat = atr.next()
        P.add("dve", lambda e, at=at, atp=atp, mask=mask: e.copy_predicated(at[:], mask[:], atp[:]),
              [batp, b_mask], [bat])
        yield
        ops, bops = psf.next()
        for hh in range(4):
            sl = slice(hh * 128, (hh + 1) * 128)
            k.mm(ops[:, sl], at[:, sl], v_[:, sl], start=True, stop=False, r=[bat, bv], w=[bops])
            k.mm(ops[:, sl], qkT[:, sl], sb_[:, sl], start=False, stop=True, r=[bqkT, bsb], w=[bops])
        for hh in range(4):
            sl = slice(hh * 128, (hh + 1) * 128)
            P.add("dve", lambda e, sl=sl, ebl=ebl, snp=snp, hh=hh: e.scalar_tensor_tensor(
                Sst[:, sl], Sst[:, sl], ebl[:, 2 * hh:2 * hh + 1], snp[:, sl], ALU.mult, ALU.add),
                [b_S, bebl, bsnp], [b_S])
        prog_state["s_done"] = my + 1
        yield
        os_, bos = osr.next()
        if d == 0:
            k.cp("dve", os_[:], ops[:], r=[bops], w=[bos])
            k.store(OF[r0:r0 + 128, :], os_[:], r=[bos], w=[bOF])
            return
        k.tt("dve", os_[:], ops[:], of_[:], ALU.add, r=[bops, bof], w=[bos])
        k.tt("dve", e3[:], e3[:], hg_[:, 2048:2560], ALU.mult, r=[be3, bhg], w=[be3])
        yield
        sq, bsq = ssq.next()
        for hh in range(4):
            junk, b_junk = junkr.next()
            k.act(junk[:], os_[:, hh * 128:(hh + 1) * 128], AF.Square, r=[bos], w=[b_junk, bsq],
                  accum_out=sq[:, hh:hh + 1])
        k.act(sq[:, 4:8], sq[:, 0:4], AF.Ln, r=[bsq, b_eps], w=[bsq], bias=eps128[:, 0:1])
        k.act(sq[:, 4:8], sq[:, 4:8], AF.Exp, r=[bsq], w=[bsq], scale=-0.5)
        yield
        for hh in range(4):
            k.ts("dve", os_[:, hh * 128:(hh + 1) * 128], os_[:, hh * 128:(hh + 1) * 128], sq[:, 4 + hh:5 + hh],
                 None, ALU.mult, r=[bos, bsq], w=[bos])
        k.tt("dve", os_[:], os_[:], gn[:].rearrange("p h c -> p (h c)"), ALU.mult, r=[bos, b_gn], w=[bos])
        rb, brb = rbr.next()
        k.tt("dve", rb[:], os_[:], e3[:], ALU.mult, r=[bos, be3], w=[brb])
        yield
        pb, bpb = psb.next()
        for hh in range(4):
            k.tr(pb[:, hh * 128:(hh + 1) * 128], rb[:, hh * 128:(hh + 1) * 128], identb[:],
                 r=[brb, b_identb], w=[bpb])
        yield
        rT, brT = rTr.next()
        k.cp("dve", rT[:], pb[:, 0:512], r=[bpb], w=[brT])
        k.store(MIXr[:, :, r0:r0 + 128], rT[:].rearrange("p (c t) -> p c t", c=4), r=[brT], w=[bMIXT])
    return step


def hgrn_order(NT):
    return [(0, ti, ti == 0) for ti in range(NT)] + [(1, ti, ti == NT - 1) for ti in range(NT - 1, -1, -1)]


def phase_hgrn(k):
    nc, P = k.nc, k.P
    with ExitStack() as es:
        psf = ps_ring(es, nc, "h_psf", 6)
        psb = ps_ring(es, nc, "h_psb", 2, (128, 1024), BF16)
        step = hgrn_build(k, es, psf, psb)
        order = hgrn_order(k.NT)
        for d in range(2):
            run_pipelined((step(*o) for o in order if o[0] == d), 2)
        k.flush()
        P.drain_dmas()
        P.emit()


def phase_route(k):
    nc, P, S, NT = k.nc, k.P, k.S, k.NT
    CAP = moe_cap(S)
    DUMP = NE * CAP
    PSL = DUMP + 128
    k.CAP, k.PSL = CAP, PSL
    MIXT, bMIXT = k.scr["MIXT"], k.scr_buf["MIXT"]
    H1 = k.scratch("H1", [S, D], F32)
    H2N = k.scratch("H2N", [S, D], BF16)
    XS, ROUTE = k.scr["XS"], k.scr["ROUTE"]
    GATE = k.scratch("GATE", [128, NT * 2], F32)
    bH1, bH2N, bXS, bROUTE, bGATE = (k.scr_buf[n] for n in ("H1", "H2N", "XS", "ROUTE", "GATE"))
    MIXv = MIXT.rearrange("(c p) t -> p c t", p=128)
    with ExitStack() as es:
        def sb(name, shape, dt):
            return es.enter_context(nc.sbuf_tensor("sb_" + name, list(shape), dt)), Buf(name)
        identf, b_identf = sb("r_identf", [128, 128], F32)
        onesf, b_onesf = sb("r_onesf", [128, 128], F32)
        ustr, b_ustr = sb("r_ustr", [128, 128], F32)
        id32, b_id32 = sb("r_id32", [32, 32], F32)
        ecap, b_ecap = sb("r_ecap", [32, 2], F32)
        limr, b_limr = sb("r_limr", [128, 32], F32)
        piota, b_piota = sb("r_piota", [128, 1], F32)
        toki, b_toki = sb("r_toki", [128, NT], I32)
        for t_, b_, nm in ((identf, b_identf, "ident_f"), (onesf, b_onesf, "ones_f"), (ustr, b_ustr, "ustrict"),
                           (id32, b_id32, "ident32"), (ecap, b_ecap, "blk_iota"), (limr, b_limr, "lim_row"),
                           (piota, b_piota, "p_iota"), (toki, b_toki, "tok_iota")):
            k.dma("sp", t_[:], k.din[nm], w=[b_])
        gff, b_gff = sb("r_gff", [128, D], F32)
        k.dma("sp", gff[:], k.din["norm_ffn_l"][0:1, :].partition_broadcast(128), w=[b_gff])
        k.ts("dve", gff[:], gff[:], 32.0, None, ALU.mult, r=[b_gff], w=[b_gff])
        brt, b_brt = sb("r_brt", [128, 36], F32)
        k.dma("sp", brt[:], k.din["b_rt"][0:1, :].partition_broadcast(128), w=[b_brt])
        wrt, b_wrt = sb("r_wrt", [128, 8, 36], F32)
        k.dma("sp", wrt[:], k.din["w_rt"].rearrange("(c p) n -> p c n", p=128), w=[b_wrt])
        eps1k, b_eps = sb("r_eps", [128, 1], F32)
        P.add("pool", lambda e: e.memset(eps1k[:], float(D * EPS)), (), [b_eps])
        wout, b_wout = sb("r_wout", [128, 8, D], BF16)
        wst = sb_ring(es, nc, "r_wst", 2, [128, D], F32)
        w_out_v = k.din["w_out"].rearrange("(c p) n -> p c n", p=128)
        for c in range(8):
            st, bst = wst.next()
            k.dma("sp", st[:], w_out_v[:, c, :], w=[bst])
            k.cp("dve" if c % 2 == 0 else "pool", wout[:, c, :], st[:], r=[bst], w=[b_wout])
        dcol, b_dcol = sb("r_dcol", [128, 2], F32)
        k.ts("dve", dcol[:, 0:1], piota[:, 0:1], float(2 * S), None, ALU.add, r=[b_piota], w=[b_dcol])
        k.ts("dve", dcol[:, 1:2], piota[:, 0:1], float(DUMP), None, ALU.add, r=[b_piota], w=[b_dcol])
        tokst, b_tokst = sb("r_tokst", [128, NT, 2, 16], I32)
        P.add("pool", lambda e: e.memset(tokst[:], 0), (), [b_tokst])
        k.ts("dve", tokst[:, :, 0, 0], toki[:], 1, None, ALU.logical_shift_left, r=[b_toki, b_tokst], w=[b_tokst])
        k.ts("dve", tokst[:, :, 1, 0], tokst[:, :, 0, 0], 1, None, ALU.add, r=[b_tokst], w=[b_tokst])

        k.fence([bXS, bROUTE])
        Aall, b_A = sb("r_A", [128, NT, 32], F32)
        M12, b_M = sb("r_M12", [128, NT, 2, 32], F32)
        gates, b_gates = sb("r_gates", [128, NT, 2], F32)
        dest, b_dest = sb("r_dest", [128, NT, 2], F32)
        desti, b_desti = sb("r_desti", [128, NT, 2], I32)

        mixr = sb_free(es, nc, "r_mix", 3, [128, 8, 128], BF16)
        xr = sb_free(es, nc, "r_x", 3, [128, D], F32)
        h1r = sb_free(es, nc, "r_h1", 3, [128, D], F32)
        junkr = sb_free(es, nc, "r_junk", 2, [128, D], BF16)
        h2r = sb_free(es, nc, "r_h2", 3, [128, D], F32)
        h2bf = sb_free(es, nc, "r_h2bf", 3, [128, D], BF16)
        h2br = sb_ring(es, nc, "r_h2b", 3, [128, D], BF16)
        h2Tr = sb_free(es, nc, "r_h2T", 2, [128, 8, 128], F32)
        smr = sb_free(es, nc, "r_sm", 4, [128, 16], F32)
        lgr = sb_free(es, nc, "r_lg", 4, [128, 36], F32)
        emr = sb_free(es, nc, "r_em", 4, [128, 32], F32)
        t8r = sb_free(es, nc, "r_t8", 4, [128, 8], F32)
        g4r = sb_free(es, nc, "r_g4", 4, [128, 12], F32)
        psf = ps_free(es, nc, "r_psf", 8)

        def tile_gen(ti):
            r0 = ti * 128
            mx, bmx, imx = yield from mixr.get()
            k.load(mx[:], MIXv[:, :, r0:r0 + 128], r=[bMIXT], w=[bmx])
            xt, bx, ix = yield from xr.get()
            k.load(xt[:], k.x[r0:r0 + 128, :], w=[bx])
            yield
            h1, bh1, ih1 = yield from h1r.get()
            pss = []
            for half in range(2):
                ps, bps, ips = yield from psf.get()
                for c in range(8):
                    k.mm(ps[:], mx[:, c, :], wout[:, c, half * 512:(half + 1) * 512], start=(c == 0), stop=(c == 7),
                         r=[bmx, b_wout], w=[bps])
                pss.append((ps, bps, ips))
            mixr.put(imx)
            yield
            for half, (ps, bps, ips) in enumerate(pss):
                k.tt("dve", h1[:, half * 512:(half + 1) * 512], ps[:], xt[:, half * 512:(half + 1) * 512], ALU.add,
                     r=[bps, bx], w=[bh1])
                psf.put(ips)
            xr.put(ix)
            k.store(H1[r0:r0 + 128, :], h1[:], r=[bh1], w=[bH1])
            yield
            sm, bsm, ism = yield from smr.get()
            junk, bjunk, ijunk = yield from junkr.get()
            k.act(junk[:], h1[:], AF.Square, r=[bh1], w=[bjunk, bsm], accum_out=sm[:, 0:1])
            junkr.put(ijunk)
            k.act(sm[:, 1:2], sm[:, 0:1], AF.Ln, r=[bsm, b_eps], w=[bsm], bias=eps1k[:, 0:1])
            k.act(sm[:, 2:3], sm[:, 1:2], AF.Exp, r=[bsm], w=[bsm], scale=-0.5)
            yield
            h2, bh2, ih2 = yield from h2r.get()
            k.ts("dve", h2[:], h1[:], sm[:, 2:3], None, ALU.mult, r=[bh1, bsm], w=[bh2])
            yield
            k.tt("pool", h2[:], h2[:], gff[:], ALU.mult, r=[bh2, b_gff], w=[bh2])
            yield
            h2b, bh2b, ih2b = yield from h2bf.get()
            k.cp("act", h2b[:], h2[:], r=[bh2], w=[bh2b])
            k.store(H2N[r0:r0 + 128, :], h2b[:], r=[bh2b], w=[bH2N])
            h2T, bh2T, ih2T = yield from h2Tr.get()
            pss = []
            for half in range(2):
                ps, bps, ips = yield from psf.get()
                for c4 in range(4):
                    c = half * 4 + c4
                    k.tr(ps[:, c4 * 128:(c4 + 1) * 128], h2[:, c * 128:(c + 1) * 128], identf[:],
                         r=[bh2, b_identf], w=[bps])
                pss.append((ps, bps, ips))
            h2r.put(ih2)
            yield
            for half, (ps, bps, ips) in enumerate(pss):
                k.cp("dve" if half == 0 else "act", h2T[:, half * 4:(half + 1) * 4, :].rearrange("p c t -> p (c t)"), ps[:],
                     r=[bps], w=[bh2T])
                psf.put(ips)
            yield
            lps, blps, ilps = yield from psf.get()
            for c in range(8):
                k.mm(lps[:, 0:36], h2T[:, c, :], wrt[:, c, :], start=(c == 0), stop=(c == 7), r=[bh2T, b_wrt], w=[blps])
            h2Tr.put(ih2T)
            yield
            lg, blg, ilg = yield from lgr.get()
            k.tt("dve", lg[:], lps[:, 0:36], brt[:], ALU.add, r=[blps, b_brt], w=[blg])
            psf.put(ilps)
            g4, bg4, ig4 = yield from g4r.get()
            P.add("dve", lambda e, g4=g4, lg=lg: e.reduce_max(g4[:, 0:1], lg[:, 0:4], AX.X), [blg], [bg4])
            k.ts("dve", g4[:, 1:2], g4[:, 0:1], -1.0, None, ALU.mult, r=[bg4], w=[bg4])
            yield
            k.act(sm[:, 4:8], lg[:, 0:4], AF.Exp, r=[blg, bg4], w=[bsm, bg4], bias=g4[:, 1:2], accum_out=g4[:, 2:3])
            k.ts("dve", g4[:, 4:8], lg[:, 0:4], g4[:, 0:1], None, ALU.is_equal, r=[blg, bg4], w=[bg4])
            k.ts("dve", g4[:, 8:12], g4[:, 4:8], 1.0e30, -1.0e30, ALU.mult, ALU.add, r=[bg4], w=[bg4])
            em, bem, iem = yield from emr.get()
            for g in range(NG):
                k.ts("dve", em[:, g * 8:(g + 1) * 8], lg[:, 4 + g * 8: 12 + g * 8], g4[:, 4 + g:5 + g], g4[:, 8 + g:9 + g],
                     ALU.mult, ALU.add, r=[blg, bg4], w=[bem])
            lgr.put(ilg)
            yield
            P.add("dve", lambda e, g4=g4: e.reciprocal(g4[:, 3:4], g4[:, 2:3]), [bg4], [bg4])
            t8, bt8, it8 = yield from t8r.get()
            P.add("dve", lambda e, t8=t8, em=em: e.max(t8[:], em[:]), [bem], [bt8])
            yield
            k.ts("dve", M12[:, ti, 0, :], em[:], t8[:, 0:1], None, ALU.is_equal, r=[bem, bt8], w=[b_M])
            k.ts("dve", M12[:, ti, 1, :], em[:], t8[:, 1:2], None, ALU.is_equal, r=[bem, bt8], w=[b_M])
            emr.put(iem)
            k.tt("dve", sm[:, 8:9], t8[:, 1:2], t8[:, 0:1], ALU.subtract, r=[bt8], w=[bsm])
            t8r.put(it8)
            yield
            k.tt("dve", Aall[:, ti, :], M12[:, ti, 0, :], M12[:, ti, 1, :], ALU.add, r=[b_M], w=[b_A])
            k.act(sm[:, 9:10], sm[:, 8:9], AF.Exp, r=[bsm], w=[bsm])
            yield
            k.ts("dve", sm[:, 9:10], sm[:, 9:10], 1.0, None, ALU.add, r=[bsm], w=[bsm])
            yield
            P.add("dve", lambda e, sm=sm: e.reciprocal(sm[:, 10:11], sm[:, 9:10]), [bsm], [bsm])
            yield
            k.tt("dve", gates[:, ti, 0:1], sm[:, 10:11], g4[:, 3:4], ALU.mult, r=[bsm, bg4], w=[b_gates])
            yield
            k.tt("dve", gates[:, ti, 1:2], g4[:, 3:4], gates[:, ti, 0:1], ALU.subtract, r=[bg4, b_gates], w=[b_gates])
            smr.put(ism)
            g4r.put(ig4)
            h1r.put(ih1)
            h2bf.put(ih2b)

        run_pipelined([tile_gen(ti) for ti in range(NT)], 3)

        cps, bcps, icps = psf.take()
        for ti in range(NT):
            k.mm(cps[0:32, ti:ti + 1], Aall[:, ti, :], onesf[:, 0:1], r=[b_A, b_onesf], w=[bcps])
        cnt, b_cnt = sb("r_cnt", [32, NT], F32)
        k.cp("dve", cnt[:], cps[0:32, 0:NT], r=[bcps], w=[b_cnt])
        inc, b_inc = sb("r_inc", [32, NT], F32)
        onesr, b_onesr = sb("r_onesr", [32, NT], F32)
        P.add("pool", lambda e: e.memset(onesr[:], 1.0), (), [b_onesr])
        P.add("dve", lambda e: e.tensor_tensor_scan(inc[:], onesr[:], cnt[:], 0.0, ALU.mult, ALU.add),
              [b_onesr, b_cnt], [b_inc])
        off, b_off = sb("r_off", [32, NT], F32)
        k.tt("dve", off[:], inc[:], cnt[:], ALU.subtract, r=[b_inc, b_cnt], w=[b_off])
        k.ts("dve", off[:], off[:], ecap[:, 0:1], None, ALU.add, r=[b_off, b_ecap], w=[b_off])
        dgr = sb_free(es, nc, "r_dg", 3, [32, 32], F32)
        tmr = sb_free(es, nc, "r_tm", 3, [128, 4, 32], F32)
        okr = sb_free(es, nc, "r_ok", 3, [128, 3, 32], F32)
        gkr = sb_free(es, nc, "r_gk", 3, [128, 2], F32)

        def slot_gen(ti):
            h2b, bh2b, ih2b = yield from h2bf.get()
            k.load(h2b[:], H2N[ti * 128:(ti + 1) * 128, :], r=[bH2N], w=[bh2b])
            dg, bdg, idg = yield from dgr.get()
            k.ts("dve", dg[:], id32[:], off[:, ti:ti + 1], None, ALU.mult, r=[b_id32, b_off], w=[bdg])
            yield
            sps, bsps, isps = yield from psf.get()
            k.mm(sps[:, 0:32], ustr[:], Aall[:, ti, :], start=True, stop=False, r=[b_ustr, b_A], w=[bsps])
            k.mm(sps[:, 0:32], onesf[0:32, :], dg[:], start=False, stop=True, r=[b_onesf, bdg], w=[bsps])
            dgr.put(idg)
            yield
            ok, bok, iok = yield from okr.get()
            k.tt("dve", ok[:, 0, :], sps[:, 0:32], limr[:], ALU.is_lt, r=[bsps, b_limr], w=[bok])
            yield
            k.tt("dve", ok[:, 1, :], sps[:, 0:32], ok[:, 0, :], ALU.mult, r=[bsps, bok], w=[bok])
            psf.put(isps)
            k.ts("dve", ok[:, 2, :], ok[:, 0, :], -1.0, 1.0, ALU.mult, ALU.add, r=[bok], w=[bok])
            yield
            k.ts("dve", ok[:, 2, :], ok[:, 2, :], dcol[:, 1:2], None, ALU.mult, r=[bok, b_dcol], w=[bok])
            yield
            k.tt("dve", ok[:, 1, :], ok[:, 1, :], ok[:, 2, :], ALU.add, r=[bok], w=[bok])
            yield
            tm, btm, itm = yield from tmr.get()
            for j in range(2):
                k.tt("dve", tm[:, j, :], M12[:, ti, j, :], ok[:, 1, :], ALU.mult, r=[b_M, bok], w=[btm])
                k.tt("dve", tm[:, 2 + j, :], M12[:, ti, j, :], ok[:, 0, :], ALU.mult, r=[b_M, bok], w=[btm])
            okr.put(iok)
            yield
            P.add("dve", lambda e, tm=tm, ti=ti: e.reduce_sum(dest[:, ti, :], tm[:, 0:2, :], AX.X), [btm], [b_dest])
            gk, bgk, igk = yield from gkr.get()
            P.add("dve", lambda e, tm=tm, gk=gk: e.reduce_sum(gk[:], tm[:, 2:4, :], AX.X), [btm], [bgk])
            tmr.put(itm)
            yield
            k.tt("dve", gates[:, ti, :], gates[:, ti, :], gk[:], ALU.mult, r=[b_gates, bgk], w=[b_gates])
            gkr.put(igk)
            k.cp("dve", desti[:, ti, :], dest[:, ti, :], r=[b_dest], w=[b_desti])
            yield
            for j in range(2):
                P.add("pool", lambda e, ti=ti, j=j: e.indirect_dma_start(
                    out=ROUTE, out_offset=bass.IndirectOffsetOnAxis(ap=desti[:, ti, j:j + 1], axis=0),
                    in_=tokst[:, ti, j, :], in_offset=None), [b_desti, b_tokst], [bROUTE], dma=True)
                P.add("pool", lambda e, ti=ti, j=j, h2b=h2b: e.indirect_dma_start(
                    out=XS, out_offset=bass.IndirectOffsetOnAxis(ap=desti[:, ti, j:j + 1], axis=0),
                    in_=h2b[:], in_offset=None), [b_desti, bh2b], [bXS], dma=True)
                yield
            h2bf.put(ih2b)

        run_pipelined([slot_gen(ti) for ti in range(NT)], 3)
        k.dma("sp", GATE, gates[:].rearrange("p t j -> p (t j)"), r=[b_gates], w=[bGATE])
        k.flush()
        P.drain_dmas()
        P.emit()


def phase_moe(k):
    nc, P, S, NT = k.nc, k.P, k.S, k.NT
    B, CAP, PSL = MOE_B, k.CAP, k.PSL
    NS = B // 128
    H1, XS, ROUTE, GATE = (k.scr[n] for n in ("H1", "XS", "ROUTE", "GATE"))
    bH1, bXS, bROUTE, bGATE = (k.scr_buf[n] for n in ("H1", "XS", "ROUTE", "GATE"))
    YT = k.scr["YT"]
    bYT = k.scr_buf["YT"]
    WG = k.din["w_gate"].rearrange("e (p c) n -> e p (c n)", p=128)
    WU = k.din["w_up"].rearrange("e (p c) n -> e p (c n)", p=128)
    WD = k.din["w_down"].rearrange("e (p c) n -> e p (c n)", p=128)
    with ExitStack() as es:
        def sb(name, shape, dt):
            return es.enter_context(nc.sbuf_tensor("sb_" + name, list(shape), dt)), Buf(name)
        identb, b_identb = sb("m_ident", [128, 128], BF16)
        k.dma("sp", identb[:], k.din["ident_bf"], w=[b_identb])
        k.fence([bYT])
        tkr = sb_free(es, nc, "m_tk", 6, [128, 16], I32)
        xgr = sb_free(es, nc, "m_xg", 6, [128, D], BF16)
        wfr = sb_free(es, nc, "m_wf", 3, [128, 2048], F32)
        wgr = sb_free(es, nc, "m_wg", 2, [128, 8, DE], BF16)
        wur = sb_free(es, nc, "m_wu", 2, [128, 8, DE], BF16)
        wdr = sb_free(es, nc, "m_wd", 2, [128, 2, D], BF16)
        xTr = sb_free(es, nc, "m_xT", 3, [128, 8, B], BF16)
        sgr = sb_free(es, nc, "m_sg", 3, [128, B], F32)
        hTr = sb_free(es, nc, "m_hT", 3, [128, 2, B], BF16)
        ysr = sb_free(es, nc, "m_ys", 6, [128, D], BF16)
        psf = ps_free(es, nc, "m_psf", 6)
        psb = ps_free(es, nc, "m_psb", 2, (128, 1024), BF16)
        ceng = ["dve", "pool", "act"]
        wtiles = {}

        def wprep_gen(ex):
            wts = []
            for wi_, (src, pool_) in enumerate(((WG, wgr), (WU, wur), (WD, wdr))):
                wf, bwf, iwf = yield from wfr.get()
                k.load(wf[:], src[ex], w=[bwf])
                wb, bwb, iwb = yield from pool_.get()
                wts.append((wf, bwf, iwf, wb, bwb, iwb, pool_))
            wtiles[ex] = [(wb, bwb, iwb, pool_) for (_, _, _, wb, bwb, iwb, pool_) in wts]
            wtiles[(ex, "left")] = CAP // B
            yield
            for wi_, (wf, bwf, iwf, wb, bwb, iwb, pool_) in enumerate(wts):
                k.cp(ceng[wi_], wb[:].rearrange("p c n -> p (c n)"), wf[:], r=[bwf], w=[bwb])
                wfr.put(iwf)
                yield

        def blk_gen(ex, blk):
            (wg, bwg, _, _), (wu, bwu, _, _), (wd, bwd, _, _) = wtiles[ex]
            wgv = wg[:].rearrange("p c (q j) -> p c j q", j=2)
            wuv = wu[:].rearrange("p c (q j) -> p c j q", j=2)
            s0 = ex * CAP + blk * B
            xT, bxT, ixT = yield from xTr.get()
            tks, xgs = [], []
            for s_ in range(NS):
                tk, btk, itk = yield from tkr.get()
                k.load(tk[:], ROUTE[s0 + s_ * 128: s0 + (s_ + 1) * 128, :], r=[bROUTE], w=[btk])
                tks.append((tk, btk, itk))
                xg, bxg, ixg = yield from xgr.get()
                k.load(xg[:], XS[s0 + s_ * 128: s0 + (s_ + 1) * 128, :], r=[bXS], w=[bxg])
                xgs.append((xg, bxg, ixg))
            yield
            for s_ in range(NS):
                xg, bxg, ixg = xgs[s_]
                pb, bpb, ipb = yield from psb.get()
                xgv = xg[:].rearrange("p (q c) -> p c q", c=8)
                for c in range(8):
                    k.tr(pb[:, c * 128:(c + 1) * 128], xgv[:, c, :], identb[:], r=[bxg, b_identb], w=[bpb])
                xgr.put(ixg)
                yield
                k.cp("act" if s_ % 2 == 0 else "dve", xT[:, :, s_ * 128:(s_ + 1) * 128],
                     pb[:].rearrange("p (c t) -> p c t", c=8), r=[bpb], w=[bxT])
                psb.put(ipb)
            yield
            hT, bhT, ihT = yield from hTr.get()
            for c in range(2):
                gps, bgps, igps = yield from psf.get()
                ups, bups, iups = yield from psf.get()
                for kc in range(8):
                    k.mm(gps[:, 0:B], wgv[:, kc, c, :], xT[:, kc, :], start=(kc == 0), stop=(kc == 7),
                         r=[bwg, bxT], w=[bgps])
                for kc in range(8):
                    k.mm(ups[:, 0:B], wuv[:, kc, c, :], xT[:, kc, :], start=(kc == 0), stop=(kc == 7),
                         r=[bwu, bxT], w=[bups])
                yield
                sg, bsg, isg = yield from sgr.get()
                k.act(sg[:], gps[:, 0:B], AF.Silu, r=[bgps], w=[bsg])
                psf.put(igps)
                yield
                k.tt("dve", hT[:, c, :], sg[:], ups[:, 0:B], ALU.mult, r=[bsg, bups], w=[bhT])
                psf.put(iups)
                sgr.put(isg)
            xTr.put(ixT)
            yield
            for s_ in range(NS):
                ys, bys, iys = yield from ysr.get()
                for half in range(2):
                    yps, byps, iyps = yield from psf.get()
                    for c in range(2):
                        k.mm(yps[:], hT[:, c, s_ * 128:(s_ + 1) * 128], wd[:, c, half * 512:(half + 1) * 512],
                             start=(c == 0), stop=(c == 1), r=[bhT, bwd], w=[byps])
                    yield
                    k.cp("act" if half == 0 else "dve", ys[:, half * 512:(half + 1) * 512], yps[:], r=[byps], w=[bys])
                    psf.put(iyps)
                tk, btk, itk = tks[s_]
                P.add("pool", lambda e, ys=ys, tk=tk: e.indirect_dma_start(
                    out=YT, out_offset=bass.IndirectOffsetOnAxis(ap=tk[:, 0:1], axis=0),
                    in_=ys[:], in_offset=None), [btk, bys], [bYT], dma=True)
                tkr.put(itk)
                ysr.put(iys)
            hTr.put(ihT)
            wtiles[(ex, "left")] -= 1
            if wtiles[(ex, "left")] == 0:
                for (_, _, iwb, pool_) in wtiles[ex]:
                    pool_.put(iwb)

        for _ in wprep_gen(0):
            pass
        gens = []
        for ex in range(NE):
            if ex + 1 < NE:
                gens.append(wprep_gen(ex + 1))
            gens += [blk_gen(ex, blk) for blk in range(CAP // B)]
        run_pipelined(gens, 3)
        k.flush()
        P.drain_dmas()
        P.emit()
    with ExitStack() as es:
        def sb(name, shape, dt):
            return es.enter_context(nc.sbuf_tensor("sb_" + name, list(shape), dt)), Buf(name)
        gates, b_gates = sb("c_gate", [128, NT * 2], F32)
        k.dma("sp", gates[:], GATE, r=[bGATE], w=[b_gates])
        h1r = sb_ring(es, nc, "c_h1", 3, [128, D], F32)
        ytr = sb_ring(es, nc, "c_yt", 3, [128, 2, D], BF16)
        by = Buf("y")
        YTv = YT[0:2 * S, :].rearrange("(t j) n -> t j n", j=2)
        for ti in range(NT):
            r0 = ti * 128
            h1, bh1 = h1r.next()
            k.load(h1[:], H1[r0:r0 + 128, :], r=[bH1], w=[bh1])
            yt, byt = ytr.next()
            k.load(yt[:], YTv[r0:r0 + 128, :, :], r=[bYT], w=[byt])
            for j in range(2):
                P.add("dve", lambda e, yt=yt, h1=h1, ti=ti, j=j: e.scalar_tensor_tensor(
                    h1[:], yt[:, j, :], gates[:, 2 * ti + j:2 * ti + j + 1], h1[:], ALU.mult, ALU.add),
                    [byt, b_gates, bh1], [bh1])
            k.store(k.y[r0:r0 + 128, :], h1[:], r=[bh1], w=[by])
        k.flush()
        P.drain_dmas()
        P.emit()


def build_program(S):
    k = K(S)
    phase1(k)
    phase_attn(k)
    phase_hgrn(k)
    phase_route(k)
    phase_moe(k)
    return k


def kernel(**inputs):
    x = np.asarray(inputs["x"], dtype=np.float32)
    nb, S, _ = x.shape
    k = build_program(S)
    par = layout_params(inputs)
    con = make_consts(S)
    in_maps = []
    for c in range(nb):
        m = {"x": np.ascontiguousarray(x[c])}
        m.update(par)
        m.update(con)
        in_maps.append(m)
    res = run_bass_kernel_spmd(k.nc, in_maps, core_ids=list(range(nb)))
    return np.stack([np.asarray(r["y"], dtype=np.float32) for r in res.results], axis=0)
```

```python
import numpy as np
from contextlib import ExitStack
import concourse.bass as bass
import concourse.mybir as mybir
from concourse.bass_utils import run_bass_kernel_spmd

F32 = mybir.dt.float32
BF16 = mybir.dt.bfloat16
I32 = mybir.dt.int32
U32 = mybir.dt.uint32
U8 = mybir.dt.uint8
AF = mybir.ActivationFunctionType
ALU = mybir.AluOpType
AX = mybir.AxisListType

ENGS = ("pe", "act", "dve", "pool", "sp")
EPS = 1e-6
D = 1024
NIN = 2976
H = 8
QK = 96
NOPE = 64
ROPE = 32
DV = 64
HGH = 4
NE = 32
NG = 4
DE = 256
MOE_B = 256
MOE_LOG2B = 8
import os
MOE_DBG = int(os.environ.get('MOE_DBG', '0'))
class Buf:
    __slots__ = ("name", "last_w", "readers", "multi", "writers")

    def __init__(self, name="", multi=False):
        self.name = name
        self.last_w = None
        self.readers = {}
        self.multi = multi
        self.writers = []


class Op:
    __slots__ = ("eng", "fn", "waits", "signal", "ticket", "is_dma", "sem", "target", "idx", "emitted", "drained")


class Prog:
    def __init__(self, nc, n_dma_sems=40, n_sw_sems=8):
        self.nc = nc
        self.eng_obj = {"pe": nc.tensor, "act": nc.scalar, "dve": nc.vector,
                        "pool": nc.gpsimd, "sp": nc.sync}
        self.sem = {}
        self.count = {e: 0 for e in ENGS}
        self._stack = []
        for e in ENGS:
            g = nc.semaphore("s_" + e)
            self.sem[e] = g.__enter__()
            self._stack.append(g)
        self.dma_sems = []
        self.dma_uses = []
        self.dma_last = []
        for i in range(n_dma_sems):
            g = nc.semaphore("d_%d" % i)
            self.dma_sems.append(g.__enter__())
            self._stack.append(g)
            self.dma_uses.append(0)
            self.dma_last.append(None)
        self.dma_rr = 0
        self.sw_sems = []
        self.sw_uses = []
        self.sw_last = []
        for i in range(n_sw_sems):
            g = nc.semaphore("w_%d" % i)
            self.sw_sems.append(g.__enter__())
            self._stack.append(g)
            self.sw_uses.append(0)
            self.sw_last.append(None)
        self.sw_rr = 0
        self.ops = {e: [] for e in ENGS}
        self.waited = {e: {} for e in ENGS}
        self.nops = 0
        self.pending_dma = []
        self._deferred = []
        self._def_src = set()

    def defer_dma(self, eng, out, in_, reads=(), writes=(), **kw):
        self._deferred.append((eng, out, in_, tuple(reads), tuple(writes), kw))
        for b in reads:
            self._def_src.add(id(b))

    def flush(self):
        d, self._deferred = self._deferred, []
        self._def_src = set()
        for eng, out, in_, r, w, kw in d:
            self.dma(eng, out, in_, r, w, **kw)

    def add(self, eng, fn, reads=(), writes=(), dma=False):
        if self._deferred and any(id(b) in self._def_src for b in writes):
            self.flush()
        op = Op()
        op.eng = eng
        op.fn = fn
        op.waits = []
        op.signal = False
        op.ticket = None
        op.is_dma = dma
        op.sem = None
        op.target = None
        op.idx = self.nops
        op.emitted = False
        op.drained = False
        self.nops += 1
        deps = {}
        raw = set()
        for b in reads:
            if b.multi:
                b.writers = [w_ for w_ in b.writers if not (w_.emitted and (w_.drained or not w_.is_dma))]
                for w_ in b.writers:
                    deps[id(w_)] = w_
            elif b.last_w is not None:
                deps[id(b.last_w)] = b.last_w
                raw.add(id(b.last_w))
        for b in writes:
            if (not b.multi) and b.last_w is not None:
                deps[id(b.last_w)] = b.last_w
            for r in b.readers.values():
                deps[id(r)] = r
        for k, d in deps.items():
            if d is op:
                continue
            if d.is_dma:
                if not (d.emitted and d.drained):
                    op.waits.append(d)
            elif d.emitted:
                continue
            elif d.eng == eng and not dma:
                if eng == "pe":
                    continue
                d.signal = True
                op.waits.append(d)
            else:
                d.signal = True
                op.waits.append(d)
        if dma and eng == "pool":
            i = self.sw_rr
            self.sw_rr = (self.sw_rr + 1) % len(self.sw_sems)
            prev = self.sw_last[i]
            if prev is not None:
                op.waits.append(prev)
            self.sw_uses[i] += 1
            op.sem = self.sw_sems[i]
            op.target = 16 * self.sw_uses[i]
            self.sw_last[i] = op
            self.pending_dma.append(op)
        elif dma:
            i = self.dma_rr
            self.dma_rr = (self.dma_rr + 1) % len(self.dma_sems)
            prev = self.dma_last[i]
            if prev is not None:
                op.waits.append(prev)
            self.dma_uses[i] += 1
            op.sem = self.dma_sems[i]
            op.target = 16 * self.dma_uses[i]
            self.dma_last[i] = op
            self.pending_dma.append(op)
        for b in reads:
            key = ("d", op.idx) if dma else eng
            b.readers[key] = op
        for b in writes:
            if b.multi:
                b.writers.append(op)
                b.readers = {k_: r_ for k_, r_ in b.readers.items() if not (r_.emitted and (r_.drained or not r_.is_dma))}
            else:
                b.last_w = op
                b.readers = {}
        self.ops[eng].append(op)
        return op

    def dma(self, eng, out, in_, reads=(), writes=(), **kw):
        return self.add(eng, lambda e: e.dma_start(out=out, in_=in_, **kw), reads, writes, dma=True)

    def drain_dmas(self, eng="sp"):
        op = self.add(eng, None)
        seen = {}
        for d in self.pending_dma:
            d.drained = True
            seen[id(d.sem)] = d
        op.waits.extend(seen.values())
        self.pending_dma = []
        return op

    def emit(self, name=None):
        for e in ENGS:
            c = self.count[e]
            for op in self.ops[e]:
                if op.is_dma:
                    continue
                if op.signal:
                    c += 1
                    op.ticket = c
            self.count[e] = c
        prog = self

        def run(e, engine):
            waited = prog.waited[e]
            for op in prog.ops[e]:
                for d in op.waits:
                    if d.is_dma:
                        key, sem, val = id(d.sem), d.sem, d.target
                    else:
                        key, sem, val = d.eng, prog.sem[d.eng], d.ticket
                    if waited.get(key, 0) >= val:
                        continue
                    waited[key] = val
                    engine.wait_ge(sem, val)
                if op.fn is None:
                    continue
                ins = op.fn(engine)
                if op.is_dma:
                    ins.then_inc(op.sem, 16)
                elif op.signal:
                    ins.then_inc(prog.sem[e], 1)

        with self.nc.Block() as block:
            @block.tensor
            def _(eng):
                run("pe", eng)

            @block.scalar
            def _(eng):
                run("act", eng)

            @block.vector
            def _(eng):
                run("dve", eng)

            @block.gpsimd
            def _(eng):
                run("pool", eng)

            @block.sync
            def _(eng):
                run("sp", eng)
        for e in ENGS:
            for op in self.ops[e]:
                op.emitted = True
        self.ops = {e: [] for e in ENGS}


def _bf(a):
    import ml_dtypes
    return np.asarray(a, dtype=np.float32).astype(ml_dtypes.bfloat16)


def moe_cap(S):
    return max(MOE_B, S // 8)


def make_consts(S):
    c = {}
    c["ident_bf"] = _bf(np.eye(128))
    c["ident_f"] = np.eye(128, dtype=np.float32)
    c["ones_bf"] = _bf(np.ones((128, 128)))
    c["ones_f"] = np.ones((128, 128), dtype=np.float32)
    rot = np.zeros((96, 96), np.float32)
    for i in range(16):
        rot[80 + i, 64 + i] = -1.0
        rot[64 + i, 80 + i] = 1.0
    c["rotT"] = _bf(rot)
    sel = np.zeros((32, 96), np.float32)
    for i in range(32):
        sel[i, 64 + i] = 1.0
    c["kr_sel"] = _bf(sel)
    half = 16
    inv = (1.0 / (10000.0 ** (np.arange(half, dtype=np.float32) / half))).astype(np.float32)
    ang = (np.arange(S, dtype=np.float32)[None, :] * inv[:, None]).astype(np.float32)
    cs = np.zeros((96, S), np.float32)
    sn = np.zeros((96, S), np.float32)
    cs[64:80] = np.cos(ang); cs[80:96] = np.cos(ang)
    sn[64:80] = np.sin(ang); sn[80:96] = np.sin(ang)
    c["rope_cos"] = cs
    c["rope_sin"] = sn
    s_ = np.arange(128)[:, None]
    t_ = np.arange(128)[None, :]
    c["hg_Lc_f"] = ((s_ <= t_).astype(np.float32) - (s_ <= 63).astype(np.float32))
    c["hg_Lr_f"] = (s_ > t_).astype(np.float32)
    c["hg_Lc_b"] = ((s_ >= t_).astype(np.float32) - (s_ >= 64).astype(np.float32))
    c["hg_Lr_b"] = (s_ < t_).astype(np.float32)
    c["hg_selm_f"] = np.stack([np.ones(128), (np.arange(128) <= 63)], 1).astype(np.float32)
    c["hg_selm_b"] = np.stack([np.ones(128), (np.arange(128) >= 64)], 1).astype(np.float32)
    mf = (s_ <= t_).astype(np.uint32)
    mb = (s_ >= t_).astype(np.uint32)
    c["hg_mask_f"] = np.tile(mf, (1, 4))
    c["hg_mask_b"] = np.tile(mb, (1, 4))
    c["ustrict"] = (s_ < t_).astype(np.float32)
    c["u32strict"] = (np.arange(32)[:, None] < np.arange(32)[None, :]).astype(np.float32)
    c["ident32"] = np.eye(32, dtype=np.float32)
    cap = moe_cap(S)
    c["blk_iota"] = np.stack([np.arange(32) * cap, np.zeros(32)], 1).astype(np.float32)
    c["lim_row"] = np.tile(((np.arange(32) + 1) * cap).astype(np.float32)[None, :], (128, 1))
    c["p_iota"] = np.arange(128, dtype=np.float32).reshape(128, 1)
    c["tok_iota"] = (np.arange(S // 128, dtype=np.int32)[None, :] * 128 + np.arange(128, dtype=np.int32)[:, None]).astype(np.int32)
    return c


CONST_SPECS = {
    "ustrict": ([128, 128], F32), "u32strict": ([32, 32], F32), "ident32": ([32, 32], F32),
    "blk_iota": ([32, 2], F32), "lim_row": ([128, 32], F32), "p_iota": ([128, 1], F32), "tok_iota": "tok",
    "ident_bf": ([128, 128], BF16), "ident_f": ([128, 128], F32),
    "ones_bf": ([128, 128], BF16), "ones_f": ([128, 128], F32),
    "rotT": ([96, 96], BF16), "kr_sel": ([32, 96], BF16),
    "rope_cos": None, "rope_sin": None,
    "hg_Lc_f": ([128, 128], F32), "hg_Lr_f": ([128, 128], F32),
    "hg_Lc_b": ([128, 128], F32), "hg_Lr_b": ([128, 128], F32),
    "hg_selm_f": ([128, 2], F32), "hg_selm_b": ([128, 2], F32),
    "hg_mask_f": ([128, 512], U32), "hg_mask_b": ([128, 512], U32),
}


def layout_params(inp):
    f = lambda a: np.ascontiguousarray(np.asarray(a, dtype=np.float32))
    p = {}
    p["norm_mix_l"] = f(inp["norm_mix"][0].reshape(8, 128).T)
    p["q_lat_norm_l"] = f(inp["q_lat_norm"][0].reshape(2, 128).T)
    p["kv_lat_norm_l"] = f(inp["kv_lat_norm"][0].reshape(1, 128).T)
    p["q_norm_l"] = f(inp["q_norm"][0].reshape(96, 1))
    p["k_norm_l"] = f(inp["k_norm"][0].reshape(96, 1))
    p["lb_logits_l"] = f(inp["lb_logits"].reshape(2, 2 * 512))
    p["hg_out_norm_l"] = f(inp["hg_out_norm"][0].reshape(1, 128))
    p["norm_ffn_l"] = f(inp["norm_ffn"][0].reshape(1, 1024))
    p["w_rt"] = f(np.concatenate([inp["w_group"][0], inp["w_router"][0]], axis=1))
    p["b_rt"] = f(np.concatenate([inp["b_group"][0], inp["b_router"][0]]).reshape(1, 36))
    p["w_in"] = f(inp["w_in"][0])
    p["w_uq"] = f(inp["w_uq"][0])
    p["w_ukv"] = f(inp["w_ukv"][0])
    p["w_out"] = f(inp["w_out"][0])
    p["w_gate"] = f(inp["w_gate"][0])
    p["w_up"] = f(inp["w_up"][0])
    p["w_down"] = f(inp["w_down"][0])
    return p


PARAM_SPECS = {
    "norm_mix_l": [128, 8], "q_lat_norm_l": [128, 2], "kv_lat_norm_l": [128, 1],
    "q_norm_l": [96, 1], "k_norm_l": [96, 1], "lb_logits_l": [2, 1024],
    "hg_out_norm_l": [1, 128], "norm_ffn_l": [1, 1024], "w_rt": [1024, 36], "b_rt": [1, 36],
    "w_in": [D, NIN], "w_uq": [256, 768], "w_ukv": [128, 1024], "w_out": [1024, 1024],
    "w_gate": [NE, D, DE], "w_up": [NE, D, DE], "w_down": [NE, DE, D],
}


class K:
    def __init__(self, S, debug=(), phases=None):
        self.S = S
        self.NT = S // 128
        self.NB = S // 512
        self.debug = set(debug)
        self.phases = phases
        self.nc = nc = bass.Bass("TRN2", target_bir_lowering=False)
        self.P = Prog(nc)
        self.din = {}
        self.x = nc.dram_tensor("x", [S, D], F32, kind="ExternalInput").ap()
        for k, shp in PARAM_SPECS.items():
            self.din[k] = nc.dram_tensor(k, shp, F32, kind="ExternalInput").ap()
        for k, spec in CONST_SPECS.items():
            if spec is None:
                shp, dt = [96, S], F32
            elif spec == "tok":
                shp, dt = [128, S // 128], I32
            else:
                shp, dt = spec
            self.din[k] = nc.dram_tensor(k, shp, dt, kind="ExternalInput").ap()
        self.y = nc.dram_tensor("y", [S, D], F32, kind="ExternalOutput").ap()
        self.scr = {}
        self.scr_buf = {}
        self.deferred = []
        self.fence_t = nc.alloc_sbuf_tensor("fence_scratch", [128, 1], F32)
        self.b_fence = Buf("fence")

    def scratch(self, name, shape, dt):
        kind = "ExternalOutput" if name in self.debug else "Internal"
        t = self.nc.dram_tensor(name, shape, dt, kind=kind).ap()
        self.scr[name] = t
        self.scr_buf[name] = Buf(name, multi=True)
        return t

    def mm(self, out, lhsT, rhs, start=True, stop=True, r=(), w=()):
        return self.P.add("pe", lambda e: e.matmul(out, lhsT, rhs, start=start, stop=stop), r, w)

    def tr(self, out, in_, ident, r=(), w=()):
        return self.P.add("pe", lambda e: e.transpose(out, in_, ident), r, w)

    def act(self, out, in_, func, r=(), w=(), eng="act", **kw):
        return self.P.add(eng, lambda e: e.activation(out, in_, func, **kw), r, w)

    def ts(self, eng, out, in0, s1, s2, op0, op1=None, r=(), w=(), **kw):
        if op1 is None:
            return self.P.add(eng, lambda e: e.tensor_scalar(out, in0, s1, s2, op0, **kw), r, w)
        return self.P.add(eng, lambda e: e.tensor_scalar(out, in0, s1, s2, op0, op1, **kw), r, w)

    def tt(self, eng, out, in0, in1, op, r=(), w=()):
        return self.P.add(eng, lambda e: e.tensor_tensor(out, in0, in1, op), r, w)

    def cp(self, eng, out, in_, r=(), w=()):
        if eng == "act":
            return self.P.add(eng, lambda e: e.copy(out, in_), r, w)
        return self.P.add(eng, lambda e: e.tensor_copy(out, in_), r, w)

    def dma(self, eng, out, in_, r=(), w=(), **kw):
        return self.P.dma(eng, out, in_, r, w, **kw)

    def load(self, out, in_, r=(), w=(), **kw):
        op = self.P.dma("sp", out, in_, r, w, **kw)
        self.flush()
        return op

    def store(self, out, in_, r=(), w=(), **kw):
        self.P.defer_dma("sp", out, in_, r, w, **kw)

    def fence(self, bufs):
        t = self.fence_t
        self.P.add("pool", lambda e: e.memset(t[0:1, 0:1], 0.0), list(bufs), [self.b_fence])

    def flush(self):
        self.P.flush()

    def make_eps(self, es):
        self.epst = {}
        self.b_epst = Buf("eps")
        for n in (96, 128, 256, 1024):
            t = es.enter_context(self.nc.sbuf_tensor("sb_eps%d" % n, [128, 1], F32))
            self.epst[n] = t
            self.P.add("pool", lambda e, t=t, n=n: e.memset(t[:], float(n * EPS)), (), [self.b_epst])

    def rstd(self, out, in_, neps, bout, r=()):
        np_ = out.shape[0]
        self.P.add("act", lambda e: e.activation(out, in_, AF.Ln, bias=self.epst[neps][0:np_, 0:1]), list(r) + [self.b_epst], [bout])
        self.P.add("act", lambda e: e.activation(out, out, AF.Exp, scale=-0.5), [bout], [bout])


def run_pipelined(gens, depth):
    active = []
    it = iter(gens)
    while True:
        while len(active) < depth:
            g = next(it, None)
            if g is None:
                break
            active.append(g)
        if not active:
            break
        for g in list(active):
            if next(g, "done") == "done":
                active.remove(g)


class Pool_:
    def __init__(self, tiles):
        self.tiles = tiles
        self.bufs = [Buf() for _ in tiles]
        self.i = 0

    def next(self):
        i = self.i
        self.i = (self.i + 1) % len(self.tiles)
        return self.tiles[i], self.bufs[i]


class FreePool:
    def __init__(self, tiles):
        self.tiles = tiles
        self.bufs = [Buf() for _ in tiles]
        self.free = list(range(len(tiles)))

    def get(self):
        while not self.free:
            yield
        i = self.free.pop(0)
        return self.tiles[i], self.bufs[i], i

    def put(self, i):
        self.free.append(i)

    def take(self):
        i = self.free.pop(0)
        return self.tiles[i], self.bufs[i], i


def sb_free(es, nc, name, n, shape, dt):
    return FreePool([es.enter_context(nc.sbuf_tensor("sb_%s%d" % (name, i), shape, dt)) for i in range(n)])


def ps_free(es, nc, name, n, shape=(128, 512), dt=F32):
    return FreePool([es.enter_context(nc.psum_tensor("%s%d" % (name, i), list(shape), dt)) for i in range(n)])


def sb_ring(es, nc, name, n, shape, dt):
    return Pool_([es.enter_context(nc.sbuf_tensor("sb_%s%d" % (name, i), shape, dt)) for i in range(n)])


def ps_ring(es, nc, name, n, shape=(128, 512), dt=F32):
    return Pool_([es.enter_context(nc.psum_tensor("%s%d" % (name, i), list(shape), dt)) for i in range(n)])


def phase1(k):
    nc, P, S = k.nc, k.P, k.S
    QT = k.scratch("QT", [H, QK, S], BF16)
    KT = k.scratch("KT", [H, QK, S], BF16)
    VV = k.scratch("VV", [S, H * DV], BF16)
    HG = k.scratch("HG", [S, 2560], F32)
    bQT, bKT, bVV, bHG = (k.scr_buf[n] for n in ("QT", "KT", "VV", "HG"))
    with ExitStack() as es:
        def sb(name, shape, dt):
            return es.enter_context(nc.sbuf_tensor("sb_" + name, list(shape), dt)), Buf(name)
        ident, b_ident = sb("ident", [128, 128], BF16)
        ones, b_ones = sb("ones", [128, 128], BF16)
        rotT, b_rotT = sb("rotT", [96, 96], BF16)
        krsel, b_krsel = sb("krsel", [32, 96], BF16)
        for t_, b_, nm in ((ident, b_ident, "ident_bf"), (ones, b_ones, "ones_bf"),
                           (rotT, b_rotT, "rotT"), (krsel, b_krsel, "kr_sel")):
            k.dma("sp", t_[:], k.din[nm], w=[b_])
        k.make_eps(es)
        gmix, b_gmix = sb("gmix", [128, 8], F32)
        gql, b_gql = sb("gql", [128, 2], F32)
        gkvl, b_gkvl = sb("gkvl", [128, 1], F32)
        gq, b_gq = sb("gq", [96, 1], F32)
        gk, b_gk = sb("gk", [96, 1], F32)
        for t_, b_, nm in ((gmix, b_gmix, "norm_mix_l"), (gql, b_gql, "q_lat_norm_l"),
                           (gkvl, b_gkvl, "kv_lat_norm_l"), (gq, b_gq, "q_norm_l"), (gk, b_gk, "k_norm_l")):
            k.dma("sp", t_[:], k.din[nm], w=[b_])
        k.ts("dve", gmix[:], gmix[:], 32.0, None, ALU.mult, r=[b_gmix], w=[b_gmix])
        k.ts("dve", gql[:], gql[:], 16.0, None, ALU.mult, r=[b_gql], w=[b_gql])
        k.ts("dve", gkvl[:], gkvl[:], float(np.sqrt(128.0)), None, ALU.mult, r=[b_gkvl], w=[b_gkvl])
        k.ts("dve", gq[:], gq[:], float(np.sqrt(96.0) * 96.0 ** -0.5), None, ALU.mult, r=[b_gq], w=[b_gq])
        k.ts("dve", gk[:], gk[:], float(np.sqrt(96.0)), None, ALU.mult, r=[b_gk], w=[b_gk])

        win, b_win = sb("win", [128, 8, NIN], BF16)
        wst = sb_ring(es, nc, "wst", 1, [128, NIN], F32)
        w_in_v = k.din["w_in"].rearrange("(kc p) n -> p kc n", p=128)
        for kc in range(8):
            st, bst = wst.next()
            k.dma("sp", st[:], w_in_v[:, kc, :], w=[bst])
            k.ts("dve" if kc % 2 == 0 else "pool", win[:, kc, :], st[:], gmix[:, kc:kc + 1], None, ALU.mult,
                 r=[bst, b_gmix], w=[b_win])
        wuq, b_wuq = sb("wuq", [128, 2, 768], BF16)
        w_uq_v = k.din["w_uq"].rearrange("(kc p) n -> p kc n", p=128)
        for kc in range(2):
            st, bst = wst.next()
            k.dma("sp", st[:, 0:768], w_uq_v[:, kc, :], w=[bst])
            k.ts("dve", wuq[:, kc, :], st[:, 0:768], gql[:, kc:kc + 1], None, ALU.mult, r=[bst, b_gql], w=[b_wuq])
        wk, b_wk = sb("wk", [128, 8, 96], BF16)
        wv, b_wv = sb("wv", [128, 8, 64], BF16)
        st, bst = wst.next()
        k.dma("sp", st[:, 0:1024], k.din["w_ukv"], w=[bst])
        P.add("pool", lambda e: e.memset(wk[:], 0.0), (), [b_wk])
        stv = st[:, 0:1024].rearrange("p (h c) -> p h c", c=128)
        k.ts("dve", wk[:, :, 0:64], stv[:, :, 0:64], gkvl[:, 0:1], None, ALU.mult, r=[bst, b_gkvl], w=[b_wk])
        k.ts("dve", wv[:], stv[:, :, 64:128], gkvl[:, 0:1], None, ALU.mult, r=[bst, b_gkvl], w=[b_wv])

        xr = sb_ring(es, nc, "xt", 3, [128, D], F32)
        junkr = sb_ring(es, nc, "junk", 3, [128, D], BF16)
        ssr = sb_ring(es, nc, "ss", 4, [128, 2], F32)
        nr = sb_ring(es, nc, "nbf", 2, [128, D], BF16)
        nTr = sb_ring(es, nc, "nT", 2, [128, 8, 512], BF16)
        stg = sb_ring(es, nc, "stg", 2, [128, 2560], F32)
        sqq, b_sqq = sb("sqq", [128, 3, 512], BF16)
        rsl, b_rsl = sb("rsl", [128, 2, 512], F32)
        qnT, b_qnT = sb("qnT", [128, 2, 512], BF16)
        kvnT, b_kvnT = sb("kvnT", [128, 512], BF16)
        krT, b_krT = sb("krT", [32, 512], BF16)
        vsb = sb_ring(es, nc, "vsb", 2, [128, 512], BF16)
        cosr = sb_ring(es, nc, "cos", 2, [96, 512], F32)
        sinr = sb_ring(es, nc, "sin", 2, [96, 512], F32)
        sqh = sb_ring(es, nc, "sqh", 3, [96, 512], BF16)
        qgh = sb_ring(es, nc, "qgh", 3, [96, 512], BF16)
        qg32 = sb_ring(es, nc, "qg32", 3, [96, 512], F32)
        rsh = sb_ring(es, nc, "rsh", 3, [96, 512], F32)
        t1r = sb_ring(es, nc, "t1r", 3, [96, 512], F32)
        t2r = sb_ring(es, nc, "t2r", 3, [96, 512], F32)
        qfr = sb_ring(es, nc, "qfr", 3, [96, 512], BF16)
        psf = ps_ring(es, nc, "psf", 6)
        psb = ps_ring(es, nc, "psb", 2, (128, 1024), BF16)

        eflip = [0]

        def evac_eng():
            eflip[0] ^= 1
            return "act" if eflip[0] else "dve"

        for j in range(k.NB):
            tok0 = j * 512
            nT, b_nT = nTr.next()
            def xt_gen(t, tok0=tok0, nT=nT, b_nT=b_nT):
                r0 = tok0 + t * 128
                xt, bx = xr.next()
                k.load(xt[:], k.x[r0:r0 + 128, :], w=[bx])
                yield
                ss, bss = ssr.next()
                junk, b_junk = junkr.next()
                k.act(junk[:], xt[:], AF.Square, r=[bx], w=[b_junk, bss], accum_out=ss[:, 0:1])
                k.rstd(ss[:, 1:2], ss[:, 0:1], 1024, bss, r=[bss])
                yield
                nb_, bn = nr.next()
                k.act(nb_[:], xt[:], AF.Copy, r=[bx, bss], w=[bn], scale=ss[:, 1:2])
                yield
                pb, bpb = psb.next()
                for kc in range(8):
                    k.tr(pb[:, kc * 128:(kc + 1) * 128], nb_[:, kc * 128:(kc + 1) * 128], ident[:],
                         r=[bn, b_ident], w=[bpb])
                yield
                k.cp("act" if t % 2 == 0 else "dve", nT[:, :, t * 128:(t + 1) * 128],
                     pb[:].rearrange("p (kc t) -> p kc t", kc=8), r=[bpb], w=[b_nT])

            run_pipelined([xt_gen(t) for t in range(4)], 2)
            lat = []
            for (c0, ncol) in ((0, 128), (128, 128), (256, 128), (384, 32)):
                ps, bps = psf.next()
                for kc in range(8):
                    k.mm(ps[0:ncol, :], win[:, kc, c0:c0 + ncol], nT[:, kc, :], start=(kc == 0), stop=(kc == 7),
                         r=[b_win, b_nT], w=[bps])
                lat.append((ps, bps))
            for i in range(3):
                k.act(sqq[:, i, :], lat[i][0][:], AF.Square, r=[lat[i][1]], w=[b_sqq])
            k.cp("dve", krT[:], lat[3][0][0:32, :], r=[lat[3][1]], w=[b_krT])
            msq, bmsq = psf.next()
            k.mm(msq[:], ones[:], sqq[:, 0, :], start=True, stop=False, r=[b_ones, b_sqq], w=[bmsq])
            k.mm(msq[:], ones[:], sqq[:, 1, :], start=False, stop=True, r=[b_ones, b_sqq], w=[bmsq])
            msk, bmsk = psf.next()
            k.mm(msk[:], ones[:], sqq[:, 2, :], r=[b_ones, b_sqq], w=[bmsk])
            k.rstd(rsl[:, 0, :], msq[:], 256, b_rsl, r=[bmsq])
            k.rstd(rsl[:, 1, :], msk[:], 128, b_rsl, r=[bmsk])
            k.tt("dve", qnT[:, 0, :], lat[0][0][:], rsl[:, 0, :], ALU.mult, r=[lat[0][1], b_rsl], w=[b_qnT])
            k.tt("dve", qnT[:, 1, :], lat[1][0][:], rsl[:, 0, :], ALU.mult, r=[lat[1][1], b_rsl], w=[b_qnT])
            k.tt("dve", kvnT[:], lat[2][0][:], rsl[:, 1, :], ALU.mult, r=[lat[2][1], b_rsl], w=[b_kvnT])
            for t in range(4):
                ps, bps = psf.next()
                k.mm(ps[:], kvnT[:, t * 128:(t + 1) * 128], wv[:].rearrange("p h c -> p (h c)"),
                     r=[b_kvnT, b_wv], w=[bps])
                v_, bv = vsb.next()
                k.cp("dve", v_[:], ps[:], r=[bps], w=[bv])
                k.store(VV[tok0 + t * 128: tok0 + (t + 1) * 128, :], v_[:], r=[bv], w=[bVV])
            cs, bcs = cosr.next()
            sn, bsn = sinr.next()
            k.load(cs[64:96, :], k.din["rope_cos"][64:96, tok0:tok0 + 512], w=[bcs])
            k.load(sn[64:96, :], k.din["rope_sin"][64:96, tok0:tok0 + 512], w=[bsn])

            def head_gen(h, which, tok0=tok0, cs=cs, bcs=bcs, sn=sn, bsn=bsn):
                ps, bps = psf.next()
                if which == 0:
                    k.mm(ps[0:96, :], wuq[:, 0, h * 96:(h + 1) * 96], qnT[:, 0, :], start=True, stop=False,
                         r=[b_wuq, b_qnT], w=[bps])
                    k.mm(ps[0:96, :], wuq[:, 1, h * 96:(h + 1) * 96], qnT[:, 1, :], start=False, stop=True,
                         r=[b_wuq, b_qnT], w=[bps])
                    g_, bg_, dst, bdst = gq, b_gq, QT, bQT
                else:
                    k.mm(ps[0:96, :], wk[:, h, :], kvnT[:], start=True, stop=False, r=[b_wk, b_kvnT], w=[bps])
                    k.mm(ps[0:96, :], krsel[:], krT[:], start=False, stop=True, r=[b_krsel, b_krT], w=[bps])
                    g_, bg_, dst, bdst = gk, b_gk, KT, bKT
                yield
                sq, bsq = sqh.next()
                k.act(sq[:], ps[0:96, :], AF.Square, r=[bps], w=[bsq])
                q32, bq32 = qg32.next()
                k.act(q32[:], ps[0:96, :], AF.Copy, r=[bps, bg_], w=[bq32], scale=g_[:, 0:1])
                yield
                ms, bms = psf.next()
                k.mm(ms[0:96, :], ones[0:96, 0:96], sq[:], r=[b_ones, bsq], w=[bms])
                qg, bqg = qgh.next()
                k.act(qg[:], ps[0:96, :], AF.Copy, r=[bps, bg_], w=[bqg], scale=g_[:, 0:1])
                t1, bt1 = t1r.next()
                k.tt("pool", t1[64:96, :], q32[64:96, :], cs[64:96, :], ALU.mult, r=[bq32, bcs], w=[bt1])
                yield
                rt, brt = psf.next()
                k.mm(rt[0:96, :], rotT[:], qg[:], r=[b_rotT, bqg], w=[brt])
                rs, brs = rsh.next()
                k.rstd(rs[:], ms[0:96, :], 96, brs, r=[bms])
                yield
                t2, bt2 = t2r.next()
                k.tt("dve", t2[64:96, :], rt[64:96, :], sn[64:96, :], ALU.mult, r=[brt, bsn], w=[bt2])
                yield
                k.tt("pool", q32[64:96, :], t1[64:96, :], t2[64:96, :], ALU.add, r=[bt1, bt2], w=[bq32])
                yield
                qf, bqf = qfr.next()
                k.tt("dve", qf[:], q32[:], rs[:], ALU.mult, r=[bq32, brs], w=[bqf])
                k.store(dst[h, :, tok0:tok0 + 512], qf[:], r=[bqf], w=[bdst])

            def tm_gen(t, tok0=tok0, nT=nT, b_nT=b_nT):
                sg, bsg = stg.next()
                for g in range(5):
                    ps, bps = psf.next()
                    c0 = 416 + g * 512
                    for kc in range(8):
                        k.mm(ps[:], nT[:, kc, t * 128:(t + 1) * 128], win[:, kc, c0:c0 + 512],
                             start=(kc == 0), stop=(kc == 7), r=[b_nT, b_win], w=[bps])
                    yield
                    k.cp(evac_eng(), sg[:, g * 512:(g + 1) * 512], ps[:], r=[bps], w=[bsg])
                k.store(HG[tok0 + t * 128: tok0 + (t + 1) * 128, :], sg[:], r=[bsg], w=[bHG])

            gens = []
            hw = [(h, w_) for h in range(H) for w_ in range(2)]
            for t in range(4):
                gens += [head_gen(h, w_) for (h, w_) in hw[4 * t:4 * t + 4]]
                gens.append(tm_gen(t))
            run_pipelined(gens, 3)
        k.flush()
        P.drain_dmas()
        P.emit()


def moe_scratch_init(k, es):
    nc, P, S = k.nc, k.P, k.S
    CAP = moe_cap(S)
    DUMP = NE * CAP
    PSL = DUMP + 128
    k.CAP, k.PSL = CAP, PSL
    XS = k.scratch("XS", [PSL, D], BF16)
    ROUTE = k.scratch("ROUTE", [PSL, 16], I32)
    YT = k.scratch("YT", [2 * S + PSL, D], BF16)
    bXS, bROUTE, bYT = (k.scr_buf[n] for n in ("XS", "ROUTE", "YT"))
    RW = PSL // 128 * 16
    zt = es.enter_context(nc.sbuf_tensor("sb_i_zero", [128, RW], I32))
    b_zt = Buf("i_zero")
    P.add("pool", lambda e: e.iota(zt[:], pattern=[[128, PSL // 128], [0, 16]], base=2 * S, channel_multiplier=1),
          (), [b_zt])
    k.dma("sp", ROUTE.rearrange("(r p) c -> p r c", p=128), zt[:].rearrange("p (r c) -> p r c", c=16),
          r=[b_zt], w=[bROUTE])
    zx = es.enter_context(nc.sbuf_tensor("sb_i_zx", [128, 4 * D], BF16))
    b_zx = Buf("i_zx")
    P.add("pool", lambda e: e.memset(zx[:], 0.0), (), [b_zx])
    XSz = XS.rearrange("(p r) n -> p r n", p=128)
    RX = PSL // 128
    for r_ in range(0, RX, 4):
        n_ = min(4, RX - r_)
        k.dma("sp", XSz[:, r_:r_ + n_, :], zx[:, 0:n_ * D].rearrange("p (r n) -> p r n", r=n_), r=[b_zx], w=[bXS])
    YTz = YT[0:2 * S, :].rearrange("(p r) n -> p r n", p=128)
    RZ = 2 * S // 128
    for r_ in range(0, RZ, 4):
        n_ = min(4, RZ - r_)
        k.dma("sp", YTz[:, r_:r_ + n_, :], zx[:, 0:n_ * D].rearrange("p (r n) -> p r n", r=n_), r=[b_zx], w=[bYT])


def phase_attn(k, with_hgrn=False):
    nc, P, S, NT = k.nc, k.P, k.S, k.NT
    QT, KT, VV = k.scr["QT"], k.scr["KT"], k.scr["VV"]
    bQT, bKT, bVV = k.scr_buf["QT"], k.scr_buf["KT"], k.scr_buf["VV"]
    MIXT = k.scratch("MIXT", [D, S], BF16)
    bMIXT = k.scr_buf["MIXT"]
    VVv = VV.rearrange("(t p) (h c) -> p t h c", p=128, c=DV)
    RD = k.scratch("RD", [H * (S // 512), 512], F32)
    bRD = [Buf("rd%d" % i) for i in range(4)]
    with ExitStack() as es:
        def sb(name, shape, dt):
            return es.enter_context(nc.sbuf_tensor("sb_" + name, list(shape), dt)), Buf(name)
        onesf, b_onesf = sb("a_onesf", [128, 128], F32)
        k.dma("sp", onesf[:], k.din["ones_f"], w=[b_onesf])
        kth = sb_ring(es, nc, "a_kt", 2, [96, S], BF16)
        qth = sb_ring(es, nc, "a_qt", 3, [96, 512], BF16)
        vh = sb_ring(es, nc, "a_v", 2, [128, NT, DV + 1], BF16)
        for t_, b_ in zip(vh.tiles, vh.bufs):
            P.add("pool", lambda e, t_=t_: e.memset(t_[:], 1.0), (), [b_])
        ptr = sb_ring(es, nc, "a_pt", 3 if with_hgrn else 4, [128, 1024], BF16)
        ocr = sb_ring(es, nc, "a_oc", 2, [128, 512], F32)
        osb = sb_ring(es, nc, "a_o", 2, [64, 512], F32)
        aout = sb_ring(es, nc, "a_a", 2, [64, 512], BF16)
        scr_ = ps_ring(es, nc, "a_sc", 2 if with_hgrn else 3, (128, 1024), F32)
        accr = ps_ring(es, nc, "a_acc", 1 if with_hgrn else 2)
        hstep = None
        if with_hgrn:
            h_psf = ps_free(es, nc, "h_psf", 2)
            h_psb = ps_free(es, nc, "h_psb", 1, (128, 1024), BF16)
            hstep = hgrn_build(k, es, h_psf, h_psb)
            hsteps = hgrn_order(NT)
        NP2 = NT // 2
        NQB = S // 512
        heads = {}

        def load_head(h):
            kt_, bkt = kth.next()
            v_, bv = vh.next()
            k.load(kt_[:], KT[h], r=[bKT], w=[bkt])
            k.load(v_[:, :, 0:DV], VVv[:, :, h, :], r=[bVV], w=[bv])
            heads[h] = (kt_, bkt, v_, bv)

        qbs = {}

        def load_q(h, qb):
            qt_, bqt = qth.next()
            k.load(qt_[:], QT[h, :, qb * 512:(qb + 1) * 512], r=[bQT], w=[bqt])
            qbs[(h, qb)] = (qt_, bqt)

        steps = [(h, qb, kp) for h in range(H) for qb in range(NQB) for kp in range(NP2)]
        scs = {}

        def emit_qk(i):
            h, qb, kp = steps[i]
            if kp == 0:
                if qb == 0 and h not in heads:
                    load_head(h)
                if (h, qb) not in qbs:
                    load_q(h, qb)
                nxt = (h, qb + 1) if qb + 1 < NQB else ((h + 1, 0) if h + 1 < H else None)
                if nxt is not None:
                    if nxt[1] == 0 and nxt[0] not in heads:
                        load_head(nxt[0])
                    if nxt not in qbs:
                        load_q(*nxt)
            kt_, bkt, v_, bv = heads[h]
            qt_, bqt = qbs[(h, qb)]
            sc, bsc = scr_.next()
            for u in range(2):
                kt = 2 * kp + u
                k.mm(sc[:, u * 512:(u + 1) * 512], kt_[:, kt * 128:(kt + 1) * 128], qt_[:],
                     r=[bkt, bqt], w=[bsc])
            scs[i] = (sc, bsc)

        LOOK = 1 if with_hgrn else 2
        for i in range(min(LOOK, len(steps))):
            emit_qk(i)
        if "XS" not in k.scr:
            moe_scratch_init(k, es)
        acc = bacc = None
        gen = None
        for i, (h, qb, kp) in enumerate(steps):
            kt_, bkt, v_, bv = heads[h]
            if kp == 0:
                acc, bacc = accr.next()
                if hstep is not None and h * NQB + qb < len(hsteps):
                    gen = hstep(*hsteps[h * NQB + qb])
            sc, bsc = scs.pop(i)
            pt, bpt = ptr.next()
            k.act(pt[:], sc[:], AF.Exp, r=[bsc], w=[bpt])
            if i + LOOK < len(steps):
                emit_qk(i + LOOK)
            for u in range(2):
                kt = 2 * kp + u
                k.mm(acc[0:DV + 1, :], v_[:, kt, :], pt[:, u * 512:(u + 1) * 512],
                     start=(kt == 0), stop=(kt == NT - 1), r=[bv, bpt], w=[bacc])
            if gen is not None:
                if next(gen, "done") == "done":
                    gen = None
            if kp == NP2 - 1:
                if gen is not None:
                    for _ in gen:
                        pass
                    gen = None
                oc, boc = ocr.next()
                k.cp("dve", oc[0:DV + 1, :], acc[0:DV + 1, :], r=[bacc], w=[boc])
                P.add("dve", lambda e, oc=oc: e.reciprocal(oc[64:65, :], oc[64:65, :]), [boc], [boc])
                o_, bo = osb.next()
                ridx = h * NQB + qb
                k.dma("sp", RD[ridx:ridx + 1, :], oc[64:65, :], r=[boc], w=[bRD[ridx % 4]])
                k.dma("sp", o_[:], RD[ridx:ridx + 1, :].partition_broadcast(64), r=[bRD[ridx % 4]], w=[bo])
                a_, ba = aout.next()
                k.tt("dve", a_[:], oc[0:64, :], o_[:], ALU.mult, r=[boc, bo], w=[ba])
                k.store(MIXT[h * 64:(h + 1) * 64, qb * 512:(qb + 1) * 512], a_[:], r=[ba], w=[bMIXT])
        k.flush()
        P.drain_dmas()
        P.emit()


def hgrn_build(k, es, psf, psb):
    nc, P, S, NT = k.nc, k.P, k.S, k.NT
    HG, MIXT = k.scr["HG"], k.scr["MIXT"]
    bHG, bMIXT = k.scr_buf["HG"], k.scr_buf["MIXT"]
    OF = k.scratch("OF", [S, 512], F32)
    bOF = k.scr_buf["OF"]
    MIXr = MIXT[512:1024, :].rearrange("(c p) t -> p c t", p=128)
    def sb(name, shape, dt):
        return es.enter_context(nc.sbuf_tensor("sb_" + name, list(shape), dt)), Buf(name)
    identb, b_identb = sb("h_ident", [128, 128], BF16)
    k.dma("sp", identb[:], k.din["ident_bf"], w=[b_identb])
    cst = {}
    for nm, shp, dt in (("hg_Lc_f", [128, 128], F32), ("hg_Lr_f", [128, 128], F32), ("hg_Lc_b", [128, 128], F32),
                        ("hg_Lr_b", [128, 128], F32), ("hg_selm_f", [128, 2], F32), ("hg_selm_b", [128, 2], F32),
                        ("hg_mask_f", [128, 512], U32), ("hg_mask_b", [128, 512], U32)):
        t_, b_ = sb(nm, shp, dt)
        k.dma("sp", t_[:], k.din[nm], w=[b_])
        cst[nm] = (t_, b_)
    l0, b_l0 = sb("h_l0", [128, 1024], F32)
    l1, b_l1 = sb("h_l1", [128, 1024], F32)
    k.dma("sp", l0[:], k.din["lb_logits_l"][0:1, :].partition_broadcast(128), w=[b_l0])
    k.dma("sp", l1[:], k.din["lb_logits_l"][1:2, :].partition_broadcast(128), w=[b_l1])
    lbb, b_lbb = sb("h_lb", [128, 1024], F32)
    oml, b_oml = sb("h_oml", [128, 1024], F32)
    k.tt("dve", l1[:], l1[:], l0[:], ALU.subtract, r=[b_l0, b_l1], w=[b_l1])
    k.act(l1[:], l1[:], AF.Exp, r=[b_l1], w=[b_l1])
    k.ts("dve", l1[:], l1[:], 1.0, None, ALU.add, r=[b_l1], w=[b_l1])
    P.add("dve", lambda e: e.reciprocal(lbb[:], l1[:]), [b_l1], [b_lbb])
    k.ts("dve", oml[:], lbb[:], -1.0, 1.0, ALU.mult, ALU.add, r=[b_lbb], w=[b_oml])
    gn, b_gn = sb("h_gn", [128, 4, 128], F32)
    for hh in range(4):
        k.dma("sp", gn[:, hh, :], k.din["hg_out_norm_l"][0:1, :].partition_broadcast(128), w=[b_gn])
    k.ts("dve", gn[:], gn[:], float(np.sqrt(128.0)), None, ALU.mult, r=[b_gn], w=[b_gn])
    eps128, b_eps = sb("h_eps", [128, 1], F32)
    P.add("pool", lambda e: e.memset(eps128[:], float(128 * EPS)), (), [b_eps])
    one1, b_one = sb("h_one", [128, 1], F32)
    P.add("pool", lambda e: e.memset(one1[:], 1.0), (), [b_one])

    hgr = sb_ring(es, nc, "h_in", 3, [128, 2560], F32)
    e1r = sb_ring(es, nc, "h_e1", 3, [128, 512], F32)
    e2r = sb_ring(es, nc, "h_e2", 3, [128, 512], F32)
    e3r = sb_ring(es, nc, "h_e3", 3, [128, 512], F32)
    fr = sb_ring(es, nc, "h_f", 3, [128, 512], F32)
    kkr = sb_ring(es, nc, "h_kk", 3, [128, 512], F32)
    gr = sb_ring(es, nc, "h_g", 3, [128, 512], F32)
    qr = sb_ring(es, nc, "h_q", 3, [128, 512], F32)
    vr = sb_ring(es, nc, "h_v", 3, [128, 512], BF16)
    ecr = sb_ring(es, nc, "h_ec", 3, [128, 512], F32)
    encr = sb_ring(es, nc, "h_enc", 3, [128, 512], F32)
    err_ = sb_ring(es, nc, "h_er", 3, [128, 512], F32)
    eblr = sb_ring(es, nc, "h_ebl", 3, [128, 8], F32)
    qkbr = sb_ring(es, nc, "h_qkb", 3, [128, 1024], BF16)
    kdr = sb_ring(es, nc, "h_kd", 3, [128, 512], BF16)
    qkTr = sb_ring(es, nc, "h_qkT", 3, [128, 1024], BF16)
    atr = sb_ring(es, nc, "h_at", 3, [128, 512], BF16)
    sbr = sb_ring(es, nc, "h_sb", 3, [128, 512], BF16)
    Sst, b_S = sb("h_S", [128, 512], F32)
    osr = sb_ring(es, nc, "h_os", 3, [128, 512], F32)
    ofr = sb_ring(es, nc, "h_of", 3, [128, 512], F32)
    junkr = sb_ring(es, nc, "h_junk", 12, [128, 128], BF16)
    ssq = sb_ring(es, nc, "h_ssq", 3, [128, 8], F32)
    rbr = sb_ring(es, nc, "h_rb", 3, [128, 512], BF16)
    rTr = sb_ring(es, nc, "h_rT", 3, [128, 512], BF16)

    def silu_from(dst, src, rd, wr, tmp, btmp):
        k.act(tmp[:], src, AF.Exp, r=rd, w=[btmp], scale=-1.0)
        k.ts("dve", tmp[:], tmp[:], 1.0, None, ALU.add, r=[btmp], w=[btmp])
        P.add("dve", lambda e: e.reciprocal(tmp[:], tmp[:]), [btmp], [btmp])
        k.tt("dve", dst, tmp[:], src, ALU.mult, r=[btmp] + list(rd), w=wr)

    dirs = []
    for d_ in range(2):
        sfx = "_f" if d_ == 0 else "_b"
        dirs.append((cst["hg_Lc" + sfx], cst["hg_Lr" + sfx], cst["hg_selm" + sfx], cst["hg_mask" + sfx]))

    prog_state = {"s_done": 0, "started": 0}

    def step(d, ti, first):
        (Lc, b_Lc), (Lr, b_Lr), (selm, b_selm), (mask, b_mask) = dirs[d]
        my = prog_state["started"]
        prog_state["started"] += 1
        if first:
            while prog_state["s_done"] < my:
                yield
            P.add("pool", lambda e: e.memset(Sst[:], 0.0), (), [b_S])
            for t_, b_ in zip(atr.tiles, atr.bufs):
                P.add("pool", lambda e, t_=t_: e.memset(t_[:], 0.0), (), [b_])
        r0 = ti * 128
        hg_, bhg = hgr.next()
        k.load(hg_[:], HG[r0:r0 + 128, :], r=[bHG], w=[bhg])
        if d == 1:
            of_, bof = ofr.next()
            k.load(of_[:], OF[r0:r0 + 128, :], r=[bOF], w=[bof])
        z = hg_[:, 512 + d * 512: 1024 + d * 512]
        yield
        e1, be1 = e1r.next()
        e2, be2 = e2r.next()
        k.act(e1[:], z, AF.Sigmoid, r=[bhg], w=[be1])
        k.act(e2[:], hg_[:, 0:512], AF.Sigmoid, r=[bhg], w=[be2])
        if d == 1:
            e3, be3 = e3r.next()
            k.act(e3[:], hg_[:, 2048:2560], AF.Sigmoid, r=[bhg], w=[be3])
        v_, bv = vr.next()
        k.act(v_[:], hg_[:, 1536:2048], AF.Copy, r=[bhg], w=[bv], scale=-1.0)
        yield
        f_, bf_ = fr.next()
        k.tt("dve", f_[:], e1[:], oml[:, d * 512:(d + 1) * 512], ALU.mult, r=[be1, b_oml], w=[bf_])
        k.tt("dve", f_[:], f_[:], lbb[:, d * 512:(d + 1) * 512], ALU.add, r=[bf_, b_lbb], w=[bf_])
        q_, bq = qr.next()
        k.tt("dve", q_[:], e2[:], hg_[:, 0:512], ALU.mult, r=[be2, bhg], w=[bq])
        yield
        g_, bg = gr.next()
        k.act(g_[:], f_[:], AF.Ln, r=[bf_], w=[bg])
        yield
        cps, bcps, icps = yield from psf.get()
        k.mm(cps[:], Lc[:], g_[:], r=[b_Lc, bg], w=[bcps])
        rps, brps, irps = yield from psf.get()
        k.mm(rps[:], Lr[:], g_[:], r=[b_Lr, bg], w=[brps])
        yield
        ec, bec = ecr.next()
        enc, benc = encr.next()
        er, ber = err_.next()
        k.act(ec[:], cps[:], AF.Exp, r=[bcps], w=[bec])
        k.act(enc[:], cps[:], AF.Exp, r=[bcps], w=[benc], scale=-1.0)
        k.act(er[:], rps[:], AF.Exp, r=[brps], w=[ber])
        psf.put(icps)
        psf.put(irps)
        yield
        blm, bblm, iblm = yield from psf.get()
        for hh in range(4):
            k.mm(blm[:, 2 * hh:2 * hh + 2], g_[:, hh * 128:(hh + 1) * 128], selm[:], r=[bg, b_selm], w=[bblm])
        qkb, bqkb = qkbr.next()
        kd, bkd = kdr.next()
        k.tt("dve", qkb[:, 0:512], q_[:], ec[:], ALU.mult, r=[bq, bec], w=[bqkb])
        P.add("dve", lambda e, qkb=qkb, f_=f_, enc=enc: e.scalar_tensor_tensor(
            qkb[:, 512:1024], f_[:], 1.0, enc[:], ALU.subtract, ALU.mult), [bf_, benc], [bqkb])
        P.add("dve", lambda e, kd=kd, f_=f_, er=er: e.scalar_tensor_tensor(
            kd[:], f_[:], 1.0, er[:], ALU.subtract, ALU.mult), [bf_, ber], [bkd])
        yield
        ebl, bebl = eblr.next()
        k.act(ebl[:], blm[:, 0:8], AF.Exp, r=[bblm], w=[bebl])
        psf.put(iblm)
        yield
        pb, bpb, ipb = yield from psb.get()
        for c8 in range(8):
            k.tr(pb[:, c8 * 128:(c8 + 1) * 128], qkb[:, c8 * 128:(c8 + 1) * 128], identb[:],
                 r=[bqkb, b_identb], w=[bpb])
        while prog_state["s_done"] < my:
            yield
        sb_, bsb = sbr.next()
        for hh in range(4):
            k.ts("dve", sb_[:, hh * 128:(hh + 1) * 128], Sst[:, hh * 128:(hh + 1) * 128],
                 ebl[:, 2 * hh + 1:2 * hh + 2], None, ALU.mult, r=[b_S, bebl], w=[bsb])
        yield
        qkT, bqkT = qkTr.next()
        k.cp("dve", qkT[:], pb[:], r=[bpb], w=[bqkT])
        psb.put(ipb)
        yield
        atp, batp, iatp = yield from psf.get()
        for hh in range(4):
            k.mm(atp[:, hh * 128:(hh + 1) * 128], qkT[:, 512 + hh * 128: 512 + (hh + 1) * 128],
                 qkT[:, hh * 128:(hh + 1) * 128], r=[bqkT], w=[batp])
        snp, bsnp, isnp = yield from psf.get()
        for hh in range(4):
            sl = slice(hh * 128, (hh + 1) * 128)
            k.mm(snp[:, sl], kd[:, sl], v_[:, sl], r=[bkd, bv], w=[bsnp])
        yield
        at, bat = atr.next()
        P.add("dve", lambda e, at=at, atp=atp, mask=mask: e.copy_predicated(at[:], mask[:], atp[:]),
              [batp, b_mask], [bat])
        psf.put(iatp)
        yield
        ops, bops, iops = yield from psf.get()
        for hh in range(4):
            sl = slice(hh * 128, (hh + 1) * 128)
            k.mm(ops[:, sl], at[:, sl], v_[:, sl], start=True, stop=False, r=[bat, bv], w=[bops])
            k.mm(ops[:, sl], qkT[:, sl], sb_[:, sl], start=False, stop=True, r=[bqkT, bsb], w=[bops])
        for hh in range(4):
            sl = slice(hh * 128, (hh + 1) * 128)
            P.add("dve", lambda e, sl=sl, ebl=ebl, snp=snp, hh=hh: e.scalar_tensor_tensor(
                Sst[:, sl], Sst[:, sl], ebl[:, 2 * hh:2 * hh + 1], snp[:, sl], ALU.mult, ALU.add),
                [b_S, bebl, bsnp], [b_S])
        prog_state["s_done"] = my + 1
        psf.put(isnp)
        yield
        os_, bos = osr.next()
        if d == 0:
            k.cp("dve", os_[:], ops[:], r=[bops], w=[bos])
            psf.put(iops)
            k.store(OF[r0:r0 + 128, :], os_[:], r=[bos], w=[bOF])
            return
        k.tt("dve", os_[:], ops[:], of_[:], ALU.add, r=[bops, bof], w=[bos])
        psf.put(iops)
        k.tt("dve", e3[:], e3[:], hg_[:, 2048:2560], ALU.mult, r=[be3, bhg], w=[be3])
        yield
        sq, bsq = ssq.next()
        for hh in range(4):
            junk, b_junk = junkr.next()
            k.act(junk[:], os_[:, hh * 128:(hh + 1) * 128], AF.Square, r=[bos], w=[b_junk, bsq],
                  accum_out=sq[:, hh:hh + 1])
        k.act(sq[:, 4:8], sq[:, 0:4], AF.Ln, r=[bsq, b_eps], w=[bsq], bias=eps128[:, 0:1])
        k.act(sq[:, 4:8], sq[:, 4:8], AF.Exp, r=[bsq], w=[bsq], scale=-0.5)
        yield
        for hh in range(4):
            k.ts("dve", os_[:, hh * 128:(hh + 1) * 128], os_[:, hh * 128:(hh + 1) * 128], sq[:, 4 + hh:5 + hh],
                 None, ALU.mult, r=[bos, bsq], w=[bos])
        k.tt("dve", os_[:], os_[:], gn[:].rearrange("p h c -> p (h c)"), ALU.mult, r=[bos, b_gn], w=[bos])
        rb, brb = rbr.next()
        k.tt("dve", rb[:], os_[:], e3[:], ALU.mult, r=[bos, be3], w=[brb])
        yield
        pb, bpb, ipb = yield from psb.get()
        for hh in range(4):
            k.tr(pb[:, hh * 128:(hh + 1) * 128], rb[:, hh * 128:(hh + 1) * 128], identb[:],
                 r=[brb, b_identb], w=[bpb])
        yield
        rT, brT = rTr.next()
        k.cp("dve", rT[:], pb[:, 0:512], r=[bpb], w=[brT])
        psb.put(ipb)
        k.store(MIXr[:, :, r0:r0 + 128], rT[:].rearrange("p (c t) -> p c t", c=4), r=[brT], w=[bMIXT])
    return step


def hgrn_order(NT):
    return [(0, ti, ti == 0) for ti in range(NT)] + [(1, ti, ti == NT - 1) for ti in range(NT - 1, -1, -1)]


def phase_hgrn(k):
    nc, P = k.nc, k.P
    with ExitStack() as es:
        psf = ps_free(es, nc, "h_psf", 6)
        psb = ps_free(es, nc, "h_psb", 2, (128, 1024), BF16)
        step = hgrn_build(k, es, psf, psb)
        order = hgrn_order(k.NT)
        for d in range(2):
            run_pipelined((step(*o) for o in order if o[0] == d), 3)
        k.flush()
        P.drain_dmas()
        P.emit()


def phase_route(k):
    nc, P, S, NT = k.nc, k.P, k.S, k.NT
    CAP = moe_cap(S)
    DUMP = NE * CAP
    PSL = DUMP + 128
    k.CAP, k.PSL = CAP, PSL
    MIXT, bMIXT = k.scr["MIXT"], k.scr_buf["MIXT"]
    H1 = k.scratch("H1", [S, D], F32)
    H2N = k.scratch("H2N", [S, D], BF16)
    XS, ROUTE = k.scr["XS"], k.scr["ROUTE"]
    GATE = k.scratch("GATE", [128, NT * 2], F32)
    bH1, bH2N, bXS, bROUTE, bGATE = (k.scr_buf[n] for n in ("H1", "H2N", "XS", "ROUTE", "GATE"))
    MIXv = MIXT.rearrange("(c p) t -> p c t", p=128)
    with ExitStack() as es:
        def sb(name, shape, dt):
            return es.enter_context(nc.sbuf_tensor("sb_" + name, list(shape), dt)), Buf(name)
        identf, b_identf = sb("r_identf", [128, 128], F32)
        onesf, b_onesf = sb("r_onesf", [128, 128], F32)
        ustr, b_ustr = sb("r_ustr", [128, 128], F32)
        id32, b_id32 = sb("r_id32", [32, 32], F32)
        ecap, b_ecap = sb("r_ecap", [32, 2], F32)
        limr, b_limr = sb("r_limr", [128, 32], F32)
        piota, b_piota = sb("r_piota", [128, 1], F32)
        toki, b_toki = sb("r_toki", [128, NT], I32)
        for t_, b_, nm in ((identf, b_identf, "ident_f"), (onesf, b_onesf, "ones_f"), (ustr, b_ustr, "ustrict"),
                           (id32, b_id32, "ident32"), (ecap, b_ecap, "blk_iota"), (limr, b_limr, "lim_row"),
                           (piota, b_piota, "p_iota"), (toki, b_toki, "tok_iota")):
            k.dma("sp", t_[:], k.din[nm], w=[b_])
        gff, b_gff = sb("r_gff", [128, D], F32)
        k.dma("sp", gff[:], k.din["norm_ffn_l"][0:1, :].partition_broadcast(128), w=[b_gff])
        k.ts("dve", gff[:], gff[:], 32.0, None, ALU.mult, r=[b_gff], w=[b_gff])
        brt, b_brt = sb("r_brt", [128, 36], F32)
        k.dma("sp", brt[:], k.din["b_rt"][0:1, :].partition_broadcast(128), w=[b_brt])
        wrt, b_wrt = sb("r_wrt", [128, 8, 36], F32)
        k.dma("sp", wrt[:], k.din["w_rt"].rearrange("(c p) n -> p c n", p=128), w=[b_wrt])
        eps1k, b_eps = sb("r_eps", [128, 1], F32)
        P.add("pool", lambda e: e.memset(eps1k[:], float(D * EPS)), (), [b_eps])
        wout, b_wout = sb("r_wout", [128, 8, D], BF16)
        wst = sb_ring(es, nc, "r_wst", 2, [128, D], F32)
        w_out_v = k.din["w_out"].rearrange("(c p) n -> p c n", p=128)
        for c in range(8):
            st, bst = wst.next()
            k.dma("sp", st[:], w_out_v[:, c, :], w=[bst])
            k.cp("dve" if c % 2 == 0 else "pool", wout[:, c, :], st[:], r=[bst], w=[b_wout])
        dcol, b_dcol = sb("r_dcol", [128, 2], F32)
        k.ts("dve", dcol[:, 0:1], piota[:, 0:1], float(2 * S), None, ALU.add, r=[b_piota], w=[b_dcol])
        k.ts("dve", dcol[:, 1:2], piota[:, 0:1], float(DUMP), None, ALU.add, r=[b_piota], w=[b_dcol])
        tokst, b_tokst = sb("r_tokst", [128, NT, 2, 16], I32)
        P.add("pool", lambda e: e.memset(tokst[:], 0), (), [b_tokst])
        k.ts("dve", tokst[:, :, 0, 0], toki[:], 1, None, ALU.logical_shift_left, r=[b_toki, b_tokst], w=[b_tokst])
        k.ts("dve", tokst[:, :, 1, 0], tokst[:, :, 0, 0], 1, None, ALU.add, r=[b_tokst], w=[b_tokst])

        k.fence([bXS, bROUTE])
        Aall, b_A = sb("r_A", [128, NT, 32], F32)
        M12, b_M = sb("r_M12", [128, NT, 2, 32], F32)
        gates, b_gates = sb("r_gates", [128, NT, 2], F32)
        dest, b_dest = sb("r_dest", [128, NT, 2], F32)
        desti, b_desti = sb("r_desti", [128, NT, 2], I32)

        mixr = sb_free(es, nc, "r_mix", 3, [128, 8, 128], BF16)
        xr = sb_free(es, nc, "r_x", 3, [128, D], F32)
        h1r = sb_free(es, nc, "r_h1", 3, [128, D], F32)
        junkr = sb_free(es, nc, "r_junk", 2, [128, D], BF16)
        h2r = sb_free(es, nc, "r_h2", 3, [128, D], F32)
        h2bf = sb_free(es, nc, "r_h2bf", 3, [128, D], BF16)
        h2br = sb_ring(es, nc, "r_h2b", 3, [128, D], BF16)
        h2Tr = sb_free(es, nc, "r_h2T", 2, [128, 8, 128], F32)
        smr = sb_free(es, nc, "r_sm", 4, [128, 16], F32)
        lgr = sb_free(es, nc, "r_lg", 4, [128, 36], F32)
        emr = sb_free(es, nc, "r_em", 4, [128, 32], F32)
        t8r = sb_free(es, nc, "r_t8", 4, [128, 8], F32)
        g4r = sb_free(es, nc, "r_g4", 4, [128, 12], F32)
        psf = ps_free(es, nc, "r_psf", 8)

        def tile_gen(ti):
            r0 = ti * 128
            mx, bmx, imx = yield from mixr.get()
            k.load(mx[:], MIXv[:, :, r0:r0 + 128], r=[bMIXT], w=[bmx])
            xt, bx, ix = yield from xr.get()
            k.load(xt[:], k.x[r0:r0 + 128, :], w=[bx])
            yield
            h1, bh1, ih1 = yield from h1r.get()
            pss = []
            for half in range(2):
                ps, bps, ips = yield from psf.get()
                for c in range(8):
                    k.mm(ps[:], mx[:, c, :], wout[:, c, half * 512:(half + 1) * 512], start=(c == 0), stop=(c == 7),
                         r=[bmx, b_wout], w=[bps])
                pss.append((ps, bps, ips))
            mixr.put(imx)
            yield
            for half, (ps, bps, ips) in enumerate(pss):
                k.tt("dve", h1[:, half * 512:(half + 1) * 512], ps[:], xt[:, half * 512:(half + 1) * 512], ALU.add,
                     r=[bps, bx], w=[bh1])
                psf.put(ips)
            xr.put(ix)
            k.store(H1[r0:r0 + 128, :], h1[:], r=[bh1], w=[bH1])
            yield
            sm, bsm, ism = yield from smr.get()
            junk, bjunk, ijunk = yield from junkr.get()
            k.act(junk[:], h1[:], AF.Square, r=[bh1], w=[bjunk, bsm], accum_out=sm[:, 0:1])
            junkr.put(ijunk)
            k.act(sm[:, 1:2], sm[:, 0:1], AF.Ln, r=[bsm, b_eps], w=[bsm], bias=eps1k[:, 0:1])
            k.act(sm[:, 2:3], sm[:, 1:2], AF.Exp, r=[bsm], w=[bsm], scale=-0.5)
            yield
            h2, bh2, ih2 = yield from h2r.get()
            k.ts("dve", h2[:], h1[:], sm[:, 2:3], None, ALU.mult, r=[bh1, bsm], w=[bh2])
            yield
            k.tt("pool", h2[:], h2[:], gff[:], ALU.mult, r=[bh2, b_gff], w=[bh2])
            yield
            h2b, bh2b, ih2b = yield from h2bf.get()
            k.cp("act", h2b[:], h2[:], r=[bh2], w=[bh2b])
            k.store(H2N[r0:r0 + 128, :], h2b[:], r=[bh2b], w=[bH2N])
            h2T, bh2T, ih2T = yield from h2Tr.get()
            pss = []
            for half in range(2):
                ps, bps, ips = yield from psf.get()
                for c4 in range(4):
                    c = half * 4 + c4
                    k.tr(ps[:, c4 * 128:(c4 + 1) * 128], h2[:, c * 128:(c + 1) * 128], identf[:],
                         r=[bh2, b_identf], w=[bps])
                pss.append((ps, bps, ips))
            h2r.put(ih2)
            yield
            for half, (ps, bps, ips) in enumerate(pss):
                k.cp("dve" if half == 0 else "act", h2T[:, half * 4:(half + 1) * 4, :].rearrange("p c t -> p (c t)"), ps[:],
                     r=[bps], w=[bh2T])
                psf.put(ips)
            yield
            lps, blps, ilps = yield from psf.get()
            for c in range(8):
                k.mm(lps[:, 0:36], h2T[:, c, :], wrt[:, c, :], start=(c == 0), stop=(c == 7), r=[bh2T, b_wrt], w=[blps])
            h2Tr.put(ih2T)
            yield
            lg, blg, ilg = yield from lgr.get()
            k.tt("dve", lg[:], lps[:, 0:36], brt[:], ALU.add, r=[blps, b_brt], w=[blg])
            psf.put(ilps)
            g4, bg4, ig4 = yield from g4r.get()
            P.add("dve", lambda e, g4=g4, lg=lg: e.reduce_max(g4[:, 0:1], lg[:, 0:4], AX.X), [blg], [bg4])
            k.ts("dve", g4[:, 1:2], g4[:, 0:1], -1.0, None, ALU.mult, r=[bg4], w=[bg4])
            yield
            k.act(sm[:, 4:8], lg[:, 0:4], AF.Exp, r=[blg, bg4], w=[bsm, bg4], bias=g4[:, 1:2], accum_out=g4[:, 2:3])
            k.ts("dve", g4[:, 4:8], lg[:, 0:4], g4[:, 0:1], None, ALU.is_equal, r=[blg, bg4], w=[bg4])
            k.ts("dve", g4[:, 8:12], g4[:, 4:8], 1.0e30, -1.0e30, ALU.mult, ALU.add, r=[bg4], w=[bg4])
            em, bem, iem = yield from emr.get()
            for g in range(NG):
                k.ts("dve", em[:, g * 8:(g + 1) * 8], lg[:, 4 + g * 8: 12 + g * 8], g4[:, 4 + g:5 + g], g4[:, 8 + g:9 + g],
                     ALU.mult, ALU.add, r=[blg, bg4], w=[bem])
            lgr.put(ilg)
            yield
            P.add("dve", lambda e, g4=g4: e.reciprocal(g4[:, 3:4], g4[:, 2:3]), [bg4], [bg4])
            t8, bt8, it8 = yield from t8r.get()
            P.add("dve", lambda e, t8=t8, em=em: e.max(t8[:], em[:]), [bem], [bt8])
            yield
            k.ts("dve", M12[:, ti, 0, :], em[:], t8[:, 0:1], None, ALU.is_equal, r=[bem, bt8], w=[b_M])
            k.ts("dve", M12[:, ti, 1, :], em[:], t8[:, 1:2], None, ALU.is_equal, r=[bem, bt8], w=[b_M])
            emr.put(iem)
            k.tt("dve", sm[:, 8:9], t8[:, 1:2], t8[:, 0:1], ALU.subtract, r=[bt8], w=[bsm])
            t8r.put(it8)
            yield
            k.tt("dve", Aall[:, ti, :], M12[:, ti, 0, :], M12[:, ti, 1, :], ALU.add, r=[b_M], w=[b_A])
            k.act(sm[:, 9:10], sm[:, 8:9], AF.Exp, r=[bsm], w=[bsm])
            yield
            k.ts("dve", sm[:, 9:10], sm[:, 9:10], 1.0, None, ALU.add, r=[bsm], w=[bsm])
            yield
            P.add("dve", lambda e, sm=sm: e.reciprocal(sm[:, 10:11], sm[:, 9:10]), [bsm], [bsm])
            yield
            k.tt("dve", gates[:, ti, 0:1], sm[:, 10:11], g4[:, 3:4], ALU.mult, r=[bsm, bg4], w=[b_gates])
            yield
            k.tt("dve", gates[:, ti, 1:2], g4[:, 3:4], gates[:, ti, 0:1], ALU.subtract, r=[bg4, b_gates], w=[b_gates])
            smr.put(ism)
            g4r.put(ig4)
            h1r.put(ih1)
            h2bf.put(ih2b)

        run_pipelined([tile_gen(ti) for ti in range(NT)], 3)

        cps, bcps, icps = psf.take()
        for ti in range(NT):
            k.mm(cps[0:32, ti:ti + 1], Aall[:, ti, :], onesf[:, 0:1], r=[b_A, b_onesf], w=[bcps])
        cnt, b_cnt = sb("r_cnt", [32, NT], F32)
        k.cp("dve", cnt[:], cps[0:32, 0:NT], r=[bcps], w=[b_cnt])
        inc, b_inc = sb("r_inc", [32, NT], F32)
        onesr, b_onesr = sb("r_onesr", [32, NT], F32)
        P.add("pool", lambda e: e.memset(onesr[:], 1.0), (), [b_onesr])
        P.add("dve", lambda e: e.tensor_tensor_scan(inc[:], onesr[:], cnt[:], 0.0, ALU.mult, ALU.add),
              [b_onesr, b_cnt], [b_inc])
        off, b_off = sb("r_off", [32, NT], F32)
        k.tt("dve", off[:], inc[:], cnt[:], ALU.subtract, r=[b_inc, b_cnt], w=[b_off])
        k.ts("dve", off[:], off[:], ecap[:, 0:1], None, ALU.add, r=[b_off, b_ecap], w=[b_off])
        dgr = sb_free(es, nc, "r_dg", 3, [32, 32], F32)
        tmr = sb_free(es, nc, "r_tm", 3, [128, 4, 32], F32)
        okr = sb_free(es, nc, "r_ok", 3, [128, 3, 32], F32)
        gkr = sb_free(es, nc, "r_gk", 3, [128, 2], F32)

        def slot_gen(ti):
            h2b, bh2b, ih2b = yield from h2bf.get()
            k.load(h2b[:], H2N[ti * 128:(ti + 1) * 128, :], r=[bH2N], w=[bh2b])
            dg, bdg, idg = yield from dgr.get()
            k.ts("dve", dg[:], id32[:], off[:, ti:ti + 1], None, ALU.mult, r=[b_id32, b_off], w=[bdg])
            yield
            sps, bsps, isps = yield from psf.get()
            k.mm(sps[:, 0:32], ustr[:], Aall[:, ti, :], start=True, stop=False, r=[b_ustr, b_A], w=[bsps])
            k.mm(sps[:, 0:32], onesf[0:32, :], dg[:], start=False, stop=True, r=[b_onesf, bdg], w=[bsps])
            dgr.put(idg)
            yield
            ok, bok, iok = yield from okr.get()
            k.tt("dve", ok[:, 0, :], sps[:, 0:32], limr[:], ALU.is_lt, r=[bsps, b_limr], w=[bok])
            yield
            k.tt("dve", ok[:, 1, :], sps[:, 0:32], ok[:, 0, :], ALU.mult, r=[bsps, bok], w=[bok])
            psf.put(isps)
            k.ts("dve", ok[:, 2, :], ok[:, 0, :], -1.0, 1.0, ALU.mult, ALU.add, r=[bok], w=[bok])
            yield
            k.ts("dve", ok[:, 2, :], ok[:, 2, :], dcol[:, 1:2], None, ALU.mult, r=[bok, b_dcol], w=[bok])
            yield
            k.tt("dve", ok[:, 1, :], ok[:, 1, :], ok[:, 2, :], ALU.add, r=[bok], w=[bok])
            yield
            tm, btm, itm = yield from tmr.get()
            for j in range(2):
                k.tt("dve", tm[:, j, :], M12[:, ti, j, :], ok[:, 1, :], ALU.mult, r=[b_M, bok], w=[btm])
                k.tt("dve", tm[:, 2 + j, :], M12[:, ti, j, :], ok[:, 0, :], ALU.mult, r=[b_M, bok], w=[btm])
            okr.put(iok)
            yield
            P.add("dve", lambda e, tm=tm, ti=ti: e.reduce_sum(dest[:, ti, :], tm[:, 0:2, :], AX.X), [btm], [b_dest])
            gk, bgk, igk = yield from gkr.get()
            P.add("dve", lambda e, tm=tm, gk=gk: e.reduce_sum(gk[:], tm[:, 2:4, :], AX.X), [btm], [bgk])
            tmr.put(itm)
            yield
            k.tt("dve", gates[:, ti, :], gates[:, ti, :], gk[:], ALU.mult, r=[b_gates, bgk], w=[b_gates])
            gkr.put(igk)
            k.cp("dve", desti[:, ti, :], dest[:, ti, :], r=[b_dest], w=[b_desti])
            yield
            for j in range(2):
                P.add("pool", lambda e, ti=ti, j=j: e.indirect_dma_start(
                    out=ROUTE, out_offset=bass.IndirectOffsetOnAxis(ap=desti[:, ti, j:j + 1], axis=0),
                    in_=tokst[:, ti, j, :], in_offset=None), [b_desti, b_tokst], [bROUTE], dma=True)
                P.add("pool", lambda e, ti=ti, j=j, h2b=h2b: e.indirect_dma_start(
                    out=XS, out_offset=bass.IndirectOffsetOnAxis(ap=desti[:, ti, j:j + 1], axis=0),
                    in_=h2b[:], in_offset=None), [b_desti, bh2b], [bXS], dma=True)
                yield
            h2bf.put(ih2b)

        run_pipelined([slot_gen(ti) for ti in range(NT)], 3)
        k.dma("sp", GATE, gates[:].rearrange("p t j -> p (t j)"), r=[b_gates], w=[bGATE])
        k.flush()
        P.drain_dmas()
        P.emit()


def phase_moe(k):
    nc, P, S, NT = k.nc, k.P, k.S, k.NT
    B, CAP, PSL = MOE_B, k.CAP, k.PSL
    NS = B // 128
    H1, XS, ROUTE, GATE = (k.scr[n] for n in ("H1", "XS", "ROUTE", "GATE"))
    bH1, bXS, bROUTE, bGATE = (k.scr_buf[n] for n in ("H1", "XS", "ROUTE", "GATE"))
    YT = k.scr["YT"]
    bYT = k.scr_buf["YT"]
    WG = k.din["w_gate"].rearrange("e (p c) n -> e p (c n)", p=128)
    WU = k.din["w_up"].rearrange("e (p c) n -> e p (c n)", p=128)
    WD = k.din["w_down"].rearrange("e (p c) n -> e p (c n)", p=128)
    with ExitStack() as es:
        def sb(name, shape, dt):
            return es.enter_context(nc.sbuf_tensor("sb_" + name, list(shape), dt)), Buf(name)
        identb, b_identb = sb("m_ident", [128, 128], BF16)
        k.dma("sp", identb[:], k.din["ident_bf"], w=[b_identb])
        k.fence([bYT])
        tkr = sb_free(es, nc, "m_tk", 6, [128, 16], I32)
        xgr = sb_free(es, nc, "m_xg", 6, [128, D], BF16)
        wfr = sb_free(es, nc, "m_wf", 3, [128, 2048], F32)
        wgr = sb_free(es, nc, "m_wg", 2, [128, 8, DE], BF16)
        wur = sb_free(es, nc, "m_wu", 2, [128, 8, DE], BF16)
        wdr = sb_free(es, nc, "m_wd", 2, [128, 2, D], BF16)
        xTr = sb_free(es, nc, "m_xT", 3, [128, 8, B], BF16)
        sgr = sb_free(es, nc, "m_sg", 3, [128, B], F32)
        hTr = sb_free(es, nc, "m_hT", 3, [128, 2, B], BF16)
        ysr = sb_free(es, nc, "m_ys", 6, [128, D], BF16)
        psf = ps_free(es, nc, "m_psf", 6)
        psb = ps_free(es, nc, "m_psb", 2, (128, 1024), BF16)
        ceng = ["dve", "pool", "act"]
        wtiles = {}

        def wprep_gen(ex):
            wts = []
            for wi_, (src, pool_) in enumerate(((WG, wgr), (WU, wur), (WD, wdr))):
                wf, bwf, iwf = yield from wfr.get()
                k.load(wf[:], src[ex], w=[bwf])
                wb, bwb, iwb = yield from pool_.get()
                wts.append((wf, bwf, iwf, wb, bwb, iwb, pool_))
            wtiles[ex] = [(wb, bwb, iwb, pool_) for (_, _, _, wb, bwb, iwb, pool_) in wts]
            wtiles[(ex, "left")] = CAP // B
            yield
            for wi_, (wf, bwf, iwf, wb, bwb, iwb, pool_) in enumerate(wts):
                k.cp(ceng[wi_], wb[:].rearrange("p c n -> p (c n)"), wf[:], r=[bwf], w=[bwb])
                wfr.put(iwf)
                yield

        def blk_gen(ex, blk):
            (wg, bwg, _, _), (wu, bwu, _, _), (wd, bwd, _, _) = wtiles[ex]
            wgv = wg[:].rearrange("p c (q j) -> p c j q", j=2)
            wuv = wu[:].rearrange("p c (q j) -> p c j q", j=2)
            s0 = ex * CAP + blk * B
            xT, bxT, ixT = yield from xTr.get()
            tks, xgs = [], []
            for s_ in range(NS):
                tk, btk, itk = yield from tkr.get()
                k.load(tk[:], ROUTE[s0 + s_ * 128: s0 + (s_ + 1) * 128, :], r=[bROUTE], w=[btk])
                tks.append((tk, btk, itk))
                xg, bxg, ixg = yield from xgr.get()
                k.load(xg[:], XS[s0 + s_ * 128: s0 + (s_ + 1) * 128, :], r=[bXS], w=[bxg])
                xgs.append((xg, bxg, ixg))
            yield
            for s_ in range(NS):
                xg, bxg, ixg = xgs[s_]
                pb, bpb, ipb = yield from psb.get()
                xgv = xg[:].rearrange("p (q c) -> p c q", c=8)
                for c in range(8):
                    k.tr(pb[:, c * 128:(c + 1) * 128], xgv[:, c, :], identb[:], r=[bxg, b_identb], w=[bpb])
                xgr.put(ixg)
                yield
                k.cp("act" if s_ % 2 == 0 else "dve", xT[:, :, s_ * 128:(s_ + 1) * 128],
                     pb[:].rearrange("p (c t) -> p c t", c=8), r=[bpb], w=[bxT])
                psb.put(ipb)
            yield
            hT, bhT, ihT = yield from hTr.get()
            for c in range(2):
                gps, bgps, igps = yield from psf.get()
                ups, bups, iups = yield from psf.get()
                for kc in range(8):
                    k.mm(gps[:, 0:B], wgv[:, kc, c, :], xT[:, kc, :], start=(kc == 0), stop=(kc == 7),
                         r=[bwg, bxT], w=[bgps])
                for kc in range(8):
                    k.mm(ups[:, 0:B], wuv[:, kc, c, :], xT[:, kc, :], start=(kc == 0), stop=(kc == 7),
                         r=[bwu, bxT], w=[bups])
                yield
                sg, bsg, isg = yield from sgr.get()
                k.act(sg[:], gps[:, 0:B], AF.Silu, r=[bgps], w=[bsg])
                psf.put(igps)
                yield
                k.tt("dve", hT[:, c, :], sg[:], ups[:, 0:B], ALU.mult, r=[bsg, bups], w=[bhT])
                psf.put(iups)
                sgr.put(isg)
            xTr.put(ixT)
            yield
            for s_ in range(NS):
                ys, bys, iys = yield from ysr.get()
                for half in range(2):
                    yps, byps, iyps = yield from psf.get()
                    for c in range(2):
                        k.mm(yps[:], hT[:, c, s_ * 128:(s_ + 1) * 128], wd[:, c, half * 512:(half + 1) * 512],
                             start=(c == 0), stop=(c == 1), r=[bhT, bwd], w=[byps])
                    yield
                    k.cp("act" if half == 0 else "dve", ys[:, half * 512:(half + 1) * 512], yps[:], r=[byps], w=[bys])
                    psf.put(iyps)
                tk, btk, itk = tks[s_]
                P.add("pool", lambda e, ys=ys, tk=tk: e.indirect_dma_start(
                    out=YT, out_offset=bass.IndirectOffsetOnAxis(ap=tk[:, 0:1], axis=0),
                    in_=ys[:], in_offset=None), [btk, bys], [bYT], dma=True)
                tkr.put(itk)
                ysr.put(iys)
            hTr.put(ihT)
            wtiles[(ex, "left")] -= 1
            if wtiles[(ex, "left")] == 0:
                for (_, _, iwb, pool_) in wtiles[ex]:
                    pool_.put(iwb)

        for _ in wprep_gen(0):
            pass
        gens = []
        for ex in range(NE):
            if ex + 1 < NE:
                gens.append(wprep_gen(ex + 1))
            gens += [blk_gen(ex, blk) for blk in range(CAP // B)]
        run_pipelined(gens, 3)
        k.flush()
        P.drain_dmas()
        P.emit()
    with ExitStack() as es:
        def sb(name, shape, dt):
            return es.enter_context(nc.sbuf_tensor("sb_" + name, list(shape), dt)), Buf(name)
        gates, b_gates = sb("c_gate", [128, NT * 2], F32)
        k.dma("sp", gates[:], GATE, r=[bGATE], w=[b_gates])
        h1r = sb_ring(es, nc, "c_h1", 3, [128, D], F32)
        ytr = sb_ring(es, nc, "c_yt", 3, [128, 2, D], BF16)
        by = Buf("y")
        YTv = YT[0:2 * S, :].rearrange("(t j) n -> t j n", j=2)
        for ti in range(NT):
            r0 = ti * 128
            h1, bh1 = h1r.next()
            k.load(h1[:], H1[r0:r0 + 128, :], r=[bH1], w=[bh1])
            yt, byt = ytr.next()
            k.load(yt[:], YTv[r0:r0 + 128, :, :], r=[bYT], w=[byt])
            for j in range(2):
                P.add("dve", lambda e, yt=yt, h1=h1, ti=ti, j=j: e.scalar_tensor_tensor(
                    h1[:], yt[:, j, :], gates[:, 2 * ti + j:2 * ti + j + 1], h1[:], ALU.mult, ALU.add),
                    [byt, b_gates, bh1], [bh1])
            k.store(k.y[r0:r0 + 128, :], h1[:], r=[bh1], w=[by])
        k.flush()
        P.drain_dmas()
        P.emit()


def build_program(S):
    k = K(S)
    phase1(k)
    phase_attn(k)
    phase_hgrn(k)
    phase_route(k)
    phase_moe(k)
    return k


def kernel(**inputs):
    x = np.asarray(inputs["x"], dtype=np.float32)
    nb, S, _ = x.shape
    k = build_program(S)
    par = layout_params(inputs)
    con = make_consts(S)
    in_maps = []
    for c in range(nb):
        m = {"x": np.ascontiguousarray(x[c])}
        m.update(par)
        m.update(con)
        in_maps.append(m)
    res = run_bass_kernel_spmd(k.nc, in_maps, core_ids=list(range(nb)))
    return np.stack([np.asarray(r["y"], dtype=np.float32) for r in res.results], axis=0)
```

```python
import numpy as np
from contextlib import ExitStack
import concourse.bass as bass
import concourse.mybir as mybir
from concourse.bass_utils import run_bass_kernel_spmd

F32 = mybir.dt.float32
BF16 = mybir.dt.bfloat16
I32 = mybir.dt.int32
U32 = mybir.dt.uint32
U8 = mybir.dt.uint8
AF = mybir.ActivationFunctionType
ALU = mybir.AluOpType
AX = mybir.AxisListType

ENGS = ("pe", "act", "dve", "pool", "sp")
EPS = 1e-6
D = 1024
NIN = 2976
H = 8
QK = 96
NOPE = 64
ROPE = 32
DV = 64
HGH = 4
NE = 32
NG = 4
DE = 256
MOE_B = 256
MOE_LOG2B = 8
import os
MOE_DBG = int(os.environ.get('MOE_DBG', '0'))
class Buf:
    __slots__ = ("name", "last_w", "readers", "multi", "writers")

    def __init__(self, name="", multi=False):
        self.name = name
        self.last_w = None
        self.readers = {}
        self.multi = multi
        self.writers = []


class Op:
    __slots__ = ("eng", "fn", "waits", "signal", "ticket", "is_dma", "sem", "target", "idx", "emitted", "drained")


class Prog:
    def __init__(self, nc, n_dma_sems=40, n_sw_sems=8):
        self.nc = nc
        self.eng_obj = {"pe": nc.tensor, "act": nc.scalar, "dve": nc.vector,
                        "pool": nc.gpsimd, "sp": nc.sync}
        self.sem = {}
        self.count = {e: 0 for e in ENGS}
        self._stack = []
        for e in ENGS:
            g = nc.semaphore("s_" + e)
            self.sem[e] = g.__enter__()
            self._stack.append(g)
        self.dma_sems = []
        self.dma_uses = []
        self.dma_last = []
        for i in range(n_dma_sems):
            g = nc.semaphore("d_%d" % i)
            self.dma_sems.append(g.__enter__())
            self._stack.append(g)
            self.dma_uses.append(0)
            self.dma_last.append(None)
        self.dma_rr = 0
        self.sw_sems = []
        self.sw_uses = []
        self.sw_last = []
        for i in range(n_sw_sems):
            g = nc.semaphore("w_%d" % i)
            self.sw_sems.append(g.__enter__())
            self._stack.append(g)
            self.sw_uses.append(0)
            self.sw_last.append(None)
        self.sw_rr = 0
        self.ops = {e: [] for e in ENGS}
        self.waited = {e: {} for e in ENGS}
        self.nops = 0
        self.pending_dma = []
        self._deferred = []
        self._def_src = set()

    def defer_dma(self, eng, out, in_, reads=(), writes=(), **kw):
        self._deferred.append((eng, out, in_, tuple(reads), tuple(writes), kw))
        for b in reads:
            self._def_src.add(id(b))

    def flush(self):
        d, self._deferred = self._deferred, []
        self._def_src = set()
        for eng, out, in_, r, w, kw in d:
            self.dma(eng, out, in_, r, w, **kw)

    def add(self, eng, fn, reads=(), writes=(), dma=False):
        if self._deferred and any(id(b) in self._def_src for b in writes):
            self.flush()
        op = Op()
        op.eng = eng
        op.fn = fn
        op.waits = []
        op.signal = False
        op.ticket = None
        op.is_dma = dma
        op.sem = None
        op.target = None
        op.idx = self.nops
        op.emitted = False
        op.drained = False
        self.nops += 1
        deps = {}
        raw = set()
        for b in reads:
            if b.multi:
                b.writers = [w_ for w_ in b.writers if not (w_.emitted and (w_.drained or not w_.is_dma))]
                for w_ in b.writers:
                    deps[id(w_)] = w_
            elif b.last_w is not None:
                deps[id(b.last_w)] = b.last_w
                raw.add(id(b.last_w))
        for b in writes:
            if (not b.multi) and b.last_w is not None:
                deps[id(b.last_w)] = b.last_w
            for r in b.readers.values():
                deps[id(r)] = r
        for k, d in deps.items():
            if d is op:
                continue
            if d.is_dma:
                if not (d.emitted and d.drained):
                    op.waits.append(d)
            elif d.emitted:
                continue
            elif d.eng == eng and not dma:
                if eng == "pe":
                    continue
                d.signal = True
                op.waits.append(d)
            else:
                d.signal = True
                op.waits.append(d)
        if dma and eng == "pool":
            i = self.sw_rr
            self.sw_rr = (self.sw_rr + 1) % len(self.sw_sems)
            prev = self.sw_last[i]
            if prev is not None:
                op.waits.append(prev)
            self.sw_uses[i] += 1
            op.sem = self.sw_sems[i]
            op.target = 16 * self.sw_uses[i]
            self.sw_last[i] = op
            self.pending_dma.append(op)
        elif dma:
            i = self.dma_rr
            self.dma_rr = (self.dma_rr + 1) % len(self.dma_sems)
            prev = self.dma_last[i]
            if prev is not None:
                op.waits.append(prev)
            self.dma_uses[i] += 1
            op.sem = self.dma_sems[i]
            op.target = 16 * self.dma_uses[i]
            self.dma_last[i] = op
            self.pending_dma.append(op)
        for b in reads:
            key = ("d", op.idx) if dma else eng
            b.readers[key] = op
        for b in writes:
            if b.multi:
                b.writers.append(op)
                b.readers = {k_: r_ for k_, r_ in b.readers.items() if not (r_.emitted and (r_.drained or not r_.is_dma))}
            else:
                b.last_w = op
                b.readers = {}
        self.ops[eng].append(op)
        return op

    def dma(self, eng, out, in_, reads=(), writes=(), **kw):
        return self.add(eng, lambda e: e.dma_start(out=out, in_=in_, **kw), reads, writes, dma=True)

    def drain_dmas(self, eng="sp"):
        op = self.add(eng, None)
        seen = {}
        for d in self.pending_dma:
            d.drained = True
            seen[id(d.sem)] = d
        op.waits.extend(seen.values())
        self.pending_dma = []
        return op

    def emit(self, name=None):
        for e in ENGS:
            c = self.count[e]
            for op in self.ops[e]:
                if op.is_dma:
                    continue
                if op.signal:
                    c += 1
                    op.ticket = c
            self.count[e] = c
        prog = self

        def run(e, engine):
            waited = prog.waited[e]
            for op in prog.ops[e]:
                for d in op.waits:
                    if d.is_dma:
                        key, sem, val = id(d.sem), d.sem, d.target
                    else:
                        key, sem, val = d.eng, prog.sem[d.eng], d.ticket
                    if waited.get(key, 0) >= val:
                        continue
                    waited[key] = val
                    engine.wait_ge(sem, val)
                if op.fn is None:
                    continue
                ins = op.fn(engine)
                if op.is_dma:
                    ins.then_inc(op.sem, 16)
                elif op.signal:
                    ins.then_inc(prog.sem[e], 1)

        with self.nc.Block() as block:
            @block.tensor
            def _(eng):
                run("pe", eng)

            @block.scalar
            def _(eng):
                run("act", eng)

            @block.vector
            def _(eng):
                run("dve", eng)

            @block.gpsimd
            def _(eng):
                run("pool", eng)

            @block.sync
            def _(eng):
                run("sp", eng)
        for e in ENGS:
            for op in self.ops[e]:
                op.emitted = True
        self.ops = {e: [] for e in ENGS}


def _bf(a):
    import ml_dtypes
    return np.asarray(a, dtype=np.float32).astype(ml_dtypes.bfloat16)


def moe_cap(S):
    return max(MOE_B, ((3 * S // 32 + MOE_B - 1) // MOE_B) * MOE_B)


def make_consts(S):
    c = {}
    c["ident_bf"] = _bf(np.eye(128))
    c["ident_f"] = np.eye(128, dtype=np.float32)
    c["ones_bf"] = _bf(np.ones((128, 128)))
    c["ones_f"] = np.ones((128, 128), dtype=np.float32)
    rot = np.zeros((96, 96), np.float32)
    for i in range(16):
        rot[80 + i, 64 + i] = -1.0
        rot[64 + i, 80 + i] = 1.0
    c["rotT"] = _bf(rot)
    sel = np.zeros((32, 96), np.float32)
    for i in range(32):
        sel[i, 64 + i] = 1.0
    c["kr_sel"] = _bf(sel)
    half = 16
    inv = (1.0 / (10000.0 ** (np.arange(half, dtype=np.float32) / half))).astype(np.float32)
    ang = (np.arange(S, dtype=np.float32)[None, :] * inv[:, None]).astype(np.float32)
    cs = np.zeros((96, S), np.float32)
    sn = np.zeros((96, S), np.float32)
    cs[64:80] = np.cos(ang); cs[80:96] = np.cos(ang)
    sn[64:80] = np.sin(ang); sn[80:96] = np.sin(ang)
    c["rope_cos"] = cs
    c["rope_sin"] = sn
    s_ = np.arange(128)[:, None]
    t_ = np.arange(128)[None, :]
    c["hg_Lc_f"] = ((s_ <= t_).astype(np.float32) - (s_ <= 63).astype(np.float32))
    c["hg_Lr_f"] = (s_ > t_).astype(np.float32)
    c["hg_Lc_b"] = ((s_ >= t_).astype(np.float32) - (s_ >= 64).astype(np.float32))
    c["hg_Lr_b"] = (s_ < t_).astype(np.float32)
    c["hg_selm_f"] = np.stack([np.ones(128), (np.arange(128) <= 63)], 1).astype(np.float32)
    c["hg_selm_b"] = np.stack([np.ones(128), (np.arange(128) >= 64)], 1).astype(np.float32)
    mf = (s_ <= t_).astype(np.uint32)
    mb = (s_ >= t_).astype(np.uint32)
    c["hg_mask_f"] = np.tile(mf, (1, 4))
    c["hg_mask_b"] = np.tile(mb, (1, 4))
    c["ustrict"] = (s_ < t_).astype(np.float32)
    c["u32strict"] = (np.arange(32)[:, None] < np.arange(32)[None, :]).astype(np.float32)
    c["ident32"] = np.eye(32, dtype=np.float32)
    cap = moe_cap(S)
    c["blk_iota"] = np.stack([np.arange(32) * cap, np.zeros(32)], 1).astype(np.float32)
    c["lim_row"] = np.tile(((np.arange(32) + 1) * cap).astype(np.float32)[None, :], (128, 1))
    c["p_iota"] = np.arange(128, dtype=np.float32).reshape(128, 1)
    c["tok_iota"] = (np.arange(S // 128, dtype=np.int32)[None, :] * 128 + np.arange(128, dtype=np.int32)[:, None]).astype(np.int32)
    return c


CONST_SPECS = {
    "ustrict": ([128, 128], F32), "u32strict": ([32, 32], F32), "ident32": ([32, 32], F32),
    "blk_iota": ([32, 2], F32), "lim_row": ([128, 32], F32), "p_iota": ([128, 1], F32), "tok_iota": "tok",
    "ident_bf": ([128, 128], BF16), "ident_f": ([128, 128], F32),
    "ones_bf": ([128, 128], BF16), "ones_f": ([128, 128], F32),
    "rotT": ([96, 96], BF16), "kr_sel": ([32, 96], BF16),
    "rope_cos": None, "rope_sin": None,
    "hg_Lc_f": ([128, 128], F32), "hg_Lr_f": ([128, 128], F32),
    "hg_Lc_b": ([128, 128], F32), "hg_Lr_b": ([128, 128], F32),
    "hg_selm_f": ([128, 2], F32), "hg_selm_b": ([128, 2], F32),
    "hg_mask_f": ([128, 512], U32), "hg_mask_b": ([128, 512], U32),
}


def layout_params(inp):
    f = lambda a: np.ascontiguousarray(np.asarray(a, dtype=np.float32))
    p = {}
    p["norm_mix_l"] = f(inp["norm_mix"][0].reshape(8, 128).T)
    p["q_lat_norm_l"] = f(inp["q_lat_norm"][0].reshape(2, 128).T)
    p["kv_lat_norm_l"] = f(inp["kv_lat_norm"][0].reshape(1, 128).T)
    p["q_norm_l"] = f(inp["q_norm"][0].reshape(96, 1))
    p["k_norm_l"] = f(inp["k_norm"][0].reshape(96, 1))
    p["lb_logits_l"] = f(inp["lb_logits"].reshape(2, 2 * 512))
    p["hg_out_norm_l"] = f(inp["hg_out_norm"][0].reshape(1, 128))
    p["norm_ffn_l"] = f(inp["norm_ffn"][0].reshape(1, 1024))
    p["w_rt"] = f(np.concatenate([inp["w_group"][0], inp["w_router"][0]], axis=1))
    p["b_rt"] = f(np.concatenate([inp["b_group"][0], inp["b_router"][0]]).reshape(1, 36))
    p["w_in"] = f(inp["w_in"][0])
    p["w_uq"] = f(inp["w_uq"][0])
    p["w_ukv"] = f(inp["w_ukv"][0])
    p["w_out"] = f(inp["w_out"][0])
    p["w_gate"] = f(inp["w_gate"][0])
    p["w_up"] = f(inp["w_up"][0])
    p["w_down"] = f(inp["w_down"][0])
    return p


PARAM_SPECS = {
    "norm_mix_l": [128, 8], "q_lat_norm_l": [128, 2], "kv_lat_norm_l": [128, 1],
    "q_norm_l": [96, 1], "k_norm_l": [96, 1], "lb_logits_l": [2, 1024],
    "hg_out_norm_l": [1, 128], "norm_ffn_l": [1, 1024], "w_rt": [1024, 36], "b_rt": [1, 36],
    "w_in": [D, NIN], "w_uq": [256, 768], "w_ukv": [128, 1024], "w_out": [1024, 1024],
    "w_gate": [NE, D, DE], "w_up": [NE, D, DE], "w_down": [NE, DE, D],
}


class K:
    def __init__(self, S, debug=(), phases=None):
        self.S = S
        self.NT = S // 128
        self.NB = S // 512
        self.debug = set(debug)
        self.phases = phases
        self.nc = nc = bass.Bass("TRN2", target_bir_lowering=False)
        self.P = Prog(nc)
        self.din = {}
        self.x = nc.dram_tensor("x", [S, D], F32, kind="ExternalInput").ap()
        for k, shp in PARAM_SPECS.items():
            self.din[k] = nc.dram_tensor(k, shp, F32, kind="ExternalInput").ap()
        for k, spec in CONST_SPECS.items():
            if spec is None:
                shp, dt = [96, S], F32
            elif spec == "tok":
                shp, dt = [128, S // 128], I32
            else:
                shp, dt = spec
            self.din[k] = nc.dram_tensor(k, shp, dt, kind="ExternalInput").ap()
        self.y = nc.dram_tensor("y", [S, D], F32, kind="ExternalOutput").ap()
        self.scr = {}
        self.scr_buf = {}
        self.deferred = []
        self.fence_t = nc.alloc_sbuf_tensor("fence_scratch", [128, 1], F32)
        self.b_fence = Buf("fence")

    def scratch(self, name, shape, dt):
        kind = "ExternalOutput" if name in self.debug else "Internal"
        t = self.nc.dram_tensor(name, shape, dt, kind=kind).ap()
        self.scr[name] = t
        self.scr_buf[name] = Buf(name, multi=True)
        return t

    def mm(self, out, lhsT, rhs, start=True, stop=True, r=(), w=()):
        return self.P.add("pe", lambda e: e.matmul(out, lhsT, rhs, start=start, stop=stop), r, w)

    def tr(self, out, in_, ident, r=(), w=()):
        return self.P.add("pe", lambda e: e.transpose(out, in_, ident), r, w)

    def act(self, out, in_, func, r=(), w=(), eng="act", **kw):
        return self.P.add(eng, lambda e: e.activation(out, in_, func, **kw), r, w)

    def ts(self, eng, out, in0, s1, s2, op0, op1=None, r=(), w=(), **kw):
        if op1 is None:
            return self.P.add(eng, lambda e: e.tensor_scalar(out, in0, s1, s2, op0, **kw), r, w)
        return self.P.add(eng, lambda e: e.tensor_scalar(out, in0, s1, s2, op0, op1, **kw), r, w)

    def tt(self, eng, out, in0, in1, op, r=(), w=()):
        return self.P.add(eng, lambda e: e.tensor_tensor(out, in0, in1, op), r, w)

    def cp(self, eng, out, in_, r=(), w=()):
        if eng == "act":
            return self.P.add(eng, lambda e: e.copy(out, in_), r, w)
        return self.P.add(eng, lambda e: e.tensor_copy(out, in_), r, w)

    def dma(self, eng, out, in_, r=(), w=(), **kw):
        return self.P.dma(eng, out, in_, r, w, **kw)

    def load(self, out, in_, r=(), w=(), **kw):
        op = self.P.dma("sp", out, in_, r, w, **kw)
        self.flush()
        return op

    def store(self, out, in_, r=(), w=(), **kw):
        self.P.defer_dma("sp", out, in_, r, w, **kw)

    def fence(self, bufs):
        t = self.fence_t
        self.P.add("pool", lambda e: e.memset(t[0:1, 0:1], 0.0), list(bufs), [self.b_fence])

    def flush(self):
        self.P.flush()

    def make_eps(self, es):
        self.epst = {}
        self.b_epst = Buf("eps")
        for n in (96, 128, 256, 1024):
            t = es.enter_context(self.nc.sbuf_tensor("sb_eps%d" % n, [128, 1], F32))
            self.epst[n] = t
            self.P.add("pool", lambda e, t=t, n=n: e.memset(t[:], float(n * EPS)), (), [self.b_epst])

    def rstd(self, out, in_, neps, bout, r=()):
        np_ = out.shape[0]
        self.P.add("act", lambda e: e.activation(out, in_, AF.Ln, bias=self.epst[neps][0:np_, 0:1]), list(r) + [self.b_epst], [bout])
        self.P.add("act", lambda e: e.activation(out, out, AF.Exp, scale=-0.5), [bout], [bout])


def run_pipelined(gens, depth):
    active = []
    it = iter(gens)
    while True:
        while len(active) < depth:
            g = next(it, None)
            if g is None:
                break
            active.append(g)
        if not active:
            break
        for g in list(active):
            if next(g, "done") == "done":
                active.remove(g)


class Pool_:
    def __init__(self, tiles):
        self.tiles = tiles
        self.bufs = [Buf() for _ in tiles]
        self.i = 0

    def next(self):
        i = self.i
        self.i = (self.i + 1) % len(self.tiles)
        return self.tiles[i], self.bufs[i]


class FreePool:
    def __init__(self, tiles):
        self.tiles = tiles
        self.bufs = [Buf() for _ in tiles]
        self.free = list(range(len(tiles)))

    def get(self):
        while not self.free:
            yield
        i = self.free.pop(0)
        return self.tiles[i], self.bufs[i], i

    def put(self, i):
        self.free.append(i)

    def take(self):
        i = self.free.pop(0)
        return self.tiles[i], self.bufs[i], i


def sb_free(es, nc, name, n, shape, dt):
    return FreePool([es.enter_context(nc.sbuf_tensor("sb_%s%d" % (name, i), shape, dt)) for i in range(n)])


def ps_free(es, nc, name, n, shape=(128, 512), dt=F32):
    return FreePool([es.enter_context(nc.psum_tensor("%s%d" % (name, i), list(shape), dt)) for i in range(n)])


def sb_ring(es, nc, name, n, shape, dt):
    return Pool_([es.enter_context(nc.sbuf_tensor("sb_%s%d" % (name, i), shape, dt)) for i in range(n)])


def ps_ring(es, nc, name, n, shape=(128, 512), dt=F32):
    return Pool_([es.enter_context(nc.psum_tensor("%s%d" % (name, i), list(shape), dt)) for i in range(n)])


def phase1(k):
    nc, P, S = k.nc, k.P, k.S
    QT = k.scratch("QT", [H, QK, S], BF16)
    KT = k.scratch("KT", [H, QK, S], BF16)
    VV = k.scratch("VV", [S, H * DV], BF16)
    HG = k.scratch("HG", [S, 2560], F32)
    bQT, bKT, bVV, bHG = (k.scr_buf[n] for n in ("QT", "KT", "VV", "HG"))
    with ExitStack() as es:
        def sb(name, shape, dt):
            return es.enter_context(nc.sbuf_tensor("sb_" + name, list(shape), dt)), Buf(name)
        ident, b_ident = sb("ident", [128, 128], BF16)
        ones, b_ones = sb("ones", [128, 128], BF16)
        rotT, b_rotT = sb("rotT", [96, 96], BF16)
        krsel, b_krsel = sb("krsel", [32, 96], BF16)
        for t_, b_, nm in ((ident, b_ident, "ident_bf"), (ones, b_ones, "ones_bf"),
                           (rotT, b_rotT, "rotT"), (krsel, b_krsel, "kr_sel")):
            k.dma("sp", t_[:], k.din[nm], w=[b_])
        k.make_eps(es)
        gmix, b_gmix = sb("gmix", [128, 8], F32)
        gql, b_gql = sb("gql", [128, 2], F32)
        gkvl, b_gkvl = sb("gkvl", [128, 1], F32)
        gq, b_gq = sb("gq", [96, 1], F32)
        gk, b_gk = sb("gk", [96, 1], F32)
        for t_, b_, nm in ((gmix, b_gmix, "norm_mix_l"), (gql, b_gql, "q_lat_norm_l"),
                           (gkvl, b_gkvl, "kv_lat_norm_l"), (gq, b_gq, "q_norm_l"), (gk, b_gk, "k_norm_l")):
            k.dma("sp", t_[:], k.din[nm], w=[b_])
        k.ts("dve", gmix[:], gmix[:], 32.0, None, ALU.mult, r=[b_gmix], w=[b_gmix])
        k.ts("dve", gql[:], gql[:], 16.0, None, ALU.mult, r=[b_gql], w=[b_gql])
        k.ts("dve", gkvl[:], gkvl[:], float(np.sqrt(128.0)), None, ALU.mult, r=[b_gkvl], w=[b_gkvl])
        k.ts("dve", gq[:], gq[:], float(np.sqrt(96.0) * 96.0 ** -0.5), None, ALU.mult, r=[b_gq], w=[b_gq])
        k.ts("dve", gk[:], gk[:], float(np.sqrt(96.0)), None, ALU.mult, r=[b_gk], w=[b_gk])

        win, b_win = sb("win", [128, 8, NIN], BF16)
        wst = sb_ring(es, nc, "wst", 1, [128, NIN], F32)
        w_in_v = k.din["w_in"].rearrange("(kc p) n -> p kc n", p=128)
        for kc in range(8):
            st, bst = wst.next()
            k.dma("sp", st[:], w_in_v[:, kc, :], w=[bst])
            k.ts("dve" if kc % 2 == 0 else "pool", win[:, kc, :], st[:], gmix[:, kc:kc + 1], None, ALU.mult,
                 r=[bst, b_gmix], w=[b_win])
        wuq, b_wuq = sb("wuq", [128, 2, 768], BF16)
        w_uq_v = k.din["w_uq"].rearrange("(kc p) n -> p kc n", p=128)
        for kc in range(2):
            st, bst = wst.next()
            k.dma("sp", st[:, 0:768], w_uq_v[:, kc, :], w=[bst])
            k.ts("dve", wuq[:, kc, :], st[:, 0:768], gql[:, kc:kc + 1], None, ALU.mult, r=[bst, b_gql], w=[b_wuq])
        wk, b_wk = sb("wk", [128, 8, 96], BF16)
        wv, b_wv = sb("wv", [128, 8, 64], BF16)
        st, bst = wst.next()
        k.dma("sp", st[:, 0:1024], k.din["w_ukv"], w=[bst])
        P.add("pool", lambda e: e.memset(wk[:], 0.0), (), [b_wk])
        stv = st[:, 0:1024].rearrange("p (h c) -> p h c", c=128)
        k.ts("dve", wk[:, :, 0:64], stv[:, :, 0:64], gkvl[:, 0:1], None, ALU.mult, r=[bst, b_gkvl], w=[b_wk])
        k.ts("dve", wv[:], stv[:, :, 64:128], gkvl[:, 0:1], None, ALU.mult, r=[bst, b_gkvl], w=[b_wv])

        xr = sb_ring(es, nc, "xt", 3, [128, D], F32)
        junkr = sb_ring(es, nc, "junk", 3, [128, D], BF16)
        ssr = sb_ring(es, nc, "ss", 4, [128, 2], F32)
        nr = sb_ring(es, nc, "nbf", 2, [128, D], BF16)
        nTr = sb_ring(es, nc, "nT", 2, [128, 8, 512], BF16)
        stg = sb_ring(es, nc, "stg", 2, [128, 2560], F32)
        sqq, b_sqq = sb("sqq", [128, 3, 512], BF16)
        rsl, b_rsl = sb("rsl", [128, 2, 512], F32)
        qnT, b_qnT = sb("qnT", [128, 2, 512], BF16)
        kvnT, b_kvnT = sb("kvnT", [128, 512], BF16)
        krT, b_krT = sb("krT", [32, 512], BF16)
        vsb = sb_ring(es, nc, "vsb", 2, [128, 512], BF16)
        cosr = sb_ring(es, nc, "cos", 2, [96, 512], F32)
        sinr = sb_ring(es, nc, "sin", 2, [96, 512], F32)
        sqh = sb_ring(es, nc, "sqh", 3, [96, 512], BF16)
        qgh = sb_ring(es, nc, "qgh", 3, [96, 512], BF16)
        qg32 = sb_ring(es, nc, "qg32", 3, [96, 512], F32)
        rsh = sb_ring(es, nc, "rsh", 3, [96, 512], F32)
        t1r = sb_ring(es, nc, "t1r", 3, [96, 512], F32)
        t2r = sb_ring(es, nc, "t2r", 3, [96, 512], F32)
        qfr = sb_ring(es, nc, "qfr", 3, [96, 512], BF16)
        psf = ps_ring(es, nc, "psf", 6)
        psb = ps_ring(es, nc, "psb", 2, (128, 1024), BF16)

        eflip = [0]

        def evac_eng():
            eflip[0] ^= 1
            return "act" if eflip[0] else "dve"

        for j in range(k.NB):
            tok0 = j * 512
            nT, b_nT = nTr.next()
            def xt_gen(t, tok0=tok0, nT=nT, b_nT=b_nT):
                r0 = tok0 + t * 128
                xt, bx = xr.next()
                k.load(xt[:], k.x[r0:r0 + 128, :], w=[bx])
                yield
                ss, bss = ssr.next()
                junk, b_junk = junkr.next()
                k.act(junk[:], xt[:], AF.Square, r=[bx], w=[b_junk, bss], accum_out=ss[:, 0:1])
                k.rstd(ss[:, 1:2], ss[:, 0:1], 1024, bss, r=[bss])
                yield
                nb_, bn = nr.next()
                k.act(nb_[:], xt[:], AF.Copy, r=[bx, bss], w=[bn], scale=ss[:, 1:2])
                yield
                pb, bpb = psb.next()
                for kc in range(8):
                    k.tr(pb[:, kc * 128:(kc + 1) * 128], nb_[:, kc * 128:(kc + 1) * 128], ident[:],
                         r=[bn, b_ident], w=[bpb])
                yield
                k.cp("act" if t % 2 == 0 else "dve", nT[:, :, t * 128:(t + 1) * 128],
                     pb[:].rearrange("p (kc t) -> p kc t", kc=8), r=[bpb], w=[b_nT])

            run_pipelined([xt_gen(t) for t in range(4)], 2)
            lat = []
            for (c0, ncol) in ((0, 128), (128, 128), (256, 128), (384, 32)):
                ps, bps = psf.next()
                for kc in range(8):
                    k.mm(ps[0:ncol, :], win[:, kc, c0:c0 + ncol], nT[:, kc, :], start=(kc == 0), stop=(kc == 7),
                         r=[b_win, b_nT], w=[bps])
                lat.append((ps, bps))
            for i in range(3):
                k.act(sqq[:, i, :], lat[i][0][:], AF.Square, r=[lat[i][1]], w=[b_sqq])
            k.cp("dve", krT[:], lat[3][0][0:32, :], r=[lat[3][1]], w=[b_krT])
            msq, bmsq = psf.next()
            k.mm(msq[:], ones[:], sqq[:, 0, :], start=True, stop=False, r=[b_ones, b_sqq], w=[bmsq])
            k.mm(msq[:], ones[:], sqq[:, 1, :], start=False, stop=True, r=[b_ones, b_sqq], w=[bmsq])
            msk, bmsk = psf.next()
            k.mm(msk[:], ones[:], sqq[:, 2, :], r=[b_ones, b_sqq], w=[bmsk])
            k.rstd(rsl[:, 0, :], msq[:], 256, b_rsl, r=[bmsq])
            k.rstd(rsl[:, 1, :], msk[:], 128, b_rsl, r=[bmsk])
            k.tt("dve", qnT[:, 0, :], lat[0][0][:], rsl[:, 0, :], ALU.mult, r=[lat[0][1], b_rsl], w=[b_qnT])
            k.tt("dve", qnT[:, 1, :], lat[1][0][:], rsl[:, 0, :], ALU.mult, r=[lat[1][1], b_rsl], w=[b_qnT])
            k.tt("dve", kvnT[:], lat[2][0][:], rsl[:, 1, :], ALU.mult, r=[lat[2][1], b_rsl], w=[b_kvnT])
            for t in range(4):
                ps, bps = psf.next()
                k.mm(ps[:], kvnT[:, t * 128:(t + 1) * 128], wv[:].rearrange("p h c -> p (h c)"),
                     r=[b_kvnT, b_wv], w=[bps])
                v_, bv = vsb.next()
                k.cp("dve", v_[:], ps[:], r=[bps], w=[bv])
                k.store(VV[tok0 + t * 128: tok0 + (t + 1) * 128, :], v_[:], r=[bv], w=[bVV])
            cs, bcs = cosr.next()
            sn, bsn = sinr.next()
            k.load(cs[64:96, :], k.din["rope_cos"][64:96, tok0:tok0 + 512], w=[bcs])
            k.load(sn[64:96, :], k.din["rope_sin"][64:96, tok0:tok0 + 512], w=[bsn])

            def head_gen(h, which, tok0=tok0, cs=cs, bcs=bcs, sn=sn, bsn=bsn):
                ps, bps = psf.next()
                if which == 0:
                    k.mm(ps[0:96, :], wuq[:, 0, h * 96:(h + 1) * 96], qnT[:, 0, :], start=True, stop=False,
                         r=[b_wuq, b_qnT], w=[bps])
                    k.mm(ps[0:96, :], wuq[:, 1, h * 96:(h + 1) * 96], qnT[:, 1, :], start=False, stop=True,
                         r=[b_wuq, b_qnT], w=[bps])
                    g_, bg_, dst, bdst = gq, b_gq, QT, bQT
                else:
                    k.mm(ps[0:96, :], wk[:, h, :], kvnT[:], start=True, stop=False, r=[b_wk, b_kvnT], w=[bps])
                    k.mm(ps[0:96, :], krsel[:], krT[:], start=False, stop=True, r=[b_krsel, b_krT], w=[bps])
                    g_, bg_, dst, bdst = gk, b_gk, KT, bKT
                yield
                sq, bsq = sqh.next()
                k.act(sq[:], ps[0:96, :], AF.Square, r=[bps], w=[bsq])
                q32, bq32 = qg32.next()
                k.act(q32[:], ps[0:96, :], AF.Copy, r=[bps, bg_], w=[bq32], scale=g_[:, 0:1])
                yield
                ms, bms = psf.next()
                k.mm(ms[0:96, :], ones[0:96, 0:96], sq[:], r=[b_ones, bsq], w=[bms])
                qg, bqg = qgh.next()
                k.act(qg[:], ps[0:96, :], AF.Copy, r=[bps, bg_], w=[bqg], scale=g_[:, 0:1])
                t1, bt1 = t1r.next()
                k.tt("pool", t1[64:96, :], q32[64:96, :], cs[64:96, :], ALU.mult, r=[bq32, bcs], w=[bt1])
                yield
                rt, brt = psf.next()
                k.mm(rt[0:96, :], rotT[:], qg[:], r=[b_rotT, bqg], w=[brt])
                rs, brs = rsh.next()
                k.rstd(rs[:], ms[0:96, :], 96, brs, r=[bms])
                yield
                t2, bt2 = t2r.next()
                k.tt("dve", t2[64:96, :], rt[64:96, :], sn[64:96, :], ALU.mult, r=[brt, bsn], w=[bt2])
                yield
                k.tt("pool", q32[64:96, :], t1[64:96, :], t2[64:96, :], ALU.add, r=[bt1, bt2], w=[bq32])
                yield
                qf, bqf = qfr.next()
                k.tt("dve", qf[:], q32[:], rs[:], ALU.mult, r=[bq32, brs], w=[bqf])
                k.store(dst[h, :, tok0:tok0 + 512], qf[:], r=[bqf], w=[bdst])

            def tm_gen(t, tok0=tok0, nT=nT, b_nT=b_nT):
                sg, bsg = stg.next()
                for g in range(5):
                    ps, bps = psf.next()
                    c0 = 416 + g * 512
                    for kc in range(8):
                        k.mm(ps[:], nT[:, kc, t * 128:(t + 1) * 128], win[:, kc, c0:c0 + 512],
                             start=(kc == 0), stop=(kc == 7), r=[b_nT, b_win], w=[bps])
                    yield
                    k.cp(evac_eng(), sg[:, g * 512:(g + 1) * 512], ps[:], r=[bps], w=[bsg])
                k.store(HG[tok0 + t * 128: tok0 + (t + 1) * 128, :], sg[:], r=[bsg], w=[bHG])

            gens = []
            hw = [(h, w_) for h in range(H) for w_ in range(2)]
            for t in range(4):
                gens += [head_gen(h, w_) for (h, w_) in hw[4 * t:4 * t + 4]]
                gens.append(tm_gen(t))
            run_pipelined(gens, 3)
        k.flush()
        P.drain_dmas()
        P.emit()


def moe_scratch_init(k, es):
    nc, P, S = k.nc, k.P, k.S
    CAP = moe_cap(S)
    DUMP = NE * CAP
    PSL = DUMP + 128
    k.CAP, k.PSL = CAP, PSL
    XS = k.scratch("XS", [PSL, D], BF16)
    ROUTE = k.scratch("ROUTE", [PSL, 16], I32)
    YT = k.scratch("YT", [2 * S + PSL, D], BF16)
    bXS, bROUTE, bYT = (k.scr_buf[n] for n in ("XS", "ROUTE", "YT"))
    RW = PSL // 128 * 16
    zt = es.enter_context(nc.sbuf_tensor("sb_i_zero", [128, RW], I32))
    b_zt = Buf("i_zero")
    P.add("pool", lambda e: e.iota(zt[:], pattern=[[128, PSL // 128], [0, 16]], base=2 * S, channel_multiplier=1),
          (), [b_zt])
    ROUTEv = ROUTE.rearrange("(r p) c -> p r c", p=128)
    ztv = zt[:].rearrange("p (r c) -> p r c", c=16)
    for r_ in range(0, PSL // 128, 64):
        n_ = min(64, PSL // 128 - r_)
        k.dma("pool", ROUTEv[:, r_:r_ + n_, :], ztv[:, r_:r_ + n_, :], r=[b_zt], w=[bROUTE])
    zx = es.enter_context(nc.sbuf_tensor("sb_i_zx", [128, 4 * D], BF16))
    b_zx = Buf("i_zx")
    P.add("pool", lambda e: e.memset(zx[:], 0.0), (), [b_zx])
    XSz = XS.rearrange("(p r) n -> p r n", p=128)
    RX = PSL // 128
    for r_ in range(0, RX, 4):
        n_ = min(4, RX - r_)
        k.dma("pool", XSz[:, r_:r_ + n_, :], zx[:, 0:n_ * D].rearrange("p (r n) -> p r n", r=n_), r=[b_zx], w=[bXS])
    YTz = YT[0:2 * S, :].rearrange("(p r) n -> p r n", p=128)
    RZ = 2 * S // 128
    for r_ in range(0, RZ, 4):
        n_ = min(4, RZ - r_)
        k.dma("pool", YTz[:, r_:r_ + n_, :], zx[:, 0:n_ * D].rearrange("p (r n) -> p r n", r=n_), r=[b_zx], w=[bYT])


def phase_attn(k, with_hgrn=False):
    nc, P, S, NT = k.nc, k.P, k.S, k.NT
    QT, KT, VV = k.scr["QT"], k.scr["KT"], k.scr["VV"]
    bQT, bKT, bVV = k.scr_buf["QT"], k.scr_buf["KT"], k.scr_buf["VV"]
    MIXT = k.scratch("MIXT", [D, S], BF16)
    bMIXT = k.scr_buf["MIXT"]
    VVv = VV.rearrange("(t p) (h c) -> p t h c", p=128, c=DV)
    RD = k.scratch("RD", [H * (S // 512), 512], F32)
    bRD = [Buf("rd%d" % i) for i in range(4)]
    with ExitStack() as es:
        def sb(name, shape, dt):
            return es.enter_context(nc.sbuf_tensor("sb_" + name, list(shape), dt)), Buf(name)
        onesf, b_onesf = sb("a_onesf", [128, 128], F32)
        k.dma("sp", onesf[:], k.din["ones_f"], w=[b_onesf])
        kth = sb_ring(es, nc, "a_kt", 2, [96, S], BF16)
        qth = sb_ring(es, nc, "a_qt", 3, [96, 512], BF16)
        vh = sb_ring(es, nc, "a_v", 2, [128, NT, DV + 1], BF16)
        for t_, b_ in zip(vh.tiles, vh.bufs):
            P.add("pool", lambda e, t_=t_: e.memset(t_[:], 1.0), (), [b_])
        ptr = sb_ring(es, nc, "a_pt", 3 if with_hgrn else 4, [128, 1024], BF16)
        ocr = sb_ring(es, nc, "a_oc", 2, [128, 512], F32)
        osb = sb_ring(es, nc, "a_o", 2, [64, 512], F32)
        aout = sb_ring(es, nc, "a_a", 2, [64, 512], BF16)
        scr_ = ps_ring(es, nc, "a_sc", 2 if with_hgrn else 3, (128, 1024), F32)
        accr = ps_ring(es, nc, "a_acc", 1 if with_hgrn else 2)
        hstep = None
        if with_hgrn:
            h_psf = ps_free(es, nc, "h_psf", 2)
            h_psb = ps_free(es, nc, "h_psb", 1, (128, 1024), BF16)
            hstep = hgrn_build(k, es, h_psf, h_psb)
            hsteps = hgrn_order(NT)
        NP2 = NT // 2
        NQB = S // 512
        heads = {}

        def load_head(h):
            kt_, bkt = kth.next()
            v_, bv = vh.next()
            k.load(kt_[:], KT[h], r=[bKT], w=[bkt])
            k.load(v_[:, :, 0:DV], VVv[:, :, h, :], r=[bVV], w=[bv])
            heads[h] = (kt_, bkt, v_, bv)

        qbs = {}

        def load_q(h, qb):
            qt_, bqt = qth.next()
            k.load(qt_[:], QT[h, :, qb * 512:(qb + 1) * 512], r=[bQT], w=[bqt])
            qbs[(h, qb)] = (qt_, bqt)

        steps = [(h, qb, kp) for h in range(H) for qb in range(NQB) for kp in range(NP2)]
        scs = {}

        def emit_qk(i):
            h, qb, kp = steps[i]
            if kp == 0:
                if qb == 0 and h not in heads:
                    load_head(h)
                if (h, qb) not in qbs:
                    load_q(h, qb)
                nxt = (h, qb + 1) if qb + 1 < NQB else ((h + 1, 0) if h + 1 < H else None)
                if nxt is not None:
                    if nxt[1] == 0 and nxt[0] not in heads:
                        load_head(nxt[0])
                    if nxt not in qbs:
                        load_q(*nxt)
            kt_, bkt, v_, bv = heads[h]
            qt_, bqt = qbs[(h, qb)]
            sc, bsc = scr_.next()
            for u in range(2):
                kt = 2 * kp + u
                k.mm(sc[:, u * 512:(u + 1) * 512], kt_[:, kt * 128:(kt + 1) * 128], qt_[:],
                     r=[bkt, bqt], w=[bsc])
            scs[i] = (sc, bsc)

        LOOK = 1 if with_hgrn else 2
        for i in range(min(LOOK, len(steps))):
            emit_qk(i)
        if "XS" not in k.scr:
            moe_scratch_init(k, es)
        acc = bacc = None
        gen = None
        for i, (h, qb, kp) in enumerate(steps):
            kt_, bkt, v_, bv = heads[h]
            if kp == 0:
                acc, bacc = accr.next()
                if hstep is not None and h * NQB + qb < len(hsteps):
                    gen = hstep(*hsteps[h * NQB + qb])
            sc, bsc = scs.pop(i)
            pt, bpt = ptr.next()
            k.act(pt[:], sc[:], AF.Exp, r=[bsc], w=[bpt])
            if i + LOOK < len(steps):
                emit_qk(i + LOOK)
            for u in range(2):
                kt = 2 * kp + u
                k.mm(acc[0:DV + 1, :], v_[:, kt, :], pt[:, u * 512:(u + 1) * 512],
                     start=(kt == 0), stop=(kt == NT - 1), r=[bv, bpt], w=[bacc])
            if gen is not None:
                if next(gen, "done") == "done":
                    gen = None
            if kp == NP2 - 1:
                if gen is not None:
                    for _ in gen:
                        pass
                    gen = None
                oc, boc = ocr.next()
                k.cp("dve", oc[0:DV + 1, :], acc[0:DV + 1, :], r=[bacc], w=[boc])
                P.add("dve", lambda e, oc=oc: e.reciprocal(oc[64:65, :], oc[64:65, :]), [boc], [boc])
                o_, bo = osb.next()
                ridx = h * NQB + qb
                k.dma("sp", RD[ridx:ridx + 1, :], oc[64:65, :], r=[boc], w=[bRD[ridx % 4]])
                k.dma("sp", o_[:], RD[ridx:ridx + 1, :].partition_broadcast(64), r=[bRD[ridx % 4]], w=[bo])
                a_, ba = aout.next()
                k.tt("dve", a_[:], oc[0:64, :], o_[:], ALU.mult, r=[boc, bo], w=[ba])
                k.store(MIXT[h * 64:(h + 1) * 64, qb * 512:(qb + 1) * 512], a_[:], r=[ba], w=[bMIXT])
        k.flush()
        P.drain_dmas()
        P.emit()


def hgrn_build(k, es, psf, psb):
    nc, P, S, NT = k.nc, k.P, k.S, k.NT
    HG, MIXT = k.scr["HG"], k.scr["MIXT"]
    bHG, bMIXT = k.scr_buf["HG"], k.scr_buf["MIXT"]
    OF = k.scratch("OF", [S, 512], F32)
    bOF = k.scr_buf["OF"]
    MIXr = MIXT[512:1024, :].rearrange("(c p) t -> p c t", p=128)
    def sb(name, shape, dt):
        return es.enter_context(nc.sbuf_tensor("sb_" + name, list(shape), dt)), Buf(name)
    identb, b_identb = sb("h_ident", [128, 128], BF16)
    k.dma("sp", identb[:], k.din["ident_bf"], w=[b_identb])
    cst = {}
    for nm, shp, dt in (("hg_Lc_f", [128, 128], F32), ("hg_Lr_f", [128, 128], F32), ("hg_Lc_b", [128, 128], F32),
                        ("hg_Lr_b", [128, 128], F32), ("hg_selm_f", [128, 2], F32), ("hg_selm_b", [128, 2], F32),
                        ("hg_mask_f", [128, 512], U32), ("hg_mask_b", [128, 512], U32)):
        t_, b_ = sb(nm, shp, dt)
        k.dma("sp", t_[:], k.din[nm], w=[b_])
        cst[nm] = (t_, b_)
    l0, b_l0 = sb("h_l0", [128, 1024], F32)
    l1, b_l1 = sb("h_l1", [128, 1024], F32)
    k.dma("sp", l0[:], k.din["lb_logits_l"][0:1, :].partition_broadcast(128), w=[b_l0])
    k.dma("sp", l1[:], k.din["lb_logits_l"][1:2, :].partition_broadcast(128), w=[b_l1])
    lbb, b_lbb = sb("h_lb", [128, 1024], F32)
    oml, b_oml = sb("h_oml", [128, 1024], F32)
    k.tt("dve", l1[:], l1[:], l0[:], ALU.subtract, r=[b_l0, b_l1], w=[b_l1])
    k.act(l1[:], l1[:], AF.Exp, r=[b_l1], w=[b_l1])
    k.ts("dve", l1[:], l1[:], 1.0, None, ALU.add, r=[b_l1], w=[b_l1])
    P.add("dve", lambda e: e.reciprocal(lbb[:], l1[:]), [b_l1], [b_lbb])
    k.ts("dve", oml[:], lbb[:], -1.0, 1.0, ALU.mult, ALU.add, r=[b_lbb], w=[b_oml])
    gn, b_gn = sb("h_gn", [128, 4, 128], F32)
    for hh in range(4):
        k.dma("sp", gn[:, hh, :], k.din["hg_out_norm_l"][0:1, :].partition_broadcast(128), w=[b_gn])
    k.ts("dve", gn[:], gn[:], float(np.sqrt(128.0)), None, ALU.mult, r=[b_gn], w=[b_gn])
    eps128, b_eps = sb("h_eps", [128, 1], F32)
    P.add("pool", lambda e: e.memset(eps128[:], float(128 * EPS)), (), [b_eps])
    one1, b_one = sb("h_one", [128, 1], F32)
    P.add("pool", lambda e: e.memset(one1[:], 1.0), (), [b_one])

    hgr = sb_ring(es, nc, "h_in", 3, [128, 2560], F32)
    e1r = sb_ring(es, nc, "h_e1", 3, [128, 512], F32)
    e2r = sb_ring(es, nc, "h_e2", 3, [128, 512], F32)
    e3r = sb_ring(es, nc, "h_e3", 3, [128, 512], F32)
    fr = sb_ring(es, nc, "h_f", 3, [128, 512], F32)
    kkr = sb_ring(es, nc, "h_kk", 3, [128, 512], F32)
    gr = sb_ring(es, nc, "h_g", 3, [128, 512], F32)
    qr = sb_ring(es, nc, "h_q", 3, [128, 512], F32)
    vr = sb_ring(es, nc, "h_v", 3, [128, 512], BF16)
    ecr = sb_ring(es, nc, "h_ec", 3, [128, 512], F32)
    encr = sb_ring(es, nc, "h_enc", 3, [128, 512], F32)
    err_ = sb_ring(es, nc, "h_er", 3, [128, 512], F32)
    eblr = sb_ring(es, nc, "h_ebl", 3, [128, 8], F32)
    qkbr = sb_ring(es, nc, "h_qkb", 3, [128, 1024], BF16)
    kdr = sb_ring(es, nc, "h_kd", 3, [128, 512], BF16)
    qkTr = sb_ring(es, nc, "h_qkT", 3, [128, 1024], BF16)
    atr = sb_ring(es, nc, "h_at", 3, [128, 512], BF16)
    sbr = sb_ring(es, nc, "h_sb", 3, [128, 512], BF16)
    Sst, b_S = sb("h_S", [128, 512], F32)
    osr = sb_ring(es, nc, "h_os", 3, [128, 512], F32)
    ofr = sb_ring(es, nc, "h_of", 3, [128, 512], F32)
    junkr = sb_ring(es, nc, "h_junk", 12, [128, 128], BF16)
    ssq = sb_ring(es, nc, "h_ssq", 3, [128, 8], F32)
    rbr = sb_ring(es, nc, "h_rb", 3, [128, 512], BF16)
    rTr = sb_ring(es, nc, "h_rT", 3, [128, 512], BF16)

    def silu_from(dst, src, rd, wr, tmp, btmp):
        k.act(tmp[:], src, AF.Exp, r=rd, w=[btmp], scale=-1.0)
        k.ts("dve", tmp[:], tmp[:], 1.0, None, ALU.add, r=[btmp], w=[btmp])
        P.add("dve", lambda e: e.reciprocal(tmp[:], tmp[:]), [btmp], [btmp])
        k.tt("dve", dst, tmp[:], src, ALU.mult, r=[btmp] + list(rd), w=wr)

    dirs = []
    for d_ in range(2):
        sfx = "_f" if d_ == 0 else "_b"
        dirs.append((cst["hg_Lc" + sfx], cst["hg_Lr" + sfx], cst["hg_selm" + sfx], cst["hg_mask" + sfx]))

    prog_state = {"s_done": 0, "started": 0}

    def step(d, ti, first):
        (Lc, b_Lc), (Lr, b_Lr), (selm, b_selm), (mask, b_mask) = dirs[d]
        my = prog_state["started"]
        prog_state["started"] += 1
        if first:
            while prog_state["s_done"] < my:
                yield
            P.add("pool", lambda e: e.memset(Sst[:], 0.0), (), [b_S])
            for t_, b_ in zip(atr.tiles, atr.bufs):
                P.add("pool", lambda e, t_=t_: e.memset(t_[:], 0.0), (), [b_])
        r0 = ti * 128
        hg_, bhg = hgr.next()
        k.load(hg_[:], HG[r0:r0 + 128, :], r=[bHG], w=[bhg])
        if d == 1:
            of_, bof = ofr.next()
            k.load(of_[:], OF[r0:r0 + 128, :], r=[bOF], w=[bof])
        z = hg_[:, 512 + d * 512: 1024 + d * 512]
        yield
        e1, be1 = e1r.next()
        e2, be2 = e2r.next()
        k.act(e1[:], z, AF.Sigmoid, r=[bhg], w=[be1])
        k.act(e2[:], hg_[:, 0:512], AF.Sigmoid, r=[bhg], w=[be2])
        if d == 1:
            e3, be3 = e3r.next()
            k.act(e3[:], hg_[:, 2048:2560], AF.Sigmoid, r=[bhg], w=[be3])
        v_, bv = vr.next()
        k.act(v_[:], hg_[:, 1536:2048], AF.Copy, r=[bhg], w=[bv], scale=-1.0)
        yield
        f_, bf_ = fr.next()
        k.tt("dve", f_[:], e1[:], oml[:, d * 512:(d + 1) * 512], ALU.mult, r=[be1, b_oml], w=[bf_])
        k.tt("dve", f_[:], f_[:], lbb[:, d * 512:(d + 1) * 512], ALU.add, r=[bf_, b_lbb], w=[bf_])
        q_, bq = qr.next()
        k.tt("dve", q_[:], e2[:], hg_[:, 0:512], ALU.mult, r=[be2, bhg], w=[bq])
        yield
        g_, bg = gr.next()
        k.act(g_[:], f_[:], AF.Ln, r=[bf_], w=[bg])
        yield
        cps, bcps, icps = yield from psf.get()
        k.mm(cps[:], Lc[:], g_[:], r=[b_Lc, bg], w=[bcps])
        rps, brps, irps = yield from psf.get()
        k.mm(rps[:], Lr[:], g_[:], r=[b_Lr, bg], w=[brps])
        yield
        ec, bec = ecr.next()
        enc, benc = encr.next()
        er, ber = err_.next()
        k.act(ec[:], cps[:], AF.Exp, r=[bcps], w=[bec])
        k.act(enc[:], cps[:], AF.Exp, r=[bcps], w=[benc], scale=-1.0)
        k.act(er[:], rps[:], AF.Exp, r=[brps], w=[ber])
        psf.put(icps)
        psf.put(irps)
        yield
        blm, bblm, iblm = yield from psf.get()
        for hh in range(4):
            k.mm(blm[:, 2 * hh:2 * hh + 2], g_[:, hh * 128:(hh + 1) * 128], selm[:], r=[bg, b_selm], w=[bblm])
        qkb, bqkb = qkbr.next()
        kd, bkd = kdr.next()
        k.tt("dve", qkb[:, 0:512], q_[:], ec[:], ALU.mult, r=[bq, bec], w=[bqkb])
        P.add("dve", lambda e, qkb=qkb, f_=f_, enc=enc: e.scalar_tensor_tensor(
            qkb[:, 512:1024], f_[:], 1.0, enc[:], ALU.subtract, ALU.mult), [bf_, benc], [bqkb])
        P.add("dve", lambda e, kd=kd, f_=f_, er=er: e.scalar_tensor_tensor(
            kd[:], f_[:], 1.0, er[:], ALU.subtract, ALU.mult), [bf_, ber], [bkd])
        yield
        ebl, bebl = eblr.next()
        k.act(ebl[:], blm[:, 0:8], AF.Exp, r=[bblm], w=[bebl])
        psf.put(iblm)
        yield
        pb, bpb, ipb = yield from psb.get()
        for c8 in range(8):
            k.tr(pb[:, c8 * 128:(c8 + 1) * 128], qkb[:, c8 * 128:(c8 + 1) * 128], identb[:],
                 r=[bqkb, b_identb], w=[bpb])
        while prog_state["s_done"] < my:
            yield
        sb_, bsb = sbr.next()
        for hh in range(4):
            k.ts("dve", sb_[:, hh * 128:(hh + 1) * 128], Sst[:, hh * 128:(hh + 1) * 128],
                 ebl[:, 2 * hh + 1:2 * hh + 2], None, ALU.mult, r=[b_S, bebl], w=[bsb])
        yield
        qkT, bqkT = qkTr.next()
        k.cp("dve", qkT[:], pb[:], r=[bpb], w=[bqkT])
        psb.put(ipb)
        yield
        atp, batp, iatp = yield from psf.get()
        for hh in range(4):
            k.mm(atp[:, hh * 128:(hh + 1) * 128], qkT[:, 512 + hh * 128: 512 + (hh + 1) * 128],
                 qkT[:, hh * 128:(hh + 1) * 128], r=[bqkT], w=[batp])
        snp, bsnp, isnp = yield from psf.get()
        for hh in range(4):
            sl = slice(hh * 128, (hh + 1) * 128)
            k.mm(snp[:, sl], kd[:, sl], v_[:, sl], r=[bkd, bv], w=[bsnp])
        yield
        at, bat = atr.next()
        P.add("dve", lambda e, at=at, atp=atp, mask=mask: e.copy_predicated(at[:], mask[:], atp[:]),
              [batp, b_mask], [bat])
        psf.put(iatp)
        yield
        ops, bops, iops = yield from psf.get()
        for hh in range(4):
            sl = slice(hh * 128, (hh + 1) * 128)
            k.mm(ops[:, sl], at[:, sl], v_[:, sl], start=True, stop=False, r=[bat, bv], w=[bops])
            k.mm(ops[:, sl], qkT[:, sl], sb_[:, sl], start=False, stop=True, r=[bqkT, bsb], w=[bops])
        for hh in range(4):
            sl = slice(hh * 128, (hh + 1) * 128)
            P.add("dve", lambda e, sl=sl, ebl=ebl, snp=snp, hh=hh: e.scalar_tensor_tensor(
                Sst[:, sl], Sst[:, sl], ebl[:, 2 * hh:2 * hh + 1], snp[:, sl], ALU.mult, ALU.add),
                [b_S, bebl, bsnp], [b_S])
        prog_state["s_done"] = my + 1
        psf.put(isnp)
        yield
        os_, bos = osr.next()
        if d == 0:
            k.cp("dve", os_[:], ops[:], r=[bops], w=[bos])
            psf.put(iops)
            k.store(OF[r0:r0 + 128, :], os_[:], r=[bos], w=[bOF])
            return
        k.tt("dve", os_[:], ops[:], of_[:], ALU.add, r=[bops, bof], w=[bos])
        psf.put(iops)
        k.tt("dve", e3[:], e3[:], hg_[:, 2048:2560], ALU.mult, r=[be3, bhg], w=[be3])
        yield
        sq, bsq = ssq.next()
        for hh in range(4):
            junk, b_junk = junkr.next()
            k.act(junk[:], os_[:, hh * 128:(hh + 1) * 128], AF.Square, r=[bos], w=[b_junk, bsq],
                  accum_out=sq[:, hh:hh + 1])
        k.act(sq[:, 4:8], sq[:, 0:4], AF.Ln, r=[bsq, b_eps], w=[bsq], bias=eps128[:, 0:1])
        k.act(sq[:, 4:8], sq[:, 4:8], AF.Exp, r=[bsq], w=[bsq], scale=-0.5)
        yield
        for hh in range(4):
            k.ts("dve", os_[:, hh * 128:(hh + 1) * 128], os_[:, hh * 128:(hh + 1) * 128], sq[:, 4 + hh:5 + hh],
                 None, ALU.mult, r=[bos, bsq], w=[bos])
        k.tt("dve", os_[:], os_[:], gn[:].rearrange("p h c -> p (h c)"), ALU.mult, r=[bos, b_gn], w=[bos])
        rb, brb = rbr.next()
        k.tt("dve", rb[:], os_[:], e3[:], ALU.mult, r=[bos, be3], w=[brb])
        yield
        pb, bpb, ipb = yield from psb.get()
        for hh in range(4):
            k.tr(pb[:, hh * 128:(hh + 1) * 128], rb[:, hh * 128:(hh + 1) * 128], identb[:],
                 r=[brb, b_identb], w=[bpb])
        yield
        rT, brT = rTr.next()
        k.cp("dve", rT[:], pb[:, 0:512], r=[bpb], w=[brT])
        psb.put(ipb)
        k.store(MIXr[:, :, r0:r0 + 128], rT[:].rearrange("p (c t) -> p c t", c=4), r=[brT], w=[bMIXT])
    return step


def hgrn_order(NT):
    return [(0, ti, ti == 0) for ti in range(NT)] + [(1, ti, ti == NT - 1) for ti in range(NT - 1, -1, -1)]


def phase_hgrn(k):
    nc, P = k.nc, k.P
    with ExitStack() as es:
        psf = ps_free(es, nc, "h_psf", 6)
        psb = ps_free(es, nc, "h_psb", 2, (128, 1024), BF16)
        step = hgrn_build(k, es, psf, psb)
        order = hgrn_order(k.NT)
        for d in range(2):
            run_pipelined((step(*o) for o in order if o[0] == d), 3)
        k.flush()
        P.drain_dmas()
        P.emit()


def phase_route(k):
    nc, P, S, NT = k.nc, k.P, k.S, k.NT
    CAP = moe_cap(S)
    DUMP = NE * CAP
    PSL = DUMP + 128
    k.CAP, k.PSL = CAP, PSL
    MIXT, bMIXT = k.scr["MIXT"], k.scr_buf["MIXT"]
    H1 = k.scratch("H1", [S, D], F32)
    H2N = k.scratch("H2N", [S, D], BF16)
    XS, ROUTE = k.scr["XS"], k.scr["ROUTE"]
    GATE = k.scratch("GATE", [128, NT * 2], F32)
    bH1, bH2N, bXS, bROUTE, bGATE = (k.scr_buf[n] for n in ("H1", "H2N", "XS", "ROUTE", "GATE"))
    MIXv = MIXT.rearrange("(c p) t -> p c t", p=128)
    with ExitStack() as es:
        def sb(name, shape, dt):
            return es.enter_context(nc.sbuf_tensor("sb_" + name, list(shape), dt)), Buf(name)
        identf, b_identf = sb("r_identf", [128, 128], F32)
        onesf, b_onesf = sb("r_onesf", [128, 128], F32)
        ustr, b_ustr = sb("r_ustr", [128, 128], F32)
        id32, b_id32 = sb("r_id32", [32, 32], F32)
        ecap, b_ecap = sb("r_ecap", [32, 2], F32)
        limr, b_limr = sb("r_limr", [128, 32], F32)
        piota, b_piota = sb("r_piota", [128, 1], F32)
        toki, b_toki = sb("r_toki", [128, NT], I32)
        for t_, b_, nm in ((identf, b_identf, "ident_f"), (onesf, b_onesf, "ones_f"), (ustr, b_ustr, "ustrict"),
                           (id32, b_id32, "ident32"), (ecap, b_ecap, "blk_iota"), (limr, b_limr, "lim_row"),
                           (piota, b_piota, "p_iota"), (toki, b_toki, "tok_iota")):
            k.dma("sp", t_[:], k.din[nm], w=[b_])
        gff, b_gff = sb("r_gff", [128, D], F32)
        k.dma("sp", gff[:], k.din["norm_ffn_l"][0:1, :].partition_broadcast(128), w=[b_gff])
        k.ts("dve", gff[:], gff[:], 32.0, None, ALU.mult, r=[b_gff], w=[b_gff])
        brt, b_brt = sb("r_brt", [128, 36], F32)
        k.dma("sp", brt[:], k.din["b_rt"][0:1, :].partition_broadcast(128), w=[b_brt])
        wrt, b_wrt = sb("r_wrt", [128, 8, 36], F32)
        k.dma("sp", wrt[:], k.din["w_rt"].rearrange("(c p) n -> p c n", p=128), w=[b_wrt])
        eps1k, b_eps = sb("r_eps", [128, 1], F32)
        P.add("pool", lambda e: e.memset(eps1k[:], float(D * EPS)), (), [b_eps])
        wout, b_wout = sb("r_wout", [128, 8, D], BF16)
        wst = sb_ring(es, nc, "r_wst", 2, [128, D], F32)
        w_out_v = k.din["w_out"].rearrange("(c p) n -> p c n", p=128)
        for c in range(8):
            st, bst = wst.next()
            k.dma("sp", st[:], w_out_v[:, c, :], w=[bst])
            k.cp("dve" if c % 2 == 0 else "pool", wout[:, c, :], st[:], r=[bst], w=[b_wout])
        dcol, b_dcol = sb("r_dcol", [128, 2], F32)
        k.ts("dve", dcol[:, 0:1], piota[:, 0:1], float(2 * S), None, ALU.add, r=[b_piota], w=[b_dcol])
        k.ts("dve", dcol[:, 1:2], piota[:, 0:1], float(DUMP), None, ALU.add, r=[b_piota], w=[b_dcol])
        tokst, b_tokst = sb("r_tokst", [128, NT, 2, 16], I32)
        P.add("pool", lambda e: e.memset(tokst[:], 0), (), [b_tokst])
        k.ts("dve", tokst[:, :, 0, 0], toki[:], 1, None, ALU.logical_shift_left, r=[b_toki, b_tokst], w=[b_tokst])
        k.ts("dve", tokst[:, :, 1, 0], tokst[:, :, 0, 0], 1, None, ALU.add, r=[b_tokst], w=[b_tokst])

        k.fence([bXS, bROUTE])
        Aall, b_A = sb("r_A", [128, NT, 32], F32)
        M12, b_M = sb("r_M12", [128, NT, 2, 32], F32)
        gates, b_gates = sb("r_gates", [128, NT, 2], F32)
        dest, b_dest = sb("r_dest", [128, NT, 2], F32)
        desti, b_desti = sb("r_desti", [128, NT, 2], I32)

        mixr = sb_free(es, nc, "r_mix", 3, [128, 8, 128], BF16)
        xr = sb_free(es, nc, "r_x", 3, [128, D], F32)
        h1r = sb_free(es, nc, "r_h1", 3, [128, D], F32)
        junkr = sb_free(es, nc, "r_junk", 2, [128, D], BF16)
        h2r = sb_free(es, nc, "r_h2", 3, [128, D], F32)
        h2bf = sb_free(es, nc, "r_h2bf", 3, [128, D], BF16)
        h2br = sb_ring(es, nc, "r_h2b", 3, [128, D], BF16)
        h2Tr = sb_free(es, nc, "r_h2T", 2, [128, 8, 128], F32)
        smr = sb_free(es, nc, "r_sm", 4, [128, 16], F32)
        lgr = sb_free(es, nc, "r_lg", 4, [128, 36], F32)
        emr = sb_free(es, nc, "r_em", 4, [128, 32], F32)
        t8r = sb_free(es, nc, "r_t8", 4, [128, 8], F32)
        g4r = sb_free(es, nc, "r_g4", 4, [128, 12], F32)
        psf = ps_free(es, nc, "r_psf", 8)

        def tile_gen(ti):
            r0 = ti * 128
            mx, bmx, imx = yield from mixr.get()
            k.load(mx[:], MIXv[:, :, r0:r0 + 128], r=[bMIXT], w=[bmx])
            xt, bx, ix = yield from xr.get()
            k.load(xt[:], k.x[r0:r0 + 128, :], w=[bx])
            yield
            h1, bh1, ih1 = yield from h1r.get()
            pss = []
            for half in range(2):
                ps, bps, ips = yield from psf.get()
                for c in range(8):
                    k.mm(ps[:], mx[:, c, :], wout[:, c, half * 512:(half + 1) * 512], start=(c == 0), stop=(c == 7),
                         r=[bmx, b_wout], w=[bps])
                pss.append((ps, bps, ips))
            mixr.put(imx)
            yield
            for half, (ps, bps, ips) in enumerate(pss):
                k.tt("dve", h1[:, half * 512:(half + 1) * 512], ps[:], xt[:, half * 512:(half + 1) * 512], ALU.add,
                     r=[bps, bx], w=[bh1])
                psf.put(ips)
            xr.put(ix)
            k.store(H1[r0:r0 + 128, :], h1[:], r=[bh1], w=[bH1])
            yield
            sm, bsm, ism = yield from smr.get()
            junk, bjunk, ijunk = yield from junkr.get()
            k.act(junk[:], h1[:], AF.Square, r=[bh1], w=[bjunk, bsm], accum_out=sm[:, 0:1])
            junkr.put(ijunk)
            k.act(sm[:, 1:2], sm[:, 0:1], AF.Ln, r=[bsm, b_eps], w=[bsm], bias=eps1k[:, 0:1])
            k.act(sm[:, 2:3], sm[:, 1:2], AF.Exp, r=[bsm], w=[bsm], scale=-0.5)
            yield
            h2, bh2, ih2 = yield from h2r.get()
            k.ts("dve", h2[:], h1[:], sm[:, 2:3], None, ALU.mult, r=[bh1, bsm], w=[bh2])
            yield
            k.tt("pool", h2[:], h2[:], gff[:], ALU.mult, r=[bh2, b_gff], w=[bh2])
            yield
            h2b, bh2b, ih2b = yield from h2bf.get()
            k.cp("act", h2b[:], h2[:], r=[bh2], w=[bh2b])
            k.store(H2N[r0:r0 + 128, :], h2b[:], r=[bh2b], w=[bH2N])
            h2T, bh2T, ih2T = yield from h2Tr.get()
            pss = []
            for half in range(2):
                ps, bps, ips = yield from psf.get()
                for c4 in range(4):
                    c = half * 4 + c4
                    k.tr(ps[:, c4 * 128:(c4 + 1) * 128], h2[:, c * 128:(c + 1) * 128], identf[:],
                         r=[bh2, b_identf], w=[bps])
                pss.append((ps, bps, ips))
            h2r.put(ih2)
            yield
            for half, (ps, bps, ips) in enumerate(pss):
                k.cp("dve" if half == 0 else "act", h2T[:, half * 4:(half + 1) * 4, :].rearrange("p c t -> p (c t)"), ps[:],
                     r=[bps], w=[bh2T])
                psf.put(ips)
            yield
            lps, blps, ilps = yield from psf.get()
            for c in range(8):
                k.mm(lps[:, 0:36], h2T[:, c, :], wrt[:, c, :], start=(c == 0), stop=(c == 7), r=[bh2T, b_wrt], w=[blps])
            h2Tr.put(ih2T)
            yield
            lg, blg, ilg = yield from lgr.get()
            k.tt("dve", lg[:], lps[:, 0:36], brt[:], ALU.add, r=[blps, b_brt], w=[blg])
            psf.put(ilps)
            g4, bg4, ig4 = yield from g4r.get()
            P.add("dve", lambda e, g4=g4, lg=lg: e.reduce_max(g4[:, 0:1], lg[:, 0:4], AX.X), [blg], [bg4])
            k.ts("dve", g4[:, 1:2], g4[:, 0:1], -1.0, None, ALU.mult, r=[bg4], w=[bg4])
            yield
            k.act(sm[:, 4:8], lg[:, 0:4], AF.Exp, r=[blg, bg4], w=[bsm, bg4], bias=g4[:, 1:2], accum_out=g4[:, 2:3])
            k.ts("dve", g4[:, 4:8], lg[:, 0:4], g4[:, 0:1], None, ALU.is_equal, r=[blg, bg4], w=[bg4])
            k.ts("dve", g4[:, 8:12], g4[:, 4:8], 1.0e30, -1.0e30, ALU.mult, ALU.add, r=[bg4], w=[bg4])
            em, bem, iem = yield from emr.get()
            for g in range(NG):
                k.ts("dve", em[:, g * 8:(g + 1) * 8], lg[:, 4 + g * 8: 12 + g * 8], g4[:, 4 + g:5 + g], g4[:, 8 + g:9 + g],
                     ALU.mult, ALU.add, r=[blg, bg4], w=[bem])
            lgr.put(ilg)
            yield
            P.add("dve", lambda e, g4=g4: e.reciprocal(g4[:, 3:4], g4[:, 2:3]), [bg4], [bg4])
            t8, bt8, it8 = yield from t8r.get()
            P.add("dve", lambda e, t8=t8, em=em: e.max(t8[:], em[:]), [bem], [bt8])
            yield
            k.ts("dve", M12[:, ti, 0, :], em[:], t8[:, 0:1], None, ALU.is_equal, r=[bem, bt8], w=[b_M])
            k.ts("dve", M12[:, ti, 1, :], em[:], t8[:, 1:2], None, ALU.is_equal, r=[bem, bt8], w=[b_M])
            emr.put(iem)
            k.tt("dve", sm[:, 8:9], t8[:, 1:2], t8[:, 0:1], ALU.subtract, r=[bt8], w=[bsm])
            t8r.put(it8)
            yield
            k.tt("dve", Aall[:, ti, :], M12[:, ti, 0, :], M12[:, ti, 1, :], ALU.add, r=[b_M], w=[b_A])
            k.act(sm[:, 9:10], sm[:, 8:9], AF.Exp, r=[bsm], w=[bsm])
            yield
            k.ts("dve", sm[:, 9:10], sm[:, 9:10], 1.0, None, ALU.add, r=[bsm], w=[bsm])
            yield
            P.add("dve", lambda e, sm=sm: e.reciprocal(sm[:, 10:11], sm[:, 9:10]), [bsm], [bsm])
            yield
            k.tt("dve", gates[:, ti, 0:1], sm[:, 10:11], g4[:, 3:4], ALU.mult, r=[bsm, bg4], w=[b_gates])
            yield
            k.tt("dve", gates[:, ti, 1:2], g4[:, 3:4], gates[:, ti, 0:1], ALU.subtract, r=[bg4, b_gates], w=[b_gates])
            smr.put(ism)
            g4r.put(ig4)
            h1r.put(ih1)
            h2bf.put(ih2b)

        run_pipelined([tile_gen(ti) for ti in range(NT)], 3)

        cps, bcps, icps = psf.take()
        for ti in range(NT):
            k.mm(cps[0:32, ti:ti + 1], Aall[:, ti, :], onesf[:, 0:1], r=[b_A, b_onesf], w=[bcps])
        cnt, b_cnt = sb("r_cnt", [32, NT], F32)
        k.cp("dve", cnt[:], cps[0:32, 0:NT], r=[bcps], w=[b_cnt])
        inc, b_inc = sb("r_inc", [32, NT], F32)
        onesr, b_onesr = sb("r_onesr", [32, NT], F32)
        P.add("pool", lambda e: e.memset(onesr[:], 1.0), (), [b_onesr])
        P.add("dve", lambda e: e.tensor_tensor_scan(inc[:], onesr[:], cnt[:], 0.0, ALU.mult, ALU.add),
              [b_onesr, b_cnt], [b_inc])
        off, b_off = sb("r_off", [32, NT], F32)
        k.tt("dve", off[:], inc[:], cnt[:], ALU.subtract, r=[b_inc, b_cnt], w=[b_off])
        k.ts("dve", off[:], off[:], ecap[:, 0:1], None, ALU.add, r=[b_off, b_ecap], w=[b_off])
        dgr = sb_free(es, nc, "r_dg", 3, [32, 32], F32)
        tmr = sb_free(es, nc, "r_tm", 3, [128, 4, 32], F32)
        okr = sb_free(es, nc, "r_ok", 3, [128, 3, 32], F32)
        gkr = sb_free(es, nc, "r_gk", 3, [128, 2], F32)

        def slot_gen(ti):
            h2b, bh2b, ih2b = yield from h2bf.get()
            k.load(h2b[:], H2N[ti * 128:(ti + 1) * 128, :], r=[bH2N], w=[bh2b])
            dg, bdg, idg = yield from dgr.get()
            k.ts("dve", dg[:], id32[:], off[:, ti:ti + 1], None, ALU.mult, r=[b_id32, b_off], w=[bdg])
            yield
            sps, bsps, isps = yield from psf.get()
            k.mm(sps[:, 0:32], ustr[:], Aall[:, ti, :], start=True, stop=False, r=[b_ustr, b_A], w=[bsps])
            k.mm(sps[:, 0:32], onesf[0:32, :], dg[:], start=False, stop=True, r=[b_onesf, bdg], w=[bsps])
            dgr.put(idg)
            yield
            ok, bok, iok = yield from okr.get()
            k.tt("dve", ok[:, 0, :], sps[:, 0:32], limr[:], ALU.is_lt, r=[bsps, b_limr], w=[bok])
            yield
            k.tt("dve", ok[:, 1, :], sps[:, 0:32], ok[:, 0, :], ALU.mult, r=[bsps, bok], w=[bok])
            psf.put(isps)
            k.ts("dve", ok[:, 2, :], ok[:, 0, :], -1.0, 1.0, ALU.mult, ALU.add, r=[bok], w=[bok])
            yield
            k.ts("dve", ok[:, 2, :], ok[:, 2, :], dcol[:, 1:2], None, ALU.mult, r=[bok, b_dcol], w=[bok])
            yield
            k.tt("dve", ok[:, 1, :], ok[:, 1, :], ok[:, 2, :], ALU.add, r=[bok], w=[bok])
            yield
            tm, btm, itm = yield from tmr.get()
            for j in range(2):
                k.tt("dve", tm[:, j, :], M12[:, ti, j, :], ok[:, 1, :], ALU.mult, r=[b_M, bok], w=[btm])
                k.tt("dve", tm[:, 2 + j, :], M12[:, ti, j, :], ok[:, 0, :], ALU.mult, r=[b_M, bok], w=[btm])
            okr.put(iok)
            yield
            P.add("dve", lambda e, tm=tm, ti=ti: e.reduce_sum(dest[:, ti, :], tm[:, 0:2, :], AX.X), [btm], [b_dest])
            gk, bgk, igk = yield from gkr.get()
            P.add("dve", lambda e, tm=tm, gk=gk: e.reduce_sum(gk[:], tm[:, 2:4, :], AX.X), [btm], [bgk])
            tmr.put(itm)
            yield
            k.tt("dve", gates[:, ti, :], gates[:, ti, :], gk[:], ALU.mult, r=[b_gates, bgk], w=[b_gates])
            gkr.put(igk)
            k.cp("dve", desti[:, ti, :], dest[:, ti, :], r=[b_dest], w=[b_desti])
            yield
            for j in range(2):
                P.add("pool", lambda e, ti=ti, j=j: e.indirect_dma_start(
                    out=ROUTE, out_offset=bass.IndirectOffsetOnAxis(ap=desti[:, ti, j:j + 1], axis=0),
                    in_=tokst[:, ti, j, :], in_offset=None), [b_desti, b_tokst], [bROUTE], dma=True)
                P.add("pool", lambda e, ti=ti, j=j, h2b=h2b: e.indirect_dma_start(
                    out=XS, out_offset=bass.IndirectOffsetOnAxis(ap=desti[:, ti, j:j + 1], axis=0),
                    in_=h2b[:], in_offset=None), [b_desti, bh2b], [bXS], dma=True)
                yield
            h2bf.put(ih2b)

        run_pipelined([slot_gen(ti) for ti in range(NT)], 3)
        k.dma("sp", GATE, gates[:].rearrange("p t j -> p (t j)"), r=[b_gates], w=[bGATE])
        k.flush()
        P.drain_dmas()
        P.emit()


def phase_moe(k):
    nc, P, S, NT = k.nc, k.P, k.S, k.NT
    B, CAP, PSL = MOE_B, k.CAP, k.PSL
    NS = B // 128
    H1, XS, ROUTE, GATE = (k.scr[n] for n in ("H1", "XS", "ROUTE", "GATE"))
    bH1, bXS, bROUTE, bGATE = (k.scr_buf[n] for n in ("H1", "XS", "ROUTE", "GATE"))
    YT = k.scr["YT"]
    bYT = k.scr_buf["YT"]
    WG = k.din["w_gate"].rearrange("e (p c) n -> e p (c n)", p=128)
    WU = k.din["w_up"].rearrange("e (p c) n -> e p (c n)", p=128)
    WD = k.din["w_down"].rearrange("e (p c) n -> e p (c n)", p=128)
    with ExitStack() as es:
        def sb(name, shape, dt):
            return es.enter_context(nc.sbuf_tensor("sb_" + name, list(shape), dt)), Buf(name)
        identb, b_identb = sb("m_ident", [128, 128], BF16)
        k.dma("sp", identb[:], k.din["ident_bf"], w=[b_identb])
        k.fence([bYT])
        tkr = sb_free(es, nc, "m_tk", 6, [128, 16], I32)
        xgr = sb_free(es, nc, "m_xg", 6, [128, D], BF16)
        wfr = sb_free(es, nc, "m_wf", 3, [128, 2048], F32)
        wgr = sb_free(es, nc, "m_wg", 2, [128, 8, DE], BF16)
        wur = sb_free(es, nc, "m_wu", 2, [128, 8, DE], BF16)
        wdr = sb_free(es, nc, "m_wd", 2, [128, 2, D], BF16)
        xTr = sb_free(es, nc, "m_xT", 3, [128, 8, B], BF16)
        sgr = sb_free(es, nc, "m_sg", 3, [128, B], F32)
        hTr = sb_free(es, nc, "m_hT", 3, [128, 2, B], BF16)
        ysr = sb_free(es, nc, "m_ys", 6, [128, D], BF16)
        psf = ps_free(es, nc, "m_psf", 6)
        psb = ps_free(es, nc, "m_psb", 2, (128, 1024), BF16)
        ceng = ["dve", "pool", "act"]
        wtiles = {}

        def wprep_gen(ex):
            wts = []
            for wi_, (src, pool_) in enumerate(((WG, wgr), (WU, wur), (WD, wdr))):
                wf, bwf, iwf = yield from wfr.get()
                k.load(wf[:], src[ex], w=[bwf])
                wb, bwb, iwb = yield from pool_.get()
                wts.append((wf, bwf, iwf, wb, bwb, iwb, pool_))
            wtiles[ex] = [(wb, bwb, iwb, pool_) for (_, _, _, wb, bwb, iwb, pool_) in wts]
            wtiles[(ex, "left")] = CAP // B
            yield
            for wi_, (wf, bwf, iwf, wb, bwb, iwb, pool_) in enumerate(wts):
                k.cp(ceng[wi_], wb[:].rearrange("p c n -> p (c n)"), wf[:], r=[bwf], w=[bwb])
                wfr.put(iwf)
                yield

        def blk_gen(ex, blk):
            (wg, bwg, _, _), (wu, bwu, _, _), (wd, bwd, _, _) = wtiles[ex]
            wgv = wg[:].rearrange("p c (q j) -> p c j q", j=2)
            wuv = wu[:].rearrange("p c (q j) -> p c j q", j=2)
            s0 = ex * CAP + blk * B
            xT, bxT, ixT = yield from xTr.get()
            tks, xgs = [], []
            for s_ in range(NS):
                tk, btk, itk = yield from tkr.get()
                k.load(tk[:], ROUTE[s0 + s_ * 128: s0 + (s_ + 1) * 128, :], r=[bROUTE], w=[btk])
                tks.append((tk, btk, itk))
                xg, bxg, ixg = yield from xgr.get()
                k.load(xg[:], XS[s0 + s_ * 128: s0 + (s_ + 1) * 128, :], r=[bXS], w=[bxg])
                xgs.append((xg, bxg, ixg))
            yield
            for s_ in range(NS):
                xg, bxg, ixg = xgs[s_]
                pb, bpb, ipb = yield from psb.get()
                xgv = xg[:].rearrange("p (q c) -> p c q", c=8)
                for c in range(8):
                    k.tr(pb[:, c * 128:(c + 1) * 128], xgv[:, c, :], identb[:], r=[bxg, b_identb], w=[bpb])
                xgr.put(ixg)
                yield
                k.cp("act" if s_ % 2 == 0 else "dve", xT[:, :, s_ * 128:(s_ + 1) * 128],
                     pb[:].rearrange("p (c t) -> p c t", c=8), r=[bpb], w=[bxT])
                psb.put(ipb)
            yield
            hT, bhT, ihT = yield from hTr.get()
            for c in range(2):
                gps, bgps, igps = yield from psf.get()
                ups, bups, iups = yield from psf.get()
                for kc in range(8):
                    k.mm(gps[:, 0:B], wgv[:, kc, c, :], xT[:, kc, :], start=(kc == 0), stop=(kc == 7),
                         r=[bwg, bxT], w=[bgps])
                for kc in range(8):
                    k.mm(ups[:, 0:B], wuv[:, kc, c, :], xT[:, kc, :], start=(kc == 0), stop=(kc == 7),
                         r=[bwu, bxT], w=[bups])
                yield
                sg, bsg, isg = yield from sgr.get()
                k.act(sg[:], gps[:, 0:B], AF.Silu, r=[bgps], w=[bsg])
                psf.put(igps)
                yield
                k.tt("dve", hT[:, c, :], sg[:], ups[:, 0:B], ALU.mult, r=[bsg, bups], w=[bhT])
                psf.put(iups)
                sgr.put(isg)
            xTr.put(ixT)
            yield
            for s_ in range(NS):
                ys, bys, iys = yield from ysr.get()
                for half in range(2):
                    yps, byps, iyps = yield from psf.get()
                    for c in range(2):
                        k.mm(yps[:], hT[:, c, s_ * 128:(s_ + 1) * 128], wd[:, c, half * 512:(half + 1) * 512],
                             start=(c == 0), stop=(c == 1), r=[bhT, bwd], w=[byps])
                    yield
                    k.cp("act" if half == 0 else "dve", ys[:, half * 512:(half + 1) * 512], yps[:], r=[byps], w=[bys])
                    psf.put(iyps)
                tk, btk, itk = tks[s_]
                P.add("pool", lambda e, ys=ys, tk=tk: e.indirect_dma_start(
                    out=YT, out_offset=bass.IndirectOffsetOnAxis(ap=tk[:, 0:1], axis=0),
                    in_=ys[:], in_offset=None), [btk, bys], [bYT], dma=True)
                tkr.put(itk)
                ysr.put(iys)
            hTr.put(ihT)
            wtiles[(ex, "left")] -= 1
            if wtiles[(ex, "left")] == 0:
                for (_, _, iwb, pool_) in wtiles[ex]:
                    pool_.put(iwb)

        for _ in wprep_gen(0):
            pass
        gens = []
        for ex in range(NE):
            if ex + 1 < NE:
                gens.append(wprep_gen(ex + 1))
            gens += [blk_gen(ex, blk) for blk in range(CAP // B)]
        run_pipelined(gens, 3)
        k.flush()
        P.drain_dmas()
        P.emit()
    with ExitStack() as es:
        def sb(name, shape, dt):
            return es.enter_context(nc.sbuf_tensor("sb_" + name, list(shape), dt)), Buf(name)
        gates, b_gates = sb("c_gate", [128, NT * 2], F32)
        k.dma("sp", gates[:], GATE, r=[bGATE], w=[b_gates])
        h1r = sb_ring(es, nc, "c_h1", 3, [128, D], F32)
        ytr = sb_ring(es, nc, "c_yt", 3, [128, 2, D], BF16)
        by = Buf("y")
        YTv = YT[0:2 * S, :].rearrange("(t j) n -> t j n", j=2)
        for ti in range(NT):
            r0 = ti * 128
            h1, bh1 = h1r.next()
            k.load(h1[:], H1[r0:r0 + 128, :], r=[bH1], w=[bh1])
            yt, byt = ytr.next()
            k.load(yt[:], YTv[r0:r0 + 128, :, :], r=[bYT], w=[byt])
            for j in range(2):
                P.add("dve", lambda e, yt=yt, h1=h1, ti=ti, j=j: e.scalar_tensor_tensor(
                    h1[:], yt[:, j, :], gates[:, 2 * ti + j:2 * ti + j + 1], h1[:], ALU.mult, ALU.add),
                    [byt, b_gates, bh1], [bh1])
            k.store(k.y[r0:r0 + 128, :], h1[:], r=[bh1], w=[by])
        k.flush()
        P.drain_dmas()
        P.emit()


def build_program(S):
    k = K(S)
    phase1(k)
    phase_attn(k)
    phase_hgrn(k)
    phase_route(k)
    phase_moe(k)
    return k


def kernel(**inputs):
    x = np.asarray(inputs["x"], dtype=np.float32)
    nb, S, _ = x.shape
    k = build_program(S)
    par = layout_params(inputs)
    con = make_consts(S)
    in_maps = []
    for c in range(nb):
        m = {"x": np.ascontiguousarray(x[c])}
        m.update(par)
        m.update(con)
        in_maps.append(m)
    res = run_bass_kernel_spmd(k.nc, in_maps, core_ids=list(range(nb)))
    return np.stack([np.asarray(r["y"], dtype=np.float32) for r in res.results], axis=0)
```

```python
import numpy as np
from contextlib import ExitStack
import concourse.bass as bass
import concourse.mybir as mybir
from concourse.bass_utils import run_bass_kernel_spmd

F32 = mybir.dt.float32
BF16 = mybir.dt.bfloat16
I32 = mybir.dt.int32
U32 = mybir.dt.uint32
U8 = mybir.dt.uint8
AF = mybir.ActivationFunctionType
ALU = mybir.AluOpType
AX = mybir.AxisListType

ENGS = ("pe", "act", "dve", "pool", "sp")
EPS = 1e-6
D = 1024
NIN = 2976
H = 8
QK = 96
NOPE = 64
ROPE = 32
DV = 64
HGH = 4
NE = 32
NG = 4
DE = 256
MOE_B = 256
MOE_LOG2B = 8
import os
MOE_DBG = int(os.environ.get('MOE_DBG', '0'))
class Buf:
    __slots__ = ("name", "last_w", "readers", "multi", "writers")

    def __init__(self, name="", multi=False):
        self.name = name
        self.last_w = None
        self.readers = {}
        self.multi = multi
        self.writers = []


class Op:
    __slots__ = ("eng", "fn", "waits", "signal", "ticket", "is_dma", "sem", "target", "idx", "emitted", "drained")


class Prog:
    def __init__(self, nc, n_dma_sems=40, n_sw_sems=8):
        self.nc = nc
        self.eng_obj = {"pe": nc.tensor, "act": nc.scalar, "dve": nc.vector,
                        "pool": nc.gpsimd, "sp": nc.sync}
        self.sem = {}
        self.count = {e: 0 for e in ENGS}
        self._stack = []
        for e in ENGS:
            g = nc.semaphore("s_" + e)
            self.sem[e] = g.__enter__()
            self._stack.append(g)
        self.dma_sems = []
        self.dma_uses = []
        self.dma_last = []
        for i in range(n_dma_sems):
            g = nc.semaphore("d_%d" % i)
            self.dma_sems.append(g.__enter__())
            self._stack.append(g)
            self.dma_uses.append(0)
            self.dma_last.append(None)
        self.dma_rr = 0
        self.sw_sems = []
        self.sw_uses = []
        self.sw_last = []
        for i in range(n_sw_sems):
            g = nc.semaphore("w_%d" % i)
            self.sw_sems.append(g.__enter__())
            self._stack.append(g)
            self.sw_uses.append(0)
            self.sw_last.append(None)
        self.sw_rr = 0
        self.ops = {e: [] for e in ENGS}
        self.waited = {e: {} for e in ENGS}
        self.nops = 0
        self.pending_dma = []
        self._deferred = []
        self._def_src = set()

    def defer_dma(self, eng, out, in_, reads=(), writes=(), **kw):
        self._deferred.append((eng, out, in_, tuple(reads), tuple(writes), kw))
        for b in reads:
            self._def_src.add(id(b))

    def flush(self):
        d, self._deferred = self._deferred, []
        self._def_src = set()
        for eng, out, in_, r, w, kw in d:
            self.dma(eng, out, in_, r, w, **kw)

    def add(self, eng, fn, reads=(), writes=(), dma=False):
        if self._deferred and any(id(b) in self._def_src for b in writes):
            self.flush()
        op = Op()
        op.eng = eng
        op.fn = fn
        op.waits = []
        op.signal = False
        op.ticket = None
        op.is_dma = dma
        op.sem = None
        op.target = None
        op.idx = self.nops
        op.emitted = False
        op.drained = False
        self.nops += 1
        deps = {}
        raw = set()
        for b in reads:
            if b.multi:
                b.writers = [w_ for w_ in b.writers if not (w_.emitted and (w_.drained or not w_.is_dma))]
                for w_ in b.writers:
                    deps[id(w_)] = w_
            elif b.last_w is not None:
                deps[id(b.last_w)] = b.last_w
                raw.add(id(b.last_w))
        for b in writes:
            if (not b.multi) and b.last_w is not None:
                deps[id(b.last_w)] = b.last_w
            for r in b.readers.values():
                deps[id(r)] = r
        for k, d in deps.items():
            if d is op:
                continue
            if d.is_dma:
                if not (d.emitted and d.drained):
                    op.waits.append(d)
            elif d.emitted:
                continue
            elif d.eng == eng and not dma:
                if eng == "pe":
                    continue
                d.signal = True
                op.waits.append(d)
            else:
                d.signal = True
                op.waits.append(d)
        if dma and eng == "pool":
            i = self.sw_rr
            self.sw_rr = (self.sw_rr + 1) % len(self.sw_sems)
            prev = self.sw_last[i]
            if prev is not None:
                op.waits.append(prev)
            self.sw_uses[i] += 1
            op.sem = self.sw_sems[i]
            op.target = 16 * self.sw_uses[i]
            self.sw_last[i] = op
            self.pending_dma.append(op)
        elif dma:
            i = self.dma_rr
            self.dma_rr = (self.dma_rr + 1) % len(self.dma_sems)
            prev = self.dma_last[i]
            if prev is not None:
                op.waits.append(prev)
            self.dma_uses[i] += 1
            op.sem = self.dma_sems[i]
            op.target = 16 * self.dma_uses[i]
            self.dma_last[i] = op
            self.pending_dma.append(op)
        for b in reads:
            key = ("d", op.idx) if dma else eng
            b.readers[key] = op
        for b in writes:
            if b.multi:
                b.writers.append(op)
                b.readers = {k_: r_ for k_, r_ in b.readers.items() if not (r_.emitted and (r_.drained or not r_.is_dma))}
            else:
                b.last_w = op
                b.readers = {}
        self.ops[eng].append(op)
        return op

    def dma(self, eng, out, in_, reads=(), writes=(), **kw):
        return self.add(eng, lambda e: e.dma_start(out=out, in_=in_, **kw), reads, writes, dma=True)

    def drain_dmas(self, eng="sp"):
        op = self.add(eng, None)
        seen = {}
        for d in self.pending_dma:
            d.drained = True
            seen[id(d.sem)] = d
        op.waits.extend(seen.values())
        self.pending_dma = []
        return op

    def emit(self, name=None):
        for e in ENGS:
            c = self.count[e]
            for op in self.ops[e]:
                if op.is_dma:
                    continue
                if op.signal:
                    c += 1
                    op.ticket = c
            self.count[e] = c
        prog = self

        def run(e, engine):
            waited = prog.waited[e]
            for op in prog.ops[e]:
                for d in op.waits:
                    if d.is_dma:
                        key, sem, val = id(d.sem), d.sem, d.target
                    else:
                        key, sem, val = d.eng, prog.sem[d.eng], d.ticket
                    if waited.get(key, 0) >= val:
                        continue
                    waited[key] = val
                    engine.wait_ge(sem, val)
                if op.fn is None:
                    continue
                ins = op.fn(engine)
                if op.is_dma:
                    ins.then_inc(op.sem, 16)
                elif op.signal:
                    ins.then_inc(prog.sem[e], 1)

        with self.nc.Block() as block:
            @block.tensor
            def _(eng):
                run("pe", eng)

            @block.scalar
            def _(eng):
                run("act", eng)

            @block.vector
            def _(eng):
                run("dve", eng)

            @block.gpsimd
            def _(eng):
                run("pool", eng)

            @block.sync
            def _(eng):
                run("sp", eng)
        for e in ENGS:
            for op in self.ops[e]:
                op.emitted = True
        self.ops = {e: [] for e in ENGS}


def _bf(a):
    import ml_dtypes
    return np.asarray(a, dtype=np.float32).astype(ml_dtypes.bfloat16)


def moe_cap(S):
    return max(MOE_B, ((3 * S // 32 + MOE_B - 1) // MOE_B) * MOE_B)


def make_consts(S):
    c = {}
    c["ident_bf"] = _bf(np.eye(128))
    c["ident_f"] = np.eye(128, dtype=np.float32)
    c["ones_bf"] = _bf(np.ones((128, 128)))
    c["ones_f"] = np.ones((128, 128), dtype=np.float32)
    rot = np.zeros((96, 96), np.float32)
    for i in range(16):
        rot[80 + i, 64 + i] = -1.0
        rot[64 + i, 80 + i] = 1.0
    c["rotT"] = _bf(rot)
    sel = np.zeros((32, 96), np.float32)
    for i in range(32):
        sel[i, 64 + i] = 1.0
    c["kr_sel"] = _bf(sel)
    half = 16
    inv = (1.0 / (10000.0 ** (np.arange(half, dtype=np.float32) / half))).astype(np.float32)
    ang = (np.arange(S, dtype=np.float32)[None, :] * inv[:, None]).astype(np.float32)
    cs = np.zeros((96, S), np.float32)
    sn = np.zeros((96, S), np.float32)
    cs[64:80] = np.cos(ang); cs[80:96] = np.cos(ang)
    sn[64:80] = np.sin(ang); sn[80:96] = np.sin(ang)
    c["rope_cos"] = cs
    c["rope_sin"] = sn
    s_ = np.arange(128)[:, None]
    t_ = np.arange(128)[None, :]
    c["hg_Lc_f"] = ((s_ <= t_).astype(np.float32) - (s_ <= 63).astype(np.float32))
    c["hg_Lr_f"] = (s_ > t_).astype(np.float32)
    c["hg_Lc_b"] = ((s_ >= t_).astype(np.float32) - (s_ >= 64).astype(np.float32))
    c["hg_Lr_b"] = (s_ < t_).astype(np.float32)
    c["hg_selm_f"] = np.stack([np.ones(128), (np.arange(128) <= 63)], 1).astype(np.float32)
    c["hg_selm_b"] = np.stack([np.ones(128), (np.arange(128) >= 64)], 1).astype(np.float32)
    mf = (s_ <= t_).astype(np.uint32)
    mb = (s_ >= t_).astype(np.uint32)
    c["hg_mask_f"] = np.tile(mf, (1, 4))
    c["hg_mask_b"] = np.tile(mb, (1, 4))
    c["ustrict"] = (s_ < t_).astype(np.float32)
    c["u32strict"] = (np.arange(32)[:, None] < np.arange(32)[None, :]).astype(np.float32)
    c["ident32"] = np.eye(32, dtype=np.float32)
    cap = moe_cap(S)
    c["blk_iota"] = np.stack([np.arange(32) * cap, np.zeros(32)], 1).astype(np.float32)
    c["lim_row"] = np.tile(((np.arange(32) + 1) * cap).astype(np.float32)[None, :], (128, 1))
    c["p_iota"] = np.arange(128, dtype=np.float32).reshape(128, 1)
    c["tok_iota"] = (np.arange(S // 128, dtype=np.int32)[None, :] * 128 + np.arange(128, dtype=np.int32)[:, None]).astype(np.int32)
    return c


CONST_SPECS = {
    "ustrict": ([128, 128], F32), "u32strict": ([32, 32], F32), "ident32": ([32, 32], F32),
    "blk_iota": ([32, 2], F32), "lim_row": ([128, 32], F32), "p_iota": ([128, 1], F32), "tok_iota": "tok",
    "ident_bf": ([128, 128], BF16), "ident_f": ([128, 128], F32),
    "ones_bf": ([128, 128], BF16), "ones_f": ([128, 128], F32),
    "rotT": ([96, 96], BF16), "kr_sel": ([32, 96], BF16),
    "rope_cos": None, "rope_sin": None,
    "hg_Lc_f": ([128, 128], F32), "hg_Lr_f": ([128, 128], F32),
    "hg_Lc_b": ([128, 128], F32), "hg_Lr_b": ([128, 128], F32),
    "hg_selm_f": ([128, 2], F32), "hg_selm_b": ([128, 2], F32),
    "hg_mask_f": ([128, 512], U32), "hg_mask_b": ([128, 512], U32),
}


def layout_params(inp):
    f = lambda a: np.ascontiguousarray(np.asarray(a, dtype=np.float32))
    p = {}
    p["norm_mix_l"] = f(inp["norm_mix"][0].reshape(8, 128).T)
    p["q_lat_norm_l"] = f(inp["q_lat_norm"][0].reshape(2, 128).T)
    p["kv_lat_norm_l"] = f(inp["kv_lat_norm"][0].reshape(1, 128).T)
    p["q_norm_l"] = f(inp["q_norm"][0].reshape(96, 1))
    p["k_norm_l"] = f(inp["k_norm"][0].reshape(96, 1))
    p["lb_logits_l"] = f(inp["lb_logits"].reshape(2, 2 * 512))
    p["hg_out_norm_l"] = f(inp["hg_out_norm"][0].reshape(1, 128))
    p["norm_ffn_l"] = f(inp["norm_ffn"][0].reshape(1, 1024))
    p["w_rt"] = f(np.concatenate([inp["w_group"][0], inp["w_router"][0]], axis=1))
    p["b_rt"] = f(np.concatenate([inp["b_group"][0], inp["b_router"][0]]).reshape(1, 36))
    p["w_in"] = f(inp["w_in"][0])
    p["w_uq"] = f(inp["w_uq"][0])
    p["w_ukv"] = f(inp["w_ukv"][0])
    p["w_out"] = f(inp["w_out"][0])
    p["w_gate"] = f(inp["w_gate"][0])
    p["w_up"] = f(inp["w_up"][0])
    p["w_down"] = f(inp["w_down"][0])
    return p


PARAM_SPECS = {
    "norm_mix_l": [128, 8], "q_lat_norm_l": [128, 2], "kv_lat_norm_l": [128, 1],
    "q_norm_l": [96, 1], "k_norm_l": [96, 1], "lb_logits_l": [2, 1024],
    "hg_out_norm_l": [1, 128], "norm_ffn_l": [1, 1024], "w_rt": [1024, 36], "b_rt": [1, 36],
    "w_in": [D, NIN], "w_uq": [256, 768], "w_ukv": [128, 1024], "w_out": [1024, 1024],
    "w_gate": [NE, D, DE], "w_up": [NE, D, DE], "w_down": [NE, DE, D],
}


class K:
    def __init__(self, S, debug=(), phases=None):
        self.S = S
        self.NT = S // 128
        self.NB = S // 512
        self.debug = set(debug)
        self.phases = phases
        self.nc = nc = bass.Bass("TRN2", target_bir_lowering=False)
        self.P = Prog(nc)
        self.din = {}
        self.x = nc.dram_tensor("x", [S, D], F32, kind="ExternalInput").ap()
        for k, shp in PARAM_SPECS.items():
            self.din[k] = nc.dram_tensor(k, shp, F32, kind="ExternalInput").ap()
        for k, spec in CONST_SPECS.items():
            if spec is None:
                shp, dt = [96, S], F32
            elif spec == "tok":
                shp, dt = [128, S // 128], I32
            else:
                shp, dt = spec
            self.din[k] = nc.dram_tensor(k, shp, dt, kind="ExternalInput").ap()
        self.y = nc.dram_tensor("y", [S, D], F32, kind="ExternalOutput").ap()
        self.scr = {}
        self.scr_buf = {}
        self.deferred = []
        self.fence_t = nc.alloc_sbuf_tensor("fence_scratch", [128, 1], F32)
        self.b_fence = Buf("fence")

    def scratch(self, name, shape, dt):
        kind = "ExternalOutput" if name in self.debug else "Internal"
        t = self.nc.dram_tensor(name, shape, dt, kind=kind).ap()
        self.scr[name] = t
        self.scr_buf[name] = Buf(name, multi=True)
        return t

    def mm(self, out, lhsT, rhs, start=True, stop=True, r=(), w=()):
        return self.P.add("pe", lambda e: e.matmul(out, lhsT, rhs, start=start, stop=stop), r, w)

    def tr(self, out, in_, ident, r=(), w=()):
        return self.P.add("pe", lambda e: e.transpose(out, in_, ident), r, w)

    def act(self, out, in_, func, r=(), w=(), eng="act", **kw):
        return self.P.add(eng, lambda e: e.activation(out, in_, func, **kw), r, w)

    def ts(self, eng, out, in0, s1, s2, op0, op1=None, r=(), w=(), **kw):
        if op1 is None:
            return self.P.add(eng, lambda e: e.tensor_scalar(out, in0, s1, s2, op0, **kw), r, w)
        return self.P.add(eng, lambda e: e.tensor_scalar(out, in0, s1, s2, op0, op1, **kw), r, w)

    def tt(self, eng, out, in0, in1, op, r=(), w=()):
        return self.P.add(eng, lambda e: e.tensor_tensor(out, in0, in1, op), r, w)

    def cp(self, eng, out, in_, r=(), w=()):
        if eng == "act":
            return self.P.add(eng, lambda e: e.copy(out, in_), r, w)
        return self.P.add(eng, lambda e: e.tensor_copy(out, in_), r, w)

    def dma(self, eng, out, in_, r=(), w=(), **kw):
        return self.P.dma(eng, out, in_, r, w, **kw)

    def load(self, out, in_, r=(), w=(), **kw):
        op = self.P.dma("sp", out, in_, r, w, **kw)
        self.flush()
        return op

    def store(self, out, in_, r=(), w=(), **kw):
        self.P.defer_dma("sp", out, in_, r, w, **kw)

    def fence(self, bufs):
        t = self.fence_t
        self.P.add("pool", lambda e: e.memset(t[0:1, 0:1], 0.0), list(bufs), [self.b_fence])

    def flush(self):
        self.P.flush()

    def make_eps(self, es):
        self.epst = {}
        self.b_epst = Buf("eps")
        for n in (96, 128, 256, 1024):
            t = es.enter_context(self.nc.sbuf_tensor("sb_eps%d" % n, [128, 1], F32))
            self.epst[n] = t
            self.P.add("pool", lambda e, t=t, n=n: e.memset(t[:], float(n * EPS)), (), [self.b_epst])

    def rstd(self, out, in_, neps, bout, r=()):
        np_ = out.shape[0]
        self.P.add("act", lambda e: e.activation(out, in_, AF.Ln, bias=self.epst[neps][0:np_, 0:1]), list(r) + [self.b_epst], [bout])
        self.P.add("act", lambda e: e.activation(out, out, AF.Exp, scale=-0.5), [bout], [bout])


def run_pipelined(gens, depth):
    active = []
    it = iter(gens)
    while True:
        while len(active) < depth:
            g = next(it, None)
            if g is None:
                break
            active.append(g)
        if not active:
            break
        for g in list(active):
            if next(g, "done") == "done":
                active.remove(g)


class Pool_:
    def __init__(self, tiles):
        self.tiles = tiles
        self.bufs = [Buf() for _ in tiles]
        self.i = 0

    def next(self):
        i = self.i
        self.i = (self.i + 1) % len(self.tiles)
        return self.tiles[i], self.bufs[i]


class FreePool:
    def __init__(self, tiles):
        self.tiles = tiles
        self.bufs = [Buf() for _ in tiles]
        self.free = list(range(len(tiles)))

    def get(self):
        while not self.free:
            yield
        i = self.free.pop(0)
        return self.tiles[i], self.bufs[i], i

    def put(self, i):
        self.free.append(i)

    def take(self):
        i = self.free.pop(0)
        return self.tiles[i], self.bufs[i], i


def sb_free(es, nc, name, n, shape, dt):
    return FreePool([es.enter_context(nc.sbuf_tensor("sb_%s%d" % (name, i), shape, dt)) for i in range(n)])


def ps_free(es, nc, name, n, shape=(128, 512), dt=F32):
    return FreePool([es.enter_context(nc.psum_tensor("%s%d" % (name, i), list(shape), dt)) for i in range(n)])


def sb_ring(es, nc, name, n, shape, dt):
    return Pool_([es.enter_context(nc.sbuf_tensor("sb_%s%d" % (name, i), shape, dt)) for i in range(n)])


def ps_ring(es, nc, name, n, shape=(128, 512), dt=F32):
    return Pool_([es.enter_context(nc.psum_tensor("%s%d" % (name, i), list(shape), dt)) for i in range(n)])


def phase1(k):
    nc, P, S = k.nc, k.P, k.S
    QT = k.scratch("QT", [H, QK, S], BF16)
    KT = k.scratch("KT", [H, QK, S], BF16)
    VV = k.scratch("VV", [S, H * DV], BF16)
    HG = k.scratch("HG", [S, 2560], F32)
    bQT, bKT, bVV, bHG = (k.scr_buf[n] for n in ("QT", "KT", "VV", "HG"))
    with ExitStack() as es:
        def sb(name, shape, dt):
            return es.enter_context(nc.sbuf_tensor("sb_" + name, list(shape), dt)), Buf(name)
        ident, b_ident = sb("ident", [128, 128], BF16)
        ones, b_ones = sb("ones", [128, 128], BF16)
        rotT, b_rotT = sb("rotT", [96, 96], BF16)
        krsel, b_krsel = sb("krsel", [32, 96], BF16)
        for t_, b_, nm in ((ident, b_ident, "ident_bf"), (ones, b_ones, "ones_bf"),
                           (rotT, b_rotT, "rotT"), (krsel, b_krsel, "kr_sel")):
            k.dma("sp", t_[:], k.din[nm], w=[b_])
        k.make_eps(es)
        gmix, b_gmix = sb("gmix", [128, 8], F32)
        gql, b_gql = sb("gql", [128, 2], F32)
        gkvl, b_gkvl = sb("gkvl", [128, 1], F32)
        gq, b_gq = sb("gq", [96, 1], F32)
        gk, b_gk = sb("gk", [96, 1], F32)
        for t_, b_, nm in ((gmix, b_gmix, "norm_mix_l"), (gql, b_gql, "q_lat_norm_l"),
                           (gkvl, b_gkvl, "kv_lat_norm_l"), (gq, b_gq, "q_norm_l"), (gk, b_gk, "k_norm_l")):
            k.dma("sp", t_[:], k.din[nm], w=[b_])
        k.ts("dve", gmix[:], gmix[:], 32.0, None, ALU.mult, r=[b_gmix], w=[b_gmix])
        k.ts("dve", gql[:], gql[:], 16.0, None, ALU.mult, r=[b_gql], w=[b_gql])
        k.ts("dve", gkvl[:], gkvl[:], float(np.sqrt(128.0)), None, ALU.mult, r=[b_gkvl], w=[b_gkvl])
        k.ts("dve", gq[:], gq[:], float(np.sqrt(96.0) * 96.0 ** -0.5), None, ALU.mult, r=[b_gq], w=[b_gq])
        k.ts("dve", gk[:], gk[:], float(np.sqrt(96.0)), None, ALU.mult, r=[b_gk], w=[b_gk])

        win, b_win = sb("win", [128, 8, NIN], BF16)
        wst = sb_ring(es, nc, "wst", 1, [128, NIN], F32)
        w_in_v = k.din["w_in"].rearrange("(kc p) n -> p kc n", p=128)
        for kc in range(8):
            st, bst = wst.next()
            k.dma("sp", st[:], w_in_v[:, kc, :], w=[bst])
            k.ts("dve" if kc % 2 == 0 else "pool", win[:, kc, :], st[:], gmix[:, kc:kc + 1], None, ALU.mult,
                 r=[bst, b_gmix], w=[b_win])
        wuq, b_wuq = sb("wuq", [128, 2, 768], BF16)
        w_uq_v = k.din["w_uq"].rearrange("(kc p) n -> p kc n", p=128)
        for kc in range(2):
            st, bst = wst.next()
            k.dma("sp", st[:, 0:768], w_uq_v[:, kc, :], w=[bst])
            k.ts("dve", wuq[:, kc, :], st[:, 0:768], gql[:, kc:kc + 1], None, ALU.mult, r=[bst, b_gql], w=[b_wuq])
        wk, b_wk = sb("wk", [128, 8, 96], BF16)
        wv, b_wv = sb("wv", [128, 8, 64], BF16)
        st, bst = wst.next()
        k.dma("sp", st[:, 0:1024], k.din["w_ukv"], w=[bst])
        P.add("pool", lambda e: e.memset(wk[:], 0.0), (), [b_wk])
        stv = st[:, 0:1024].rearrange("p (h c) -> p h c", c=128)
        k.ts("dve", wk[:, :, 0:64], stv[:, :, 0:64], gkvl[:, 0:1], None, ALU.mult, r=[bst, b_gkvl], w=[b_wk])
        k.ts("dve", wv[:], stv[:, :, 64:128], gkvl[:, 0:1], None, ALU.mult, r=[bst, b_gkvl], w=[b_wv])

        xr = sb_ring(es, nc, "xt", 3, [128, D], F32)
        junkr = sb_ring(es, nc, "junk", 3, [128, D], BF16)
        ssr = sb_ring(es, nc, "ss", 4, [128, 2], F32)
        nr = sb_ring(es, nc, "nbf", 2, [128, D], BF16)
        nTr = sb_ring(es, nc, "nT", 2, [128, 8, 512], BF16)
        stg = sb_ring(es, nc, "stg", 2, [128, 2560], F32)
        sqq, b_sqq = sb("sqq", [128, 3, 512], BF16)
        rsl, b_rsl = sb("rsl", [128, 2, 512], F32)
        qnT, b_qnT = sb("qnT", [128, 2, 512], BF16)
        kvnT, b_kvnT = sb("kvnT", [128, 512], BF16)
        krT, b_krT = sb("krT", [32, 512], BF16)
        vsb = sb_ring(es, nc, "vsb", 2, [128, 512], BF16)
        cosr = sb_ring(es, nc, "cos", 2, [96, 512], F32)
        sinr = sb_ring(es, nc, "sin", 2, [96, 512], F32)
        sqh = sb_ring(es, nc, "sqh", 3, [96, 512], BF16)
        qgh = sb_ring(es, nc, "qgh", 3, [96, 512], BF16)
        qg32 = sb_ring(es, nc, "qg32", 3, [96, 512], F32)
        rsh = sb_ring(es, nc, "rsh", 3, [96, 512], F32)
        t1r = sb_ring(es, nc, "t1r", 3, [96, 512], F32)
        t2r = sb_ring(es, nc, "t2r", 3, [96, 512], F32)
        qfr = sb_ring(es, nc, "qfr", 3, [96, 512], BF16)
        psf = ps_ring(es, nc, "psf", 6)
        psb = ps_ring(es, nc, "psb", 2, (128, 1024), BF16)

        eflip = [0]

        def evac_eng():
            eflip[0] ^= 1
            return "act" if eflip[0] else "dve"

        for j in range(k.NB):
            tok0 = j * 512
            nT, b_nT = nTr.next()
            def xt_gen(t, tok0=tok0, nT=nT, b_nT=b_nT):
                r0 = tok0 + t * 128
                xt, bx = xr.next()
                k.load(xt[:], k.x[r0:r0 + 128, :], w=[bx])
                yield
                ss, bss = ssr.next()
                junk, b_junk = junkr.next()
                k.act(junk[:], xt[:], AF.Square, r=[bx], w=[b_junk, bss], accum_out=ss[:, 0:1])
                k.rstd(ss[:, 1:2], ss[:, 0:1], 1024, bss, r=[bss])
                yield
                nb_, bn = nr.next()
                k.act(nb_[:], xt[:], AF.Copy, r=[bx, bss], w=[bn], scale=ss[:, 1:2])
                yield
                pb, bpb = psb.next()
                for kc in range(8):
                    k.tr(pb[:, kc * 128:(kc + 1) * 128], nb_[:, kc * 128:(kc + 1) * 128], ident[:],
                         r=[bn, b_ident], w=[bpb])
                yield
                k.cp("act" if t % 2 == 0 else "dve", nT[:, :, t * 128:(t + 1) * 128],
                     pb[:].rearrange("p (kc t) -> p kc t", kc=8), r=[bpb], w=[b_nT])

            run_pipelined([xt_gen(t) for t in range(4)], 2)
            lat = []
            for (c0, ncol) in ((0, 128), (128, 128), (256, 128), (384, 32)):
                ps, bps = psf.next()
                for kc in range(8):
                    k.mm(ps[0:ncol, :], win[:, kc, c0:c0 + ncol], nT[:, kc, :], start=(kc == 0), stop=(kc == 7),
                         r=[b_win, b_nT], w=[bps])
                lat.append((ps, bps))
            for i in range(3):
                k.act(sqq[:, i, :], lat[i][0][:], AF.Square, r=[lat[i][1]], w=[b_sqq])
            k.cp("dve", krT[:], lat[3][0][0:32, :], r=[lat[3][1]], w=[b_krT])
            msq, bmsq = psf.next()
            k.mm(msq[:], ones[:], sqq[:, 0, :], start=True, stop=False, r=[b_ones, b_sqq], w=[bmsq])
            k.mm(msq[:], ones[:], sqq[:, 1, :], start=False, stop=True, r=[b_ones, b_sqq], w=[bmsq])
            msk, bmsk = psf.next()
            k.mm(msk[:], ones[:], sqq[:, 2, :], r=[b_ones, b_sqq], w=[bmsk])
            k.rstd(rsl[:, 0, :], msq[:], 256, b_rsl, r=[bmsq])
            k.rstd(rsl[:, 1, :], msk[:], 128, b_rsl, r=[bmsk])
            k.tt("dve", qnT[:, 0, :], lat[0][0][:], rsl[:, 0, :], ALU.mult, r=[lat[0][1], b_rsl], w=[b_qnT])
            k.tt("dve", qnT[:, 1, :], lat[1][0][:], rsl[:, 0, :], ALU.mult, r=[lat[1][1], b_rsl], w=[b_qnT])
            k.tt("dve", kvnT[:], lat[2][0][:], rsl[:, 1, :], ALU.mult, r=[lat[2][1], b_rsl], w=[b_kvnT])
            for t in range(4):
                ps, bps = psf.next()
                k.mm(ps[:], kvnT[:, t * 128:(t + 1) * 128], wv[:].rearrange("p h c -> p (h c)"),
                     r=[b_kvnT, b_wv], w=[bps])
                v_, bv = vsb.next()
                k.cp("dve", v_[:], ps[:], r=[bps], w=[bv])
                k.store(VV[tok0 + t * 128: tok0 + (t + 1) * 128, :], v_[:], r=[bv], w=[bVV])
            cs, bcs = cosr.next()
            sn, bsn = sinr.next()
            k.load(cs[64:96, :], k.din["rope_cos"][64:96, tok0:tok0 + 512], w=[bcs])
            k.load(sn[64:96, :], k.din["rope_sin"][64:96, tok0:tok0 + 512], w=[bsn])

            def head_gen(h, which, tok0=tok0, cs=cs, bcs=bcs, sn=sn, bsn=bsn):
                ps, bps = psf.next()
                if which == 0:
                    k.mm(ps[0:96, :], wuq[:, 0, h * 96:(h + 1) * 96], qnT[:, 0, :], start=True, stop=False,
                         r=[b_wuq, b_qnT], w=[bps])
                    k.mm(ps[0:96, :], wuq[:, 1, h * 96:(h + 1) * 96], qnT[:, 1, :], start=False, stop=True,
                         r=[b_wuq, b_qnT], w=[bps])
                    g_, bg_, dst, bdst = gq, b_gq, QT, bQT
                else:
                    k.mm(ps[0:96, :], wk[:, h, :], kvnT[:], start=True, stop=False, r=[b_wk, b_kvnT], w=[bps])
                    k.mm(ps[0:96, :], krsel[:], krT[:], start=False, stop=True, r=[b_krsel, b_krT], w=[bps])
                    g_, bg_, dst, bdst = gk, b_gk, KT, bKT
                yield
                sq, bsq = sqh.next()
                k.act(sq[:], ps[0:96, :], AF.Square, r=[bps], w=[bsq])
                q32, bq32 = qg32.next()
                k.act(q32[:], ps[0:96, :], AF.Copy, r=[bps, bg_], w=[bq32], scale=g_[:, 0:1])
                yield
                ms, bms = psf.next()
                k.mm(ms[0:96, :], ones[0:96, 0:96], sq[:], r=[b_ones, bsq], w=[bms])
                qg, bqg = qgh.next()
                k.act(qg[:], ps[0:96, :], AF.Copy, r=[bps, bg_], w=[bqg], scale=g_[:, 0:1])
                t1, bt1 = t1r.next()
                k.tt("pool", t1[64:96, :], q32[64:96, :], cs[64:96, :], ALU.mult, r=[bq32, bcs], w=[bt1])
                yield
                rt, brt = psf.next()
                k.mm(rt[0:96, :], rotT[:], qg[:], r=[b_rotT, bqg], w=[brt])
                rs, brs = rsh.next()
                k.rstd(rs[:], ms[0:96, :], 96, brs, r=[bms])
                yield
                t2, bt2 = t2r.next()
                k.tt("dve", t2[64:96, :], rt[64:96, :], sn[64:96, :], ALU.mult, r=[brt, bsn], w=[bt2])
                yield
                k.tt("pool", q32[64:96, :], t1[64:96, :], t2[64:96, :], ALU.add, r=[bt1, bt2], w=[bq32])
                yield
                qf, bqf = qfr.next()
                k.tt("dve", qf[:], q32[:], rs[:], ALU.mult, r=[bq32, brs], w=[bqf])
                k.store(dst[h, :, tok0:tok0 + 512], qf[:], r=[bqf], w=[bdst])

            def tm_gen(t, tok0=tok0, nT=nT, b_nT=b_nT):
                sg, bsg = stg.next()
                for g in range(5):
                    ps, bps = psf.next()
                    c0 = 416 + g * 512
                    for kc in range(8):
                        k.mm(ps[:], nT[:, kc, t * 128:(t + 1) * 128], win[:, kc, c0:c0 + 512],
                             start=(kc == 0), stop=(kc == 7), r=[b_nT, b_win], w=[bps])
                    yield
                    k.cp(evac_eng(), sg[:, g * 512:(g + 1) * 512], ps[:], r=[bps], w=[bsg])
                k.store(HG[tok0 + t * 128: tok0 + (t + 1) * 128, :], sg[:], r=[bsg], w=[bHG])

            gens = []
            hw = [(h, w_) for h in range(H) for w_ in range(2)]
            for t in range(4):
                gens += [head_gen(h, w_) for (h, w_) in hw[4 * t:4 * t + 4]]
                gens.append(tm_gen(t))
            run_pipelined(gens, 3)
        k.flush()
        P.drain_dmas()
        P.emit()


def moe_scratch_init(k, es):
    nc, P, S = k.nc, k.P, k.S
    CAP = moe_cap(S)
    DUMP = NE * CAP
    PSL = DUMP + 128
    k.CAP, k.PSL = CAP, PSL
    XS = k.scratch("XS", [PSL, D], BF16)
    YB = k.scratch("YB", [PSL, D], BF16)
    bXS, bYB = (k.scr_buf[n] for n in ("XS", "YB"))
    zx = es.enter_context(nc.sbuf_tensor("sb_i_zx", [128, 4 * D], BF16))
    b_zx = Buf("i_zx")
    P.add("pool", lambda e: e.memset(zx[:], 0.0), (), [b_zx])
    XSz = XS.rearrange("(p r) n -> p r n", p=128)
    RX = PSL // 128
    for r_ in range(0, RX, 4):
        n_ = min(4, RX - r_)
        k.dma("pool", XSz[:, r_:r_ + n_, :], zx[:, 0:n_ * D].rearrange("p (r n) -> p r n", r=n_), r=[b_zx], w=[bXS])
    k.dma("pool", YB[DUMP:DUMP + 128, :], zx[:, 0:D], r=[b_zx], w=[bYB])


def phase_attn(k, with_hgrn=False):
    nc, P, S, NT = k.nc, k.P, k.S, k.NT
    QT, KT, VV = k.scr["QT"], k.scr["KT"], k.scr["VV"]
    bQT, bKT, bVV = k.scr_buf["QT"], k.scr_buf["KT"], k.scr_buf["VV"]
    MIXT = k.scratch("MIXT", [D, S], BF16)
    bMIXT = k.scr_buf["MIXT"]
    VVv = VV.rearrange("(t p) (h c) -> p t h c", p=128, c=DV)
    RD = k.scratch("RD", [H * (S // 512), 512], F32)
    bRD = [Buf("rd%d" % i) for i in range(4)]
    with ExitStack() as es:
        def sb(name, shape, dt):
            return es.enter_context(nc.sbuf_tensor("sb_" + name, list(shape), dt)), Buf(name)
        onesf, b_onesf = sb("a_onesf", [128, 128], F32)
        k.dma("sp", onesf[:], k.din["ones_f"], w=[b_onesf])
        kth = sb_ring(es, nc, "a_kt", 2, [96, S], BF16)
        qth = sb_ring(es, nc, "a_qt", 3, [96, 512], BF16)
        vh = sb_ring(es, nc, "a_v", 2, [128, NT, DV + 1], BF16)
        for t_, b_ in zip(vh.tiles, vh.bufs):
            P.add("pool", lambda e, t_=t_: e.memset(t_[:], 1.0), (), [b_])
        ptr = sb_ring(es, nc, "a_pt", 3 if with_hgrn else 4, [128, 1024], BF16)
        ocr = sb_ring(es, nc, "a_oc", 2, [128, 512], F32)
        osb = sb_ring(es, nc, "a_o", 2, [64, 512], F32)
        aout = sb_ring(es, nc, "a_a", 2, [64, 512], BF16)
        scr_ = ps_ring(es, nc, "a_sc", 2 if with_hgrn else 3, (128, 1024), F32)
        accr = ps_ring(es, nc, "a_acc", 1 if with_hgrn else 2)
        hstep = None
        if with_hgrn:
            h_psf = ps_free(es, nc, "h_psf", 2)
            h_psb = ps_free(es, nc, "h_psb", 1, (128, 1024), BF16)
            hstep = hgrn_build(k, es, h_psf, h_psb)
            hsteps = hgrn_order(NT)
        NP2 = NT // 2
        NQB = S // 512
        heads = {}

        def load_head(h):
            kt_, bkt = kth.next()
            v_, bv = vh.next()
            k.load(kt_[:], KT[h], r=[bKT], w=[bkt])
            k.load(v_[:, :, 0:DV], VVv[:, :, h, :], r=[bVV], w=[bv])
            heads[h] = (kt_, bkt, v_, bv)

        qbs = {}

        def load_q(h, qb):
            qt_, bqt = qth.next()
            k.load(qt_[:], QT[h, :, qb * 512:(qb + 1) * 512], r=[bQT], w=[bqt])
            qbs[(h, qb)] = (qt_, bqt)

        steps = [(h, qb, kp) for h in range(H) for qb in range(NQB) for kp in range(NP2)]
        scs = {}

        def emit_qk(i):
            h, qb, kp = steps[i]
            if kp == 0:
                if qb == 0 and h not in heads:
                    load_head(h)
                if (h, qb) not in qbs:
                    load_q(h, qb)
                nxt = (h, qb + 1) if qb + 1 < NQB else ((h + 1, 0) if h + 1 < H else None)
                if nxt is not None:
                    if nxt[1] == 0 and nxt[0] not in heads:
                        load_head(nxt[0])
                    if nxt not in qbs:
                        load_q(*nxt)
            kt_, bkt, v_, bv = heads[h]
            qt_, bqt = qbs[(h, qb)]
            sc, bsc = scr_.next()
            for u in range(2):
                kt = 2 * kp + u
                k.mm(sc[:, u * 512:(u + 1) * 512], kt_[:, kt * 128:(kt + 1) * 128], qt_[:],
                     r=[bkt, bqt], w=[bsc])
            scs[i] = (sc, bsc)

        LOOK = 1 if with_hgrn else 2
        for i in range(min(LOOK, len(steps))):
            emit_qk(i)
        if "XS" not in k.scr:
            moe_scratch_init(k, es)
        acc = bacc = None
        gen = None
        for i, (h, qb, kp) in enumerate(steps):
            kt_, bkt, v_, bv = heads[h]
            if kp == 0:
                acc, bacc = accr.next()
                if hstep is not None and h * NQB + qb < len(hsteps):
                    gen = hstep(*hsteps[h * NQB + qb])
            sc, bsc = scs.pop(i)
            pt, bpt = ptr.next()
            k.act(pt[:], sc[:], AF.Exp, r=[bsc], w=[bpt])
            if i + LOOK < len(steps):
                emit_qk(i + LOOK)
            for u in range(2):
                kt = 2 * kp + u
                k.mm(acc[0:DV + 1, :], v_[:, kt, :], pt[:, u * 512:(u + 1) * 512],
                     start=(kt == 0), stop=(kt == NT - 1), r=[bv, bpt], w=[bacc])
            if gen is not None:
                if next(gen, "done") == "done":
                    gen = None
            if kp == NP2 - 1:
                if gen is not None:
                    for _ in gen:
                        pass
                    gen = None
                oc, boc = ocr.next()
                k.cp("dve", oc[0:DV + 1, :], acc[0:DV + 1, :], r=[bacc], w=[boc])
                P.add("dve", lambda e, oc=oc: e.reciprocal(oc[64:65, :], oc[64:65, :]), [boc], [boc])
                o_, bo = osb.next()
                ridx = h * NQB + qb
                k.dma("sp", RD[ridx:ridx + 1, :], oc[64:65, :], r=[boc], w=[bRD[ridx % 4]])
                k.dma("sp", o_[:], RD[ridx:ridx + 1, :].partition_broadcast(64), r=[bRD[ridx % 4]], w=[bo])
                a_, ba = aout.next()
                k.tt("dve", a_[:], oc[0:64, :], o_[:], ALU.mult, r=[boc, bo], w=[ba])
                k.store(MIXT[h * 64:(h + 1) * 64, qb * 512:(qb + 1) * 512], a_[:], r=[ba], w=[bMIXT])
        k.flush()
        P.drain_dmas()
        P.emit()


def hgrn_build(k, es, psf, psb):
    nc, P, S, NT = k.nc, k.P, k.S, k.NT
    HG, MIXT = k.scr["HG"], k.scr["MIXT"]
    bHG, bMIXT = k.scr_buf["HG"], k.scr_buf["MIXT"]
    OF = k.scratch("OF", [S, 512], F32)
    bOF = k.scr_buf["OF"]
    MIXr = MIXT[512:1024, :].rearrange("(c p) t -> p c t", p=128)
    def sb(name, shape, dt):
        return es.enter_context(nc.sbuf_tensor("sb_" + name, list(shape), dt)), Buf(name)
    identb, b_identb = sb("h_ident", [128, 128], BF16)
    k.dma("sp", identb[:], k.din["ident_bf"], w=[b_identb])
    cst = {}
    for nm, shp, dt in (("hg_Lc_f", [128, 128], F32), ("hg_Lr_f", [128, 128], F32), ("hg_Lc_b", [128, 128], F32),
                        ("hg_Lr_b", [128, 128], F32), ("hg_selm_f", [128, 2], F32), ("hg_selm_b", [128, 2], F32),
                        ("hg_mask_f", [128, 512], U32), ("hg_mask_b", [128, 512], U32)):
        t_, b_ = sb(nm, shp, dt)
        k.dma("sp", t_[:], k.din[nm], w=[b_])
        cst[nm] = (t_, b_)
    l0, b_l0 = sb("h_l0", [128, 1024], F32)
    l1, b_l1 = sb("h_l1", [128, 1024], F32)
    k.dma("sp", l0[:], k.din["lb_logits_l"][0:1, :].partition_broadcast(128), w=[b_l0])
    k.dma("sp", l1[:], k.din["lb_logits_l"][1:2, :].partition_broadcast(128), w=[b_l1])
    lbb, b_lbb = sb("h_lb", [128, 1024], F32)
    oml, b_oml = sb("h_oml", [128, 1024], F32)
    k.tt("dve", l1[:], l1[:], l0[:], ALU.subtract, r=[b_l0, b_l1], w=[b_l1])
    k.act(l1[:], l1[:], AF.Exp, r=[b_l1], w=[b_l1])
    k.ts("dve", l1[:], l1[:], 1.0, None, ALU.add, r=[b_l1], w=[b_l1])
    P.add("dve", lambda e: e.reciprocal(lbb[:], l1[:]), [b_l1], [b_lbb])
    k.ts("dve", oml[:], lbb[:], -1.0, 1.0, ALU.mult, ALU.add, r=[b_lbb], w=[b_oml])
    gn, b_gn = sb("h_gn", [128, 4, 128], F32)
    for hh in range(4):
        k.dma("sp", gn[:, hh, :], k.din["hg_out_norm_l"][0:1, :].partition_broadcast(128), w=[b_gn])
    k.ts("dve", gn[:], gn[:], float(np.sqrt(128.0)), None, ALU.mult, r=[b_gn], w=[b_gn])
    eps128, b_eps = sb("h_eps", [128, 1], F32)
    P.add("pool", lambda e: e.memset(eps128[:], float(128 * EPS)), (), [b_eps])
    one1, b_one = sb("h_one", [128, 1], F32)
    P.add("pool", lambda e: e.memset(one1[:], 1.0), (), [b_one])

    hgr = sb_ring(es, nc, "h_in", 3, [128, 2560], F32)
    e1r = sb_ring(es, nc, "h_e1", 3, [128, 512], F32)
    e2r = sb_ring(es, nc, "h_e2", 3, [128, 512], F32)
    e3r = sb_ring(es, nc, "h_e3", 3, [128, 512], F32)
    fr = sb_ring(es, nc, "h_f", 3, [128, 512], F32)
    kkr = sb_ring(es, nc, "h_kk", 3, [128, 512], F32)
    gr = sb_ring(es, nc, "h_g", 3, [128, 512], F32)
    qr = sb_ring(es, nc, "h_q", 3, [128, 512], F32)
    vr = sb_ring(es, nc, "h_v", 3, [128, 512], BF16)
    ecr = sb_ring(es, nc, "h_ec", 3, [128, 512], F32)
    encr = sb_ring(es, nc, "h_enc", 3, [128, 512], F32)
    err_ = sb_ring(es, nc, "h_er", 3, [128, 512], F32)
    eblr = sb_ring(es, nc, "h_ebl", 3, [128, 8], F32)
    qkbr = sb_ring(es, nc, "h_qkb", 3, [128, 1024], BF16)
    kdr = sb_ring(es, nc, "h_kd", 3, [128, 512], BF16)
    qkTr = sb_ring(es, nc, "h_qkT", 3, [128, 1024], BF16)
    atr = sb_ring(es, nc, "h_at", 3, [128, 512], BF16)
    sbr = sb_ring(es, nc, "h_sb", 3, [128, 512], BF16)
    Sst, b_S = sb("h_S", [128, 512], F32)
    osr = sb_ring(es, nc, "h_os", 3, [128, 512], F32)
    ofr = sb_ring(es, nc, "h_of", 3, [128, 512], F32)
    junkr = sb_ring(es, nc, "h_junk", 12, [128, 128], BF16)
    ssq = sb_ring(es, nc, "h_ssq", 3, [128, 8], F32)
    rbr = sb_ring(es, nc, "h_rb", 3, [128, 512], BF16)
    rTr = sb_ring(es, nc, "h_rT", 3, [128, 512], BF16)

    def silu_from(dst, src, rd, wr, tmp, btmp):
        k.act(tmp[:], src, AF.Exp, r=rd, w=[btmp], scale=-1.0)
        k.ts("dve", tmp[:], tmp[:], 1.0, None, ALU.add, r=[btmp], w=[btmp])
        P.add("dve", lambda e: e.reciprocal(tmp[:], tmp[:]), [btmp], [btmp])
        k.tt("dve", dst, tmp[:], src, ALU.mult, r=[btmp] + list(rd), w=wr)

    dirs = []
    for d_ in range(2):
        sfx = "_f" if d_ == 0 else "_b"
        dirs.append((cst["hg_Lc" + sfx], cst["hg_Lr" + sfx], cst["hg_selm" + sfx], cst["hg_mask" + sfx]))

    prog_state = {"s_done": 0, "started": 0}

    def step(d, ti, first):
        (Lc, b_Lc), (Lr, b_Lr), (selm, b_selm), (mask, b_mask) = dirs[d]
        my = prog_state["started"]
        prog_state["started"] += 1
        if first:
            while prog_state["s_done"] < my:
                yield
            P.add("pool", lambda e: e.memset(Sst[:], 0.0), (), [b_S])
            for t_, b_ in zip(atr.tiles, atr.bufs):
                P.add("pool", lambda e, t_=t_: e.memset(t_[:], 0.0), (), [b_])
        r0 = ti * 128
        hg_, bhg = hgr.next()
        k.load(hg_[:], HG[r0:r0 + 128, :], r=[bHG], w=[bhg])
        if d == 1:
            of_, bof = ofr.next()
            k.load(of_[:], OF[r0:r0 + 128, :], r=[bOF], w=[bof])
        z = hg_[:, 512 + d * 512: 1024 + d * 512]
        yield
        e1, be1 = e1r.next()
        e2, be2 = e2r.next()
        k.act(e1[:], z, AF.Sigmoid, r=[bhg], w=[be1])
        k.act(e2[:], hg_[:, 0:512], AF.Sigmoid, r=[bhg], w=[be2])
        if d == 1:
            e3, be3 = e3r.next()
            k.act(e3[:], hg_[:, 2048:2560], AF.Sigmoid, r=[bhg], w=[be3])
        v_, bv = vr.next()
        k.act(v_[:], hg_[:, 1536:2048], AF.Copy, r=[bhg], w=[bv], scale=-1.0)
        yield
        f_, bf_ = fr.next()
        k.tt("dve", f_[:], e1[:], oml[:, d * 512:(d + 1) * 512], ALU.mult, r=[be1, b_oml], w=[bf_])
        k.tt("dve", f_[:], f_[:], lbb[:, d * 512:(d + 1) * 512], ALU.add, r=[bf_, b_lbb], w=[bf_])
        q_, bq = qr.next()
        k.tt("dve", q_[:], e2[:], hg_[:, 0:512], ALU.mult, r=[be2, bhg], w=[bq])
        yield
        g_, bg = gr.next()
        k.act(g_[:], f_[:], AF.Ln, r=[bf_], w=[bg])
        yield
        cps, bcps, icps = yield from psf.get()
        k.mm(cps[:], Lc[:], g_[:], r=[b_Lc, bg], w=[bcps])
        rps, brps, irps = yield from psf.get()
        k.mm(rps[:], Lr[:], g_[:], r=[b_Lr, bg], w=[brps])
        yield
        ec, bec = ecr.next()
        enc, benc = encr.next()
        er, ber = err_.next()
        k.act(ec[:], cps[:], AF.Exp, r=[bcps], w=[bec])
        k.act(enc[:], cps[:], AF.Exp, r=[bcps], w=[benc], scale=-1.0)
        k.act(er[:], rps[:], AF.Exp, r=[brps], w=[ber])
        psf.put(icps)
        psf.put(irps)
        yield
        blm, bblm, iblm = yield from psf.get()
        for hh in range(4):
            k.mm(blm[:, 2 * hh:2 * hh + 2], g_[:, hh * 128:(hh + 1) * 128], selm[:], r=[bg, b_selm], w=[bblm])
        qkb, bqkb = qkbr.next()
        kd, bkd = kdr.next()
        k.tt("dve", qkb[:, 0:512], q_[:], ec[:], ALU.mult, r=[bq, bec], w=[bqkb])
        P.add("dve", lambda e, qkb=qkb, f_=f_, enc=enc: e.scalar_tensor_tensor(
            qkb[:, 512:1024], f_[:], 1.0, enc[:], ALU.subtract, ALU.mult), [bf_, benc], [bqkb])
        P.add("dve", lambda e, kd=kd, f_=f_, er=er: e.scalar_tensor_tensor(
            kd[:], f_[:], 1.0, er[:], ALU.subtract, ALU.mult), [bf_, ber], [bkd])
        yield
        ebl, bebl = eblr.next()
        k.act(ebl[:], blm[:, 0:8], AF.Exp, r=[bblm], w=[bebl])
        psf.put(iblm)
        yield
        pb, bpb, ipb = yield from psb.get()
        for c8 in range(8):
            k.tr(pb[:, c8 * 128:(c8 + 1) * 128], qkb[:, c8 * 128:(c8 + 1) * 128], identb[:],
                 r=[bqkb, b_identb], w=[bpb])
        while prog_state["s_done"] < my:
            yield
        sb_, bsb = sbr.next()
        for hh in range(4):
            k.ts("dve", sb_[:, hh * 128:(hh + 1) * 128], Sst[:, hh * 128:(hh + 1) * 128],
                 ebl[:, 2 * hh + 1:2 * hh + 2], None, ALU.mult, r=[b_S, bebl], w=[bsb])
        yield
        qkT, bqkT = qkTr.next()
        k.cp("dve", qkT[:], pb[:], r=[bpb], w=[bqkT])
        psb.put(ipb)
        yield
        atp, batp, iatp = yield from psf.get()
        for hh in range(4):
            k.mm(atp[:, hh * 128:(hh + 1) * 128], qkT[:, 512 + hh * 128: 512 + (hh + 1) * 128],
                 qkT[:, hh * 128:(hh + 1) * 128], r=[bqkT], w=[batp])
        snp, bsnp, isnp = yield from psf.get()
        for hh in range(4):
            sl = slice(hh * 128, (hh + 1) * 128)
            k.mm(snp[:, sl], kd[:, sl], v_[:, sl], r=[bkd, bv], w=[bsnp])
        yield
        at, bat = atr.next()
        P.add("dve", lambda e, at=at, atp=atp, mask=mask: e.copy_predicated(at[:], mask[:], atp[:]),
              [batp, b_mask], [bat])
        psf.put(iatp)
        yield
        ops, bops, iops = yield from psf.get()
        for hh in range(4):
            sl = slice(hh * 128, (hh + 1) * 128)
            k.mm(ops[:, sl], at[:, sl], v_[:, sl], start=True, stop=False, r=[bat, bv], w=[bops])
            k.mm(ops[:, sl], qkT[:, sl], sb_[:, sl], start=False, stop=True, r=[bqkT, bsb], w=[bops])
        for hh in range(4):
            sl = slice(hh * 128, (hh + 1) * 128)
            P.add("dve", lambda e, sl=sl, ebl=ebl, snp=snp, hh=hh: e.scalar_tensor_tensor(
                Sst[:, sl], Sst[:, sl], ebl[:, 2 * hh:2 * hh + 1], snp[:, sl], ALU.mult, ALU.add),
                [b_S, bebl, bsnp], [b_S])
        prog_state["s_done"] = my + 1
        psf.put(isnp)
        yield
        os_, bos = osr.next()
        if d == 0:
            k.cp("dve", os_[:], ops[:], r=[bops], w=[bos])
            psf.put(iops)
            k.store(OF[r0:r0 + 128, :], os_[:], r=[bos], w=[bOF])
            return
        k.tt("dve", os_[:], ops[:], of_[:], ALU.add, r=[bops, bof], w=[bos])
        psf.put(iops)
        k.tt("dve", e3[:], e3[:], hg_[:, 2048:2560], ALU.mult, r=[be3, bhg], w=[be3])
        yield
        sq, bsq = ssq.next()
        for hh in range(4):
            junk, b_junk = junkr.next()
            k.act(junk[:], os_[:, hh * 128:(hh + 1) * 128], AF.Square, r=[bos], w=[b_junk, bsq],
                  accum_out=sq[:, hh:hh + 1])
        k.act(sq[:, 4:8], sq[:, 0:4], AF.Ln, r=[bsq, b_eps], w=[bsq], bias=eps128[:, 0:1])
        k.act(sq[:, 4:8], sq[:, 4:8], AF.Exp, r=[bsq], w=[bsq], scale=-0.5)
        yield
        for hh in range(4):
            k.ts("dve", os_[:, hh * 128:(hh + 1) * 128], os_[:, hh * 128:(hh + 1) * 128], sq[:, 4 + hh:5 + hh],
                 None, ALU.mult, r=[bos, bsq], w=[bos])
        k.tt("dve", os_[:], os_[:], gn[:].rearrange("p h c -> p (h c)"), ALU.mult, r=[bos, b_gn], w=[bos])
        rb, brb = rbr.next()
        k.tt("dve", rb[:], os_[:], e3[:], ALU.mult, r=[bos, be3], w=[brb])
        yield
        pb, bpb, ipb = yield from psb.get()
        for hh in range(4):
            k.tr(pb[:, hh * 128:(hh + 1) * 128], rb[:, hh * 128:(hh + 1) * 128], identb[:],
                 r=[brb, b_identb], w=[bpb])
        yield
        rT, brT = rTr.next()
        k.cp("dve", rT[:], pb[:, 0:512], r=[bpb], w=[brT])
        psb.put(ipb)
        k.store(MIXr[:, :, r0:r0 + 128], rT[:].rearrange("p (c t) -> p c t", c=4), r=[brT], w=[bMIXT])
    return step


def hgrn_order(NT):
    return [(0, ti, ti == 0) for ti in range(NT)] + [(1, ti, ti == NT - 1) for ti in range(NT - 1, -1, -1)]


def phase_hgrn(k):
    nc, P = k.nc, k.P
    with ExitStack() as es:
        psf = ps_free(es, nc, "h_psf", 6)
        psb = ps_free(es, nc, "h_psb", 2, (128, 1024), BF16)
        step = hgrn_build(k, es, psf, psb)
        order = hgrn_order(k.NT)
        for d in range(2):
            run_pipelined((step(*o) for o in order if o[0] == d), 3)
        k.flush()
        P.drain_dmas()
        P.emit()


def phase_route(k):
    nc, P, S, NT = k.nc, k.P, k.S, k.NT
    CAP = moe_cap(S)
    DUMP = NE * CAP
    PSL = DUMP + 128
    k.CAP, k.PSL = CAP, PSL
    MIXT, bMIXT = k.scr["MIXT"], k.scr_buf["MIXT"]
    H1 = k.scratch("H1", [S, D], F32)
    H2N = k.scratch("H2N", [S, D], BF16)
    XS = k.scr["XS"]
    DEST = k.scratch("DEST", [128, NT * 2], I32)
    GATE = k.scratch("GATE", [128, NT * 2], F32)
    bH1, bH2N, bXS, bDEST, bGATE = (k.scr_buf[n] for n in ("H1", "H2N", "XS", "DEST", "GATE"))
    MIXv = MIXT.rearrange("(c p) t -> p c t", p=128)
    with ExitStack() as es:
        def sb(name, shape, dt):
            return es.enter_context(nc.sbuf_tensor("sb_" + name, list(shape), dt)), Buf(name)
        identf, b_identf = sb("r_identf", [128, 128], F32)
        onesf, b_onesf = sb("r_onesf", [128, 128], F32)
        ustr, b_ustr = sb("r_ustr", [128, 128], F32)
        id32, b_id32 = sb("r_id32", [32, 32], F32)
        ecap, b_ecap = sb("r_ecap", [32, 2], F32)
        limr, b_limr = sb("r_limr", [128, 32], F32)
        piota, b_piota = sb("r_piota", [128, 1], F32)
        toki, b_toki = sb("r_toki", [128, NT], I32)
        for t_, b_, nm in ((identf, b_identf, "ident_f"), (onesf, b_onesf, "ones_f"), (ustr, b_ustr, "ustrict"),
                           (id32, b_id32, "ident32"), (ecap, b_ecap, "blk_iota"), (limr, b_limr, "lim_row"),
                           (piota, b_piota, "p_iota"), (toki, b_toki, "tok_iota")):
            k.dma("sp", t_[:], k.din[nm], w=[b_])
        gff, b_gff = sb("r_gff", [128, D], F32)
        k.dma("sp", gff[:], k.din["norm_ffn_l"][0:1, :].partition_broadcast(128), w=[b_gff])
        k.ts("dve", gff[:], gff[:], 32.0, None, ALU.mult, r=[b_gff], w=[b_gff])
        brt, b_brt = sb("r_brt", [128, 36], F32)
        k.dma("sp", brt[:], k.din["b_rt"][0:1, :].partition_broadcast(128), w=[b_brt])
        wrt, b_wrt = sb("r_wrt", [128, 8, 36], F32)
        k.dma("sp", wrt[:], k.din["w_rt"].rearrange("(c p) n -> p c n", p=128), w=[b_wrt])
        eps1k, b_eps = sb("r_eps", [128, 1], F32)
        P.add("pool", lambda e: e.memset(eps1k[:], float(D * EPS)), (), [b_eps])
        wout, b_wout = sb("r_wout", [128, 8, D], BF16)
        wst = sb_ring(es, nc, "r_wst", 2, [128, D], F32)
        w_out_v = k.din["w_out"].rearrange("(c p) n -> p c n", p=128)
        for c in range(8):
            st, bst = wst.next()
            k.dma("sp", st[:], w_out_v[:, c, :], w=[bst])
            k.cp("dve" if c % 2 == 0 else "pool", wout[:, c, :], st[:], r=[bst], w=[b_wout])
        dcol, b_dcol = sb("r_dcol", [128, 2], F32)
        k.ts("dve", dcol[:, 0:1], piota[:, 0:1], float(2 * S), None, ALU.add, r=[b_piota], w=[b_dcol])
        k.ts("dve", dcol[:, 1:2], piota[:, 0:1], float(DUMP), None, ALU.add, r=[b_piota], w=[b_dcol])
        k.fence([bXS])
        Aall, b_A = sb("r_A", [128, NT, 32], F32)
        M12, b_M = sb("r_M12", [128, NT, 2, 32], F32)
        gates, b_gates = sb("r_gates", [128, NT, 2], F32)
        dest, b_dest = sb("r_dest", [128, NT, 2], F32)
        desti, b_desti = sb("r_desti", [128, NT, 2], I32)

        mixr = sb_free(es, nc, "r_mix", 3, [128, 8, 128], BF16)
        xr = sb_free(es, nc, "r_x", 3, [128, D], F32)
        h1r = sb_free(es, nc, "r_h1", 3, [128, D], F32)
        junkr = sb_free(es, nc, "r_junk", 2, [128, D], BF16)
        h2r = sb_free(es, nc, "r_h2", 3, [128, D], F32)
        h2bf = sb_free(es, nc, "r_h2bf", 3, [128, D], BF16)
        h2br = sb_ring(es, nc, "r_h2b", 3, [128, D], BF16)
        h2Tr = sb_free(es, nc, "r_h2T", 2, [128, 8, 128], F32)
        smr = sb_free(es, nc, "r_sm", 4, [128, 16], F32)
        lgr = sb_free(es, nc, "r_lg", 4, [128, 36], F32)
        emr = sb_free(es, nc, "r_em", 4, [128, 32], F32)
        t8r = sb_free(es, nc, "r_t8", 4, [128, 8], F32)
        g4r = sb_free(es, nc, "r_g4", 4, [128, 12], F32)
        psf = ps_free(es, nc, "r_psf", 8)

        def tile_gen(ti):
            r0 = ti * 128
            mx, bmx, imx = yield from mixr.get()
            k.load(mx[:], MIXv[:, :, r0:r0 + 128], r=[bMIXT], w=[bmx])
            xt, bx, ix = yield from xr.get()
            k.load(xt[:], k.x[r0:r0 + 128, :], w=[bx])
            yield
            h1, bh1, ih1 = yield from h1r.get()
            pss = []
            for half in range(2):
                ps, bps, ips = yield from psf.get()
                for c in range(8):
                    k.mm(ps[:], mx[:, c, :], wout[:, c, half * 512:(half + 1) * 512], start=(c == 0), stop=(c == 7),
                         r=[bmx, b_wout], w=[bps])
                pss.append((ps, bps, ips))
            mixr.put(imx)
            yield
            for half, (ps, bps, ips) in enumerate(pss):
                k.tt("dve", h1[:, half * 512:(half + 1) * 512], ps[:], xt[:, half * 512:(half + 1) * 512], ALU.add,
                     r=[bps, bx], w=[bh1])
                psf.put(ips)
            xr.put(ix)
            k.store(H1[r0:r0 + 128, :], h1[:], r=[bh1], w=[bH1])
            yield
            sm, bsm, ism = yield from smr.get()
            junk, bjunk, ijunk = yield from junkr.get()
            k.act(junk[:], h1[:], AF.Square, r=[bh1], w=[bjunk, bsm], accum_out=sm[:, 0:1])
            junkr.put(ijunk)
            k.act(sm[:, 1:2], sm[:, 0:1], AF.Ln, r=[bsm, b_eps], w=[bsm], bias=eps1k[:, 0:1])
            k.act(sm[:, 2:3], sm[:, 1:2], AF.Exp, r=[bsm], w=[bsm], scale=-0.5)
            yield
            h2, bh2, ih2 = yield from h2r.get()
            k.ts("dve", h2[:], h1[:], sm[:, 2:3], None, ALU.mult, r=[bh1, bsm], w=[bh2])
            yield
            k.tt("pool", h2[:], h2[:], gff[:], ALU.mult, r=[bh2, b_gff], w=[bh2])
            yield
            h2b, bh2b, ih2b = yield from h2bf.get()
            k.cp("act", h2b[:], h2[:], r=[bh2], w=[bh2b])
            k.store(H2N[r0:r0 + 128, :], h2b[:], r=[bh2b], w=[bH2N])
            h2T, bh2T, ih2T = yield from h2Tr.get()
            pss = []
            for half in range(2):
                ps, bps, ips = yield from psf.get()
                for c4 in range(4):
                    c = half * 4 + c4
                    k.tr(ps[:, c4 * 128:(c4 + 1) * 128], h2[:, c * 128:(c + 1) * 128], identf[:],
                         r=[bh2, b_identf], w=[bps])
                pss.append((ps, bps, ips))
            h2r.put(ih2)
            yield
            for half, (ps, bps, ips) in enumerate(pss):
                k.cp("dve" if half == 0 else "act", h2T[:, half * 4:(half + 1) * 4, :].rearrange("p c t -> p (c t)"), ps[:],
                     r=[bps], w=[bh2T])
                psf.put(ips)
            yield
            lps, blps, ilps = yield from psf.get()
            for c in range(8):
                k.mm(lps[:, 0:36], h2T[:, c, :], wrt[:, c, :], start=(c == 0), stop=(c == 7), r=[bh2T, b_wrt], w=[blps])
            h2Tr.put(ih2T)
            yield
            lg, blg, ilg = yield from lgr.get()
            k.tt("dve", lg[:], lps[:, 0:36], brt[:], ALU.add, r=[blps, b_brt], w=[blg])
            psf.put(ilps)
            g4, bg4, ig4 = yield from g4r.get()
            P.add("dve", lambda e, g4=g4, lg=lg: e.reduce_max(g4[:, 0:1], lg[:, 0:4], AX.X), [blg], [bg4])
            k.ts("dve", g4[:, 1:2], g4[:, 0:1], -1.0, None, ALU.mult, r=[bg4], w=[bg4])
            yield
            k.act(sm[:, 4:8], lg[:, 0:4], AF.Exp, r=[blg, bg4], w=[bsm, bg4], bias=g4[:, 1:2], accum_out=g4[:, 2:3])
            k.ts("dve", g4[:, 4:8], lg[:, 0:4], g4[:, 0:1], None, ALU.is_equal, r=[blg, bg4], w=[bg4])
            k.ts("dve", g4[:, 8:12], g4[:, 4:8], 1.0e30, -1.0e30, ALU.mult, ALU.add, r=[bg4], w=[bg4])
            em, bem, iem = yield from emr.get()
            for g in range(NG):
                k.ts("dve", em[:, g * 8:(g + 1) * 8], lg[:, 4 + g * 8: 12 + g * 8], g4[:, 4 + g:5 + g], g4[:, 8 + g:9 + g],
                     ALU.mult, ALU.add, r=[blg, bg4], w=[bem])
            lgr.put(ilg)
            yield
            P.add("dve", lambda e, g4=g4: e.reciprocal(g4[:, 3:4], g4[:, 2:3]), [bg4], [bg4])
            t8, bt8, it8 = yield from t8r.get()
            P.add("dve", lambda e, t8=t8, em=em: e.max(t8[:], em[:]), [bem], [bt8])
            yield
            k.ts("dve", M12[:, ti, 0, :], em[:], t8[:, 0:1], None, ALU.is_equal, r=[bem, bt8], w=[b_M])
            k.ts("dve", M12[:, ti, 1, :], em[:], t8[:, 1:2], None, ALU.is_equal, r=[bem, bt8], w=[b_M])
            emr.put(iem)
            k.tt("dve", sm[:, 8:9], t8[:, 1:2], t8[:, 0:1], ALU.subtract, r=[bt8], w=[bsm])
            t8r.put(it8)
            yield
            k.tt("dve", Aall[:, ti, :], M12[:, ti, 0, :], M12[:, ti, 1, :], ALU.add, r=[b_M], w=[b_A])
            k.act(sm[:, 9:10], sm[:, 8:9], AF.Exp, r=[bsm], w=[bsm])
            yield
            k.ts("dve", sm[:, 9:10], sm[:, 9:10], 1.0, None, ALU.add, r=[bsm], w=[bsm])
            yield
            P.add("dve", lambda e, sm=sm: e.reciprocal(sm[:, 10:11], sm[:, 9:10]), [bsm], [bsm])
            yield
            k.tt("dve", gates[:, ti, 0:1], sm[:, 10:11], g4[:, 3:4], ALU.mult, r=[bsm, bg4], w=[b_gates])
            yield
            k.tt("dve", gates[:, ti, 1:2], g4[:, 3:4], gates[:, ti, 0:1], ALU.subtract, r=[bg4, b_gates], w=[b_gates])
            smr.put(ism)
            g4r.put(ig4)
            h1r.put(ih1)
            h2bf.put(ih2b)

        run_pipelined([tile_gen(ti) for ti in range(NT)], 3)

        cps, bcps, icps = psf.take()
        for ti in range(NT):
            k.mm(cps[0:32, ti:ti + 1], Aall[:, ti, :], onesf[:, 0:1], r=[b_A, b_onesf], w=[bcps])
        cnt, b_cnt = sb("r_cnt", [32, NT], F32)
        k.cp("dve", cnt[:], cps[0:32, 0:NT], r=[bcps], w=[b_cnt])
        inc, b_inc = sb("r_inc", [32, NT], F32)
        onesr, b_onesr = sb("r_onesr", [32, NT], F32)
        P.add("pool", lambda e: e.memset(onesr[:], 1.0), (), [b_onesr])
        P.add("dve", lambda e: e.tensor_tensor_scan(inc[:], onesr[:], cnt[:], 0.0, ALU.mult, ALU.add),
              [b_onesr, b_cnt], [b_inc])
        off, b_off = sb("r_off", [32, NT], F32)
        k.tt("dve", off[:], inc[:], cnt[:], ALU.subtract, r=[b_inc, b_cnt], w=[b_off])
        k.ts("dve", off[:], off[:], ecap[:, 0:1], None, ALU.add, r=[b_off, b_ecap], w=[b_off])
        dgr = sb_free(es, nc, "r_dg", 3, [32, 32], F32)
        tmr = sb_free(es, nc, "r_tm", 3, [128, 4, 32], F32)
        okr = sb_free(es, nc, "r_ok", 3, [128, 3, 32], F32)
        gkr = sb_free(es, nc, "r_gk", 3, [128, 2], F32)

        def slot_gen(ti):
            h2b, bh2b, ih2b = yield from h2bf.get()
            k.load(h2b[:], H2N[ti * 128:(ti + 1) * 128, :], r=[bH2N], w=[bh2b])
            dg, bdg, idg = yield from dgr.get()
            k.ts("dve", dg[:], id32[:], off[:, ti:ti + 1], None, ALU.mult, r=[b_id32, b_off], w=[bdg])
            yield
            sps, bsps, isps = yield from psf.get()
            k.mm(sps[:, 0:32], ustr[:], Aall[:, ti, :], start=True, stop=False, r=[b_ustr, b_A], w=[bsps])
            k.mm(sps[:, 0:32], onesf[0:32, :], dg[:], start=False, stop=True, r=[b_onesf, bdg], w=[bsps])
            dgr.put(idg)
            yield
            ok, bok, iok = yield from okr.get()
            k.tt("dve", ok[:, 0, :], sps[:, 0:32], limr[:], ALU.is_lt, r=[bsps, b_limr], w=[bok])
            yield
            k.tt("dve", ok[:, 1, :], sps[:, 0:32], ok[:, 0, :], ALU.mult, r=[bsps, bok], w=[bok])
            psf.put(isps)
            k.ts("dve", ok[:, 2, :], ok[:, 0, :], -1.0, 1.0, ALU.mult, ALU.add, r=[bok], w=[bok])
            yield
            k.ts("dve", ok[:, 2, :], ok[:, 2, :], dcol[:, 1:2], None, ALU.mult, r=[bok, b_dcol], w=[bok])
            yield
            k.tt("dve", ok[:, 1, :], ok[:, 1, :], ok[:, 2, :], ALU.add, r=[bok], w=[bok])
            yield
            tm, btm, itm = yield from tmr.get()
            for j in range(2):
                k.tt("dve", tm[:, j, :], M12[:, ti, j, :], ok[:, 1, :], ALU.mult, r=[b_M, bok], w=[btm])
                k.tt("dve", tm[:, 2 + j, :], M12[:, ti, j, :], ok[:, 0, :], ALU.mult, r=[b_M, bok], w=[btm])
            okr.put(iok)
            yield
            P.add("dve", lambda e, tm=tm, ti=ti: e.reduce_sum(dest[:, ti, :], tm[:, 0:2, :], AX.X), [btm], [b_dest])
            gk, bgk, igk = yield from gkr.get()
            P.add("dve", lambda e, tm=tm, gk=gk: e.reduce_sum(gk[:], tm[:, 2:4, :], AX.X), [btm], [bgk])
            tmr.put(itm)
            yield
            k.tt("dve", gates[:, ti, :], gates[:, ti, :], gk[:], ALU.mult, r=[b_gates, bgk], w=[b_gates])
            gkr.put(igk)
            k.cp("dve", desti[:, ti, :], dest[:, ti, :], r=[b_dest], w=[b_desti])
            yield
            for j in range(2):
                P.add("pool", lambda e, ti=ti, j=j, h2b=h2b: e.indirect_dma_start(
                    out=XS, out_offset=bass.IndirectOffsetOnAxis(ap=desti[:, ti, j:j + 1], axis=0),
                    in_=h2b[:], in_offset=None), [b_desti, bh2b], [bXS], dma=True)
                yield
            h2bf.put(ih2b)

        run_pipelined([slot_gen(ti) for ti in range(NT)], 3)
        k.dma("sp", GATE, gates[:].rearrange("p t j -> p (t j)"), r=[b_gates], w=[bGATE])
        k.dma("sp", DEST, desti[:].rearrange("p t j -> p (t j)"), r=[b_desti], w=[bDEST])
        k.flush()
        P.drain_dmas()
        P.emit()


def phase_moe(k):
    nc, P, S, NT = k.nc, k.P, k.S, k.NT
    B, CAP, PSL = MOE_B, k.CAP, k.PSL
    NS = B // 128
    H1, XS, DEST, GATE, YB = (k.scr[n] for n in ("H1", "XS", "DEST", "GATE", "YB"))
    bH1, bXS, bDEST, bGATE, bYB = (k.scr_buf[n] for n in ("H1", "XS", "DEST", "GATE", "YB"))
    WG = k.din["w_gate"].rearrange("e (p c) n -> e p (c n)", p=128)
    WU = k.din["w_up"].rearrange("e (p c) n -> e p (c n)", p=128)
    WD = k.din["w_down"].rearrange("e (p c) n -> e p (c n)", p=128)
    with ExitStack() as es:
        def sb(name, shape, dt):
            return es.enter_context(nc.sbuf_tensor("sb_" + name, list(shape), dt)), Buf(name)
        identb, b_identb = sb("m_ident", [128, 128], BF16)
        k.dma("sp", identb[:], k.din["ident_bf"], w=[b_identb])
        tkr = sb_free(es, nc, "m_tk", 6, [128, 16], I32)
        xgr = sb_free(es, nc, "m_xg", 6, [128, D], BF16)
        wfr = sb_free(es, nc, "m_wf", 3, [128, 2048], F32)
        wgr = sb_free(es, nc, "m_wg", 2, [128, 8, DE], BF16)
        wur = sb_free(es, nc, "m_wu", 2, [128, 8, DE], BF16)
        wdr = sb_free(es, nc, "m_wd", 2, [128, 2, D], BF16)
        xTr = sb_free(es, nc, "m_xT", 3, [128, 8, B], BF16)
        sgr = sb_free(es, nc, "m_sg", 3, [128, B], F32)
        hTr = sb_free(es, nc, "m_hT", 3, [128, 2, B], BF16)
        ysr = sb_free(es, nc, "m_ys", 6, [128, D], BF16)
        psf = ps_free(es, nc, "m_psf", 6)
        psb = ps_free(es, nc, "m_psb", 2, (128, 1024), BF16)
        ceng = ["dve", "pool", "act"]
        wtiles = {}

        def wprep_gen(ex):
            wts = []
            for wi_, (src, pool_) in enumerate(((WG, wgr), (WU, wur), (WD, wdr))):
                wf, bwf, iwf = yield from wfr.get()
                k.load(wf[:], src[ex], w=[bwf])
                wb, bwb, iwb = yield from pool_.get()
                wts.append((wf, bwf, iwf, wb, bwb, iwb, pool_))
            wtiles[ex] = [(wb, bwb, iwb, pool_) for (_, _, _, wb, bwb, iwb, pool_) in wts]
            wtiles[(ex, "left")] = CAP // B
            yield
            for wi_, (wf, bwf, iwf, wb, bwb, iwb, pool_) in enumerate(wts):
                k.cp(ceng[wi_], wb[:].rearrange("p c n -> p (c n)"), wf[:], r=[bwf], w=[bwb])
                wfr.put(iwf)
                yield

        def blk_gen(ex, blk):
            (wg, bwg, _, _), (wu, bwu, _, _), (wd, bwd, _, _) = wtiles[ex]
            wgv = wg[:].rearrange("p c (q j) -> p c j q", j=2)
            wuv = wu[:].rearrange("p c (q j) -> p c j q", j=2)
            s0 = ex * CAP + blk * B
            xT, bxT, ixT = yield from xTr.get()
            tks, xgs = [], []
            for s_ in range(NS):
                xg, bxg, ixg = yield from xgr.get()
                k.load(xg[:], XS[s0 + s_ * 128: s0 + (s_ + 1) * 128, :], r=[bXS], w=[bxg])
                xgs.append((xg, bxg, ixg))
            yield
            for s_ in range(NS):
                xg, bxg, ixg = xgs[s_]
                pb, bpb, ipb = yield from psb.get()
                xgv = xg[:].rearrange("p (q c) -> p c q", c=8)
                for c in range(8):
                    k.tr(pb[:, c * 128:(c + 1) * 128], xgv[:, c, :], identb[:], r=[bxg, b_identb], w=[bpb])
                xgr.put(ixg)
                yield
                k.cp("act" if s_ % 2 == 0 else "dve", xT[:, :, s_ * 128:(s_ + 1) * 128],
                     pb[:].rearrange("p (c t) -> p c t", c=8), r=[bpb], w=[bxT])
                psb.put(ipb)
            yield
            hT, bhT, ihT = yield from hTr.get()
            for c in range(2):
                gps, bgps, igps = yield from psf.get()
                ups, bups, iups = yield from psf.get()
                for kc in range(8):
                    k.mm(gps[:, 0:B], wgv[:, kc, c, :], xT[:, kc, :], start=(kc == 0), stop=(kc == 7),
                         r=[bwg, bxT], w=[bgps])
                for kc in range(8):
                    k.mm(ups[:, 0:B], wuv[:, kc, c, :], xT[:, kc, :], start=(kc == 0), stop=(kc == 7),
                         r=[bwu, bxT], w=[bups])
                yield
                sg, bsg, isg = yield from sgr.get()
                k.act(sg[:], gps[:, 0:B], AF.Silu, r=[bgps], w=[bsg])
                psf.put(igps)
                yield
                k.tt("dve", hT[:, c, :], sg[:], ups[:, 0:B], ALU.mult, r=[bsg, bups], w=[bhT])
                psf.put(iups)
                sgr.put(isg)
            xTr.put(ixT)
            yield
            for s_ in range(NS):
                ys, bys, iys = yield from ysr.get()
                for half in range(2):
                    yps, byps, iyps = yield from psf.get()
                    for c in range(2):
                        k.mm(yps[:], hT[:, c, s_ * 128:(s_ + 1) * 128], wd[:, c, half * 512:(half + 1) * 512],
                             start=(c == 0), stop=(c == 1), r=[bhT, bwd], w=[byps])
                    yield
                    k.cp("act" if half == 0 else "dve", ys[:, half * 512:(half + 1) * 512], yps[:], r=[byps], w=[bys])
                    psf.put(iyps)
                k.store(YB[s0 + s_ * 128: s0 + (s_ + 1) * 128, :], ys[:], r=[bys], w=[bYB])
                ysr.put(iys)
            hTr.put(ihT)
            wtiles[(ex, "left")] -= 1
            if wtiles[(ex, "left")] == 0:
                for (_, _, iwb, pool_) in wtiles[ex]:
                    pool_.put(iwb)

        for _ in wprep_gen(0):
            pass
        gens = []
        for ex in range(NE):
            if ex + 1 < NE:
                gens.append(wprep_gen(ex + 1))
            gens += [blk_gen(ex, blk) for blk in range(CAP // B)]
        run_pipelined(gens, 3)
        k.flush()
        P.drain_dmas()
        P.emit()
    with ExitStack() as es:
        def sb(name, shape, dt):
            return es.enter_context(nc.sbuf_tensor("sb_" + name, list(shape), dt)), Buf(name)
        gates, b_gates = sb("c_gate", [128, NT * 2], F32)
        desti, b_desti = sb("c_dest", [128, NT * 2], I32)
        k.dma("sp", gates[:], GATE, r=[bGATE], w=[b_gates])
        k.dma("sp", desti[:], DEST, r=[bDEST], w=[b_desti])
        h1r = sb_ring(es, nc, "c_h1", 4, [128, D], F32)
        ytr = sb_ring(es, nc, "c_yt", 8, [128, D], BF16)
        by = Buf("y", multi=True)
        for ti in range(NT):
            r0 = ti * 128
            h1, bh1 = h1r.next()
            k.load(h1[:], H1[r0:r0 + 128, :], r=[bH1], w=[bh1])
            yts = []
            for j in range(2):
                yt, byt = ytr.next()
                P.add("pool", lambda e, yt=yt, ti=ti, j=j: e.indirect_dma_start(
                    out=yt[:], out_offset=None, in_=YB,
                    in_offset=bass.IndirectOffsetOnAxis(ap=desti[:, 2 * ti + j:2 * ti + j + 1], axis=0)),
                    [b_desti, bYB], [byt], dma=True)
                yts.append((yt, byt))
            for j, (yt, byt) in enumerate(yts):
                P.add("dve", lambda e, yt=yt, h1=h1, ti=ti, j=j: e.scalar_tensor_tensor(
                    h1[:], yt[:], gates[:, 2 * ti + j:2 * ti + j + 1], h1[:], ALU.mult, ALU.add),
                    [byt, b_gates, bh1], [bh1])
            k.store(k.y[r0:r0 + 128, :], h1[:], r=[bh1], w=[by])
        k.flush()
        P.drain_dmas()
        P.emit()


def build_program(S):
    k = K(S)
    phase1(k)
    phase_attn(k)
    phase_hgrn(k)
    phase_route(k)
    phase_moe(k)
    return k


def kernel(**inputs):
    x = np.asarray(inputs["x"], dtype=np.float32)
    nb, S, _ = x.shape
    k = build_program(S)
    par = layout_params(inputs)
    con = make_consts(S)
    in_maps = []
    for c in range(nb):
        m = {"x": np.ascontiguousarray(x[c])}
        m.update(par)
        m.update(con)
        in_maps.append(m)
    res = run_bass_kernel_spmd(k.nc, in_maps, core_ids=list(range(nb)))
    return np.stack([np.asarray(r["y"], dtype=np.float32) for r in res.results], axis=0)
```

```python
import numpy as np
from contextlib import ExitStack
import concourse.bass as bass
import concourse.mybir as mybir
from concourse.bass_utils import run_bass_kernel_spmd

F32 = mybir.dt.float32
BF16 = mybir.dt.bfloat16
I32 = mybir.dt.int32
U32 = mybir.dt.uint32
U8 = mybir.dt.uint8
AF = mybir.ActivationFunctionType
ALU = mybir.AluOpType
AX = mybir.AxisListType

ENGS = ("pe", "act", "dve", "pool", "sp")
EPS = 1e-6
D = 1024
NIN = 2976
H = 8
QK = 96
NOPE = 64
ROPE = 32
DV = 64
HGH = 4
NE = 32
NG = 4
DE = 256
MOE_B = 256
MOE_LOG2B = 8
import os
MOE_DBG = int(os.environ.get('MOE_DBG', '0'))
class Buf:
    __slots__ = ("name", "last_w", "readers", "multi", "writers")

    def __init__(self, name="", multi=False):
        self.name = name
        self.last_w = None
        self.readers = {}
        self.multi = multi
        self.writers = []


class Op:
    __slots__ = ("eng", "fn", "waits", "signal", "ticket", "is_dma", "sem", "target", "idx", "emitted", "drained")


class Prog:
    def __init__(self, nc, n_dma_sems=40, n_sw_sems=8):
        self.nc = nc
        self.eng_obj = {"pe": nc.tensor, "act": nc.scalar, "dve": nc.vector,
                        "pool": nc.gpsimd, "sp": nc.sync}
        self.sem = {}
        self.count = {e: 0 for e in ENGS}
        self._stack = []
        for e in ENGS:
            g = nc.semaphore("s_" + e)
            self.sem[e] = g.__enter__()
            self._stack.append(g)
        self.dma_sems = []
        self.dma_uses = []
        self.dma_last = []
        for i in range(n_dma_sems):
            g = nc.semaphore("d_%d" % i)
            self.dma_sems.append(g.__enter__())
            self._stack.append(g)
            self.dma_uses.append(0)
            self.dma_last.append(None)
        self.dma_rr = 0
        self.sw_sems = []
        self.sw_uses = []
        self.sw_last = []
        for i in range(n_sw_sems):
            g = nc.semaphore("w_%d" % i)
            self.sw_sems.append(g.__enter__())
            self._stack.append(g)
            self.sw_uses.append(0)
            self.sw_last.append(None)
        self.sw_rr = 0
        self.ops = {e: [] for e in ENGS}
        self.waited = {e: {} for e in ENGS}
        self.nops = 0
        self.pending_dma = []
        self._deferred = []
        self._def_src = set()

    def defer_dma(self, eng, out, in_, reads=(), writes=(), **kw):
        self._deferred.append((eng, out, in_, tuple(reads), tuple(writes), kw))
        for b in reads:
            self._def_src.add(id(b))

    def flush(self):
        d, self._deferred = self._deferred, []
        self._def_src = set()
        for eng, out, in_, r, w, kw in d:
            self.dma(eng, out, in_, r, w, **kw)

    def add(self, eng, fn, reads=(), writes=(), dma=False):
        if self._deferred and any(id(b) in self._def_src for b in writes):
            self.flush()
        op = Op()
        op.eng = eng
        op.fn = fn
        op.waits = []
        op.signal = False
        op.ticket = None
        op.is_dma = dma
        op.sem = None
        op.target = None
        op.idx = self.nops
        op.emitted = False
        op.drained = False
        self.nops += 1
        deps = {}
        raw = set()
        for b in reads:
            if b.multi:
                b.writers = [w_ for w_ in b.writers if not (w_.emitted and (w_.drained or not w_.is_dma))]
                for w_ in b.writers:
                    deps[id(w_)] = w_
            elif b.last_w is not None:
                deps[id(b.last_w)] = b.last_w
                raw.add(id(b.last_w))
        for b in writes:
            if (not b.multi) and b.last_w is not None:
                deps[id(b.last_w)] = b.last_w
            for r in b.readers.values():
                deps[id(r)] = r
        for k, d in deps.items():
            if d is op:
                continue
            if d.is_dma:
                if not (d.emitted and d.drained):
                    op.waits.append(d)
            elif d.emitted:
                continue
            elif d.eng == eng and not dma:
                if eng == "pe":
                    continue
                d.signal = True
                op.waits.append(d)
            else:
                d.signal = True
                op.waits.append(d)
        if dma and eng == "pool":
            i = self.sw_rr
            self.sw_rr = (self.sw_rr + 1) % len(self.sw_sems)
            prev = self.sw_last[i]
            if prev is not None:
                op.waits.append(prev)
            self.sw_uses[i] += 1
            op.sem = self.sw_sems[i]
            op.target = 16 * self.sw_uses[i]
            self.sw_last[i] = op
            self.pending_dma.append(op)
        elif dma:
            i = self.dma_rr
            self.dma_rr = (self.dma_rr + 1) % len(self.dma_sems)
            prev = self.dma_last[i]
            if prev is not None:
                op.waits.append(prev)
            self.dma_uses[i] += 1
            op.sem = self.dma_sems[i]
            op.target = 16 * self.dma_uses[i]
            self.dma_last[i] = op
            self.pending_dma.append(op)
        for b in reads:
            key = ("d", op.idx) if dma else eng
            b.readers[key] = op
        for b in writes:
            if b.multi:
                b.writers.append(op)
                b.readers = {k_: r_ for k_, r_ in b.readers.items() if not (r_.emitted and (r_.drained or not r_.is_dma))}
            else:
                b.last_w = op
                b.readers = {}
        self.ops[eng].append(op)
        return op

    def dma(self, eng, out, in_, reads=(), writes=(), **kw):
        return self.add(eng, lambda e: e.dma_start(out=out, in_=in_, **kw), reads, writes, dma=True)

    def drain_dmas(self, eng="sp"):
        op = self.add(eng, None)
        seen = {}
        for d in self.pending_dma:
            d.drained = True
            seen[id(d.sem)] = d
        op.waits.extend(seen.values())
        self.pending_dma = []
        return op

    def emit(self, name=None):
        for e in ENGS:
            c = self.count[e]
            for op in self.ops[e]:
                if op.is_dma:
                    continue
                if op.signal:
                    c += 1
                    op.ticket = c
            self.count[e] = c
        prog = self

        def run(e, engine):
            waited = prog.waited[e]
            for op in prog.ops[e]:
                for d in op.waits:
                    if d.is_dma:
                        key, sem, val = id(d.sem), d.sem, d.target
                    else:
                        key, sem, val = d.eng, prog.sem[d.eng], d.ticket
                    if waited.get(key, 0) >= val:
                        continue
                    waited[key] = val
                    engine.wait_ge(sem, val)
                if op.fn is None:
                    continue
                ins = op.fn(engine)
                if op.is_dma:
                    ins.then_inc(op.sem, 16)
                elif op.signal:
                    ins.then_inc(prog.sem[e], 1)

        with self.nc.Block() as block:
            @block.tensor
            def _(eng):
                run("pe", eng)

            @block.scalar
            def _(eng):
                run("act", eng)

            @block.vector
            def _(eng):
                run("dve", eng)

            @block.gpsimd
            def _(eng):
                run("pool", eng)

            @block.sync
            def _(eng):
                run("sp", eng)
        for e in ENGS:
            for op in self.ops[e]:
                op.emitted = True
        self.ops = {e: [] for e in ENGS}


def _bf(a):
    import ml_dtypes
    return np.asarray(a, dtype=np.float32).astype(ml_dtypes.bfloat16)


def moe_cap(S):
    return max(MOE_B, ((3 * S // 32 + MOE_B - 1) // MOE_B) * MOE_B)


def make_consts(S):
    c = {}
    c["ident_bf"] = _bf(np.eye(128))
    c["ident_f"] = np.eye(128, dtype=np.float32)
    c["ones_bf"] = _bf(np.ones((128, 128)))
    c["ones_f"] = np.ones((128, 128), dtype=np.float32)
    rot = np.zeros((96, 96), np.float32)
    for i in range(16):
        rot[80 + i, 64 + i] = -1.0
        rot[64 + i, 80 + i] = 1.0
    c["rotT"] = _bf(rot)
    sel = np.zeros((32, 96), np.float32)
    for i in range(32):
        sel[i, 64 + i] = 1.0
    c["kr_sel"] = _bf(sel)
    half = 16
    inv = (1.0 / (10000.0 ** (np.arange(half, dtype=np.float32) / half))).astype(np.float32)
    ang = (np.arange(S, dtype=np.float32)[None, :] * inv[:, None]).astype(np.float32)
    cs = np.zeros((96, S), np.float32)
    sn = np.zeros((96, S), np.float32)
    cs[64:80] = np.cos(ang); cs[80:96] = np.cos(ang)
    sn[64:80] = np.sin(ang); sn[80:96] = np.sin(ang)
    c["rope_cos"] = cs
    c["rope_sin"] = sn
    s_ = np.arange(128)[:, None]
    t_ = np.arange(128)[None, :]
    c["hg_Lc_f"] = ((s_ <= t_).astype(np.float32) - (s_ <= 63).astype(np.float32))
    c["hg_Lr_f"] = (s_ > t_).astype(np.float32)
    c["hg_Lc_b"] = ((s_ >= t_).astype(np.float32) - (s_ >= 64).astype(np.float32))
    c["hg_Lr_b"] = (s_ < t_).astype(np.float32)
    c["hg_selm_f"] = np.stack([np.ones(128), (np.arange(128) <= 63)], 1).astype(np.float32)
    c["hg_selm_b"] = np.stack([np.ones(128), (np.arange(128) >= 64)], 1).astype(np.float32)
    mf = (s_ <= t_).astype(np.uint32)
    mb = (s_ >= t_).astype(np.uint32)
    c["hg_mask_f"] = np.tile(mf, (1, 4))
    c["hg_mask_b"] = np.tile(mb, (1, 4))
    c["ustrict"] = (s_ < t_).astype(np.float32)
    c["u32strict"] = (np.arange(32)[:, None] < np.arange(32)[None, :]).astype(np.float32)
    c["ident32"] = np.eye(32, dtype=np.float32)
    cap = moe_cap(S)
    c["blk_iota"] = np.stack([np.arange(32) * cap, np.zeros(32)], 1).astype(np.float32)
    c["lim_row"] = np.tile(((np.arange(32) + 1) * cap).astype(np.float32)[None, :], (128, 1))
    c["p_iota"] = np.arange(128, dtype=np.float32).reshape(128, 1)
    c["tok_iota"] = (np.arange(S // 128, dtype=np.int32)[None, :] * 128 + np.arange(128, dtype=np.int32)[:, None]).astype(np.int32)
    return c


CONST_SPECS = {
    "ustrict": ([128, 128], F32), "u32strict": ([32, 32], F32), "ident32": ([32, 32], F32),
    "blk_iota": ([32, 2], F32), "lim_row": ([128, 32], F32), "p_iota": ([128, 1], F32), "tok_iota": "tok",
    "ident_bf": ([128, 128], BF16), "ident_f": ([128, 128], F32),
    "ones_bf": ([128, 128], BF16), "ones_f": ([128, 128], F32),
    "rotT": ([96, 96], BF16), "kr_sel": ([32, 96], BF16),
    "rope_cos": None, "rope_sin": None,
    "hg_Lc_f": ([128, 128], F32), "hg_Lr_f": ([128, 128], F32),
    "hg_Lc_b": ([128, 128], F32), "hg_Lr_b": ([128, 128], F32),
    "hg_selm_f": ([128, 2], F32), "hg_selm_b": ([128, 2], F32),
    "hg_mask_f": ([128, 512], U32), "hg_mask_b": ([128, 512], U32),
}


def layout_params(inp):
    f = lambda a: np.ascontiguousarray(np.asarray(a, dtype=np.float32))
    p = {}
    p["norm_mix_l"] = f(inp["norm_mix"][0].reshape(8, 128).T)
    p["q_lat_norm_l"] = f(inp["q_lat_norm"][0].reshape(2, 128).T)
    p["kv_lat_norm_l"] = f(inp["kv_lat_norm"][0].reshape(1, 128).T)
    p["q_norm_l"] = f(inp["q_norm"][0].reshape(96, 1))
    p["k_norm_l"] = f(inp["k_norm"][0].reshape(96, 1))
    p["lb_logits_l"] = f(inp["lb_logits"].reshape(2, 2 * 512))
    p["hg_out_norm_l"] = f(inp["hg_out_norm"][0].reshape(1, 128))
    p["norm_ffn_l"] = f(inp["norm_ffn"][0].reshape(1, 1024))
    p["w_rt"] = f(np.concatenate([inp["w_group"][0], inp["w_router"][0]], axis=1))
    p["b_rt"] = f(np.concatenate([inp["b_group"][0], inp["b_router"][0]]).reshape(1, 36))
    p["w_in"] = f(inp["w_in"][0])
    p["w_uq"] = f(inp["w_uq"][0])
    p["w_ukv"] = f(inp["w_ukv"][0])
    p["w_out"] = f(inp["w_out"][0])
    p["w_gate"] = f(inp["w_gate"][0])
    p["w_up"] = f(inp["w_up"][0])
    p["w_down"] = f(inp["w_down"][0])
    return p


PARAM_SPECS = {
    "norm_mix_l": [128, 8], "q_lat_norm_l": [128, 2], "kv_lat_norm_l": [128, 1],
    "q_norm_l": [96, 1], "k_norm_l": [96, 1], "lb_logits_l": [2, 1024],
    "hg_out_norm_l": [1, 128], "norm_ffn_l": [1, 1024], "w_rt": [1024, 36], "b_rt": [1, 36],
    "w_in": [D, NIN], "w_uq": [256, 768], "w_ukv": [128, 1024], "w_out": [1024, 1024],
    "w_gate": [NE, D, DE], "w_up": [NE, D, DE], "w_down": [NE, DE, D],
}


class K:
    def __init__(self, S, debug=(), phases=None):
        self.S = S
        self.NT = S // 128
        self.NB = S // 512
        self.debug = set(debug)
        self.phases = phases
        self.nc = nc = bass.Bass("TRN2", target_bir_lowering=False)
        self.P = Prog(nc)
        self.din = {}
        self.x = nc.dram_tensor("x", [S, D], F32, kind="ExternalInput").ap()
        for k, shp in PARAM_SPECS.items():
            self.din[k] = nc.dram_tensor(k, shp, F32, kind="ExternalInput").ap()
        for k, spec in CONST_SPECS.items():
            if spec is None:
                shp, dt = [96, S], F32
            elif spec == "tok":
                shp, dt = [128, S // 128], I32
            else:
                shp, dt = spec
            self.din[k] = nc.dram_tensor(k, shp, dt, kind="ExternalInput").ap()
        self.y = nc.dram_tensor("y", [S, D], F32, kind="ExternalOutput").ap()
        self.scr = {}
        self.scr_buf = {}
        self.deferred = []
        self.fence_t = nc.alloc_sbuf_tensor("fence_scratch", [128, 1], F32)
        self.b_fence = Buf("fence")

    def scratch(self, name, shape, dt):
        kind = "ExternalOutput" if name in self.debug else "Internal"
        t = self.nc.dram_tensor(name, shape, dt, kind=kind).ap()
        self.scr[name] = t
        self.scr_buf[name] = Buf(name, multi=True)
        return t

    def mm(self, out, lhsT, rhs, start=True, stop=True, r=(), w=()):
        return self.P.add("pe", lambda e: e.matmul(out, lhsT, rhs, start=start, stop=stop), r, w)

    def tr(self, out, in_, ident, r=(), w=()):
        return self.P.add("pe", lambda e: e.transpose(out, in_, ident), r, w)

    def act(self, out, in_, func, r=(), w=(), eng="act", **kw):
        return self.P.add(eng, lambda e: e.activation(out, in_, func, **kw), r, w)

    def ts(self, eng, out, in0, s1, s2, op0, op1=None, r=(), w=(), **kw):
        if op1 is None:
            return self.P.add(eng, lambda e: e.tensor_scalar(out, in0, s1, s2, op0, **kw), r, w)
        return self.P.add(eng, lambda e: e.tensor_scalar(out, in0, s1, s2, op0, op1, **kw), r, w)

    def tt(self, eng, out, in0, in1, op, r=(), w=()):
        return self.P.add(eng, lambda e: e.tensor_tensor(out, in0, in1, op), r, w)

    def cp(self, eng, out, in_, r=(), w=()):
        if eng == "act":
            return self.P.add(eng, lambda e: e.copy(out, in_), r, w)
        return self.P.add(eng, lambda e: e.tensor_copy(out, in_), r, w)

    def dma(self, eng, out, in_, r=(), w=(), **kw):
        return self.P.dma(eng, out, in_, r, w, **kw)

    def load(self, out, in_, r=(), w=(), **kw):
        op = self.P.dma("sp", out, in_, r, w, **kw)
        self.flush()
        return op

    def store(self, out, in_, r=(), w=(), **kw):
        self.P.defer_dma("sp", out, in_, r, w, **kw)

    def fence(self, bufs):
        t = self.fence_t
        self.P.add("pool", lambda e: e.memset(t[0:1, 0:1], 0.0), list(bufs), [self.b_fence])

    def flush(self):
        self.P.flush()

    def make_eps(self, es):
        self.epst = {}
        self.b_epst = Buf("eps")
        for n in (96, 128, 256, 1024):
            t = es.enter_context(self.nc.sbuf_tensor("sb_eps%d" % n, [128, 1], F32))
            self.epst[n] = t
            self.P.add("pool", lambda e, t=t, n=n: e.memset(t[:], float(n * EPS)), (), [self.b_epst])

    def rstd(self, out, in_, neps, bout, r=()):
        np_ = out.shape[0]
        self.P.add("act", lambda e: e.activation(out, in_, AF.Ln, bias=self.epst[neps][0:np_, 0:1]), list(r) + [self.b_epst], [bout])
        self.P.add("act", lambda e: e.activation(out, out, AF.Exp, scale=-0.5), [bout], [bout])


def run_pipelined(gens, depth):
    active = []
    it = iter(gens)
    while True:
        while len(active) < depth:
            g = next(it, None)
            if g is None:
                break
            active.append(g)
        if not active:
            break
        for g in list(active):
            if next(g, "done") == "done":
                active.remove(g)


class Pool_:
    def __init__(self, tiles):
        self.tiles = tiles
        self.bufs = [Buf() for _ in tiles]
        self.i = 0

    def next(self):
        i = self.i
        self.i = (self.i + 1) % len(self.tiles)
        return self.tiles[i], self.bufs[i]


class FreePool:
    def __init__(self, tiles):
        self.tiles = tiles
        self.bufs = [Buf() for _ in tiles]
        self.free = list(range(len(tiles)))

    def get(self):
        while not self.free:
            yield
        i = self.free.pop(0)
        return self.tiles[i], self.bufs[i], i

    def put(self, i):
        self.free.append(i)

    def take(self):
        i = self.free.pop(0)
        return self.tiles[i], self.bufs[i], i


def sb_free(es, nc, name, n, shape, dt):
    return FreePool([es.enter_context(nc.sbuf_tensor("sb_%s%d" % (name, i), shape, dt)) for i in range(n)])


def ps_free(es, nc, name, n, shape=(128, 512), dt=F32):
    return FreePool([es.enter_context(nc.psum_tensor("%s%d" % (name, i), list(shape), dt)) for i in range(n)])


def sb_ring(es, nc, name, n, shape, dt):
    return Pool_([es.enter_context(nc.sbuf_tensor("sb_%s%d" % (name, i), shape, dt)) for i in range(n)])


def ps_ring(es, nc, name, n, shape=(128, 512), dt=F32):
    return Pool_([es.enter_context(nc.psum_tensor("%s%d" % (name, i), list(shape), dt)) for i in range(n)])


def phase1(k):
    nc, P, S = k.nc, k.P, k.S
    QT = k.scratch("QT", [H, QK, S], BF16)
    KT = k.scratch("KT", [H, QK, S], BF16)
    VV = k.scratch("VV", [S, H * DV], BF16)
    HG = k.scratch("HG", [S, 2560], F32)
    bQT, bKT, bVV, bHG = (k.scr_buf[n] for n in ("QT", "KT", "VV", "HG"))
    with ExitStack() as es:
        def sb(name, shape, dt):
            return es.enter_context(nc.sbuf_tensor("sb_" + name, list(shape), dt)), Buf(name)
        ident, b_ident = sb("ident", [128, 128], BF16)
        ones, b_ones = sb("ones", [128, 128], BF16)
        rotT, b_rotT = sb("rotT", [96, 96], BF16)
        krsel, b_krsel = sb("krsel", [32, 96], BF16)
        for t_, b_, nm in ((ident, b_ident, "ident_bf"), (ones, b_ones, "ones_bf"),
                           (rotT, b_rotT, "rotT"), (krsel, b_krsel, "kr_sel")):
            k.dma("sp", t_[:], k.din[nm], w=[b_])
        k.make_eps(es)
        gmix, b_gmix = sb("gmix", [128, 8], F32)
        gql, b_gql = sb("gql", [128, 2], F32)
        gkvl, b_gkvl = sb("gkvl", [128, 1], F32)
        gq, b_gq = sb("gq", [96, 1], F32)
        gk, b_gk = sb("gk", [96, 1], F32)
        for t_, b_, nm in ((gmix, b_gmix, "norm_mix_l"), (gql, b_gql, "q_lat_norm_l"),
                           (gkvl, b_gkvl, "kv_lat_norm_l"), (gq, b_gq, "q_norm_l"), (gk, b_gk, "k_norm_l")):
            k.dma("sp", t_[:], k.din[nm], w=[b_])
        k.ts("dve", gmix[:], gmix[:], 32.0, None, ALU.mult, r=[b_gmix], w=[b_gmix])
        k.ts("dve", gql[:], gql[:], 16.0, None, ALU.mult, r=[b_gql], w=[b_gql])
        k.ts("dve", gkvl[:], gkvl[:], float(np.sqrt(128.0)), None, ALU.mult, r=[b_gkvl], w=[b_gkvl])
        k.ts("dve", gq[:], gq[:], float(np.sqrt(96.0) * 96.0 ** -0.5), None, ALU.mult, r=[b_gq], w=[b_gq])
        k.ts("dve", gk[:], gk[:], float(np.sqrt(96.0)), None, ALU.mult, r=[b_gk], w=[b_gk])

        win, b_win = sb("win", [128, 8, NIN], BF16)
        wst = sb_ring(es, nc, "wst", 1, [128, NIN], F32)
        w_in_v = k.din["w_in"].rearrange("(kc p) n -> p kc n", p=128)
        for kc in range(8):
            st, bst = wst.next()
            k.dma("sp", st[:], w_in_v[:, kc, :], w=[bst])
            k.ts("dve" if kc % 2 == 0 else "pool", win[:, kc, :], st[:], gmix[:, kc:kc + 1], None, ALU.mult,
                 r=[bst, b_gmix], w=[b_win])
        wuq, b_wuq = sb("wuq", [128, 2, 768], BF16)
        w_uq_v = k.din["w_uq"].rearrange("(kc p) n -> p kc n", p=128)
        for kc in range(2):
            st, bst = wst.next()
            k.dma("sp", st[:, 0:768], w_uq_v[:, kc, :], w=[bst])
            k.ts("dve", wuq[:, kc, :], st[:, 0:768], gql[:, kc:kc + 1], None, ALU.mult, r=[bst, b_gql], w=[b_wuq])
        wk, b_wk = sb("wk", [128, 8, 96], BF16)
        wv, b_wv = sb("wv", [128, 8, 64], BF16)
        st, bst = wst.next()
        k.dma("sp", st[:, 0:1024], k.din["w_ukv"], w=[bst])
        P.add("pool", lambda e: e.memset(wk[:], 0.0), (), [b_wk])
        stv = st[:, 0:1024].rearrange("p (h c) -> p h c", c=128)
        k.ts("dve", wk[:, :, 0:64], stv[:, :, 0:64], gkvl[:, 0:1], None, ALU.mult, r=[bst, b_gkvl], w=[b_wk])
        k.ts("dve", wv[:], stv[:, :, 64:128], gkvl[:, 0:1], None, ALU.mult, r=[bst, b_gkvl], w=[b_wv])

        xr = sb_ring(es, nc, "xt", 3, [128, D], F32)
        junkr = sb_ring(es, nc, "junk", 3, [128, D], BF16)
        ssr = sb_ring(es, nc, "ss", 4, [128, 2], F32)
        nr = sb_ring(es, nc, "nbf", 2, [128, D], BF16)
        nTr = sb_ring(es, nc, "nT", 2, [128, 8, 512], BF16)
        stg = sb_ring(es, nc, "stg", 2, [128, 2560], F32)
        sqq, b_sqq = sb("sqq", [128, 3, 512], BF16)
        rsl, b_rsl = sb("rsl", [128, 2, 512], F32)
        qnT, b_qnT = sb("qnT", [128, 2, 512], BF16)
        kvnT, b_kvnT = sb("kvnT", [128, 512], BF16)
        krT, b_krT = sb("krT", [32, 512], BF16)
        vsb = sb_ring(es, nc, "vsb", 2, [128, 512], BF16)
        cosr = sb_ring(es, nc, "cos", 2, [96, 512], F32)
        sinr = sb_ring(es, nc, "sin", 2, [96, 512], F32)
        sqh = sb_ring(es, nc, "sqh", 3, [96, 512], BF16)
        qgh = sb_ring(es, nc, "qgh", 3, [96, 512], BF16)
        qg32 = sb_ring(es, nc, "qg32", 3, [96, 512], F32)
        rsh = sb_ring(es, nc, "rsh", 3, [96, 512], F32)
        t1r = sb_ring(es, nc, "t1r", 3, [96, 512], F32)
        t2r = sb_ring(es, nc, "t2r", 3, [96, 512], F32)
        qfr = sb_ring(es, nc, "qfr", 3, [96, 512], BF16)
        psf = ps_ring(es, nc, "psf", 6)
        psb = ps_ring(es, nc, "psb", 2, (128, 1024), BF16)

        eflip = [0]

        def evac_eng():
            eflip[0] ^= 1
            return "act" if eflip[0] else "dve"

        for j in range(k.NB):
            tok0 = j * 512
            nT, b_nT = nTr.next()
            def xt_gen(t, tok0=tok0, nT=nT, b_nT=b_nT):
                r0 = tok0 + t * 128
                xt, bx = xr.next()
                k.load(xt[:], k.x[r0:r0 + 128, :], w=[bx])
                yield
                ss, bss = ssr.next()
                junk, b_junk = junkr.next()
                k.act(junk[:], xt[:], AF.Square, r=[bx], w=[b_junk, bss], accum_out=ss[:, 0:1])
                k.rstd(ss[:, 1:2], ss[:, 0:1], 1024, bss, r=[bss])
                yield
                nb_, bn = nr.next()
                k.act(nb_[:], xt[:], AF.Copy, r=[bx, bss], w=[bn], scale=ss[:, 1:2])
                yield
                pb, bpb = psb.next()
                for kc in range(8):
                    k.tr(pb[:, kc * 128:(kc + 1) * 128], nb_[:, kc * 128:(kc + 1) * 128], ident[:],
                         r=[bn, b_ident], w=[bpb])
                yield
                k.cp("act" if t % 2 == 0 else "dve", nT[:, :, t * 128:(t + 1) * 128],
                     pb[:].rearrange("p (kc t) -> p kc t", kc=8), r=[bpb], w=[b_nT])

            run_pipelined([xt_gen(t) for t in range(4)], 2)
            lat = []
            for (c0, ncol) in ((0, 128), (128, 128), (256, 128), (384, 32)):
                ps, bps = psf.next()
                for kc in range(8):
                    k.mm(ps[0:ncol, :], win[:, kc, c0:c0 + ncol], nT[:, kc, :], start=(kc == 0), stop=(kc == 7),
                         r=[b_win, b_nT], w=[bps])
                lat.append((ps, bps))
            for i in range(3):
                k.act(sqq[:, i, :], lat[i][0][:], AF.Square, r=[lat[i][1]], w=[b_sqq])
            k.cp("dve", krT[:], lat[3][0][0:32, :], r=[lat[3][1]], w=[b_krT])
            msq, bmsq = psf.next()
            k.mm(msq[:], ones[:], sqq[:, 0, :], start=True, stop=False, r=[b_ones, b_sqq], w=[bmsq])
            k.mm(msq[:], ones[:], sqq[:, 1, :], start=False, stop=True, r=[b_ones, b_sqq], w=[bmsq])
            msk, bmsk = psf.next()
            k.mm(msk[:], ones[:], sqq[:, 2, :], r=[b_ones, b_sqq], w=[bmsk])
            k.rstd(rsl[:, 0, :], msq[:], 256, b_rsl, r=[bmsq])
            k.rstd(rsl[:, 1, :], msk[:], 128, b_rsl, r=[bmsk])
            k.tt("dve", qnT[:, 0, :], lat[0][0][:], rsl[:, 0, :], ALU.mult, r=[lat[0][1], b_rsl], w=[b_qnT])
            k.tt("dve", qnT[:, 1, :], lat[1][0][:], rsl[:, 0, :], ALU.mult, r=[lat[1][1], b_rsl], w=[b_qnT])
            k.tt("dve", kvnT[:], lat[2][0][:], rsl[:, 1, :], ALU.mult, r=[lat[2][1], b_rsl], w=[b_kvnT])
            for t in range(4):
                ps, bps = psf.next()
                k.mm(ps[:], kvnT[:, t * 128:(t + 1) * 128], wv[:].rearrange("p h c -> p (h c)"),
                     r=[b_kvnT, b_wv], w=[bps])
                v_, bv = vsb.next()
                k.cp("dve", v_[:], ps[:], r=[bps], w=[bv])
                k.store(VV[tok0 + t * 128: tok0 + (t + 1) * 128, :], v_[:], r=[bv], w=[bVV])
            cs, bcs = cosr.next()
            sn, bsn = sinr.next()
            k.load(cs[64:96, :], k.din["rope_cos"][64:96, tok0:tok0 + 512], w=[bcs])
            k.load(sn[64:96, :], k.din["rope_sin"][64:96, tok0:tok0 + 512], w=[bsn])

            def head_gen(h, which, tok0=tok0, cs=cs, bcs=bcs, sn=sn, bsn=bsn):
                ps, bps = psf.next()
                if which == 0:
                    k.mm(ps[0:96, :], wuq[:, 0, h * 96:(h + 1) * 96], qnT[:, 0, :], start=True, stop=False,
                         r=[b_wuq, b_qnT], w=[bps])
                    k.mm(ps[0:96, :], wuq[:, 1, h * 96:(h + 1) * 96], qnT[:, 1, :], start=False, stop=True,
                         r=[b_wuq, b_qnT], w=[bps])
                    g_, bg_, dst, bdst = gq, b_gq, QT, bQT
                else:
                    k.mm(ps[0:96, :], wk[:, h, :], kvnT[:], start=True, stop=False, r=[b_wk, b_kvnT], w=[bps])
                    k.mm(ps[0:96, :], krsel[:], krT[:], start=False, stop=True, r=[b_krsel, b_krT], w=[bps])
                    g_, bg_, dst, bdst = gk, b_gk, KT, bKT
                yield
                sq, bsq = sqh.next()
                k.act(sq[:], ps[0:96, :], AF.Square, r=[bps], w=[bsq])
                q32, bq32 = qg32.next()
                k.act(q32[:], ps[0:96, :], AF.Copy, r=[bps, bg_], w=[bq32], scale=g_[:, 0:1])
                yield
                ms, bms = psf.next()
                k.mm(ms[0:96, :], ones[0:96, 0:96], sq[:], r=[b_ones, bsq], w=[bms])
                qg, bqg = qgh.next()
                k.act(qg[:], ps[0:96, :], AF.Copy, r=[bps, bg_], w=[bqg], scale=g_[:, 0:1])
                t1, bt1 = t1r.next()
                k.tt("pool", t1[64:96, :], q32[64:96, :], cs[64:96, :], ALU.mult, r=[bq32, bcs], w=[bt1])
                yield
                rt, brt = psf.next()
                k.mm(rt[0:96, :], rotT[:], qg[:], r=[b_rotT, bqg], w=[brt])
                rs, brs = rsh.next()
                k.rstd(rs[:], ms[0:96, :], 96, brs, r=[bms])
                yield
                t2, bt2 = t2r.next()
                k.tt("dve", t2[64:96, :], rt[64:96, :], sn[64:96, :], ALU.mult, r=[brt, bsn], w=[bt2])
                yield
                k.tt("pool", q32[64:96, :], t1[64:96, :], t2[64:96, :], ALU.add, r=[bt1, bt2], w=[bq32])
                yield
                qf, bqf = qfr.next()
                k.tt("dve", qf[:], q32[:], rs[:], ALU.mult, r=[bq32, brs], w=[bqf])
                k.store(dst[h, :, tok0:tok0 + 512], qf[:], r=[bqf], w=[bdst])

            def tm_gen(t, tok0=tok0, nT=nT, b_nT=b_nT):
                sg, bsg = stg.next()
                for g in range(5):
                    ps, bps = psf.next()
                    c0 = 416 + g * 512
                    for kc in range(8):
                        k.mm(ps[:], nT[:, kc, t * 128:(t + 1) * 128], win[:, kc, c0:c0 + 512],
                             start=(kc == 0), stop=(kc == 7), r=[b_nT, b_win], w=[bps])
                    yield
                    k.cp(evac_eng(), sg[:, g * 512:(g + 1) * 512], ps[:], r=[bps], w=[bsg])
                k.store(HG[tok0 + t * 128: tok0 + (t + 1) * 128, :], sg[:], r=[bsg], w=[bHG])

            gens = []
            hw = [(h, w_) for h in range(H) for w_ in range(2)]
            for t in range(4):
                gens += [head_gen(h, w_) for (h, w_) in hw[4 * t:4 * t + 4]]
                gens.append(tm_gen(t))
            run_pipelined(gens, 3)
        k.flush()
        P.drain_dmas()
        P.emit()


def moe_scratch_init(k, es):
    nc, P, S = k.nc, k.P, k.S
    CAP = moe_cap(S)
    DUMP = NE * CAP
    PSL = DUMP + 128
    k.CAP, k.PSL = CAP, PSL
    XS = k.scratch("XS", [PSL, D], BF16)
    YB = k.scratch("YB", [PSL, D], BF16)
    bXS, bYB = (k.scr_buf[n] for n in ("XS", "YB"))
    zx = es.enter_context(nc.sbuf_tensor("sb_i_zx", [128, 4 * D], BF16))
    b_zx = Buf("i_zx")
    P.add("pool", lambda e: e.memset(zx[:], 0.0), (), [b_zx])
    XSz = XS.rearrange("(p r) n -> p r n", p=128)
    RX = PSL // 128
    for r_ in range(0, RX, 4):
        n_ = min(4, RX - r_)
        k.dma("pool", XSz[:, r_:r_ + n_, :], zx[:, 0:n_ * D].rearrange("p (r n) -> p r n", r=n_), r=[b_zx], w=[bXS])
    k.dma("pool", YB[DUMP:DUMP + 128, :], zx[:, 0:D], r=[b_zx], w=[bYB])


def phase_attn(k, with_hgrn=False):
    nc, P, S, NT = k.nc, k.P, k.S, k.NT
    QT, KT, VV = k.scr["QT"], k.scr["KT"], k.scr["VV"]
    bQT, bKT, bVV = k.scr_buf["QT"], k.scr_buf["KT"], k.scr_buf["VV"]
    MIXT = k.scratch("MIXT", [D, S], BF16)
    bMIXT = k.scr_buf["MIXT"]
    VVv = VV.rearrange("(t p) (h c) -> p t h c", p=128, c=DV)
    RD = k.scratch("RD", [H * (S // 512), 512], F32)
    bRD = [Buf("rd%d" % i) for i in range(4)]
    with ExitStack() as es:
        def sb(name, shape, dt):
            return es.enter_context(nc.sbuf_tensor("sb_" + name, list(shape), dt)), Buf(name)
        onesf, b_onesf = sb("a_onesf", [128, 128], F32)
        k.dma("sp", onesf[:], k.din["ones_f"], w=[b_onesf])
        kth = sb_ring(es, nc, "a_kt", 2, [96, S], BF16)
        qth = sb_ring(es, nc, "a_qt", 3, [96, 512], BF16)
        vh = sb_ring(es, nc, "a_v", 2, [128, NT, DV + 1], BF16)
        for t_, b_ in zip(vh.tiles, vh.bufs):
            P.add("pool", lambda e, t_=t_: e.memset(t_[:], 1.0), (), [b_])
        ptr = sb_ring(es, nc, "a_pt", 3 if with_hgrn else 4, [128, 1024], BF16)
        ocr = sb_ring(es, nc, "a_oc", 2, [128, 512], F32)
        osb = sb_ring(es, nc, "a_o", 2, [64, 512], F32)
        aout = sb_ring(es, nc, "a_a", 2, [64, 512], BF16)
        scr_ = ps_ring(es, nc, "a_sc", 2 if with_hgrn else 3, (128, 1024), F32)
        accr = ps_ring(es, nc, "a_acc", 1 if with_hgrn else 2)
        hstep = None
        if with_hgrn:
            h_psf = ps_free(es, nc, "h_psf", 2)
            h_psb = ps_free(es, nc, "h_psb", 1, (128, 1024), BF16)
            hstep = hgrn_build(k, es, h_psf, h_psb)
            hsteps = hgrn_order(NT)
        NP2 = NT // 2
        NQB = S // 512
        heads = {}

        def load_head(h):
            kt_, bkt = kth.next()
            v_, bv = vh.next()
            k.load(kt_[:], KT[h], r=[bKT], w=[bkt])
            k.load(v_[:, :, 0:DV], VVv[:, :, h, :], r=[bVV], w=[bv])
            heads[h] = (kt_, bkt, v_, bv)

        qbs = {}

        def load_q(h, qb):
            qt_, bqt = qth.next()
            k.load(qt_[:], QT[h, :, qb * 512:(qb + 1) * 512], r=[bQT], w=[bqt])
            qbs[(h, qb)] = (qt_, bqt)

        steps = [(h, qb, kp) for h in range(H) for qb in range(NQB) for kp in range(NP2)]
        scs = {}

        def emit_qk(i):
            h, qb, kp = steps[i]
            if kp == 0:
                if qb == 0 and h not in heads:
                    load_head(h)
                if (h, qb) not in qbs:
                    load_q(h, qb)
                nxt = (h, qb + 1) if qb + 1 < NQB else ((h + 1, 0) if h + 1 < H else None)
                if nxt is not None:
                    if nxt[1] == 0 and nxt[0] not in heads:
                        load_head(nxt[0])
                    if nxt not in qbs:
                        load_q(*nxt)
            kt_, bkt, v_, bv = heads[h]
            qt_, bqt = qbs[(h, qb)]
            sc, bsc = scr_.next()
            for u in range(2):
                kt = 2 * kp + u
                k.mm(sc[:, u * 512:(u + 1) * 512], kt_[:, kt * 128:(kt + 1) * 128], qt_[:],
                     r=[bkt, bqt], w=[bsc])
            scs[i] = (sc, bsc)

        LOOK = 1 if with_hgrn else 2
        for i in range(min(LOOK, len(steps))):
            emit_qk(i)
        if "XS" not in k.scr:
            moe_scratch_init(k, es)
        acc = bacc = None
        gen = None
        for i, (h, qb, kp) in enumerate(steps):
            kt_, bkt, v_, bv = heads[h]
            if kp == 0:
                acc, bacc = accr.next()
                if hstep is not None and h * NQB + qb < len(hsteps):
                    gen = hstep(*hsteps[h * NQB + qb])
            sc, bsc = scs.pop(i)
            pt, bpt = ptr.next()
            k.act(pt[:], sc[:], AF.Exp, r=[bsc], w=[bpt])
            if i + LOOK < len(steps):
                emit_qk(i + LOOK)
            for u in range(2):
                kt = 2 * kp + u
                k.mm(acc[0:DV + 1, :], v_[:, kt, :], pt[:, u * 512:(u + 1) * 512],
                     start=(kt == 0), stop=(kt == NT - 1), r=[bv, bpt], w=[bacc])
            if gen is not None:
                if next(gen, "done") == "done":
                    gen = None
            if kp == NP2 - 1:
                if gen is not None:
                    for _ in gen:
                        pass
                    gen = None
                oc, boc = ocr.next()
                k.cp("dve", oc[0:DV + 1, :], acc[0:DV + 1, :], r=[bacc], w=[boc])
                P.add("dve", lambda e, oc=oc: e.reciprocal(oc[64:65, :], oc[64:65, :]), [boc], [boc])
                o_, bo = osb.next()
                ridx = h * NQB + qb
                k.dma("sp", RD[ridx:ridx + 1, :], oc[64:65, :], r=[boc], w=[bRD[ridx % 4]])
                k.dma("sp", o_[:], RD[ridx:ridx + 1, :].partition_broadcast(64), r=[bRD[ridx % 4]], w=[bo])
                a_, ba = aout.next()
                k.tt("dve", a_[:], oc[0:64, :], o_[:], ALU.mult, r=[boc, bo], w=[ba])
                k.store(MIXT[h * 64:(h + 1) * 64, qb * 512:(qb + 1) * 512], a_[:], r=[ba], w=[bMIXT])
        k.flush()
        P.drain_dmas()
        P.emit()


def hgrn_build(k, es, psf, psb):
    nc, P, S, NT = k.nc, k.P, k.S, k.NT
    HG, MIXT = k.scr["HG"], k.scr["MIXT"]
    bHG, bMIXT = k.scr_buf["HG"], k.scr_buf["MIXT"]
    OF = k.scratch("OF", [S, 512], F32)
    bOF = k.scr_buf["OF"]
    MIXr = MIXT[512:1024, :].rearrange("(c p) t -> p c t", p=128)
    def sb(name, shape, dt):
        return es.enter_context(nc.sbuf_tensor("sb_" + name, list(shape), dt)), Buf(name)
    identb, b_identb = sb("h_ident", [128, 128], BF16)
    k.dma("sp", identb[:], k.din["ident_bf"], w=[b_identb])
    cst = {}
    for nm, shp, dt in (("hg_Lc_f", [128, 128], F32), ("hg_Lr_f", [128, 128], F32), ("hg_Lc_b", [128, 128], F32),
                        ("hg_Lr_b", [128, 128], F32), ("hg_selm_f", [128, 2], F32), ("hg_selm_b", [128, 2], F32),
                        ("hg_mask_f", [128, 512], U32), ("hg_mask_b", [128, 512], U32)):
        t_, b_ = sb(nm, shp, dt)
        k.dma("sp", t_[:], k.din[nm], w=[b_])
        cst[nm] = (t_, b_)
    l0, b_l0 = sb("h_l0", [128, 1024], F32)
    l1, b_l1 = sb("h_l1", [128, 1024], F32)
    k.dma("sp", l0[:], k.din["lb_logits_l"][0:1, :].partition_broadcast(128), w=[b_l0])
    k.dma("sp", l1[:], k.din["lb_logits_l"][1:2, :].partition_broadcast(128), w=[b_l1])
    lbb, b_lbb = sb("h_lb", [128, 1024], F32)
    oml, b_oml = sb("h_oml", [128, 1024], F32)
    k.tt("dve", l1[:], l1[:], l0[:], ALU.subtract, r=[b_l0, b_l1], w=[b_l1])
    k.act(l1[:], l1[:], AF.Exp, r=[b_l1], w=[b_l1])
    k.ts("dve", l1[:], l1[:], 1.0, None, ALU.add, r=[b_l1], w=[b_l1])
    P.add("dve", lambda e: e.reciprocal(lbb[:], l1[:]), [b_l1], [b_lbb])
    k.ts("dve", oml[:], lbb[:], -1.0, 1.0, ALU.mult, ALU.add, r=[b_lbb], w=[b_oml])
    gn, b_gn = sb("h_gn", [128, 4, 128], F32)
    for hh in range(4):
        k.dma("sp", gn[:, hh, :], k.din["hg_out_norm_l"][0:1, :].partition_broadcast(128), w=[b_gn])
    k.ts("dve", gn[:], gn[:], float(np.sqrt(128.0)), None, ALU.mult, r=[b_gn], w=[b_gn])
    eps128, b_eps = sb("h_eps", [128, 1], F32)
    P.add("pool", lambda e: e.memset(eps128[:], float(128 * EPS)), (), [b_eps])
    one1, b_one = sb("h_one", [128, 1], F32)
    P.add("pool", lambda e: e.memset(one1[:], 1.0), (), [b_one])

    hgr = sb_ring(es, nc, "h_in", 3, [128, 2560], F32)
    e1r = sb_ring(es, nc, "h_e1", 3, [128, 512], F32)
    e2r = sb_ring(es, nc, "h_e2", 3, [128, 512], F32)
    e3r = sb_ring(es, nc, "h_e3", 3, [128, 512], F32)
    fr = sb_ring(es, nc, "h_f", 3, [128, 512], F32)
    kkr = sb_ring(es, nc, "h_kk", 3, [128, 512], F32)
    gr = sb_ring(es, nc, "h_g", 3, [128, 512], F32)
    qr = sb_ring(es, nc, "h_q", 3, [128, 512], F32)
    vr = sb_ring(es, nc, "h_v", 3, [128, 512], BF16)
    ecr = sb_ring(es, nc, "h_ec", 3, [128, 512], F32)
    encr = sb_ring(es, nc, "h_enc", 3, [128, 512], F32)
    err_ = sb_ring(es, nc, "h_er", 3, [128, 512], F32)
    eblr = sb_ring(es, nc, "h_ebl", 3, [128, 8], F32)
    qkbr = sb_ring(es, nc, "h_qkb", 3, [128, 1024], BF16)
    kdr = sb_ring(es, nc, "h_kd", 3, [128, 512], BF16)
    qkTr = sb_ring(es, nc, "h_qkT", 3, [128, 1024], BF16)
    atr = sb_ring(es, nc, "h_at", 3, [128, 512], BF16)
    sbr = sb_ring(es, nc, "h_sb", 3, [128, 512], BF16)
    Sst, b_S = sb("h_S", [128, 512], F32)
    osr = sb_ring(es, nc, "h_os", 3, [128, 512], F32)
    ofr = sb_ring(es, nc, "h_of", 3, [128, 512], F32)
    junkr = sb_ring(es, nc, "h_junk", 12, [128, 128], BF16)
    ssq = sb_ring(es, nc, "h_ssq", 3, [128, 8], F32)
    rbr = sb_ring(es, nc, "h_rb", 3, [128, 512], BF16)
    rTr = sb_ring(es, nc, "h_rT", 3, [128, 512], BF16)

    def silu_from(dst, src, rd, wr, tmp, btmp):
        k.act(tmp[:], src, AF.Exp, r=rd, w=[btmp], scale=-1.0)
        k.ts("dve", tmp[:], tmp[:], 1.0, None, ALU.add, r=[btmp], w=[btmp])
        P.add("dve", lambda e: e.reciprocal(tmp[:], tmp[:]), [btmp], [btmp])
        k.tt("dve", dst, tmp[:], src, ALU.mult, r=[btmp] + list(rd), w=wr)

    dirs = []
    for d_ in range(2):
        sfx = "_f" if d_ == 0 else "_b"
        dirs.append((cst["hg_Lc" + sfx], cst["hg_Lr" + sfx], cst["hg_selm" + sfx], cst["hg_mask" + sfx]))

    prog_state = {"s_done": 0, "started": 0}

    def step(d, ti, first):
        (Lc, b_Lc), (Lr, b_Lr), (selm, b_selm), (mask, b_mask) = dirs[d]
        my = prog_state["started"]
        prog_state["started"] += 1
        if first:
            while prog_state["s_done"] < my:
                yield
            P.add("pool", lambda e: e.memset(Sst[:], 0.0), (), [b_S])
            for t_, b_ in zip(atr.tiles, atr.bufs):
                P.add("pool", lambda e, t_=t_: e.memset(t_[:], 0.0), (), [b_])
        r0 = ti * 128
        hg_, bhg = hgr.next()
        k.load(hg_[:], HG[r0:r0 + 128, :], r=[bHG], w=[bhg])
        if d == 1:
            of_, bof = ofr.next()
            k.load(of_[:], OF[r0:r0 + 128, :], r=[bOF], w=[bof])
        z = hg_[:, 512 + d * 512: 1024 + d * 512]
        yield
        e1, be1 = e1r.next()
        e2, be2 = e2r.next()
        k.act(e1[:], z, AF.Sigmoid, r=[bhg], w=[be1])
        k.act(e2[:], hg_[:, 0:512], AF.Sigmoid, r=[bhg], w=[be2])
        if d == 1:
            e3, be3 = e3r.next()
            k.act(e3[:], hg_[:, 2048:2560], AF.Sigmoid, r=[bhg], w=[be3])
        v_, bv = vr.next()
        k.act(v_[:], hg_[:, 1536:2048], AF.Copy, r=[bhg], w=[bv], scale=-1.0)
        yield
        f_, bf_ = fr.next()
        k.tt("dve", f_[:], e1[:], oml[:, d * 512:(d + 1) * 512], ALU.mult, r=[be1, b_oml], w=[bf_])
        k.tt("dve", f_[:], f_[:], lbb[:, d * 512:(d + 1) * 512], ALU.add, r=[bf_, b_lbb], w=[bf_])
        q_, bq = qr.next()
        k.tt("dve", q_[:], e2[:], hg_[:, 0:512], ALU.mult, r=[be2, bhg], w=[bq])
        yield
        g_, bg = gr.next()
        k.act(g_[:], f_[:], AF.Ln, r=[bf_], w=[bg])
        yield
        cps, bcps, icps = yield from psf.get()
        k.mm(cps[:], Lc[:], g_[:], r=[b_Lc, bg], w=[bcps])
        rps, brps, irps = yield from psf.get()
        k.mm(rps[:], Lr[:], g_[:], r=[b_Lr, bg], w=[brps])
        yield
        ec, bec = ecr.next()
        enc, benc = encr.next()
        er, ber = err_.next()
        k.act(ec[:], cps[:], AF.Exp, r=[bcps], w=[bec])
        k.act(enc[:], cps[:], AF.Exp, r=[bcps], w=[benc], scale=-1.0)
        k.act(er[:], rps[:], AF.Exp, r=[brps], w=[ber])
        psf.put(icps)
        psf.put(irps)
        yield
        blm, bblm, iblm = yield from psf.get()
        for hh in range(4):
            k.mm(blm[:, 2 * hh:2 * hh + 2], g_[:, hh * 128:(hh + 1) * 128], selm[:], r=[bg, b_selm], w=[bblm])
        qkb, bqkb = qkbr.next()
        kd, bkd = kdr.next()
        k.tt("dve", qkb[:, 0:512], q_[:], ec[:], ALU.mult, r=[bq, bec], w=[bqkb])
        P.add("dve", lambda e, qkb=qkb, f_=f_, enc=enc: e.scalar_tensor_tensor(
            qkb[:, 512:1024], f_[:], 1.0, enc[:], ALU.subtract, ALU.mult), [bf_, benc], [bqkb])
        P.add("dve", lambda e, kd=kd, f_=f_, er=er: e.scalar_tensor_tensor(
            kd[:], f_[:], 1.0, er[:], ALU.subtract, ALU.mult), [bf_, ber], [bkd])
        yield
        ebl, bebl = eblr.next()
        k.act(ebl[:], blm[:, 0:8], AF.Exp, r=[bblm], w=[bebl])
        psf.put(iblm)
        yield
        pb, bpb, ipb = yield from psb.get()
        for c8 in range(8):
            k.tr(pb[:, c8 * 128:(c8 + 1) * 128], qkb[:, c8 * 128:(c8 + 1) * 128], identb[:],
                 r=[bqkb, b_identb], w=[bpb])
        while prog_state["s_done"] < my:
            yield
        sb_, bsb = sbr.next()
        for hh in range(4):
            k.act(sb_[:, hh * 128:(hh + 1) * 128], Sst[:, hh * 128:(hh + 1) * 128], AF.Copy,
                  r=[b_S, bebl], w=[bsb], scale=ebl[:, 2 * hh + 1:2 * hh + 2])
        yield
        qkT, bqkT = qkTr.next()
        k.cp("dve", qkT[:], pb[:], r=[bpb], w=[bqkT])
        psb.put(ipb)
        yield
        atp, batp, iatp = yield from psf.get()
        for hh in range(4):
            k.mm(atp[:, hh * 128:(hh + 1) * 128], qkT[:, 512 + hh * 128: 512 + (hh + 1) * 128],
                 qkT[:, hh * 128:(hh + 1) * 128], r=[bqkT], w=[batp])
        snp, bsnp, isnp = yield from psf.get()
        for hh in range(4):
            sl = slice(hh * 128, (hh + 1) * 128)
            k.mm(snp[:, sl], kd[:, sl], v_[:, sl], r=[bkd, bv], w=[bsnp])
        yield
        at, bat = atr.next()
        P.add("dve", lambda e, at=at, atp=atp, mask=mask: e.copy_predicated(at[:], mask[:], atp[:]),
              [batp, b_mask], [bat])
        psf.put(iatp)
        yield
        ops, bops, iops = yield from psf.get()
        for hh in range(4):
            sl = slice(hh * 128, (hh + 1) * 128)
            k.mm(ops[:, sl], at[:, sl], v_[:, sl], start=True, stop=False, r=[bat, bv], w=[bops])
            k.mm(ops[:, sl], qkT[:, sl], sb_[:, sl], start=False, stop=True, r=[bqkT, bsb], w=[bops])
        for hh in range(4):
            sl = slice(hh * 128, (hh + 1) * 128)
            P.add("dve", lambda e, sl=sl, ebl=ebl, snp=snp, hh=hh: e.scalar_tensor_tensor(
                Sst[:, sl], Sst[:, sl], ebl[:, 2 * hh:2 * hh + 1], snp[:, sl], ALU.mult, ALU.add),
                [b_S, bebl, bsnp], [b_S])
        prog_state["s_done"] = my + 1
        psf.put(isnp)
        yield
        os_, bos = osr.next()
        if d == 0:
            k.cp("dve", os_[:], ops[:], r=[bops], w=[bos])
            psf.put(iops)
            k.store(OF[r0:r0 + 128, :], os_[:], r=[bos], w=[bOF])
            return
        k.tt("dve", os_[:], ops[:], of_[:], ALU.add, r=[bops, bof], w=[bos])
        psf.put(iops)
        k.tt("dve", e3[:], e3[:], hg_[:, 2048:2560], ALU.mult, r=[be3, bhg], w=[be3])
        yield
        sq, bsq = ssq.next()
        for hh in range(4):
            junk, b_junk = junkr.next()
            k.act(junk[:], os_[:, hh * 128:(hh + 1) * 128], AF.Square, r=[bos], w=[b_junk, bsq],
                  accum_out=sq[:, hh:hh + 1])
        k.act(sq[:, 4:8], sq[:, 0:4], AF.Ln, r=[bsq, b_eps], w=[bsq], bias=eps128[:, 0:1])
        k.act(sq[:, 4:8], sq[:, 4:8], AF.Exp, r=[bsq], w=[bsq], scale=-0.5)
        yield
        for hh in range(4):
            k.ts("dve", os_[:, hh * 128:(hh + 1) * 128], os_[:, hh * 128:(hh + 1) * 128], sq[:, 4 + hh:5 + hh],
                 None, ALU.mult, r=[bos, bsq], w=[bos])
        k.tt("dve", os_[:], os_[:], gn[:].rearrange("p h c -> p (h c)"), ALU.mult, r=[bos, b_gn], w=[bos])
        rb, brb = rbr.next()
        k.tt("dve", rb[:], os_[:], e3[:], ALU.mult, r=[bos, be3], w=[brb])
        yield
        pb, bpb, ipb = yield from psb.get()
        for hh in range(4):
            k.tr(pb[:, hh * 128:(hh + 1) * 128], rb[:, hh * 128:(hh + 1) * 128], identb[:],
                 r=[brb, b_identb], w=[bpb])
        yield
        rT, brT = rTr.next()
        k.cp("dve", rT[:], pb[:, 0:512], r=[bpb], w=[brT])
        psb.put(ipb)
        k.store(MIXr[:, :, r0:r0 + 128], rT[:].rearrange("p (c t) -> p c t", c=4), r=[brT], w=[bMIXT])
    return step


def hgrn_order(NT):
    return [(0, ti, ti == 0) for ti in range(NT)] + [(1, ti, ti == NT - 1) for ti in range(NT - 1, -1, -1)]


def phase_hgrn(k):
    nc, P = k.nc, k.P
    with ExitStack() as es:
        psf = ps_free(es, nc, "h_psf", 6)
        psb = ps_free(es, nc, "h_psb", 2, (128, 1024), BF16)
        step = hgrn_build(k, es, psf, psb)
        order = hgrn_order(k.NT)
        for d in range(2):
            run_pipelined((step(*o) for o in order if o[0] == d), 3)
        k.flush()
        P.drain_dmas()
        P.emit()


def phase_route(k):
    nc, P, S, NT = k.nc, k.P, k.S, k.NT
    CAP = moe_cap(S)
    DUMP = NE * CAP
    PSL = DUMP + 128
    k.CAP, k.PSL = CAP, PSL
    MIXT, bMIXT = k.scr["MIXT"], k.scr_buf["MIXT"]
    H1 = k.scratch("H1", [S, D], F32)
    H2N = k.scratch("H2N", [S, D], BF16)
    XS = k.scr["XS"]
    DEST = k.scratch("DEST", [128, NT * 2], I32)
    GATE = k.scratch("GATE", [128, NT * 2], F32)
    bH1, bH2N, bXS, bDEST, bGATE = (k.scr_buf[n] for n in ("H1", "H2N", "XS", "DEST", "GATE"))
    MIXv = MIXT.rearrange("(c p) t -> p c t", p=128)
    with ExitStack() as es:
        def sb(name, shape, dt):
            return es.enter_context(nc.sbuf_tensor("sb_" + name, list(shape), dt)), Buf(name)
        identf, b_identf = sb("r_identf", [128, 128], F32)
        onesf, b_onesf = sb("r_onesf", [128, 128], F32)
        ustr, b_ustr = sb("r_ustr", [128, 128], F32)
        id32, b_id32 = sb("r_id32", [32, 32], F32)
        ecap, b_ecap = sb("r_ecap", [32, 2], F32)
        limr, b_limr = sb("r_limr", [128, 32], F32)
        piota, b_piota = sb("r_piota", [128, 1], F32)
        toki, b_toki = sb("r_toki", [128, NT], I32)
        for t_, b_, nm in ((identf, b_identf, "ident_f"), (onesf, b_onesf, "ones_f"), (ustr, b_ustr, "ustrict"),
                           (id32, b_id32, "ident32"), (ecap, b_ecap, "blk_iota"), (limr, b_limr, "lim_row"),
                           (piota, b_piota, "p_iota"), (toki, b_toki, "tok_iota")):
            k.dma("sp", t_[:], k.din[nm], w=[b_])
        gff, b_gff = sb("r_gff", [128, D], F32)
        k.dma("sp", gff[:], k.din["norm_ffn_l"][0:1, :].partition_broadcast(128), w=[b_gff])
        k.ts("dve", gff[:], gff[:], 32.0, None, ALU.mult, r=[b_gff], w=[b_gff])
        brt, b_brt = sb("r_brt", [128, 36], F32)
        k.dma("sp", brt[:], k.din["b_rt"][0:1, :].partition_broadcast(128), w=[b_brt])
        wrt, b_wrt = sb("r_wrt", [128, 8, 36], F32)
        k.dma("sp", wrt[:], k.din["w_rt"].rearrange("(c p) n -> p c n", p=128), w=[b_wrt])
        eps1k, b_eps = sb("r_eps", [128, 1], F32)
        P.add("pool", lambda e: e.memset(eps1k[:], float(D * EPS)), (), [b_eps])
        wout, b_wout = sb("r_wout", [128, 8, D], BF16)
        wst = sb_ring(es, nc, "r_wst", 2, [128, D], F32)
        w_out_v = k.din["w_out"].rearrange("(c p) n -> p c n", p=128)
        for c in range(8):
            st, bst = wst.next()
            k.dma("sp", st[:], w_out_v[:, c, :], w=[bst])
            k.cp("dve" if c % 2 == 0 else "pool", wout[:, c, :], st[:], r=[bst], w=[b_wout])
        dcol, b_dcol = sb("r_dcol", [128, 2], F32)
        k.ts("dve", dcol[:, 0:1], piota[:, 0:1], float(2 * S), None, ALU.add, r=[b_piota], w=[b_dcol])
        k.ts("dve", dcol[:, 1:2], piota[:, 0:1], float(DUMP), None, ALU.add, r=[b_piota], w=[b_dcol])
        k.fence([bXS])
        Aall, b_A = sb("r_A", [128, NT, 32], F32)
        M12, b_M = sb("r_M12", [128, NT, 2, 32], F32)
        gates, b_gates = sb("r_gates", [128, NT, 2], F32)
        dest, b_dest = sb("r_dest", [128, NT, 2], F32)
        desti, b_desti = sb("r_desti", [128, NT, 2], I32)

        mixr = sb_free(es, nc, "r_mix", 3, [128, 8, 128], BF16)
        xr = sb_free(es, nc, "r_x", 3, [128, D], F32)
        h1r = sb_free(es, nc, "r_h1", 3, [128, D], F32)
        junkr = sb_free(es, nc, "r_junk", 2, [128, D], BF16)
        h2r = sb_free(es, nc, "r_h2", 3, [128, D], F32)
        h2bf = sb_free(es, nc, "r_h2bf", 3, [128, D], BF16)
        h2br = sb_ring(es, nc, "r_h2b", 3, [128, D], BF16)
        h2Tr = sb_free(es, nc, "r_h2T", 2, [128, 8, 128], F32)
        smr = sb_free(es, nc, "r_sm", 4, [128, 16], F32)
        lgr = sb_free(es, nc, "r_lg", 4, [128, 36], F32)
        emr = sb_free(es, nc, "r_em", 4, [128, 32], F32)
        t8r = sb_free(es, nc, "r_t8", 4, [128, 8], F32)
        g4r = sb_free(es, nc, "r_g4", 4, [128, 12], F32)
        psf = ps_free(es, nc, "r_psf", 8)

        def tile_gen(ti):
            r0 = ti * 128
            mx, bmx, imx = yield from mixr.get()
            k.load(mx[:], MIXv[:, :, r0:r0 + 128], r=[bMIXT], w=[bmx])
            xt, bx, ix = yield from xr.get()
            k.load(xt[:], k.x[r0:r0 + 128, :], w=[bx])
            yield
            h1, bh1, ih1 = yield from h1r.get()
            pss = []
            for half in range(2):
                ps, bps, ips = yield from psf.get()
                for c in range(8):
                    k.mm(ps[:], mx[:, c, :], wout[:, c, half * 512:(half + 1) * 512], start=(c == 0), stop=(c == 7),
                         r=[bmx, b_wout], w=[bps])
                pss.append((ps, bps, ips))
            mixr.put(imx)
            yield
            for half, (ps, bps, ips) in enumerate(pss):
                k.tt("dve", h1[:, half * 512:(half + 1) * 512], ps[:], xt[:, half * 512:(half + 1) * 512], ALU.add,
                     r=[bps, bx], w=[bh1])
                psf.put(ips)
            xr.put(ix)
            k.store(H1[r0:r0 + 128, :], h1[:], r=[bh1], w=[bH1])
            yield
            sm, bsm, ism = yield from smr.get()
            junk, bjunk, ijunk = yield from junkr.get()
            k.act(junk[:], h1[:], AF.Square, r=[bh1], w=[bjunk, bsm], accum_out=sm[:, 0:1])
            junkr.put(ijunk)
            k.act(sm[:, 1:2], sm[:, 0:1], AF.Ln, r=[bsm, b_eps], w=[bsm], bias=eps1k[:, 0:1])
            k.act(sm[:, 2:3], sm[:, 1:2], AF.Exp, r=[bsm], w=[bsm], scale=-0.5)
            yield
            h2, bh2, ih2 = yield from h2r.get()
            k.ts("dve", h2[:], h1[:], sm[:, 2:3], None, ALU.mult, r=[bh1, bsm], w=[bh2])
            yield
            k.tt("pool", h2[:], h2[:], gff[:], ALU.mult, r=[bh2, b_gff], w=[bh2])
            yield
            h2b, bh2b, ih2b = yield from h2bf.get()
            k.cp("act", h2b[:], h2[:], r=[bh2], w=[bh2b])
            k.store(H2N[r0:r0 + 128, :], h2b[:], r=[bh2b], w=[bH2N])
            h2T, bh2T, ih2T = yield from h2Tr.get()
            pss = []
            for half in range(2):
                ps, bps, ips = yield from psf.get()
                for c4 in range(4):
                    c = half * 4 + c4
                    k.tr(ps[:, c4 * 128:(c4 + 1) * 128], h2[:, c * 128:(c + 1) * 128], identf[:],
                         r=[bh2, b_identf], w=[bps])
                pss.append((ps, bps, ips))
            h2r.put(ih2)
            yield
            for half, (ps, bps, ips) in enumerate(pss):
                k.cp("dve" if half == 0 else "act", h2T[:, half * 4:(half + 1) * 4, :].rearrange("p c t -> p (c t)"), ps[:],
                     r=[bps], w=[bh2T])
                psf.put(ips)
            yield
            lps, blps, ilps = yield from psf.get()
            for c in range(8):
                k.mm(lps[:, 0:36], h2T[:, c, :], wrt[:, c, :], start=(c == 0), stop=(c == 7), r=[bh2T, b_wrt], w=[blps])
            h2Tr.put(ih2T)
            yield
            lg, blg, ilg = yield from lgr.get()
            k.tt("dve", lg[:], lps[:, 0:36], brt[:], ALU.add, r=[blps, b_brt], w=[blg])
            psf.put(ilps)
            g4, bg4, ig4 = yield from g4r.get()
            P.add("dve", lambda e, g4=g4, lg=lg: e.reduce_max(g4[:, 0:1], lg[:, 0:4], AX.X), [blg], [bg4])
            k.ts("dve", g4[:, 1:2], g4[:, 0:1], -1.0, None, ALU.mult, r=[bg4], w=[bg4])
            yield
            k.act(sm[:, 4:8], lg[:, 0:4], AF.Exp, r=[blg, bg4], w=[bsm, bg4], bias=g4[:, 1:2], accum_out=g4[:, 2:3])
            k.ts("dve", g4[:, 4:8], lg[:, 0:4], g4[:, 0:1], None, ALU.is_equal, r=[blg, bg4], w=[bg4])
            k.ts("dve", g4[:, 8:12], g4[:, 4:8], 1.0e30, -1.0e30, ALU.mult, ALU.add, r=[bg4], w=[bg4])
            em, bem, iem = yield from emr.get()
            for g in range(NG):
                k.ts("dve", em[:, g * 8:(g + 1) * 8], lg[:, 4 + g * 8: 12 + g * 8], g4[:, 4 + g:5 + g], g4[:, 8 + g:9 + g],
                     ALU.mult, ALU.add, r=[blg, bg4], w=[bem])
            lgr.put(ilg)
            yield
            P.add("dve", lambda e, g4=g4: e.reciprocal(g4[:, 3:4], g4[:, 2:3]), [bg4], [bg4])
            t8, bt8, it8 = yield from t8r.get()
            P.add("dve", lambda e, t8=t8, em=em: e.max(t8[:], em[:]), [bem], [bt8])
            yield
            k.ts("dve", M12[:, ti, 0, :], em[:], t8[:, 0:1], None, ALU.is_equal, r=[bem, bt8], w=[b_M])
            k.ts("dve", M12[:, ti, 1, :], em[:], t8[:, 1:2], None, ALU.is_equal, r=[bem, bt8], w=[b_M])
            emr.put(iem)
            k.tt("dve", sm[:, 8:9], t8[:, 1:2], t8[:, 0:1], ALU.subtract, r=[bt8], w=[bsm])
            t8r.put(it8)
            yield
            k.tt("dve", Aall[:, ti, :], M12[:, ti, 0, :], M12[:, ti, 1, :], ALU.add, r=[b_M], w=[b_A])
            k.act(sm[:, 9:10], sm[:, 8:9], AF.Exp, r=[bsm], w=[bsm])
            yield
            k.ts("dve", sm[:, 9:10], sm[:, 9:10], 1.0, None, ALU.add, r=[bsm], w=[bsm])
            yield
            P.add("dve", lambda e, sm=sm: e.reciprocal(sm[:, 10:11], sm[:, 9:10]), [bsm], [bsm])
            yield
            k.tt("dve", gates[:, ti, 0:1], sm[:, 10:11], g4[:, 3:4], ALU.mult, r=[bsm, bg4], w=[b_gates])
            yield
            k.tt("dve", gates[:, ti, 1:2], g4[:, 3:4], gates[:, ti, 0:1], ALU.subtract, r=[bg4, b_gates], w=[b_gates])
            smr.put(ism)
            g4r.put(ig4)
            h1r.put(ih1)
            h2bf.put(ih2b)

        run_pipelined([tile_gen(ti) for ti in range(NT)], 3)

        cps, bcps, icps = psf.take()
        for ti in range(NT):
            k.mm(cps[0:32, ti:ti + 1], Aall[:, ti, :], onesf[:, 0:1], r=[b_A, b_onesf], w=[bcps])
        cnt, b_cnt = sb("r_cnt", [32, NT], F32)
        k.cp("dve", cnt[:], cps[0:32, 0:NT], r=[bcps], w=[b_cnt])
        inc, b_inc = sb("r_inc", [32, NT], F32)
        onesr, b_onesr = sb("r_onesr", [32, NT], F32)
        P.add("pool", lambda e: e.memset(onesr[:], 1.0), (), [b_onesr])
        P.add("dve", lambda e: e.tensor_tensor_scan(inc[:], onesr[:], cnt[:], 0.0, ALU.mult, ALU.add),
              [b_onesr, b_cnt], [b_inc])
        off, b_off = sb("r_off", [32, NT], F32)
        k.tt("dve", off[:], inc[:], cnt[:], ALU.subtract, r=[b_inc, b_cnt], w=[b_off])
        k.ts("dve", off[:], off[:], ecap[:, 0:1], None, ALU.add, r=[b_off, b_ecap], w=[b_off])
        dgr = sb_free(es, nc, "r_dg", 3, [32, 32], F32)
        tmr = sb_free(es, nc, "r_tm", 3, [128, 4, 32], F32)
        okr = sb_free(es, nc, "r_ok", 3, [128, 3, 32], F32)
        gkr = sb_free(es, nc, "r_gk", 3, [128, 2], F32)

        def slot_gen(ti):
            h2b, bh2b, ih2b = yield from h2bf.get()
            k.load(h2b[:], H2N[ti * 128:(ti + 1) * 128, :], r=[bH2N], w=[bh2b])
            dg, bdg, idg = yield from dgr.get()
            k.ts("dve", dg[:], id32[:], off[:, ti:ti + 1], None, ALU.mult, r=[b_id32, b_off], w=[bdg])
            yield
            sps, bsps, isps = yield from psf.get()
            k.mm(sps[:, 0:32], ustr[:], Aall[:, ti, :], start=True, stop=False, r=[b_ustr, b_A], w=[bsps])
            k.mm(sps[:, 0:32], onesf[0:32, :], dg[:], start=False, stop=True, r=[b_onesf, bdg], w=[bsps])
            dgr.put(idg)
            yield
            ok, bok, iok = yield from okr.get()
            k.tt("dve", ok[:, 0, :], sps[:, 0:32], limr[:], ALU.is_lt, r=[bsps, b_limr], w=[bok])
            yield
            k.tt("dve", ok[:, 1, :], sps[:, 0:32], ok[:, 0, :], ALU.mult, r=[bsps, bok], w=[bok])
            psf.put(isps)
            k.ts("dve", ok[:, 2, :], ok[:, 0, :], -1.0, 1.0, ALU.mult, ALU.add, r=[bok], w=[bok])
            yield
            k.ts("dve", ok[:, 2, :], ok[:, 2, :], dcol[:, 1:2], None, ALU.mult, r=[bok, b_dcol], w=[bok])
            yield
            k.tt("dve", ok[:, 1, :], ok[:, 1, :], ok[:, 2, :], ALU.add, r=[bok], w=[bok])
            yield
            tm, btm, itm = yield from tmr.get()
            for j in range(2):
                k.tt("dve", tm[:, j, :], M12[:, ti, j, :], ok[:, 1, :], ALU.mult, r=[b_M, bok], w=[btm])
                k.tt("dve", tm[:, 2 + j, :], M12[:, ti, j, :], ok[:, 0, :], ALU.mult, r=[b_M, bok], w=[btm])
            okr.put(iok)
            yield
            P.add("dve", lambda e, tm=tm, ti=ti: e.reduce_sum(dest[:, ti, :], tm[:, 0:2, :], AX.X), [btm], [b_dest])
            gk, bgk, igk = yield from gkr.get()
            P.add("dve", lambda e, tm=tm, gk=gk: e.reduce_sum(gk[:], tm[:, 2:4, :], AX.X), [btm], [bgk])
            tmr.put(itm)
            yield
            k.tt("dve", gates[:, ti, :], gates[:, ti, :], gk[:], ALU.mult, r=[b_gates, bgk], w=[b_gates])
            gkr.put(igk)
            k.cp("dve", desti[:, ti, :], dest[:, ti, :], r=[b_dest], w=[b_desti])
            yield
            for j in range(2):
                P.add("pool", lambda e, ti=ti, j=j, h2b=h2b: e.indirect_dma_start(
                    out=XS, out_offset=bass.IndirectOffsetOnAxis(ap=desti[:, ti, j:j + 1], axis=0),
                    in_=h2b[:], in_offset=None), [b_desti, bh2b], [bXS], dma=True)
                yield
            h2bf.put(ih2b)

        run_pipelined([slot_gen(ti) for ti in range(NT)], 3)
        k.dma("sp", GATE, gates[:].rearrange("p t j -> p (t j)"), r=[b_gates], w=[bGATE])
        k.dma("sp", DEST, desti[:].rearrange("p t j -> p (t j)"), r=[b_desti], w=[bDEST])
        k.flush()
        P.drain_dmas()
        P.emit()


def phase_moe(k):
    nc, P, S, NT = k.nc, k.P, k.S, k.NT
    B, CAP, PSL = MOE_B, k.CAP, k.PSL
    NS = B // 128
    H1, XS, DEST, GATE, YB = (k.scr[n] for n in ("H1", "XS", "DEST", "GATE", "YB"))
    bH1, bXS, bDEST, bGATE, bYB = (k.scr_buf[n] for n in ("H1", "XS", "DEST", "GATE", "YB"))
    WG = k.din["w_gate"].rearrange("e (p c) n -> e p (c n)", p=128)
    WU = k.din["w_up"].rearrange("e (p c) n -> e p (c n)", p=128)
    WD = k.din["w_down"].rearrange("e (p c) n -> e p (c n)", p=128)
    with ExitStack() as es:
        def sb(name, shape, dt):
            return es.enter_context(nc.sbuf_tensor("sb_" + name, list(shape), dt)), Buf(name)
        identb, b_identb = sb("m_ident", [128, 128], BF16)
        k.dma("sp", identb[:], k.din["ident_bf"], w=[b_identb])
        tkr = sb_free(es, nc, "m_tk", 6, [128, 16], I32)
        xgr = sb_free(es, nc, "m_xg", 6, [128, D], BF16)
        wfr = sb_free(es, nc, "m_wf", 3, [128, 2048], F32)
        wgr = sb_free(es, nc, "m_wg", 2, [128, 8, DE], BF16)
        wur = sb_free(es, nc, "m_wu", 2, [128, 8, DE], BF16)
        wdr = sb_free(es, nc, "m_wd", 2, [128, 2, D], BF16)
        xTr = sb_free(es, nc, "m_xT", 3, [128, 8, B], BF16)
        sgr = sb_free(es, nc, "m_sg", 3, [128, B], F32)
        hTr = sb_free(es, nc, "m_hT", 3, [128, 2, B], BF16)
        ysr = sb_free(es, nc, "m_ys", 6, [128, D], BF16)
        psf = ps_free(es, nc, "m_psf", 6)
        psb = ps_free(es, nc, "m_psb", 2, (128, 1024), BF16)
        ceng = ["dve", "pool", "act"]
        wtiles = {}

        def wprep_gen(ex):
            wts = []
            for wi_, (src, pool_) in enumerate(((WG, wgr), (WU, wur), (WD, wdr))):
                wf, bwf, iwf = yield from wfr.get()
                k.load(wf[:], src[ex], w=[bwf])
                wb, bwb, iwb = yield from pool_.get()
                wts.append((wf, bwf, iwf, wb, bwb, iwb, pool_))
            wtiles[ex] = [(wb, bwb, iwb, pool_) for (_, _, _, wb, bwb, iwb, pool_) in wts]
            wtiles[(ex, "left")] = CAP // B
            yield
            for wi_, (wf, bwf, iwf, wb, bwb, iwb, pool_) in enumerate(wts):
                k.cp(ceng[wi_], wb[:].rearrange("p c n -> p (c n)"), wf[:], r=[bwf], w=[bwb])
                wfr.put(iwf)
                yield

        def blk_gen(ex, blk):
            (wg, bwg, _, _), (wu, bwu, _, _), (wd, bwd, _, _) = wtiles[ex]
            wgv = wg[:].rearrange("p c (q j) -> p c j q", j=2)
            wuv = wu[:].rearrange("p c (q j) -> p c j q", j=2)
            s0 = ex * CAP + blk * B
            xT, bxT, ixT = yield from xTr.get()
            tks, xgs = [], []
            for s_ in range(NS):
                xg, bxg, ixg = yield from xgr.get()
                k.load(xg[:], XS[s0 + s_ * 128: s0 + (s_ + 1) * 128, :], r=[bXS], w=[bxg])
                xgs.append((xg, bxg, ixg))
            yield
            for s_ in range(NS):
                xg, bxg, ixg = xgs[s_]
                pb, bpb, ipb = yield from psb.get()
                xgv = xg[:].rearrange("p (q c) -> p c q", c=8)
                for c in range(8):
                    k.tr(pb[:, c * 128:(c + 1) * 128], xgv[:, c, :], identb[:], r=[bxg, b_identb], w=[bpb])
                xgr.put(ixg)
                yield
                k.cp("act" if s_ % 2 == 0 else "dve", xT[:, :, s_ * 128:(s_ + 1) * 128],
                     pb[:].rearrange("p (c t) -> p c t", c=8), r=[bpb], w=[bxT])
                psb.put(ipb)
            yield
            hT, bhT, ihT = yield from hTr.get()
            for c in range(2):
                gps, bgps, igps = yield from psf.get()
                ups, bups, iups = yield from psf.get()
                for kc in range(8):
                    k.mm(gps[:, 0:B], wgv[:, kc, c, :], xT[:, kc, :], start=(kc == 0), stop=(kc == 7),
                         r=[bwg, bxT], w=[bgps])
                for kc in range(8):
                    k.mm(ups[:, 0:B], wuv[:, kc, c, :], xT[:, kc, :], start=(kc == 0), stop=(kc == 7),
                         r=[bwu, bxT], w=[bups])
                yield
                sg, bsg, isg = yield from sgr.get()
                k.act(sg[:], gps[:, 0:B], AF.Silu, r=[bgps], w=[bsg])
                psf.put(igps)
                yield
                k.tt("dve", hT[:, c, :], sg[:], ups[:, 0:B], ALU.mult, r=[bsg, bups], w=[bhT])
                psf.put(iups)
                sgr.put(isg)
            xTr.put(ixT)
            yield
            for s_ in range(NS):
                ys, bys, iys = yield from ysr.get()
                for half in range(2):
                    yps, byps, iyps = yield from psf.get()
                    for c in range(2):
                        k.mm(yps[:], hT[:, c, s_ * 128:(s_ + 1) * 128], wd[:, c, half * 512:(half + 1) * 512],
                             start=(c == 0), stop=(c == 1), r=[bhT, bwd], w=[byps])
                    yield
                    k.cp("act" if half == 0 else "dve", ys[:, half * 512:(half + 1) * 512], yps[:], r=[byps], w=[bys])
                    psf.put(iyps)
                k.store(YB[s0 + s_ * 128: s0 + (s_ + 1) * 128, :], ys[:], r=[bys], w=[bYB])
                ysr.put(iys)
            hTr.put(ihT)
            wtiles[(ex, "left")] -= 1
            if wtiles[(ex, "left")] == 0:
                for (_, _, iwb, pool_) in wtiles[ex]:
                    pool_.put(iwb)

        for _ in wprep_gen(0):
            pass
        gens = []
        for ex in range(NE):
            if ex + 1 < NE:
                gens.append(wprep_gen(ex + 1))
            gens += [blk_gen(ex, blk) for blk in range(CAP // B)]
        run_pipelined(gens, 3)
        k.flush()
        P.drain_dmas()
        P.emit()
    with ExitStack() as es:
        def sb(name, shape, dt):
            return es.enter_context(nc.sbuf_tensor("sb_" + name, list(shape), dt)), Buf(name)
        gates, b_gates = sb("c_gate", [128, NT * 2], F32)
        desti, b_desti = sb("c_dest", [128, NT * 2], I32)
        k.dma("sp", gates[:], GATE, r=[bGATE], w=[b_gates])
        k.dma("sp", desti[:], DEST, r=[bDEST], w=[b_desti])
        h1r = sb_ring(es, nc, "c_h1", 4, [128, D], F32)
        ytr = sb_ring(es, nc, "c_yt", 8, [128, D], BF16)
        by = Buf("y", multi=True)
        for ti in range(NT):
            r0 = ti * 128
            h1, bh1 = h1r.next()
            k.load(h1[:], H1[r0:r0 + 128, :], r=[bH1], w=[bh1])
            yts = []
            for j in range(2):
                yt, byt = ytr.next()
                P.add("pool", lambda e, yt=yt, ti=ti, j=j: e.indirect_dma_start(
                    out=yt[:], out_offset=None, in_=YB,
                    in_offset=bass.IndirectOffsetOnAxis(ap=desti[:, 2 * ti + j:2 * ti + j + 1], axis=0)),
                    [b_desti, bYB], [byt], dma=True)
                yts.append((yt, byt))
            for j, (yt, byt) in enumerate(yts):
                P.add("dve", lambda e, yt=yt, h1=h1, ti=ti, j=j: e.scalar_tensor_tensor(
                    h1[:], yt[:], gates[:, 2 * ti + j:2 * ti + j + 1], h1[:], ALU.mult, ALU.add),
                    [byt, b_gates, bh1], [bh1])
            k.store(k.y[r0:r0 + 128, :], h1[:], r=[bh1], w=[by])
        k.flush()
        P.drain_dmas()
        P.emit()


def build_program(S):
    k = K(S)
    phase1(k)
    phase_attn(k)
    phase_hgrn(k)
    phase_route(k)
    phase_moe(k)
    return k


def kernel(**inputs):
    x = np.asarray(inputs["x"], dtype=np.float32)
    nb, S, _ = x.shape
    k = build_program(S)
    par = layout_params(inputs)
    con = make_consts(S)
    in_maps = []
    for c in range(nb):
        m = {"x": np.ascontiguousarray(x[c])}
        m.update(par)
        m.update(con)
        in_maps.append(m)
    res = run_bass_kernel_spmd(k.nc, in_maps, core_ids=list(range(nb)))
    return np.stack([np.asarray(r["y"], dtype=np.float32) for r in res.results], axis=0)
```
